# Optimizing a Trainium2 kernel written in Bass

```python
import math
import jax, jax.numpy as jnp
from jax import lax
import numpy as np

D_MODEL = 1024
BATCH = 4
SEQ = 4096
DEPTH = 1

EPS = 1e-6
ROPE_BASE = 10000.0
N_MLA_HEADS = 8
QK_NOPE = 64
QK_ROPE = 32
V_HEAD = 64
Q_LORA = 256
KV_LORA = 128
Q_BLOCK = 128
N_RET_HEADS = 4
RET_DK = 128
RET_DV = 128
RET_CHUNK = 128
MLA_WIDTH = N_MLA_HEADS * V_HEAD
RET_WIDTH = N_RET_HEADS * RET_DV
D_MIX = MLA_WIDTH + RET_WIDTH
IN_SPLITS = (Q_LORA, KV_LORA, QK_ROPE,
             N_RET_HEADS * RET_DK, N_RET_HEADS * RET_DK,
             N_RET_HEADS * RET_DV, N_RET_HEADS * RET_DV)
IN_COLS = sum(IN_SPLITS)
MEM_LEN = 256
N_XATTN_HEADS = 4
XATTN_HEAD = D_MODEL // N_XATTN_HEADS
D_FF = 2816
CONV_W = 3

kernel_name = "hymba_mla_retnet_convffn_layer"


def rmsnorm(x, g):
    x32 = x.astype(jnp.float32)
    inv = lax.rsqrt(jnp.mean(x32 * x32, axis=-1, keepdims=True) + EPS)
    return (x32 * inv * g.astype(jnp.float32)).astype(x.dtype)


def rope(x, positions):
    d = x.shape[-1]
    inv_freq = 1.0 / (ROPE_BASE ** (jnp.arange(0, d, 2, dtype=jnp.float32) / d))
    ang = positions.astype(jnp.float32)[..., None] * inv_freq
    ang = ang.reshape(ang.shape[:2] + (1,) * (x.ndim - 3) + (d // 2,))
    cos, sin = jnp.cos(ang), jnp.sin(ang)
    x32 = x.astype(jnp.float32)
    x1, x2 = x32[..., : d // 2], x32[..., d // 2:]
    out = jnp.concatenate([x1 * cos - x2 * sin, x2 * cos + x1 * sin], axis=-1)
    return out.astype(x.dtype)


def mla_group(c_q, c_kv, k_rope_raw, positions, g_q_lat, w_uq, g_kv_lat, w_ukv):
    B, S, _ = c_q.shape
    q = (rmsnorm(c_q, g_q_lat) @ w_uq).reshape(B, S, N_MLA_HEADS, QK_NOPE + QK_ROPE)
    q_nope, q_rope = q[..., :QK_NOPE], rope(q[..., QK_NOPE:], positions)
    kv = (rmsnorm(c_kv, g_kv_lat) @ w_ukv).reshape(B, S, N_MLA_HEADS, QK_NOPE + V_HEAD)
    k_nope, v = kv[..., :QK_NOPE], kv[..., QK_NOPE:]
    k_rope = rope(k_rope_raw, positions)
    scale = 1.0 / math.sqrt(QK_NOPE + QK_ROPE)

    nb = S // Q_BLOCK
    qn_blocks = q_nope.reshape(B, nb, Q_BLOCK, N_MLA_HEADS, QK_NOPE).transpose(1, 0, 2, 3, 4)
    qr_blocks = q_rope.reshape(B, nb, Q_BLOCK, N_MLA_HEADS, QK_ROPE).transpose(1, 0, 2, 3, 4)
    starts = jnp.arange(nb, dtype=jnp.int32) * Q_BLOCK
    key_idx = jnp.arange(S, dtype=jnp.int32)

    def one_block(args):
        qn_b, qr_b, start = args
        s = (jnp.einsum('bqhd,bkhd->bhqk', qn_b, k_nope)
             + jnp.einsum('bqhd,bkd->bhqk', qr_b, k_rope)).astype(jnp.float32) * scale
        q_idx = start + jnp.arange(Q_BLOCK, dtype=jnp.int32)
        mask = key_idx[None, :] <= q_idx[:, None]
        s = jnp.where(mask[None, None], s, jnp.finfo(jnp.float32).min)
        p = jax.nn.softmax(s, axis=-1).astype(v.dtype)
        return jnp.einsum('bhqk,bkhd->bqhd', p, v)

    out = lax.map(one_block, (qn_blocks, qr_blocks, starts))
    return out.transpose(1, 0, 2, 3, 4).reshape(B, S, MLA_WIDTH)


def retention_group(q, k, v, g, positions):
    B, S, _ = q.shape
    H, L = N_RET_HEADS, RET_CHUNK
    C = S // L
    q = rope(q.reshape(B, S, H, RET_DK), positions)
    k = rope(k.reshape(B, S, H, RET_DK), positions) * (RET_DK ** -0.5)
    v = v.reshape(B, S, H, RET_DV)
    dt = q.dtype

    log_gamma = jnp.log(1.0 - 2.0 ** (-5.0 - jnp.arange(H, dtype=jnp.float32)))
    j = jnp.arange(L, dtype=jnp.float32)
    diff = j[:, None] - j[None, :]
    intra = jnp.where(diff[None] >= 0,
                      jnp.exp(jnp.maximum(diff, 0.0)[None] * log_gamma[:, None, None]),
                      0.0).astype(dt)
    k_to_end = jnp.exp((L - 1 - j)[:, None] * log_gamma[None, :]).astype(dt)
    q_from_start = jnp.exp((j + 1)[:, None] * log_gamma[None, :]).astype(dt)
    chunk_decay = jnp.exp(L * log_gamma).astype(dt)

    qc = q.reshape(B, C, L, H, RET_DK)
    kc = k.reshape(B, C, L, H, RET_DK)
    vc = v.reshape(B, C, L, H, RET_DV)

    scores = jnp.einsum('bclhd,bcmhd->bchlm', qc, kc) * intra[None, None]
    inner = jnp.einsum('bchlm,bcmhe->bclhe', scores, vc)

    chunk_kv = jnp.einsum('bclhd,bclhe->cbhde', kc * k_to_end[None, None, :, :, None], vc)

    def step(state, kv_c):
        return chunk_decay[None, :, None, None] * state + kv_c, state

    init = jnp.zeros((B, H, RET_DK, RET_DV), dtype=chunk_kv.dtype)
    _, prev_states = lax.scan(step, init, chunk_kv)
    cross = jnp.einsum('bclhd,cbhde->bclhe',
                       qc * q_from_start[None, None, :, :, None], prev_states)
    out = (inner + cross).reshape(B, S, H, RET_DV)

    o32 = out.astype(jnp.float32)
    mu = jnp.mean(o32, axis=-1, keepdims=True)
    var = jnp.mean(jnp.square(o32 - mu), axis=-1, keepdims=True)
    o = ((o32 - mu) * lax.rsqrt(var + EPS)).astype(dt).reshape(B, S, RET_WIDTH)
    return o * jax.nn.silu(g)


def memory_cross_attention(h, mem_n, w_xq, w_xkv, w_xo):
    B, S, _ = h.shape
    M = mem_n.shape[1]
    q = (h @ w_xq).reshape(B, S, N_XATTN_HEADS, XATTN_HEAD)
    kv = (mem_n @ w_xkv).reshape(B, M, 2, N_XATTN_HEADS, XATTN_HEAD)
    k, v = kv[:, :, 0], kv[:, :, 1]
    s = jnp.einsum('bqhd,bkhd->bhqk', q, k).astype(jnp.float32) / math.sqrt(XATTN_HEAD)
    p = jax.nn.softmax(s, axis=-1).astype(v.dtype)
    o = jnp.einsum('bhqk,bkhd->bqhd', p, v).reshape(B, S, N_XATTN_HEADS * XATTN_HEAD)
    return o @ w_xo


def conv_gated_mlp(h, w_ffn_in, conv_w, conv_b, w_ffn_out):
    gu = h @ w_ffn_in
    gate, up = gu[..., :D_FF], gu[..., D_FF:]
    gp = jnp.pad(gate, ((0, 0), (CONV_W - 1, 0), (0, 0)))
    S = gate.shape[1]
    conv = conv_b + sum(gp[:, i:i + S] * conv_w[i] for i in range(CONV_W))
    return (jax.nn.silu(conv) * up) @ w_ffn_out


def setup_inputs(seed: int = 0) -> dict:
    key = jax.random.key(seed)
    ks = jax.random.split(key, 24)

    def nrm(k, shape, fan_in):
        return jax.random.normal(k, shape, jnp.float32) * (fan_in ** -0.5)

    def gain(k, shape):
        return 1.0 + 0.01 * jax.random.normal(k, shape, jnp.float32)

    x = jax.random.normal(ks[0], (BATCH, SEQ, D_MODEL), jnp.float32)
    mem = jax.random.normal(ks[1], (BATCH, MEM_LEN, D_MODEL), jnp.float32)
    offset = jax.random.randint(ks[2], (BATCH, 1), 0, 1024, dtype=jnp.int32)
    positions = offset + jnp.arange(SEQ, dtype=jnp.int32)[None, :]
    return {
        "x": x,
        "mem": mem,
        "positions": positions,
        "g_mix": gain(ks[3], (DEPTH, D_MODEL)),
        "w_in": nrm(ks[4], (DEPTH, D_MODEL, IN_COLS), D_MODEL),
        "g_q_lat": gain(ks[5], (DEPTH, Q_LORA)),
        "w_uq": nrm(ks[6], (DEPTH, Q_LORA, N_MLA_HEADS * (QK_NOPE + QK_ROPE)), Q_LORA),
        "g_kv_lat": gain(ks[7], (DEPTH, KV_LORA)),
        "w_ukv": nrm(ks[8], (DEPTH, KV_LORA, N_MLA_HEADS * (QK_NOPE + V_HEAD)), KV_LORA),
        "w_out": nrm(ks[9], (DEPTH, D_MIX, D_MODEL), D_MIX),
        "g_xattn": gain(ks[10], (DEPTH, D_MODEL)),
        "g_mem": gain(ks[11], (DEPTH, D_MODEL)),
        "w_xq": nrm(ks[12], (DEPTH, D_MODEL, N_XATTN_HEADS * XATTN_HEAD), D_MODEL),
        "w_xkv": nrm(ks[13], (DEPTH, D_MODEL, 2 * N_XATTN_HEADS * XATTN_HEAD), D_MODEL),
        "w_xo": nrm(ks[14], (DEPTH, N_XATTN_HEADS * XATTN_HEAD, D_MODEL), N_XATTN_HEADS * XATTN_HEAD),
        "g_ffn": gain(ks[15], (DEPTH, D_MODEL)),
        "w_ffn_in": nrm(ks[16], (DEPTH, D_MODEL, 2 * D_FF), D_MODEL),
        "conv_w": nrm(ks[17], (DEPTH, CONV_W, D_FF), CONV_W),
        "conv_b": 0.01 * jax.random.normal(ks[18], (DEPTH, D_FF), jnp.float32),
        "w_ffn_out": nrm(ks[19], (DEPTH, D_FF, D_MODEL), D_FF),
        "g_final": gain(ks[20], (D_MODEL,)),
    }


def reference(x, mem, positions, g_mix, w_in, g_q_lat, w_uq, g_kv_lat, w_ukv, w_out,
              g_xattn, g_mem, w_xq, w_xkv, w_xo, g_ffn, w_ffn_in, conv_w, conv_b,
              w_ffn_out, g_final):
    offs = np.cumsum((0,) + IN_SPLITS)
    for l in range(DEPTH):
        h = rmsnorm(x, g_mix[l])
        proj = h @ w_in[l]
        c_q, c_kv, k_rope_raw, rq, rk, rv, rg = [proj[..., offs[i]:offs[i + 1]]
                                                 for i in range(len(IN_SPLITS))]
        y_mla = mla_group(c_q, c_kv, k_rope_raw, positions,
                          g_q_lat[l], w_uq[l], g_kv_lat[l], w_ukv[l])
        y_ret = retention_group(rq, rk, rv, rg, positions)
        x = x + jnp.concatenate([y_mla, y_ret], axis=-1) @ w_out[l]
        x = x + memory_cross_attention(rmsnorm(x, g_xattn[l]), rmsnorm(mem, g_mem[l]),
                                       w_xq[l], w_xkv[l], w_xo[l])
        x = x + conv_gated_mlp(rmsnorm(x, g_ffn[l]), w_ffn_in[l], conv_w[l], conv_b[l],
                               w_ffn_out[l])
    return rmsnorm(x, g_final)
```

```python
import math
from contextlib import ExitStack

import numpy as np
import concourse.bass as bass
import concourse.mybir as mybir
from concourse.bass_utils import run_bass_kernel_spmd

F32 = mybir.dt.float32
BF16 = mybir.dt.bfloat16
I32 = mybir.dt.int32
U8 = mybir.dt.uint8
AF = mybir.ActivationFunctionType
ALU = mybir.AluOpType
AX = mybir.AxisListType

D = 1024
SEQ = 4096
NB = 4
EPS = 1e-6
NPRE = 2048
NOWN = 2048
NTOK = NPRE + NOWN
HALO0 = NPRE - 128
NST = NOWN + 128
IN_COLS = 2464
OFF_CQ, OFF_CKV, OFF_KR, OFF_RQ, OFF_RK, OFF_RV, OFF_RG = 0, 256, 384, 416, 928, 1440, 1952
DFF = 2816
NFB = DFF // 128
MEM = 256
TWO_PI = 2.0 * math.pi
C1 = 6.28125
C2 = TWO_PI - C1
MAGIC = 12582912.0
PI_LO = 3.1415925
ARENA_BYTES = 206 * 1024
ENGS = ("pe", "act", "dve", "pool", "sp")
NDMA_MAX = 90
DT_SIZE = {F32: 4, BF16: 2, I32: 4}


class Buf:
    def __init__(self, uid, name, off, nbytes, ap, nsub):
        self.uid, self.name, self.off, self.nbytes, self.ap, self.nsub = uid, name, off, nbytes, ap, nsub

    def k(self, i=0):
        return (self.uid, i)

    def all(self):
        return [(self.uid, i) for i in range(self.nsub)]

    def __getitem__(self, idx):
        return self.ap[idx]


class Sched:
    def __init__(self, nc, es):
        self.nc, self.es = nc, es
        self.prog = {e: [] for e in ENGS}
        self.sem = {e: es.enter_context(nc.semaphore("s_" + e)) for e in ENGS if e != "sp"}
        self.cnt = {e: 0 for e in ENGS}
        self.seen = {e: {} for e in ENGS}
        self.drained = {e: 0 for e in ENGS}
        self.needed = {e: set() for e in ENGS}
        self.lastw, self.readers = {}, {}
        self.ndma = 0
        self.dma_events = []
        self.dma_pool = [es.enter_context(nc.semaphore(f"d{i}")) for i in range(NDMA_MAX)]
        self.dma_sem = {}
        self.arena = es.enter_context(nc.sbuf_tensor("arena", [128, ARENA_BYTES], U8))
        self.free = [(0, ARENA_BYTES)]
        self.freed_events = []
        self.nbuf = 0
        self.peak = 0
        self.used = 0

    def alloc(self, name, shape, dtype, nsub=1):
        n = 1
        for s in shape[1:]:
            n *= s
        nbytes = (n * DT_SIZE[dtype] + 63) // 64 * 64
        for i, (o, sz) in enumerate(self.free):
            if sz >= nbytes:
                off = o
                if sz == nbytes:
                    self.free.pop(i)
                else:
                    self.free[i] = (o + nbytes, sz - nbytes)
                break
        else:
            raise MemoryError(f"arena full allocating {name} {nbytes}B; free={self.free}")
        ap = self.arena[0:shape[0], off:off + n * DT_SIZE[dtype]].bitcast(dtype)
        if len(shape) == 3:
            ap = ap.rearrange("p (a b) -> p a b", a=shape[1])
        elif len(shape) == 4:
            ap = ap.rearrange("p (a b c) -> p a b c", a=shape[1], b=shape[2])
        self.nbuf += 1
        b = Buf(self.nbuf, name, off, nbytes, ap, nsub)
        evs, keep = [], []
        for (fo, fn, fe) in self.freed_events:
            if fo < off + nbytes and off < fo + fn:
                evs.extend(fe)
            keep.append((fo, fn, fe))
        self.freed_events = keep
        if evs:
            for kk in b.all():
                self.readers[kk] = list(evs)
        self.used += nbytes
        self.peak = max(self.peak, self.used)
        return b

    def release(self, *bufs):
        for b in bufs:
            evs = []
            for kk in b.all():
                if kk in self.lastw:
                    evs.append(self.lastw.pop(kk))
                evs.extend(self.readers.pop(kk, []))
            best = {}
            for (s, v) in evs:
                best[s] = max(best.get(s, 0), v)
            self.freed_events.append((b.off, b.nbytes, list(best.items())))
            self.free.append((b.off, b.nbytes))
            self.free.sort()
            merged = []
            for (o, sz) in self.free:
                if merged and merged[-1][0] + merged[-1][1] == o:
                    merged[-1] = (merged[-1][0], merged[-1][1] + sz)
                else:
                    merged.append((o, sz))
            self.free = merged
            self.used -= b.nbytes

    def _deps(self, eng, R, W):
        best = {}
        for r in R:
            ev = self.lastw.get(r)
            if ev is not None:
                best[ev[0]] = max(best.get(ev[0], 0), ev[1])
        for w in W:
            ev = self.lastw.get(w)
            if ev is not None:
                best[ev[0]] = max(best.get(ev[0], 0), ev[1])
            for ev in self.readers.get(w, ()):
                best[ev[0]] = max(best.get(ev[0], 0), ev[1])
        for s, v in best.items():
            if s == eng:
                if eng == "pe":
                    continue
                if eng in ("act", "dve"):
                    if self.drained[eng] < v:
                        self.prog[eng].append(("drain",))
                        self.drained[eng] = self.cnt[eng]
                    continue
            if self.seen[eng].get(s, 0) >= v:
                continue
            self.seen[eng][s] = v
            self.prog[eng].append(("wait", s, v))
            if isinstance(s, str):
                self.needed[s].add(v)

    def _commit(self, ev, R, W):
        for w in W:
            self.lastw[w] = ev
            self.readers[w] = []
        for r in R:
            if r not in W:
                self.readers.setdefault(r, []).append(ev)

    def op(self, eng, fn, R=(), W=()):
        R, W = list(R), list(W)
        self._deps(eng, R, W)
        self.cnt[eng] += 1
        ev = (eng, self.cnt[eng])
        self.prog[eng].append(("inst", fn, self.cnt[eng]))
        self._commit(ev, R, W)
        return ev

    def dma(self, eng, out, in_, R=(), W=(), key=None):
        R, W = list(R), list(W)
        self._deps(eng, R, W)
        if key is None:
            key = f"_auto{self.ndma}"
        if key not in self.dma_sem:
            self.dma_sem[key] = [self.dma_pool[len(self.dma_sem)], 0]
        ent = self.dma_sem[key]
        sem = ent[0]
        if ent[1] > 0 and self.seen[eng].get(sem, 0) < ent[1]:
            self.seen[eng][sem] = ent[1]
            self.prog[eng].append(("wait", sem, ent[1]))
        ent[1] += 16
        self.ndma += 1
        ev = (sem, ent[1])
        self.prog[eng].append(("dma", out, in_, sem))
        self._commit(ev, R, W)
        self.dma_events.append(ev)
        return ev

    def emit(self, block):
        nc = self.nc
        S = self

        rank = {en: {q: i + 1 for i, q in enumerate(sorted(S.needed[en]))} for en in ENGS}
        S.n_inc = {en: len(rank[en]) for en in ENGS}

        def run(e, name):
            for ent in S.prog[name]:
                if ent[0] == "wait":
                    s = ent[1]
                    if isinstance(s, str):
                        e.wait_ge(S.sem[s], rank[s][ent[2]])
                    else:
                        e.wait_ge(s, ent[2])
                elif ent[0] == "drain":
                    e.drain()
                elif ent[0] == "inst":
                    ins = ent[1](e)
                    if ent[2] in rank[name]:
                        ins.then_inc(S.sem[name], 1)
                else:
                    e.dma_start(out=ent[1], in_=ent[2]).then_inc(ent[3], 16)

        @block.tensor
        def _(e):
            run(e, "pe")

        @block.scalar
        def _(e):
            run(e, "act")

        @block.vector
        def _(e):
            run(e, "dve")

        @block.gpsimd
        def _(e):
            run(e, "pool")

        @block.sync
        def _(e):
            run(e, "sp")

    def final_wait(self, eng, events):
        for (s, v) in events:
            if self.seen[eng].get(s, 0) >= v:
                continue
            self.seen[eng][s] = v
            self.prog[eng].append(("wait", s, v))


class K:
    pass


def bc(ap, shape):
    return ap.broadcast_to(shape)


def build(stage="full", dbg=False, a1_tiles=None, a1_level=9):
    nc = bass.Bass("TRN2", target_bir_lowering=False)
    es = ExitStack()
    k = K()
    k.nc, k.es, k.stage, k.dbg = nc, es, stage, dbg
    k.a1_tiles, k.a1_level = a1_tiles, a1_level
    d = {}

    def din(name, shape, dt=F32):
        d[name] = nc.dram_tensor(name, list(shape), dt, kind="ExternalInput").ap()

    din("xT", [D, NTOK]); din("posi", [1, NTOK], I32); din("memT", [D, MEM]); din("flags", [128, 2])
    din("w_in", [D, IN_COLS]); din("w_uq", [256, 768]); din("w_ukv", [128, 1024]); din("w_out", [D, D])
    din("w_xq", [D, D]); din("w_xkv", [D, 2 * D]); din("w_xo", [D, D])
    din("w_ffn_in", [D, 2 * DFF]); din("w_ffn_out", [DFF, D])
    din("gv", [128, 43]); din("convp", [128, NFB * 4]); din("c_small", [128, 8])
    din("c_ident", [128, 128]); din("c_rot", [128, 128]); din("c_causal", [128, 128])
    din("c_intra", [128, 512]); din("c_qfs", [128, 512]); din("c_decay", [128, 512])
    d["outT"] = nc.dram_tensor("outT", [D, NOWN], F32, kind="ExternalOutput").ap()
    d["x2s"] = nc.dram_tensor("x2s", [D, NST], F32, kind="Internal").ap()
    k.dbg_out = {}
    k.d = d
    with es:
        S = Sched(nc, es)
        k.S = S
        k.ps = [es.enter_context(nc.psum_tensor(f"ps{i}", [128, 512], F32))[:, :] for i in range(8)]
        k.psk = [("ps", i) for i in range(8)]
        block = es.enter_context(nc.Block())
        emit_all(k)
        S.final_wait("sp", S.dma_events)
        S.emit(block)
    k.peak = S.peak
    return nc, k


def mm(k, out, lhsT, rhs, start, stop, R, W):
    k.S.op("pe", lambda e: e.matmul(out, lhsT=lhsT, rhs=rhs, start=start, stop=stop), R, W)


def tr(k, out, in_, ident, R, W):
    k.S.op("pe", lambda e: e.transpose(out=out, in_=in_, identity=ident), R, W)


def act(k, out, in_, func, R, W, scale=1.0, bias=0.0):
    k.S.op("act", lambda e: e.activation(out=out, in_=in_, func=func, scale=scale, bias=bias), R, W)


def tt(k, eng, out, in0, in1, op, R, W):
    k.S.op(eng, lambda e: e.tensor_tensor(out=out, in0=in0, in1=in1, op=op), R, W)


def ts(k, eng, out, in0, s1, s2, op0, op1, R, W):
    if op1 is None:
        k.S.op(eng, lambda e: e.tensor_scalar(out=out, in0=in0, scalar1=s1, scalar2=None, op0=op0), R, W)
    else:
        k.S.op(eng, lambda e: e.tensor_scalar(out=out, in0=in0, scalar1=s1, scalar2=s2, op0=op0, op1=op1), R, W)


def stt(k, out, in0, scalar, in1, op0, op1, R, W):
    k.S.op("dve", lambda e: e.scalar_tensor_tensor(out=out, in0=in0, scalar=scalar, in1=in1, op0=op0, op1=op1), R, W)


def cp(k, eng, out, in_, R, W):
    if eng == "act":
        k.S.op("act", lambda e: e.copy(out=out, in_=in_), R, W)
    else:
        k.S.op(eng, lambda e: e.tensor_copy(out=out, in_=in_), R, W)


def recip(k, out, in_, R, W):
    k.S.op("dve", lambda e: e.reciprocal(out=out, in_=in_), R, W)


def dump(k, name, buf_ap, shape, R, dt=F32):
    t = k.nc.dram_tensor("dbg_" + name, list(shape), dt, kind="ExternalOutput").ap()
    k.dbg_out[name] = t
    k.S.dma("sp", t, buf_ap, R=R, W=[("dbg", name)], key="dbg")


def load_consts(k):
    S, d = k.S, k.d
    c = K()
    k.c = c
    c.gv = S.alloc("gv", [128, 43], F32)
    c.convp = S.alloc("convp", [128, NFB, 4], F32)
    c.small = S.alloc("c_small", [128, 8], F32)
    c.flags = S.alloc("flags", [128, 2], F32)
    c.ident = S.alloc("ident", [128, 128], BF16)
    c.rot = S.alloc("rot", [128, 128], BF16)
    c.causal = S.alloc("causal", [128, 128], BF16)
    c.intra = S.alloc("intra", [128, 512], F32)
    c.qfs = S.alloc("qfs", [128, 512], F32)
    c.decay = S.alloc("decay", [128, 512], F32)
    c.ones = S.alloc("ones", [128, 128], BF16)
    S.dma("sp", c.gv.ap, d["gv"], W=c.gv.all())
    S.dma("sp", c.convp.ap, d["convp"].rearrange("p (a b) -> p a b", a=NFB), W=c.convp.all())
    S.dma("sp", c.small.ap, d["c_small"], W=c.small.all())
    S.dma("sp", c.flags.ap, d["flags"], W=c.flags.all())
    S.dma("sp", c.intra.ap, d["c_intra"], W=c.intra.all())
    S.dma("sp", c.qfs.ap, d["c_qfs"], W=c.qfs.all())
    S.dma("sp", c.decay.ap, d["c_decay"], W=c.decay.all())
    S.dma("pool", c.ident.ap, d["c_ident"], W=c.ident.all())
    S.dma("pool", c.rot.ap, d["c_rot"], W=c.rot.all())
    S.dma("pool", c.causal.ap, d["c_causal"], W=c.causal.all())
    S.op("pool", lambda e: e.memset(c.ones.ap, 1.0), W=c.ones.all())
    c.g_mix, c.g_xattn, c.g_mem, c.g_ffn, c.g_final, c.g_q, c.g_kv = 0, 8, 16, 24, 32, 40, 42


def rope_tables(k, posi_ap, posi_key, prange, col, cosb, sinb, tmp, n):
    c = k.c
    p0, p1 = prange
    a, b_, kk = tmp
    A = lambda buf: buf.ap[p0:p1, 0:n]
    invf = c.small.ap[p0:p1, col:col + 1]
    ts(k, "dve", A(a), posi_ap[p0:p1, 0:n], invf, None, ALU.mult, None, [posi_key] + c.small.all(), a.all())
    ts(k, "dve", A(b_), A(a), 1.0 / TWO_PI, MAGIC, ALU.mult, ALU.add, a.all(), b_.all())
    ts(k, "dve", A(kk), A(b_), -MAGIC, None, ALU.add, None, b_.all(), kk.all())
    stt(k, A(b_), A(kk), -C1, A(a), ALU.mult, ALU.add, kk.all() + a.all(), b_.all())
    stt(k, A(a), A(kk), -C2, A(b_), ALU.mult, ALU.add, kk.all() + b_.all(), a.all())
    ts(k, "dve", A(a), A(a), -PI_LO, PI_LO, ALU.max, ALU.min, a.all(), a.all())
    act(k, sinb.ap[p0:p1, 0:n], A(a), AF.Sin, a.all(), sinb.all())
    ts(k, "dve", A(b_), A(a), math.pi / 2, -TWO_PI, ALU.is_gt, ALU.mult, a.all(), b_.all())
    stt(k, A(kk), A(a), math.pi / 2, A(b_), ALU.add, ALU.add, a.all() + b_.all(), kk.all())
    ts(k, "dve", A(kk), A(kk), -PI_LO, PI_LO, ALU.max, ALU.min, kk.all(), kk.all())
    act(k, cosb.ap[p0:p1, 0:n], A(kk), AF.Sin, kk.all(), cosb.all())


def rms_stats(k, sq_chunks, nch, nfeat, ps_i, sd, rstd, n, sqkeys):
    c = k.c
    ps = k.ps[ps_i]
    for i in range(nch):
        mm(k, ps[:, 0:n], c.ones.ap, sq_chunks(i), i == 0, i == nch - 1, sqkeys + c.ones.all(), [k.psk[ps_i]])
    act(k, sd.ap[:, 0:n], ps[:, 0:n], AF.Sqrt, [k.psk[ps_i]], sd.all(), scale=1.0 / nfeat, bias=EPS)
    recip(k, rstd.ap[:, 0:n], sd.ap[:, 0:n], sd.all(), rstd.all())


def emit_all(k):
    load_consts(k)
    P = K()
    k.P = P
    S = k.S
    P.cqn = S.alloc("cqn", [128, 2, NST], BF16)
    P.ckvn = S.alloc("ckvn", [128, NTOK], BF16)
    P.krope = S.alloc("krope", [128, NTOK], BF16)
    P.oretT = S.alloc("oretT", [128, 4, NST], BF16)
    phase_A1(k)
    if k.stage == "A1":
        return
    P.memKT = S.alloc("memKT", [128, 8, MEM], BF16)
    P.memV = S.alloc("memV", [128, 2, D], BF16)
    phase_MKV(k)
    P.omlaT = S.alloc("omlaT", [128, 4, NST], BF16)
    P.w_out = S.alloc("w_out_bf", [128, 8, D], BF16)
    P.w_xq = S.alloc("w_xq_bf", [128, 8, D], BF16)
    d = k.d
    S.dma("pool", P.w_out.ap, d["w_out"].rearrange("(c p) n -> p c n", p=128), W=P.w_out.all(), key="w_out")
    S.dma("pool", P.w_xq.ap, d["w_xq"].rearrange("(c p) n -> p c n", p=128), W=P.w_xq.all(), key="w_xq")
    phase_A2(k)
    if k.stage == "A2":
        return
    S.release(P.cqn, P.ckvn, P.krope)
    P.w_xo = S.alloc("w_xo_bf", [128, 8, D], BF16)
    S.dma("pool", P.w_xo.ap, d["w_xo"].rearrange("(c p) n -> p c n", p=128), W=P.w_xo.all(), key="w_xo")
    P.w_ffn_out = S.alloc("w_ffn_out_bf", [128, NFB, D], BF16)
    S.dma("pool", P.w_ffn_out.ap, d["w_ffn_out"].rearrange("(c p) n -> p c n", p=128), W=P.w_ffn_out.all(), key="w_ffn_out")
    phase_A3X(k)
    if k.stage == "A3X":
        return
    S.release(P.oretT, P.omlaT, P.w_out, P.w_xq, P.w_xo, P.memKT, P.memV)
    P.w_ffn_in = S.alloc("w_ffn_in_bf", [128, 8, 2 * DFF], BF16)
    S.dma("pool", P.w_ffn_in.ap, d["w_ffn_in"].rearrange("(c p) n -> p c n", p=128), W=P.w_ffn_in.all(), key="w_ffn_in")
    phase_F(k)


def phase_A1(k):
    S, d, c, P = k.S, k.d, k.c, k.P
    ps, psk = k.ps, k.psk
    T = 512
    w_in = S.alloc("w_in_bf", [128, 8, IN_COLS], BF16)
    S.dma("pool", w_in.ap, d["w_in"].rearrange("(c p) n -> p c n", p=128), W=w_in.all(), key="w_in")
    w_krot = S.alloc("w_krot", [128, 8, 96], BF16)
    S.op("pool", lambda e: e.memset(w_krot.ap, 0.0), W=w_krot.all())
    ts(k, "pool", w_krot.ap[:, :, 64:80], w_in.ap[:, :, OFF_KR + 16:OFF_KR + 32], -1.0, None, ALU.mult, None,
       w_in.all(), w_krot.all())
    cp(k, "pool", w_krot.ap[:, :, 80:96], w_in.ap[:, :, OFF_KR:OFF_KR + 16], w_in.all(), w_krot.all())

    xt = [S.alloc(f"xt{i}", [128, 8, T], F32) for i in range(2)]
    posi = [S.alloc(f"posi{i}", [128, T], I32) for i in range(2)]
    hT = S.alloc("hT", [128, 8, T], BF16)
    sd = S.alloc("sd", [128, T], F32)
    rstd = S.alloc("rstd", [128, T], F32)
    ta, tb, tc = (S.alloc(n, [128, T], F32) for n in ("ta", "tb", "tc"))
    cos_r, sin_r = S.alloc("cos_r", [128, T], F32), S.alloc("sin_r", [128, T], F32)
    cos_m, sin_m = S.alloc("cos_m", [128, T], F32), S.alloc("sin_m", [128, T], F32)
    sql = S.alloc("sql", [128, 2, T], BF16)
    rstl = S.alloc("rstl", [128, T], F32)
    sdl = S.alloc("sdl", [128, T], F32)
    raw = [S.alloc(f"raw{i}", [128, T], BF16) for i in range(2)]
    t1 = [S.alloc(f"t1_{i}", [128, T], F32) for i in range(2)]
    t2 = [S.alloc(f"t2_{i}", [128, T], F32) for i in range(2)]
    rqT = S.alloc("rqT", [128, 4, T], BF16, nsub=4)
    rkT = S.alloc("rkT", [128, 4, T], BF16, nsub=4)
    v_tm = S.alloc("v_tm", [128, 4, 512], BF16, nsub=4)
    sg_tm = S.alloc("sg_tm", [128, 4, 512], BF16, nsub=4)
    kdec = S.alloc("kdec", [128, 512], BF16)
    PT = S.alloc("PT", [128, 512], BF16)
    qsT = S.alloc("qsT", [128, 4, 128], BF16)
    S_f = S.alloc("S_f", [128, 512], F32)
    S_b = S.alloc("S_b", [128, 512], BF16)
    sqo = S.alloc("sqo", [128, 512], F32)
    tn = S.alloc("tn", [128, 512], F32)
    og = S.alloc("og", [128, 512], BF16)
    oT = S.alloc("oT", [128, 512], BF16)
    sqT = S.alloc("sqT", [128, 512], BF16)
    S.op("pool", lambda e: e.memset(S_f.ap, 0.0), W=S_f.all())
    S.op("pool", lambda e: e.memset(S_b.ap, 0.0), W=S_b.all())

    xTv = d["xT"].rearrange("(c p) t -> p c t", p=128)
    ntiles = NTOK // T
    rr = 0
    tile_list = list(range(ntiles)) if k.a1_tiles is None else k.a1_tiles
    for tti in tile_list:
        tok0 = tti * T
        xb, pb = xt[tti % 2], posi[tti % 2]
        own_tile = tok0 + T > HALO0
        S.dma("sp", xb.ap, xTv[:, :, tok0:tok0 + T], W=xb.all(), key=f"xt{tti % 2}")
        S.dma("sp", pb.ap, d["posi"][:, tok0:tok0 + T].partition_broadcast(128), W=pb.all(), key=f"posi{tti % 2}")
        tt(k, "pool", hT.ap, xb.ap, xb.ap, ALU.mult, xb.all(), hT.all())
        rms_stats(k, lambda i: hT.ap[:, i, :], 8, D, 0, sd, rstd, T, hT.all())
        for ci in range(8):
            stt(k, hT.ap[:, ci, :], xb.ap[:, ci, :], c.gv.ap[:, c.g_mix + ci:c.g_mix + ci + 1], rstd.ap,
                ALU.mult, ALU.mult, xb.all() + rstd.all() + c.gv.all(), hT.all())
        if k.a1_level < 2:
            continue
        rope_tables(k, pb.ap, pb.k(), (0, 128), 0, cos_r, sin_r, (ta, tb, tc), T)
        rope_tables(k, pb.ap, pb.k(), (64, 96), 1, cos_m, sin_m, (ta, tb, tc), T)

        def proj(ps_i, m, wbuf, col0, n=T):
            for ci in range(8):
                mm(k, ps[ps_i][0:m, 0:n], wbuf.ap[:, ci, col0:col0 + m], hT.ap[:, ci, 0:n], ci == 0, ci == 7,
                   wbuf.all() + hT.all(), [psk[ps_i]])

        if k.a1_level < 3:
            continue
        proj(1, 128, w_in, OFF_CKV)
        act(k, sql.ap[:, 0, :], ps[1], AF.Square, [psk[1]], sql.all())
        rms_stats(k, lambda i: sql.ap[:, 0, :], 1, 128, 0, sdl, rstl, T, sql.all())
        stt(k, P.ckvn.ap[:, tok0:tok0 + T], ps[1], c.gv.ap[:, c.g_kv:c.g_kv + 1], rstl.ap, ALU.mult, ALU.mult,
            [psk[1]] + rstl.all() + c.gv.all(), P.ckvn.all())
        proj(2, 96, w_in, OFF_KR - 64)
        proj(3, 96, w_krot, 0)
        tt(k, "dve", t1[0].ap[64:96, :], ps[2][64:96, :], cos_m.ap[64:96, :], ALU.mult, [psk[2]] + cos_m.all(), t1[0].all())
        tt(k, "dve", t2[0].ap[64:96, :], ps[3][64:96, :], sin_m.ap[64:96, :], ALU.mult, [psk[3]] + sin_m.all(), t2[0].all())
        tt(k, "pool", P.krope.ap[64:96, tok0:tok0 + T], t1[0].ap[64:96, :], t2[0].ap[64:96, :], ALU.add,
           t1[0].all() + t2[0].all(), P.krope.all())
        if k.a1_level < 4:
            continue
        if own_tile:
            proj(1, 128, w_in, OFF_CQ)
            proj(2, 128, w_in, OFF_CQ + 128)
            act(k, sql.ap[:, 0, :], ps[1], AF.Square, [psk[1]], sql.all())
            act(k, sql.ap[:, 1, :], ps[2], AF.Square, [psk[2]], sql.all())
            rms_stats(k, lambda i: sql.ap[:, i, :], 2, 256, 0, sdl, rstl, T, sql.all())
            if tok0 < HALO0:
                lo, n_, s0 = HALO0 - tok0, 128, 0
            else:
                lo, n_, s0 = 0, T, tok0 - HALO0
            for j, pi in ((0, 1), (1, 2)):
                stt(k, P.cqn.ap[:, j, s0:s0 + n_], ps[pi][:, lo:lo + n_], c.gv.ap[:, c.g_q + j:c.g_q + j + 1],
                    rstl.ap[:, lo:lo + n_], ALU.mult, ALU.mult, [psk[pi]] + rstl.all() + c.gv.all(), P.cqn.all())
        if k.a1_level < 5:
            continue
        todo = [(rkT, OFF_RK)] + ([(rqT, OFF_RQ)] if own_tile else [])
        for (dst, off) in todo:
            for h in range(4):
                pi = 1 + (rr % 2)
                pj = 3 + (rr % 2)
                rb, a1, a2 = raw[rr % 2], t1[rr % 2], t2[rr % 2]
                rr += 1
                proj(pi, 128, w_in, off + h * 128)
                cp(k, "act", rb.ap, ps[pi], [psk[pi]], rb.all())
                mm(k, ps[pj], c.rot.ap, rb.ap, True, True, rb.all() + c.rot.all(), [psk[pj]])
                tt(k, "dve", a2.ap, ps[pj], sin_r.ap, ALU.mult, [psk[pj]] + sin_r.all(), a2.all())
                tt(k, "pool", a1.ap, rb.ap, cos_r.ap, ALU.mult, rb.all() + cos_r.all(), a1.all())
                tt(k, "pool", dst.ap[:, h, :], a1.ap, a2.ap, ALU.add, a1.all() + a2.all(), [dst.k(h)])
        if k.a1_level < 6:
            continue
        if own_tile:
            for h in range(4):
                pi = 1 + h % 2
                proj(pi, 128, w_in, OFF_RG + h * 128)
                act(k, sg_tm.ap[:, h, :], ps[pi], AF.Silu, [psk[pi]], [sg_tm.k(h)])
        for cc in range(4):
            ctok = tok0 + cc * 128
            own_chunk = ctok >= HALO0
            for ci in range(8):
                mm(k, ps[5], hT.ap[:, ci, cc * 128:(cc + 1) * 128], w_in.ap[:, ci, OFF_RV:OFF_RV + 512], ci == 0, ci == 7,
                   hT.all() + w_in.all(), [psk[5]])
            cp(k, "act", v_tm.ap[:, cc, :], ps[5], [psk[5]], [v_tm.k(cc)])
            if k.a1_level < 7:
                continue
            cs = slice(cc * 128, (cc + 1) * 128)
            p7b = ps[7].bitcast(BF16)
            for h in range(4):
                tr(k, p7b[:, h * 128:(h + 1) * 128], rkT.ap[:, h, cs], c.ident.ap, [rkT.k(h)] + c.ident.all(), [psk[7]])
            tt(k, "dve", kdec.ap.rearrange("p (h e) -> p h e", h=4), p7b[:, 0:512].rearrange("p (h e) -> p h e", h=4),
               bc(c.small.ap[:, 2:6].unsqueeze(2), [128, 4, 128]), ALU.mult, [psk[7]] + c.small.all(), kdec.all())
            if own_chunk and k.a1_level >= 8:
                for h in range(4):
                    mm(k, ps[4][:, h * 128:(h + 1) * 128], rkT.ap[:, h, cs], rqT.ap[:, h, cs], True, True,
                       [rkT.k(h), rqT.k(h)], [psk[4]])
                tt(k, "dve", PT.ap, ps[4], c.intra.ap, ALU.mult, [psk[4]] + c.intra.all(), PT.all())
                tt(k, "pool", qsT.ap, rqT.ap[:, :, cs], c.qfs.ap.rearrange("p (h e) -> p h e", h=4), ALU.mult,
                   rqT.all() + c.qfs.all(), qsT.all())
                for h in range(4):
                    hs = slice(h * 128, (h + 1) * 128)
                    mm(k, ps[6][:, hs], PT.ap[:, hs], v_tm.ap[:, cc, hs], True, False, PT.all() + [v_tm.k(cc)], [psk[6]])
                    mm(k, ps[6][:, hs], qsT.ap[:, h, :], S_b.ap[:, hs], False, True, qsT.all() + S_b.all(), [psk[6]])
            if ctok + 128 < NTOK:
                for h in range(4):
                    hs = slice(h * 128, (h + 1) * 128)
                    mm(k, ps[5][:, hs], kdec.ap[:, hs], v_tm.ap[:, cc, hs], True, True, kdec.all() + [v_tm.k(cc)], [psk[5]])
                tt(k, "pool", S_f.ap, S_f.ap, c.decay.ap, ALU.mult, S_f.all() + c.decay.all(), S_f.all())
                tt(k, "dve", S_f.ap, S_f.ap, ps[5], ALU.add, S_f.all() + [psk[5]], S_f.all())
                cp(k, "act", S_b.ap, S_f.ap, S_f.all(), S_b.all())
            if own_chunk and k.a1_level >= 9:
                cp(k, "act", og.ap, ps[6], [psk[6]], og.all())
                for h in range(4):
                    tr(k, p7b[:, 512 + h * 128:512 + (h + 1) * 128], og.ap[:, h * 128:(h + 1) * 128], c.ident.ap,
                       og.all() + c.ident.all(), [psk[7]])
                cp(k, "dve", oT.ap, p7b[:, 512:1024], [psk[7]], oT.all())
                act(k, sqT.ap, oT.ap, AF.Square, oT.all(), sqT.all())
                mm(k, ps[4], c.ones.ap, oT.ap, True, True, c.ones.all() + oT.all(), [psk[4]])
                mm(k, ps[3], c.ones.ap, sqT.ap, True, True, c.ones.all() + sqT.all(), [psk[3]])
                act(k, tn.ap, ps[4], AF.Copy, [psk[4]], tn.all(), scale=1.0 / 128)
                tt(k, "dve", sqo.ap, tn.ap, tn.ap, ALU.mult, tn.all(), sqo.all())
                stt(k, sqo.ap, ps[3], 1.0 / 128, sqo.ap, ALU.mult, ALU.subtract, [psk[3]] + sqo.all(), sqo.all())
                act(k, sqo.ap, sqo.ap, AF.Sqrt, sqo.all(), sqo.all(), scale=1.0, bias=EPS)
                recip(k, sqo.ap, sqo.ap, sqo.all(), sqo.all())
                tt(k, "dve", tn.ap, oT.ap, tn.ap, ALU.subtract, oT.all() + tn.all(), tn.all())
                tt(k, "pool", tn.ap, tn.ap, sqo.ap, ALU.mult, tn.all() + sqo.all(), tn.all())
                s0 = ctok - HALO0
                tt(k, "pool", P.oretT.ap[:, :, s0:s0 + 128], tn.ap.rearrange("p (h e) -> p h e", h=4), sg_tm.ap[:, :, cs],
                   ALU.mult, tn.all() + sg_tm.all(), P.oretT.all())
    if k.stage == "A1":
        mm(k, ps[0][:, 0:128], c.ones.ap, c.ones.ap, True, True, c.ones.all(), [psk[0]])
    if k.dbg and k.stage == "A1":
        dump(k, "ckvn", P.ckvn.ap, [128, NTOK], P.ckvn.all(), BF16)
        dump(k, "krope", P.krope.ap[64:96, :], [32, NTOK], P.krope.all(), BF16)
        dump(k, "cqn", P.cqn.ap, [128, 2, NST], P.cqn.all(), BF16)
        dump(k, "oretT", P.oretT.ap, [128, 4, NST], P.oretT.all(), BF16)
        dump(k, "hT", hT.ap, [128, 8, T], hT.all(), BF16)
    S.release(w_in, w_krot, *xt, *posi, hT, sd, rstd, ta, tb, tc, cos_r, sin_r, cos_m, sin_m, sql, rstl, sdl, *raw, *t1,
              *t2, rqT, rkT, v_tm, sg_tm, kdec, PT, qsT, S_f, S_b, sqo, tn, og, oT, sqT)


def phase_MKV(k):
    S, d, c, P, ps, psk = k.S, k.d, k.c, k.P, k.ps, k.psk
    w_xkv = S.alloc("w_xkv_bf", [128, 8, 2 * D], BF16)
    S.dma("pool", w_xkv.ap, d["w_xkv"].rearrange("(c p) n -> p c n", p=128), W=w_xkv.all(), key="w_xkv")
    mt = S.alloc("memT", [128, 8, MEM], F32)
    S.dma("sp", mt.ap, d["memT"].rearrange("(c p) t -> p c t", p=128), W=mt.all(), key="memT")
    hm = S.alloc("hmem", [128, 8, MEM], BF16)
    sd, rstd = S.alloc("sdm", [128, MEM], F32), S.alloc("rstdm", [128, MEM], F32)
    tt(k, "pool", hm.ap, mt.ap, mt.ap, ALU.mult, mt.all(), hm.all())
    rms_stats(k, lambda i: hm.ap[:, i, :], 8, D, 0, sd, rstd, MEM, hm.all())
    for ci in range(8):
        stt(k, hm.ap[:, ci, :], mt.ap[:, ci, :], c.gv.ap[:, c.g_mem + ci:c.g_mem + ci + 1], rstd.ap, ALU.mult, ALU.mult,
            mt.all() + rstd.all() + c.gv.all(), hm.all())
    for blk in range(8):
        pi = 1 + blk % 2
        for ci in range(8):
            mm(k, ps[pi][:, 0:MEM], w_xkv.ap[:, ci, blk * 128:(blk + 1) * 128], hm.ap[:, ci, :], ci == 0, ci == 7,
               w_xkv.all() + hm.all(), [psk[pi]])
        cp(k, "act", P.memKT.ap[:, blk, :], ps[pi][:, 0:MEM], [psk[pi]], P.memKT.all())
    n = 0
    for kb2 in range(2):
        for half in range(2):
            pi = 3 + n % 2
            n += 1
            for ci in range(8):
                mm(k, ps[pi], hm.ap[:, ci, kb2 * 128:(kb2 + 1) * 128], w_xkv.ap[:, ci, D + half * 512:D + (half + 1) * 512],
                   ci == 0, ci == 7, w_xkv.all() + hm.all(), [psk[pi]])
            cp(k, "dve", P.memV.ap[:, kb2, half * 512:(half + 1) * 512], ps[pi], [psk[pi]], P.memV.all())
    S.release(w_xkv, mt, hm, sd, rstd)


def phase_A2(k):
    S, d, c, P, ps, psk = k.S, k.d, k.c, k.P, k.ps, k.psk
    w_uq = S.alloc("w_uq_bf", [128, 2, 768], BF16)
    w_uqr = S.alloc("w_uqr_bf", [128, 2, 768], BF16)
    w_ukv = S.alloc("w_ukv_bf", [128, 1024], BF16)
    S.dma("pool", w_uq.ap, d["w_uq"].rearrange("(c p) n -> p c n", p=128), W=w_uq.all(), key="w_uq")
    S.dma("pool", w_ukv.ap, d["w_ukv"], W=w_ukv.all(), key="w_ukv")
    S.op("pool", lambda e: e.memset(w_uqr.ap, 0.0), W=w_uqr.all())
    q4 = w_uq.ap.rearrange("p c (h x) -> p c h x", h=8)
    r4 = w_uqr.ap.rearrange("p c (h x) -> p c h x", h=8)
    for ci in range(2):
        ts(k, "pool", r4[:, ci, :, 64:80], q4[:, ci, :, 80:96], -1.0, None, ALU.mult, None, w_uq.all(), w_uqr.all())
        cp(k, "pool", r4[:, ci, :, 80:96], q4[:, ci, :, 64:80], w_uq.all(), w_uqr.all())
    KT = S.alloc("KT", [128, 4, NTOK], BF16, nsub=4)
    Vc = S.alloc("Vc", [128, 32, 384], BF16)
    S.op("pool", lambda e: e.memset(Vc.ap, 1.0), W=Vc.all())
    for o in (64, 256):
        ts(k, "dve", Vc.ap[:, 0:16, o:o + 64], Vc.ap[:, 0:16, o:o + 64], c.flags.ap[:, 0:1], None, ALU.mult, None,
           Vc.all() + c.flags.all(), Vc.all())
    qT = [S.alloc(f"qT{i}", [128, 512], BF16) for i in range(4)]
    for q_ in qT:
        S.op("pool", lambda e, q_=q_: e.memset(q_.ap[96:128, :], 0.0), W=q_.all())
    S.op("pool", lambda e: e.memset(KT.ap[96:128, :, :], 0.0), W=KT.all())
    PTb = [S.alloc(f"PTb{i}", [128, 512], BF16) for i in range(3)]
    tq1, tq2 = S.alloc("tq1", [128, 512], F32), S.alloc("tq2", [128, 512], F32)
    ta, tb, tc = (S.alloc(n, [128, 512], F32) for n in ("ta2", "tb2", "tc2"))
    cos_m, sin_m = S.alloc("cos_m2", [128, 512], F32), S.alloc("sin_m2", [128, 512], F32)
    pq = S.alloc("posq", [128, 512], I32)
    rec = S.alloc("rec", [128, 512], F32)
    wk3 = w_ukv.ap.rearrange("p (h x) -> p h x", h=8)
    scale = 1.0 / math.sqrt(96.0)
    n_ev = 0
    for hh in range(2):
        heads = list(range(4 * hh, 4 * hh + 4))
        for kt in range(NTOK // 512):
            for hi, h in enumerate(heads):
                pi = n_ev % 2
                mm(k, ps[pi], w_ukv.ap[:, h * 128:h * 128 + 128], P.ckvn.ap[:, kt * 512:(kt + 1) * 512], True, True,
                   w_ukv.all() + P.ckvn.all(), [psk[pi]])
                cp(k, "act" if n_ev % 2 == 0 else "dve", KT.ap[0:64, hi, kt * 512:(kt + 1) * 512], ps[pi][0:64, :], [psk[pi]],
                   [KT.k(hi)])
                n_ev += 1
        for hi in range(4):
            cp(k, "pool", KT.ap[64:96, hi, :], P.krope.ap[64:96, :], P.krope.all(), [KT.k(hi)])
        for kb in range(NTOK // 128):
            pi = 2 + kb % 2
            mm(k, ps[pi], P.ckvn.ap[:, kb * 128:(kb + 1) * 128], w_ukv.ap[:, 512 * hh:512 * (hh + 1)], True, True,
               w_ukv.all() + P.ckvn.all(), [psk[pi]])
            src = ps[pi].rearrange("p (a m x) -> p a m x", a=2, m=2)
            dst = Vc.ap[:, kb, :].rearrange("p (a r) -> p a r", a=2)
            cp(k, "dve", dst[:, :, 0:64], src[:, :, 0, 64:128], [psk[pi]], Vc.all())
            cp(k, "dve", dst[:, :, 128:192], src[:, :, 1, 64:128], [psk[pi]], Vc.all())
        for qi in range(5):
            if qi == 0:
                s0, NQ, qblk0 = 0, 128, HALO0 // 128
            else:
                s0, NQ, qblk0 = 128 + 512 * (qi - 1), 512, NPRE // 128 + 4 * (qi - 1)
            nqb = NQ // 128
            g0 = HALO0 + s0
            S.dma("sp", pq.ap[:, 0:NQ], d["posi"][:, g0:g0 + NQ].partition_broadcast(128), W=pq.all(), key="posq")
            rope_tables(k, pq.ap, pq.k(), (64, 96), 1, cos_m, sin_m, (ta, tb, tc), NQ)
            for hi, h in enumerate(heads):
                for ci in range(2):
                    mm(k, ps[0][0:96, 0:NQ], w_uq.ap[:, ci, h * 96:(h + 1) * 96], P.cqn.ap[:, ci, s0:s0 + NQ], ci == 0, ci == 1,
                       w_uq.all() + P.cqn.all(), [psk[0]])
                for ci in range(2):
                    mm(k, ps[1][0:96, 0:NQ], w_uqr.ap[:, ci, h * 96:(h + 1) * 96], P.cqn.ap[:, ci, s0:s0 + NQ], ci == 0, ci == 1,
                       w_uqr.all() + P.cqn.all(), [psk[1]])
                cp(k, "act", qT[hi].ap[0:64, 0:NQ], ps[0][0:64, 0:NQ], [psk[0]], qT[hi].all())
                tt(k, "dve", tq1.ap[64:96, 0:NQ], ps[0][64:96, 0:NQ], cos_m.ap[64:96, 0:NQ], ALU.mult, [psk[0]] + cos_m.all(),
                   tq1.all())
                tt(k, "dve", tq2.ap[64:96, 0:NQ], ps[1][64:96, 0:NQ], sin_m.ap[64:96, 0:NQ], ALU.mult, [psk[1]] + sin_m.all(),
                   tq2.all())
                tt(k, "pool", qT[hi].ap[64:96, 0:NQ], tq1.ap[64:96, 0:NQ], tq2.ap[64:96, 0:NQ], ALU.add, tq1.all() + tq2.all(),
                   qT[hi].all())
            for hi, h in enumerate(heads):
                pair, mem = divmod(hi, 2)
                vcol0 = pair * 192 + mem * 64
                pob = 5 + hi % 2
                po = ps[pob]
                nkb = qblk0 + nqb
                pend = None

                def pv(pd, nkb=nkb, po=po, pob=pob, vcol0=vcol0, NQ=NQ):
                    kb_, qlo_, n_, pt_ = pd
                    mm(k, po[:, qlo_:NQ], Vc.ap[:, kb_, vcol0:vcol0 + 128], pt_.ap[:, 0:n_], kb_ == 0, kb_ == nkb - 1,
                       Vc.all() + pt_.all(), [psk[pob]])

                for kb in range(nkb):
                    r = kb - qblk0
                    q_lo = max(r, 0) * 128
                    n = NQ - q_lo
                    sb = 2 + kb % 3
                    pt = PTb[kb % 3]
                    mm(k, ps[sb][:, 0:n], KT.ap[:, hi, kb * 128:(kb + 1) * 128], qT[hi].ap[:, q_lo:NQ], True, True,
                       [KT.k(hi)] + qT[hi].all(), [psk[sb]])
                    act(k, pt.ap[:, 0:n], ps[sb][:, 0:n], AF.Exp, [psk[sb]], pt.all(), scale=scale)
                    if r >= 0:
                        tt(k, "pool", pt.ap[:, 0:128], pt.ap[:, 0:128], c.causal.ap, ALU.mult, pt.all() + c.causal.all(), pt.all())
                    if pend is not None:
                        pv(pend)
                    pend = (kb, q_lo, n, pt)
                pv(pend)
                if mem == 0:
                    o_rows, s_rows = slice(0, 64), slice(64, 128)
                else:
                    o_rows, s_rows = slice(64, 128), slice(0, 64)
                ts(k, "dve", rec.ap[o_rows, 0:NQ], po[s_rows, 0:NQ], 1e-30, None, ALU.add, None, [psk[pob]], rec.all())
                recip(k, rec.ap[o_rows, 0:NQ], rec.ap[o_rows, 0:NQ], rec.all(), rec.all())
                tt(k, "dve", P.omlaT.ap[o_rows, 2 * hh + pair, s0:s0 + NQ], po[o_rows, 0:NQ], rec.ap[o_rows, 0:NQ], ALU.mult,
                   [psk[pob]] + rec.all(), P.omlaT.all())
    if k.dbg and k.stage == "A2":
        dump(k, "omlaT", P.omlaT.ap, [128, 4, NST], P.omlaT.all(), BF16)
    S.release(w_uq, w_uqr, w_ukv, KT, Vc, *qT, *PTb, tq1, tq2, ta, tb, tc, cos_m, sin_m, pq, rec)


def _norm_to_hT(k, xb, hT, sd, rstd, gcol, n):
    c = k.c
    tt(k, "pool", hT.ap[:, :, 0:n], xb.ap[:, :, 0:n], xb.ap[:, :, 0:n], ALU.mult, xb.all(), hT.all())
    rms_stats(k, lambda i: hT.ap[:, i, 0:n], 8, D, 0, sd, rstd, n, hT.all())
    for ci in range(8):
        stt(k, hT.ap[:, ci, 0:n], xb.ap[:, ci, 0:n], c.gv.ap[:, gcol + ci:gcol + ci + 1], rstd.ap[:, 0:n], ALU.mult, ALU.mult,
            xb.all() + rstd.all() + c.gv.all(), hT.all())


def phase_A3X(k):
    S, d, c, P, ps, psk = k.S, k.d, k.c, k.P, k.ps, k.psk
    xb = S.alloc("xa", [128, 8, 512], F32)
    hT = S.alloc("hTa", [128, 8, 512], BF16)
    sd, rstd = S.alloc("sda", [128, 512], F32), S.alloc("rstda", [128, 512], F32)
    qxT = S.alloc("qxT", [128, 8, 512], BF16, nsub=8)
    PTx = [S.alloc(f"PTx{i}", [128, 512], BF16) for i in range(2)]
    oxT = S.alloc("oxT", [128, 8, 512], BF16)
    rec = S.alloc("recx", [128, 512], F32)
    xTv = d["xT"].rearrange("(c p) t -> p c t", p=128)
    x2v = d["x2s"].rearrange("(c p) t -> p c t", p=128)
    tiles = [(0, 128)] + [(128 + 512 * i, 512) for i in range(4)]
    for (s0, N) in tiles:
        g0 = HALO0 + s0
        S.dma("sp", xb.ap[:, :, 0:N], xTv[:, :, g0:g0 + N], W=xb.all(), key="xa")
        for cb in range(8):
            pi = 1 + cb % 2
            cs = slice(cb * 128, (cb + 1) * 128)
            for j in range(4):
                mm(k, ps[pi][:, 0:N], P.w_out.ap[:, j, cs], P.omlaT.ap[:, j, s0:s0 + N], j == 0, False,
                   P.w_out.all() + P.omlaT.all(), [psk[pi]])
            for j in range(4):
                mm(k, ps[pi][:, 0:N], P.w_out.ap[:, 4 + j, cs], P.oretT.ap[:, j, s0:s0 + N], False, j == 3,
                   P.w_out.all() + P.oretT.all(), [psk[pi]])
            tt(k, "dve", xb.ap[:, cb, 0:N], xb.ap[:, cb, 0:N], ps[pi][:, 0:N], ALU.add, xb.all() + [psk[pi]], xb.all())
        if k.dbg and k.stage == "A3X":
            dumpx(k, "x1", xb, s0, N)
        _norm_to_hT(k, xb, hT, sd, rstd, c.g_xattn, N)
        for blk in range(8):
            pi = 1 + blk % 2
            for ci in range(8):
                mm(k, ps[pi][:, 0:N], P.w_xq.ap[:, ci, blk * 128:(blk + 1) * 128], hT.ap[:, ci, 0:N], ci == 0, ci == 7,
                   P.w_xq.all() + hT.all(), [psk[pi]])
            cp(k, "act", qxT.ap[:, blk, 0:N], ps[pi][:, 0:N], [psk[pi]], [qxT.k(blk)])
        for h in range(4):
            for kb2 in range(2):
                sb = 3 + kb2
                for dc in range(2):
                    mm(k, ps[sb][:, 0:N], P.memKT.ap[:, 2 * h + dc, kb2 * 128:(kb2 + 1) * 128], qxT.ap[:, 2 * h + dc, 0:N],
                       dc == 0, dc == 1, P.memKT.all() + [qxT.k(2 * h + dc)], [psk[sb]])
                act(k, PTx[kb2].ap[:, 0:N], ps[sb][:, 0:N], AF.Exp, [psk[sb]], PTx[kb2].all(), scale=1.0 / 16.0)
            for kb2 in range(2):
                mm(k, ps[5][:, 0:N], c.ones.ap, PTx[kb2].ap[:, 0:N], kb2 == 0, kb2 == 1, c.ones.all() + PTx[kb2].all(), [psk[5]])
            recip(k, rec.ap[:, 0:N], ps[5][:, 0:N], [psk[5]], rec.all())
            for eb in range(2):
                pi = 6 + eb
                for kb2 in range(2):
                    mm(k, ps[pi][:, 0:N], P.memV.ap[:, kb2, h * 256 + eb * 128:h * 256 + (eb + 1) * 128], PTx[kb2].ap[:, 0:N],
                       kb2 == 0, kb2 == 1, P.memV.all() + PTx[kb2].all(), [psk[pi]])
                tt(k, "dve", oxT.ap[:, 2 * h + eb, 0:N], ps[pi][:, 0:N], rec.ap[:, 0:N], ALU.mult, [psk[pi]] + rec.all(), oxT.all())
        for cb in range(8):
            pi = 1 + cb % 2
            for j in range(8):
                mm(k, ps[pi][:, 0:N], P.w_xo.ap[:, j, cb * 128:(cb + 1) * 128], oxT.ap[:, j, 0:N], j == 0, j == 7,
                   P.w_xo.all() + oxT.all(), [psk[pi]])
            tt(k, "dve", xb.ap[:, cb, 0:N], xb.ap[:, cb, 0:N], ps[pi][:, 0:N], ALU.add, xb.all() + [psk[pi]], xb.all())
        S.dma("sp", x2v[:, :, s0:s0 + N], xb.ap[:, :, 0:N], R=xb.all(), W=[("x2s", s0)], key="xs")
        if k.dbg and k.stage == "A3X":
            dumpx(k, "x2", xb, s0, N)
    S.release(xb, hT, sd, rstd, qxT, *PTx, oxT, rec)


def dumpx(k, name, xb, s0, N):
    if name not in k.dbg_out:
        k.dbg_out[name] = k.nc.dram_tensor("dbg_" + name, [D, NST], F32, kind="ExternalOutput").ap()
    t = k.dbg_out[name].rearrange("(c p) t -> p c t", p=128)
    k.S.dma("sp", t[:, :, s0:s0 + N], xb.ap[:, :, 0:N], R=xb.all(), W=[("dbg", name, s0)], key="dbg")


def phase_F(k):
    S, d, c, P, ps, psk = k.S, k.d, k.c, k.P, k.ps, k.psk
    xf = S.alloc("xf", [128, 8, 512], F32)
    hT = S.alloc("hTf", [128, 8, 512], BF16)
    sd, rstd = S.alloc("sdf", [128, 512], F32), S.alloc("rstdf", [128, 512], F32)
    aT = S.alloc("aT", [128, NFB, 512], BF16, nsub=NFB)
    gsb = [S.alloc(f"gsb{i}", [128, 576], F32) for i in range(2)]
    cv = [S.alloc("cv0", [128, 512], F32)] * 2
    ost = [S.alloc("ost0", [128, 512], F32)] * 2
    ghalo = S.alloc("ghalo", [128, NFB, 64], F32)
    x2v = d["x2s"].rearrange("(c p) t -> p c t", p=128)
    outv = d["outT"].rearrange("(c p) t -> p c t", p=128)
    w1 = P.w_ffn_in
    S.dma("sp", xf.ap[:, :, 0:128], x2v[:, :, 0:128], R=[("x2s", 0)], W=xf.all(), key="xf")
    _norm_to_hT(k, xf, hT, sd, rstd, c.g_ffn, 128)
    for j in range(NFB):
        pi = 1 + j % 2
        for ci in range(8):
            mm(k, ps[pi][:, 0:128], w1.ap[:, ci, j * 128:(j + 1) * 128], hT.ap[:, ci, 0:128], ci == 0, ci == 7,
               w1.all() + hT.all(), [psk[pi]])
        ts(k, "dve", ghalo.ap[:, j, :], ps[pi][:, 64:128], c.flags.ap[:, 1:2], None, ALU.mult, None, [psk[pi]] + c.flags.all(),
           ghalo.all())
    for ti in range(4):
        s0 = 128 + 512 * ti
        S.dma("sp", xf.ap, x2v[:, :, s0:s0 + 512], R=[("x2s", s0)], W=xf.all(), key="xf")
        _norm_to_hT(k, xf, hT, sd, rstd, c.g_ffn, 512)
        for j in range(NFB):
            pg, pu = 1 + (j % 2) * 2, 2 + (j % 2) * 2
            for ci in range(8):
                mm(k, ps[pg], w1.ap[:, ci, j * 128:(j + 1) * 128], hT.ap[:, ci, :], ci == 0, ci == 7, w1.all() + hT.all(), [psk[pg]])
            for ci in range(8):
                mm(k, ps[pu], w1.ap[:, ci, DFF + j * 128:DFF + (j + 1) * 128], hT.ap[:, ci, :], ci == 0, ci == 7,
                   w1.all() + hT.all(), [psk[pu]])
            g, cvb = gsb[j % 2], cv[j % 2]
            cw = c.convp.ap
            cp(k, "act", g.ap[:, 64:576], ps[pg], [psk[pg]], g.all())
            cp(k, "pool", g.ap[:, 0:64], ghalo.ap[:, j, :], ghalo.all(), g.all())
            k.S.op("act", lambda e, g=g, cvb=cvb, j=j: e.activation(out=cvb.ap, in_=g.ap[:, 64:576], func=AF.Identity,
                                                                     scale=cw[:, j, 2:3], bias=cw[:, j, 3:4]),
                   g.all() + c.convp.all(), cvb.all())
            stt(k, cvb.ap, g.ap[:, 63:575], cw[:, j, 1:2], cvb.ap, ALU.mult, ALU.add, g.all() + cvb.all() + c.convp.all(), cvb.all())
            stt(k, cvb.ap, g.ap[:, 62:574], cw[:, j, 0:1], cvb.ap, ALU.mult, ALU.add, g.all() + cvb.all() + c.convp.all(), cvb.all())
            cp(k, "pool", ghalo.ap[:, j, :], g.ap[:, 512:576], g.all(), ghalo.all())
            act(k, cvb.ap, cvb.ap, AF.Silu, cvb.all(), cvb.all())
            tt(k, "dve", aT.ap[:, j, :], cvb.ap, ps[pu], ALU.mult, cvb.all() + [psk[pu]], [aT.k(j)])
        for cb in range(8):
            pi = 5 + cb % 2
            for j in range(NFB):
                mm(k, ps[pi], P.w_ffn_out.ap[:, j, cb * 128:(cb + 1) * 128], aT.ap[:, j, :], j == 0, j == NFB - 1,
                   P.w_ffn_out.all() + [aT.k(j)], [psk[pi]])
            tt(k, "dve", xf.ap[:, cb, :], xf.ap[:, cb, :], ps[pi], ALU.add, xf.all() + [psk[pi]], xf.all())
        tt(k, "pool", hT.ap, xf.ap, xf.ap, ALU.mult, xf.all(), hT.all())
        rms_stats(k, lambda i: hT.ap[:, i, :], 8, D, 0, sd, rstd, 512, hT.all())
        for cb in range(8):
            o = ost[cb % 2]
            stt(k, o.ap, xf.ap[:, cb, :], c.gv.ap[:, c.g_final + cb:c.g_final + cb + 1], rstd.ap, ALU.mult, ALU.mult,
                xf.all() + rstd.all() + c.gv.all(), o.all())
            S.dma("sp", outv[:, cb, 512 * ti:512 * (ti + 1)], o.ap, R=o.all(), W=[("out", ti, cb)], key=f"out{cb % 2}")
    S.release(xf, hT, sd, rstd, aT, *gsb, cv[0], ost[0], ghalo)


def _consts():
    f32 = np.float32
    H, L = 4, 128
    log_gamma = np.log(f32(1.0) - f32(2.0) ** (f32(-5.0) - np.arange(H, dtype=f32))).astype(f32)
    j = np.arange(L, dtype=f32)
    diff = j[:, None] - j[None, :]
    intra = np.where(diff[None] >= 0, np.exp(np.maximum(diff, 0.0)[None] * log_gamma[:, None, None]), 0.0).astype(f32)
    k_to_end = np.exp((L - 1 - j)[:, None] * log_gamma[None, :]).astype(f32)
    q_from_start = np.exp((j + 1)[:, None] * log_gamma[None, :]).astype(f32)
    chunk_decay = np.exp(f32(L) * log_gamma).astype(f32)
    dk = f32(128.0 ** -0.5)
    c_intra = np.zeros((128, 512), f32)
    for h in range(H):
        c_intra[:, h * 128:(h + 1) * 128] = intra[h].T * dk
    c_qfs = np.zeros((128, 512), f32)
    c_decay = np.zeros((128, 512), f32)
    for h in range(H):
        c_qfs[:, h * 128:(h + 1) * 128] = q_from_start[:, h][None, :]
        c_decay[:, h * 128:(h + 1) * 128] = chunk_decay[h]
    c_small = np.zeros((128, 8), f32)
    invf_r = (1.0 / (f32(10000.0) ** (np.arange(0, 128, 2, dtype=f32) / f32(128)))).astype(f32)
    invf_m = (1.0 / (f32(10000.0) ** (np.arange(0, 32, 2, dtype=f32) / f32(32)))).astype(f32)
    p = np.arange(128)
    c_small[:, 0] = invf_r[p % 64]
    c_small[:, 1] = invf_m[p % 16]
    c_small[:, 2:6] = k_to_end * dk
    c_ident = np.eye(128, dtype=f32)
    c_rot = np.zeros((128, 128), f32)
    for m in range(64):
        c_rot[m + 64, m] = -1.0
    for m in range(64, 128):
        c_rot[m - 64, m] = 1.0
    kk = np.arange(128)
    c_causal = (kk[None, :] >= kk[:, None]).astype(f32)
    return dict(c_small=c_small, c_ident=c_ident, c_rot=c_rot, c_causal=c_causal, c_intra=c_intra, c_qfs=c_qfs,
                c_decay=c_decay)


def make_in_maps(inputs):
    f32 = np.float32
    x = np.asarray(inputs["x"], f32)
    mem = np.asarray(inputs["mem"], f32)
    pos = np.asarray(inputs["positions"], np.int32)

    def col(g):
        g = np.asarray(g, f32).reshape(-1, 128)
        return np.ascontiguousarray(g.T)

    gv = np.concatenate([col(inputs["g_mix"][0]), col(inputs["g_xattn"][0]), col(inputs["g_mem"][0]),
                         col(inputs["g_ffn"][0]), col(inputs["g_final"]), col(inputs["g_q_lat"][0]),
                         col(inputs["g_kv_lat"][0])], axis=1)
    cw = np.asarray(inputs["conv_w"][0], f32)
    cb = np.asarray(inputs["conv_b"][0], f32)
    convp = np.zeros((128, NFB, 4), f32)
    for i in range(3):
        convp[:, :, i] = cw[i].reshape(NFB, 128).T
    convp[:, :, 3] = cb.reshape(NFB, 128).T
    shared = dict(
        w_in=np.ascontiguousarray(inputs["w_in"][0], f32), w_uq=np.ascontiguousarray(inputs["w_uq"][0], f32),
        w_ukv=np.ascontiguousarray(inputs["w_ukv"][0], f32), w_out=np.ascontiguousarray(inputs["w_out"][0], f32),
        w_xq=np.ascontiguousarray(inputs["w_xq"][0], f32), w_xkv=np.ascontiguousarray(inputs["w_xkv"][0], f32),
        w_xo=np.ascontiguousarray(inputs["w_xo"][0], f32), w_ffn_in=np.ascontiguousarray(inputs["w_ffn_in"][0], f32),
        w_ffn_out=np.ascontiguousarray(inputs["w_ffn_out"][0], f32), gv=np.ascontiguousarray(gv),
        convp=np.ascontiguousarray(convp.reshape(128, NFB * 4)), **_consts())
    maps = []
    for core in range(8):
        b, hf = core // 2, core % 2
        xT = np.zeros((D, NTOK), f32)
        pp = np.zeros((1, NTOK), np.int32)
        if hf == 0:
            xT[:, NPRE:] = x[b, :NOWN].T
            pp[0, NPRE:] = pos[b, :NOWN]
        else:
            xT[:, :] = x[b].T
            pp[0, :] = pos[b]
        flags = np.full((128, 2), float(hf), f32)
        m = dict(shared)
        m.update(xT=xT, posi=pp, memT=np.ascontiguousarray(mem[b].T), flags=flags)
        maps.append(m)
    return maps


_CACHE = {}


def kernel(**inputs):
    if "nc" not in _CACHE:
        _CACHE["nc"] = build("full")[0]
    nc = _CACHE["nc"]
    maps = make_in_maps(inputs)
    res = run_bass_kernel_spmd(nc, maps, core_ids=list(range(8)))
    out = np.zeros((NB, SEQ, D), np.float32)
    for core in range(8):
        b, hf = core // 2, core % 2
        out[b, hf * NOWN:(hf + 1) * NOWN, :] = res.results[core]["outT"].T
    return out
```

```python
import math
from contextlib import ExitStack

import numpy as np
import concourse.bass as bass
import concourse.mybir as mybir
from concourse.bass_utils import run_bass_kernel_spmd

F32 = mybir.dt.float32
BF16 = mybir.dt.bfloat16
I32 = mybir.dt.int32
U8 = mybir.dt.uint8
AF = mybir.ActivationFunctionType
ALU = mybir.AluOpType
AX = mybir.AxisListType

D = 1024
SEQ = 4096
NB = 4
EPS = 1e-6
NPRE = 2048
NOWN = 2048
NTOK = NPRE + NOWN
HALO0 = NPRE - 128
NST = NOWN + 128
IN_COLS = 2464
OFF_CQ, OFF_CKV, OFF_KR, OFF_RQ, OFF_RK, OFF_RV, OFF_RG = 0, 256, 384, 416, 928, 1440, 1952
DFF = 2816
NFB = DFF // 128
MEM = 256
TWO_PI = 2.0 * math.pi
C1 = 6.28125
C2 = TWO_PI - C1
MAGIC = 12582912.0
PI_LO = 3.1415925
ARENA_BYTES = 206 * 1024
ENGS = ("pe", "act", "dve", "pool", "sp")
NDMA_MAX = 90
DT_SIZE = {F32: 4, BF16: 2, I32: 4}


class Buf:
    def __init__(self, uid, name, off, nbytes, ap, nsub):
        self.uid, self.name, self.off, self.nbytes, self.ap, self.nsub = uid, name, off, nbytes, ap, nsub

    def k(self, i=0):
        return (self.uid, i)

    def all(self):
        return [(self.uid, i) for i in range(self.nsub)]

    def __getitem__(self, idx):
        return self.ap[idx]


class Sched:
    def __init__(self, nc, es):
        self.nc, self.es = nc, es
        self.prog = {e: [] for e in ENGS}
        self.sem = {e: es.enter_context(nc.semaphore("s_" + e)) for e in ENGS if e != "sp"}
        self.cnt = {e: 0 for e in ENGS}
        self.seen = {e: {} for e in ENGS}
        self.drained = {e: 0 for e in ENGS}
        self.needed = {e: set() for e in ENGS}
        self.lastw, self.readers = {}, {}
        self.ndma = 0
        self.dma_events = []
        self.dma_pool = [es.enter_context(nc.semaphore(f"d{i}")) for i in range(NDMA_MAX)]
        self.dma_sem = {}
        self.arena = es.enter_context(nc.sbuf_tensor("arena", [128, ARENA_BYTES], U8))
        self.free = [(0, ARENA_BYTES)]
        self.freed_events = []
        self.nbuf = 0
        self.peak = 0
        self.used = 0

    def alloc(self, name, shape, dtype, nsub=1):
        n = 1
        for s in shape[1:]:
            n *= s
        nbytes = (n * DT_SIZE[dtype] + 63) // 64 * 64
        for i, (o, sz) in enumerate(self.free):
            if sz >= nbytes:
                off = o
                if sz == nbytes:
                    self.free.pop(i)
                else:
                    self.free[i] = (o + nbytes, sz - nbytes)
                break
        else:
            raise MemoryError(f"arena full allocating {name} {nbytes}B; free={self.free}")
        ap = self.arena[0:shape[0], off:off + n * DT_SIZE[dtype]].bitcast(dtype)
        if len(shape) == 3:
            ap = ap.rearrange("p (a b) -> p a b", a=shape[1])
        elif len(shape) == 4:
            ap = ap.rearrange("p (a b c) -> p a b c", a=shape[1], b=shape[2])
        self.nbuf += 1
        b = Buf(self.nbuf, name, off, nbytes, ap, nsub)
        evs, keep = [], []
        for (fo, fn, fe) in self.freed_events:
            if fo < off + nbytes and off < fo + fn:
                evs.extend(fe)
            keep.append((fo, fn, fe))
        self.freed_events = keep
        if evs:
            for kk in b.all():
                self.readers[kk] = list(evs)
        self.used += nbytes
        self.peak = max(self.peak, self.used)
        return b

    def release(self, *bufs):
        for b in bufs:
            evs = []
            for kk in b.all():
                if kk in self.lastw:
                    evs.append(self.lastw.pop(kk))
                evs.extend(self.readers.pop(kk, []))
            best = {}
            for (s, v) in evs:
                best[s] = max(best.get(s, 0), v)
            self.freed_events.append((b.off, b.nbytes, list(best.items())))
            self.free.append((b.off, b.nbytes))
            self.free.sort()
            merged = []
            for (o, sz) in self.free:
                if merged and merged[-1][0] + merged[-1][1] == o:
                    merged[-1] = (merged[-1][0], merged[-1][1] + sz)
                else:
                    merged.append((o, sz))
            self.free = merged
            self.used -= b.nbytes

    def _deps(self, eng, R, W):
        best = {}
        for r in R:
            ev = self.lastw.get(r)
            if ev is not None:
                best[ev[0]] = max(best.get(ev[0], 0), ev[1])
        for w in W:
            ev = self.lastw.get(w)
            if ev is not None:
                best[ev[0]] = max(best.get(ev[0], 0), ev[1])
            for ev in self.readers.get(w, ()):
                best[ev[0]] = max(best.get(ev[0], 0), ev[1])
        for s, v in best.items():
            if s == eng:
                if eng == "pe":
                    continue
                if eng in ("act", "dve"):
                    if self.drained[eng] < v:
                        self.prog[eng].append(("drain",))
                        self.drained[eng] = self.cnt[eng]
                    continue
            if self.seen[eng].get(s, 0) >= v:
                continue
            self.seen[eng][s] = v
            self.prog[eng].append(("wait", s, v))
            if isinstance(s, str):
                self.needed[s].add(v)

    def _commit(self, ev, R, W):
        for w in W:
            self.lastw[w] = ev
            self.readers[w] = []
        for r in R:
            if r not in W:
                self.readers.setdefault(r, []).append(ev)

    def op(self, eng, fn, R=(), W=()):
        R, W = list(R), list(W)
        self._deps(eng, R, W)
        self.cnt[eng] += 1
        ev = (eng, self.cnt[eng])
        self.prog[eng].append(("inst", fn, self.cnt[eng]))
        self._commit(ev, R, W)
        return ev

    def dma(self, eng, out, in_, R=(), W=(), key=None):
        R, W = list(R), list(W)
        self._deps(eng, R, W)
        if key is None:
            key = f"_auto{self.ndma}"
        if key not in self.dma_sem:
            self.dma_sem[key] = [self.dma_pool[len(self.dma_sem)], 0]
        ent = self.dma_sem[key]
        sem = ent[0]
        if ent[1] > 0 and self.seen[eng].get(sem, 0) < ent[1]:
            self.seen[eng][sem] = ent[1]
            self.prog[eng].append(("wait", sem, ent[1]))
        ent[1] += 16
        self.ndma += 1
        ev = (sem, ent[1])
        self.prog[eng].append(("dma", out, in_, sem))
        self._commit(ev, R, W)
        self.dma_events.append(ev)
        return ev

    def emit(self, block):
        nc = self.nc
        S = self

        rank = {en: {q: i + 1 for i, q in enumerate(sorted(S.needed[en]))} for en in ENGS}
        S.n_inc = {en: len(rank[en]) for en in ENGS}

        def run(e, name):
            for ent in S.prog[name]:
                if ent[0] == "wait":
                    s = ent[1]
                    if isinstance(s, str):
                        e.wait_ge(S.sem[s], rank[s][ent[2]])
                    else:
                        e.wait_ge(s, ent[2])
                elif ent[0] == "drain":
                    e.drain()
                elif ent[0] == "inst":
                    ins = ent[1](e)
                    if ent[2] in rank[name]:
                        ins.then_inc(S.sem[name], 1)
                else:
                    e.dma_start(out=ent[1], in_=ent[2]).then_inc(ent[3], 16)

        @block.tensor
        def _(e):
            run(e, "pe")

        @block.scalar
        def _(e):
            run(e, "act")

        @block.vector
        def _(e):
            run(e, "dve")

        @block.gpsimd
        def _(e):
            run(e, "pool")

        @block.sync
        def _(e):
            run(e, "sp")

    def final_wait(self, eng, events):
        for (s, v) in events:
            if self.seen[eng].get(s, 0) >= v:
                continue
            self.seen[eng][s] = v
            self.prog[eng].append(("wait", s, v))


class K:
    pass


def bc(ap, shape):
    return ap.broadcast_to(shape)


def build(stage="full", dbg=False, a1_tiles=None, a1_level=9):
    nc = bass.Bass("TRN2", target_bir_lowering=False)
    es = ExitStack()
    k = K()
    k.nc, k.es, k.stage, k.dbg = nc, es, stage, dbg
    k.a1_tiles, k.a1_level = a1_tiles, a1_level
    d = {}

    def din(name, shape, dt=F32):
        d[name] = nc.dram_tensor(name, list(shape), dt, kind="ExternalInput").ap()

    din("xT", [D, NTOK]); din("posi", [1, NTOK], I32); din("memT", [D, MEM]); din("flags", [128, 2])
    din("w_in", [D, IN_COLS]); din("w_uq", [256, 768]); din("w_ukv", [128, 1024]); din("w_out", [D, D])
    din("w_xq", [D, D]); din("w_xkv", [D, 2 * D]); din("w_xo", [D, D])
    din("w_ffn_in", [D, 2 * DFF]); din("w_ffn_out", [DFF, D])
    din("gv", [128, 43]); din("convp", [128, NFB * 4]); din("c_small", [128, 8])
    din("c_ident", [128, 128]); din("c_rot", [128, 128]); din("c_causal", [128, 128])
    din("c_intra", [128, 512]); din("c_qfs", [128, 512]); din("c_decay", [128, 512])
    d["outT"] = nc.dram_tensor("outT", [D, NOWN], F32, kind="ExternalOutput").ap()
    d["x2s"] = nc.dram_tensor("x2s", [D, NST], F32, kind="Internal").ap()
    k.dbg_out = {}
    k.d = d
    with es:
        S = Sched(nc, es)
        k.S = S
        k.ps = [es.enter_context(nc.psum_tensor(f"ps{i}", [128, 512], F32))[:, :] for i in range(8)]
        k.psk = [("ps", i) for i in range(8)]
        block = es.enter_context(nc.Block())
        emit_all(k)
        S.final_wait("sp", S.dma_events)
        S.emit(block)
    k.peak = S.peak
    return nc, k


def mm(k, out, lhsT, rhs, start, stop, R, W):
    k.S.op("pe", lambda e: e.matmul(out, lhsT=lhsT, rhs=rhs, start=start, stop=stop), R, W)


def tr(k, out, in_, ident, R, W):
    k.S.op("pe", lambda e: e.transpose(out=out, in_=in_, identity=ident), R, W)


def act(k, out, in_, func, R, W, scale=1.0, bias=0.0):
    k.S.op("act", lambda e: e.activation(out=out, in_=in_, func=func, scale=scale, bias=bias), R, W)


def tt(k, eng, out, in0, in1, op, R, W):
    k.S.op(eng, lambda e: e.tensor_tensor(out=out, in0=in0, in1=in1, op=op), R, W)


def ts(k, eng, out, in0, s1, s2, op0, op1, R, W):
    if op1 is None:
        k.S.op(eng, lambda e: e.tensor_scalar(out=out, in0=in0, scalar1=s1, scalar2=None, op0=op0), R, W)
    else:
        k.S.op(eng, lambda e: e.tensor_scalar(out=out, in0=in0, scalar1=s1, scalar2=s2, op0=op0, op1=op1), R, W)


def stt(k, out, in0, scalar, in1, op0, op1, R, W):
    k.S.op("dve", lambda e: e.scalar_tensor_tensor(out=out, in0=in0, scalar=scalar, in1=in1, op0=op0, op1=op1), R, W)


def cp(k, eng, out, in_, R, W):
    if eng == "act":
        k.S.op("act", lambda e: e.copy(out=out, in_=in_), R, W)
    else:
        k.S.op(eng, lambda e: e.tensor_copy(out=out, in_=in_), R, W)


def recip(k, out, in_, R, W):
    k.S.op("dve", lambda e: e.reciprocal(out=out, in_=in_), R, W)


def dump(k, name, buf_ap, shape, R, dt=F32):
    t = k.nc.dram_tensor("dbg_" + name, list(shape), dt, kind="ExternalOutput").ap()
    k.dbg_out[name] = t
    k.S.dma("sp", t, buf_ap, R=R, W=[("dbg", name)], key="dbg")


def load_consts(k):
    S, d = k.S, k.d
    c = K()
    k.c = c
    c.gv = S.alloc("gv", [128, 43], F32)
    c.convp = S.alloc("convp", [128, NFB, 4], F32)
    c.small = S.alloc("c_small", [128, 8], F32)
    c.flags = S.alloc("flags", [128, 2], F32)
    c.ident = S.alloc("ident", [128, 128], BF16)
    c.rot = S.alloc("rot", [128, 128], BF16)
    c.causal = S.alloc("causal", [128, 128], BF16)
    c.intra = S.alloc("intra", [128, 512], F32)
    c.qfs = S.alloc("qfs", [128, 512], F32)
    c.decay = S.alloc("decay", [128, 512], F32)
    c.ones = S.alloc("ones", [128, 128], BF16)
    S.dma("sp", c.gv.ap, d["gv"], W=c.gv.all())
    S.dma("sp", c.convp.ap, d["convp"].rearrange("p (a b) -> p a b", a=NFB), W=c.convp.all())
    S.dma("sp", c.small.ap, d["c_small"], W=c.small.all())
    S.dma("sp", c.flags.ap, d["flags"], W=c.flags.all())
    S.dma("sp", c.intra.ap, d["c_intra"], W=c.intra.all())
    S.dma("sp", c.qfs.ap, d["c_qfs"], W=c.qfs.all())
    S.dma("sp", c.decay.ap, d["c_decay"], W=c.decay.all())
    S.dma("pool", c.ident.ap, d["c_ident"], W=c.ident.all())
    S.dma("pool", c.rot.ap, d["c_rot"], W=c.rot.all())
    S.dma("pool", c.causal.ap, d["c_causal"], W=c.causal.all())
    S.op("pool", lambda e: e.memset(c.ones.ap, 1.0), W=c.ones.all())
    c.g_mix, c.g_xattn, c.g_mem, c.g_ffn, c.g_final, c.g_q, c.g_kv = 0, 8, 16, 24, 32, 40, 42


def rope_tables(k, posi_ap, posi_key, prange, col, cosb, sinb, tmp, n):
    c = k.c
    p0, p1 = prange
    a, b_, kk = tmp
    A = lambda buf: buf.ap[p0:p1, 0:n]
    invf = c.small.ap[p0:p1, col:col + 1]
    ts(k, "dve", A(a), posi_ap[p0:p1, 0:n], invf, None, ALU.mult, None, [posi_key] + c.small.all(), a.all())
    ts(k, "dve", A(b_), A(a), 1.0 / TWO_PI, MAGIC, ALU.mult, ALU.add, a.all(), b_.all())
    ts(k, "dve", A(kk), A(b_), -MAGIC, None, ALU.add, None, b_.all(), kk.all())
    stt(k, A(b_), A(kk), -C1, A(a), ALU.mult, ALU.add, kk.all() + a.all(), b_.all())
    stt(k, A(a), A(kk), -C2, A(b_), ALU.mult, ALU.add, kk.all() + b_.all(), a.all())
    ts(k, "dve", A(a), A(a), -PI_LO, PI_LO, ALU.max, ALU.min, a.all(), a.all())
    act(k, sinb.ap[p0:p1, 0:n], A(a), AF.Sin, a.all(), sinb.all())
    ts(k, "dve", A(b_), A(a), math.pi / 2, -TWO_PI, ALU.is_gt, ALU.mult, a.all(), b_.all())
    stt(k, A(kk), A(a), math.pi / 2, A(b_), ALU.add, ALU.add, a.all() + b_.all(), kk.all())
    ts(k, "dve", A(kk), A(kk), -PI_LO, PI_LO, ALU.max, ALU.min, kk.all(), kk.all())
    act(k, cosb.ap[p0:p1, 0:n], A(kk), AF.Sin, kk.all(), cosb.all())


def rms_stats(k, sq_chunks, nch, nfeat, ps_i, sd, rstd, n, sqkeys):
    c = k.c
    ps = k.ps[ps_i]
    for i in range(nch):
        mm(k, ps[:, 0:n], c.ones.ap, sq_chunks(i), i == 0, i == nch - 1, sqkeys + c.ones.all(), [k.psk[ps_i]])
    act(k, sd.ap[:, 0:n], ps[:, 0:n], AF.Sqrt, [k.psk[ps_i]], sd.all(), scale=1.0 / nfeat, bias=EPS)
    recip(k, rstd.ap[:, 0:n], sd.ap[:, 0:n], sd.all(), rstd.all())


def emit_all(k):
    load_consts(k)
    P = K()
    k.P = P
    S = k.S
    P.cqn = S.alloc("cqn", [128, 2, NST], BF16)
    P.ckvn = S.alloc("ckvn", [128, NTOK], BF16)
    P.krope = S.alloc("krope", [128, NTOK], BF16)
    P.oretT = S.alloc("oretT", [128, 4, NST], BF16)
    phase_A1(k)
    if k.stage == "A1":
        return
    P.memKT = S.alloc("memKT", [128, 8, MEM], BF16)
    P.memV = S.alloc("memV", [128, 2, D], BF16)
    phase_MKV(k)
    P.omlaT = S.alloc("omlaT", [128, 4, NST], BF16)
    P.w_out = S.alloc("w_out_bf", [128, 8, D], BF16)
    P.w_xq = S.alloc("w_xq_bf", [128, 8, D], BF16)
    d = k.d
    S.dma("pool", P.w_out.ap, d["w_out"].rearrange("(c p) n -> p c n", p=128), W=P.w_out.all(), key="w_out")
    S.dma("pool", P.w_xq.ap, d["w_xq"].rearrange("(c p) n -> p c n", p=128), W=P.w_xq.all(), key="w_xq")
    phase_A2(k)
    if k.stage == "A2":
        return
    S.release(P.cqn, P.ckvn, P.krope)
    P.w_xo = S.alloc("w_xo_bf", [128, 8, D], BF16)
    S.dma("pool", P.w_xo.ap, d["w_xo"].rearrange("(c p) n -> p c n", p=128), W=P.w_xo.all(), key="w_xo")
    P.w_ffn_out = S.alloc("w_ffn_out_bf", [128, NFB, D], BF16)
    S.dma("pool", P.w_ffn_out.ap, d["w_ffn_out"].rearrange("(c p) n -> p c n", p=128), W=P.w_ffn_out.all(), key="w_ffn_out")
    phase_A3X(k)
    if k.stage == "A3X":
        return
    S.release(P.oretT, P.omlaT, P.w_out, P.w_xq, P.w_xo, P.memKT, P.memV)
    P.w_ffn_in = S.alloc("w_ffn_in_bf", [128, 8, 2 * DFF], BF16, nsub=4)
    wv = d["w_ffn_in"].rearrange("(c p) n -> p c n", p=128)
    HB = 11 * 128
    for sub, (a, b) in enumerate(((0, HB), (DFF, DFF + HB), (HB, DFF), (DFF + HB, 2 * DFF))):
        S.dma("pool", P.w_ffn_in.ap[:, :, a:b], wv[:, :, a:b], W=[P.w_ffn_in.k(sub)], key=f"w_ffn_in{sub}")
    phase_F(k)


def phase_A1(k):
    S, d, c, P = k.S, k.d, k.c, k.P
    ps, psk = k.ps, k.psk
    T = 512
    w_in = S.alloc("w_in_bf", [128, 8, IN_COLS], BF16)
    S.dma("pool", w_in.ap, d["w_in"].rearrange("(c p) n -> p c n", p=128), W=w_in.all(), key="w_in")
    w_krot = S.alloc("w_krot", [128, 8, 96], BF16)
    S.op("pool", lambda e: e.memset(w_krot.ap, 0.0), W=w_krot.all())
    ts(k, "pool", w_krot.ap[:, :, 64:80], w_in.ap[:, :, OFF_KR + 16:OFF_KR + 32], -1.0, None, ALU.mult, None,
       w_in.all(), w_krot.all())
    cp(k, "pool", w_krot.ap[:, :, 80:96], w_in.ap[:, :, OFF_KR:OFF_KR + 16], w_in.all(), w_krot.all())

    xt = [S.alloc(f"xt{i}", [128, 8, T], F32) for i in range(2)]
    posi = [S.alloc(f"posi{i}", [128, T], I32) for i in range(2)]
    hT = S.alloc("hT", [128, 8, T], BF16)
    sd = S.alloc("sd", [128, T], F32)
    rstd = S.alloc("rstd", [128, T], F32)
    ta, tb, tc = (S.alloc(n, [128, T], F32) for n in ("ta", "tb", "tc"))
    cos_r, sin_r = S.alloc("cos_r", [128, T], F32), S.alloc("sin_r", [128, T], F32)
    cos_m, sin_m = S.alloc("cos_m", [128, T], F32), S.alloc("sin_m", [128, T], F32)
    sql = S.alloc("sql", [128, 2, T], BF16)
    rstl = S.alloc("rstl", [128, T], F32)
    sdl = S.alloc("sdl", [128, T], F32)
    raw = [S.alloc(f"raw{i}", [128, T], BF16) for i in range(2)]
    t1 = [S.alloc(f"t1_{i}", [128, T], F32) for i in range(2)]
    t2 = [S.alloc(f"t2_{i}", [128, T], F32) for i in range(2)]
    rqT = S.alloc("rqT", [128, 4, T], BF16, nsub=4)
    rkT = S.alloc("rkT", [128, 4, T], BF16, nsub=4)
    v_tm = S.alloc("v_tm", [128, 4, 512], BF16, nsub=4)
    sg_tm = S.alloc("sg_tm", [128, 4, 512], BF16, nsub=4)
    kdec = S.alloc("kdec", [128, 512], BF16)
    PT = S.alloc("PT", [128, 512], BF16)
    qsT = S.alloc("qsT", [128, 4, 128], BF16)
    S_f = S.alloc("S_f", [128, 512], F32)
    S_b = S.alloc("S_b", [128, 512], BF16)
    sqo = S.alloc("sqo", [128, 512], F32)
    tn = S.alloc("tn", [128, 512], F32)
    og = S.alloc("og", [128, 512], BF16)
    oT = S.alloc("oT", [128, 512], BF16)
    sqT = S.alloc("sqT", [128, 512], BF16)
    S.op("pool", lambda e: e.memset(S_f.ap, 0.0), W=S_f.all())
    S.op("pool", lambda e: e.memset(S_b.ap, 0.0), W=S_b.all())

    xTv = d["xT"].rearrange("(c p) t -> p c t", p=128)
    ntiles = NTOK // T
    rr = 0
    tile_list = list(range(ntiles)) if k.a1_tiles is None else k.a1_tiles
    for tti in tile_list:
        tok0 = tti * T
        xb, pb = xt[tti % 2], posi[tti % 2]
        own_tile = tok0 + T > HALO0
        S.dma("sp", xb.ap, xTv[:, :, tok0:tok0 + T], W=xb.all(), key=f"xt{tti % 2}")
        S.dma("sp", pb.ap, d["posi"][:, tok0:tok0 + T].partition_broadcast(128), W=pb.all(), key=f"posi{tti % 2}")
        tt(k, "pool", hT.ap, xb.ap, xb.ap, ALU.mult, xb.all(), hT.all())
        rms_stats(k, lambda i: hT.ap[:, i, :], 8, D, 0, sd, rstd, T, hT.all())
        for ci in range(8):
            stt(k, hT.ap[:, ci, :], xb.ap[:, ci, :], c.gv.ap[:, c.g_mix + ci:c.g_mix + ci + 1], rstd.ap,
                ALU.mult, ALU.mult, xb.all() + rstd.all() + c.gv.all(), hT.all())
        if k.a1_level < 2:
            continue
        rope_tables(k, pb.ap, pb.k(), (0, 128), 0, cos_r, sin_r, (ta, tb, tc), T)
        rope_tables(k, pb.ap, pb.k(), (64, 96), 1, cos_m, sin_m, (ta, tb, tc), T)

        def proj(ps_i, m, wbuf, col0, n=T):
            for ci in range(8):
                mm(k, ps[ps_i][0:m, 0:n], wbuf.ap[:, ci, col0:col0 + m], hT.ap[:, ci, 0:n], ci == 0, ci == 7,
                   wbuf.all() + hT.all(), [psk[ps_i]])

        if k.a1_level < 3:
            continue
        proj(1, 128, w_in, OFF_CKV)
        act(k, sql.ap[:, 0, :], ps[1], AF.Square, [psk[1]], sql.all())
        rms_stats(k, lambda i: sql.ap[:, 0, :], 1, 128, 0, sdl, rstl, T, sql.all())
        stt(k, P.ckvn.ap[:, tok0:tok0 + T], ps[1], c.gv.ap[:, c.g_kv:c.g_kv + 1], rstl.ap, ALU.mult, ALU.mult,
            [psk[1]] + rstl.all() + c.gv.all(), P.ckvn.all())
        proj(2, 96, w_in, OFF_KR - 64)
        proj(3, 96, w_krot, 0)
        tt(k, "dve", t1[0].ap[64:96, :], ps[2][64:96, :], cos_m.ap[64:96, :], ALU.mult, [psk[2]] + cos_m.all(), t1[0].all())
        tt(k, "dve", t2[0].ap[64:96, :], ps[3][64:96, :], sin_m.ap[64:96, :], ALU.mult, [psk[3]] + sin_m.all(), t2[0].all())
        tt(k, "pool", P.krope.ap[64:96, tok0:tok0 + T], t1[0].ap[64:96, :], t2[0].ap[64:96, :], ALU.add,
           t1[0].all() + t2[0].all(), P.krope.all())
        if k.a1_level < 4:
            continue
        if own_tile:
            proj(1, 128, w_in, OFF_CQ)
            proj(2, 128, w_in, OFF_CQ + 128)
            act(k, sql.ap[:, 0, :], ps[1], AF.Square, [psk[1]], sql.all())
            act(k, sql.ap[:, 1, :], ps[2], AF.Square, [psk[2]], sql.all())
            rms_stats(k, lambda i: sql.ap[:, i, :], 2, 256, 0, sdl, rstl, T, sql.all())
            if tok0 < HALO0:
                lo, n_, s0 = HALO0 - tok0, 128, 0
            else:
                lo, n_, s0 = 0, T, tok0 - HALO0
            for j, pi in ((0, 1), (1, 2)):
                stt(k, P.cqn.ap[:, j, s0:s0 + n_], ps[pi][:, lo:lo + n_], c.gv.ap[:, c.g_q + j:c.g_q + j + 1],
                    rstl.ap[:, lo:lo + n_], ALU.mult, ALU.mult, [psk[pi]] + rstl.all() + c.gv.all(), P.cqn.all())
        if k.a1_level < 5:
            continue
        todo = [(rkT, OFF_RK)] + ([(rqT, OFF_RQ)] if own_tile else [])
        items = [(dst, off, h) for (dst, off) in todo for h in range(4)]

        def rope_tail(it, slot):
            dst, off, h = it
            pi, pj = 1 + slot, 3 + slot
            rb, a1, a2 = raw[slot], t1[slot], t2[slot]
            mm(k, ps[pj], c.rot.ap, rb.ap, True, True, rb.all() + c.rot.all(), [psk[pj]])
            tt(k, "dve", a2.ap, ps[pj], sin_r.ap, ALU.mult, [psk[pj]] + sin_r.all(), a2.all())
            tt(k, "pool", a1.ap, rb.ap, cos_r.ap, ALU.mult, rb.all() + cos_r.all(), a1.all())
            tt(k, "pool", dst.ap[:, h, :], a1.ap, a2.ap, ALU.add, a1.all() + a2.all(), [dst.k(h)])

        prev = None
        for n_, it in enumerate(items):
            slot = n_ % 2
            proj(1 + slot, 128, w_in, it[1] + it[2] * 128)
            cp(k, "act", raw[slot].ap, ps[1 + slot], [psk[1 + slot]], raw[slot].all())
            if prev is not None:
                rope_tail(*prev)
            prev = (it, slot)
        rope_tail(*prev)
        if k.a1_level < 6:
            continue
        if own_tile:
            for h in range(4):
                pi = 1 + h % 2
                proj(pi, 128, w_in, OFF_RG + h * 128)
                act(k, sg_tm.ap[:, h, :], ps[pi], AF.Silu, [psk[pi]], [sg_tm.k(h)])
        for cc in range(4):
            ctok = tok0 + cc * 128
            own_chunk = ctok >= HALO0
            for ci in range(8):
                mm(k, ps[5], hT.ap[:, ci, cc * 128:(cc + 1) * 128], w_in.ap[:, ci, OFF_RV:OFF_RV + 512], ci == 0, ci == 7,
                   hT.all() + w_in.all(), [psk[5]])
            cp(k, "act", v_tm.ap[:, cc, :], ps[5], [psk[5]], [v_tm.k(cc)])
            if k.a1_level < 7:
                continue
            cs = slice(cc * 128, (cc + 1) * 128)
            p7b = ps[7].bitcast(BF16)
            for h in range(4):
                tr(k, p7b[:, h * 128:(h + 1) * 128], rkT.ap[:, h, cs], c.ident.ap, [rkT.k(h)] + c.ident.all(), [psk[7]])
            tt(k, "dve", kdec.ap.rearrange("p (h e) -> p h e", h=4), p7b[:, 0:512].rearrange("p (h e) -> p h e", h=4),
               bc(c.small.ap[:, 2:6].unsqueeze(2), [128, 4, 128]), ALU.mult, [psk[7]] + c.small.all(), kdec.all())
            if own_chunk and k.a1_level >= 8:
                for h in range(4):
                    mm(k, ps[4][:, h * 128:(h + 1) * 128], rkT.ap[:, h, cs], rqT.ap[:, h, cs], True, True,
                       [rkT.k(h), rqT.k(h)], [psk[4]])
                tt(k, "dve", PT.ap, ps[4], c.intra.ap, ALU.mult, [psk[4]] + c.intra.all(), PT.all())
                tt(k, "pool", qsT.ap, rqT.ap[:, :, cs], c.qfs.ap.rearrange("p (h e) -> p h e", h=4), ALU.mult,
                   rqT.all() + c.qfs.all(), qsT.all())
                for h in range(4):
                    hs = slice(h * 128, (h + 1) * 128)
                    mm(k, ps[6][:, hs], PT.ap[:, hs], v_tm.ap[:, cc, hs], True, False, PT.all() + [v_tm.k(cc)], [psk[6]])
                    mm(k, ps[6][:, hs], qsT.ap[:, h, :], S_b.ap[:, hs], False, True, qsT.all() + S_b.all(), [psk[6]])
            if ctok + 128 < NTOK:
                for h in range(4):
                    hs = slice(h * 128, (h + 1) * 128)
                    mm(k, ps[5][:, hs], kdec.ap[:, hs], v_tm.ap[:, cc, hs], True, True, kdec.all() + [v_tm.k(cc)], [psk[5]])
                tt(k, "pool", S_f.ap, S_f.ap, c.decay.ap, ALU.mult, S_f.all() + c.decay.all(), S_f.all())
                tt(k, "dve", S_f.ap, S_f.ap, ps[5], ALU.add, S_f.all() + [psk[5]], S_f.all())
                cp(k, "act", S_b.ap, S_f.ap, S_f.all(), S_b.all())
            if own_chunk and k.a1_level >= 9:
                cp(k, "act", og.ap, ps[6], [psk[6]], og.all())
                for h in range(4):
                    tr(k, p7b[:, 512 + h * 128:512 + (h + 1) * 128], og.ap[:, h * 128:(h + 1) * 128], c.ident.ap,
                       og.all() + c.ident.all(), [psk[7]])
                cp(k, "dve", oT.ap, p7b[:, 512:1024], [psk[7]], oT.all())
                act(k, sqT.ap, oT.ap, AF.Square, oT.all(), sqT.all())
                mm(k, ps[4], c.ones.ap, oT.ap, True, True, c.ones.all() + oT.all(), [psk[4]])
                mm(k, ps[3], c.ones.ap, sqT.ap, True, True, c.ones.all() + sqT.all(), [psk[3]])
                act(k, tn.ap, ps[4], AF.Copy, [psk[4]], tn.all(), scale=1.0 / 128)
                tt(k, "dve", sqo.ap, tn.ap, tn.ap, ALU.mult, tn.all(), sqo.all())
                stt(k, sqo.ap, ps[3], 1.0 / 128, sqo.ap, ALU.mult, ALU.subtract, [psk[3]] + sqo.all(), sqo.all())
                act(k, sqo.ap, sqo.ap, AF.Sqrt, sqo.all(), sqo.all(), scale=1.0, bias=EPS)
                recip(k, sqo.ap, sqo.ap, sqo.all(), sqo.all())
                tt(k, "dve", tn.ap, oT.ap, tn.ap, ALU.subtract, oT.all() + tn.all(), tn.all())
                tt(k, "pool", tn.ap, tn.ap, sqo.ap, ALU.mult, tn.all() + sqo.all(), tn.all())
                s0 = ctok - HALO0
                tt(k, "pool", P.oretT.ap[:, :, s0:s0 + 128], tn.ap.rearrange("p (h e) -> p h e", h=4), sg_tm.ap[:, :, cs],
                   ALU.mult, tn.all() + sg_tm.all(), P.oretT.all())
    if k.stage == "A1":
        mm(k, ps[0][:, 0:128], c.ones.ap, c.ones.ap, True, True, c.ones.all(), [psk[0]])
    if k.dbg and k.stage == "A1":
        dump(k, "ckvn", P.ckvn.ap, [128, NTOK], P.ckvn.all(), BF16)
        dump(k, "krope", P.krope.ap[64:96, :], [32, NTOK], P.krope.all(), BF16)
        dump(k, "cqn", P.cqn.ap, [128, 2, NST], P.cqn.all(), BF16)
        dump(k, "oretT", P.oretT.ap, [128, 4, NST], P.oretT.all(), BF16)
        dump(k, "hT", hT.ap, [128, 8, T], hT.all(), BF16)
    S.release(w_in, w_krot, *xt, *posi, hT, sd, rstd, ta, tb, tc, cos_r, sin_r, cos_m, sin_m, sql, rstl, sdl, *raw, *t1,
              *t2, rqT, rkT, v_tm, sg_tm, kdec, PT, qsT, S_f, S_b, sqo, tn, og, oT, sqT)


def phase_MKV(k):
    S, d, c, P, ps, psk = k.S, k.d, k.c, k.P, k.ps, k.psk
    w_xkv = S.alloc("w_xkv_bf", [128, 8, 2 * D], BF16)
    S.dma("pool", w_xkv.ap, d["w_xkv"].rearrange("(c p) n -> p c n", p=128), W=w_xkv.all(), key="w_xkv")
    mt = S.alloc("memT", [128, 8, MEM], F32)
    S.dma("sp", mt.ap, d["memT"].rearrange("(c p) t -> p c t", p=128), W=mt.all(), key="memT")
    hm = S.alloc("hmem", [128, 8, MEM], BF16)
    sd, rstd = S.alloc("sdm", [128, MEM], F32), S.alloc("rstdm", [128, MEM], F32)
    tt(k, "pool", hm.ap, mt.ap, mt.ap, ALU.mult, mt.all(), hm.all())
    rms_stats(k, lambda i: hm.ap[:, i, :], 8, D, 0, sd, rstd, MEM, hm.all())
    for ci in range(8):
        stt(k, hm.ap[:, ci, :], mt.ap[:, ci, :], c.gv.ap[:, c.g_mem + ci:c.g_mem + ci + 1], rstd.ap, ALU.mult, ALU.mult,
            mt.all() + rstd.all() + c.gv.all(), hm.all())
    for blk in range(8):
        pi = 1 + blk % 2
        for ci in range(8):
            mm(k, ps[pi][:, 0:MEM], w_xkv.ap[:, ci, blk * 128:(blk + 1) * 128], hm.ap[:, ci, :], ci == 0, ci == 7,
               w_xkv.all() + hm.all(), [psk[pi]])
        cp(k, "act", P.memKT.ap[:, blk, :], ps[pi][:, 0:MEM], [psk[pi]], P.memKT.all())
    n = 0
    for kb2 in range(2):
        for half in range(2):
            pi = 3 + n % 2
            n += 1
            for ci in range(8):
                mm(k, ps[pi], hm.ap[:, ci, kb2 * 128:(kb2 + 1) * 128], w_xkv.ap[:, ci, D + half * 512:D + (half + 1) * 512],
                   ci == 0, ci == 7, w_xkv.all() + hm.all(), [psk[pi]])
            cp(k, "dve", P.memV.ap[:, kb2, half * 512:(half + 1) * 512], ps[pi], [psk[pi]], P.memV.all())
    S.release(w_xkv, mt, hm, sd, rstd)


def phase_A2(k):
    S, d, c, P, ps, psk = k.S, k.d, k.c, k.P, k.ps, k.psk
    w_uq = S.alloc("w_uq_bf", [128, 2, 768], BF16)
    w_uqr = S.alloc("w_uqr_bf", [128, 2, 768], BF16)
    w_ukv = S.alloc("w_ukv_bf", [128, 1024], BF16)
    S.dma("pool", w_uq.ap, d["w_uq"].rearrange("(c p) n -> p c n", p=128), W=w_uq.all(), key="w_uq")
    S.dma("pool", w_ukv.ap, d["w_ukv"], W=w_ukv.all(), key="w_ukv")
    S.op("pool", lambda e: e.memset(w_uqr.ap, 0.0), W=w_uqr.all())
    q4 = w_uq.ap.rearrange("p c (h x) -> p c h x", h=8)
    r4 = w_uqr.ap.rearrange("p c (h x) -> p c h x", h=8)
    for ci in range(2):
        ts(k, "pool", r4[:, ci, :, 64:80], q4[:, ci, :, 80:96], -1.0, None, ALU.mult, None, w_uq.all(), w_uqr.all())
        cp(k, "pool", r4[:, ci, :, 80:96], q4[:, ci, :, 64:80], w_uq.all(), w_uqr.all())
    KT = S.alloc("KT", [128, 4, NTOK], BF16, nsub=4)
    Vc = S.alloc("Vc", [128, 32, 384], BF16)
    S.op("pool", lambda e: e.memset(Vc.ap, 1.0), W=Vc.all())
    for o in (64, 256):
        ts(k, "dve", Vc.ap[:, 0:16, o:o + 64], Vc.ap[:, 0:16, o:o + 64], c.flags.ap[:, 0:1], None, ALU.mult, None,
           Vc.all() + c.flags.all(), Vc.all())
    qT = [S.alloc(f"qT{i}", [128, 512], BF16) for i in range(4)]
    for q_ in qT:
        S.op("pool", lambda e, q_=q_: e.memset(q_.ap[96:128, :], 0.0), W=q_.all())
    S.op("pool", lambda e: e.memset(KT.ap[96:128, :, :], 0.0), W=KT.all())
    PTb = [S.alloc(f"PTb{i}", [128, 512], BF16) for i in range(3)]
    tq1, tq2 = S.alloc("tq1", [128, 512], F32), S.alloc("tq2", [128, 512], F32)
    ta, tb, tc = (S.alloc(n, [128, 512], F32) for n in ("ta2", "tb2", "tc2"))
    cos_m, sin_m = S.alloc("cos_m2", [128, 512], F32), S.alloc("sin_m2", [128, 512], F32)
    pq = S.alloc("posq", [128, 512], I32)
    rec = S.alloc("rec", [128, 512], F32)
    wk3 = w_ukv.ap.rearrange("p (h x) -> p h x", h=8)
    scale = 1.0 / math.sqrt(96.0)
    n_ev = 0
    for hh in range(2):
        heads = list(range(4 * hh, 4 * hh + 4))
        for kt in range(NTOK // 512):
            for hi, h in enumerate(heads):
                pi = n_ev % 2
                mm(k, ps[pi], w_ukv.ap[:, h * 128:h * 128 + 128], P.ckvn.ap[:, kt * 512:(kt + 1) * 512], True, True,
                   w_ukv.all() + P.ckvn.all(), [psk[pi]])
                cp(k, "act" if n_ev % 2 == 0 else "dve", KT.ap[0:64, hi, kt * 512:(kt + 1) * 512], ps[pi][0:64, :], [psk[pi]],
                   [KT.k(hi)])
                n_ev += 1
        for hi in range(4):
            cp(k, "pool", KT.ap[64:96, hi, :], P.krope.ap[64:96, :], P.krope.all(), [KT.k(hi)])
        for kb in range(NTOK // 128):
            pi = 2 + kb % 2
            mm(k, ps[pi], P.ckvn.ap[:, kb * 128:(kb + 1) * 128], w_ukv.ap[:, 512 * hh:512 * (hh + 1)], True, True,
               w_ukv.all() + P.ckvn.all(), [psk[pi]])
            src = ps[pi].rearrange("p (a m x) -> p a m x", a=2, m=2)
            dst = Vc.ap[:, kb, :].rearrange("p (a r) -> p a r", a=2)
            cp(k, "dve", dst[:, :, 0:64], src[:, :, 0, 64:128], [psk[pi]], Vc.all())
            cp(k, "dve", dst[:, :, 128:192], src[:, :, 1, 64:128], [psk[pi]], Vc.all())
        for qi in range(5):
            if qi == 0:
                s0, NQ, qblk0 = 0, 128, HALO0 // 128
            else:
                s0, NQ, qblk0 = 128 + 512 * (qi - 1), 512, NPRE // 128 + 4 * (qi - 1)
            nqb = NQ // 128
            g0 = HALO0 + s0
            S.dma("sp", pq.ap[:, 0:NQ], d["posi"][:, g0:g0 + NQ].partition_broadcast(128), W=pq.all(), key="posq")
            rope_tables(k, pq.ap, pq.k(), (64, 96), 1, cos_m, sin_m, (ta, tb, tc), NQ)
            for hi, h in enumerate(heads):
                for ci in range(2):
                    mm(k, ps[0][0:96, 0:NQ], w_uq.ap[:, ci, h * 96:(h + 1) * 96], P.cqn.ap[:, ci, s0:s0 + NQ], ci == 0, ci == 1,
                       w_uq.all() + P.cqn.all(), [psk[0]])
                for ci in range(2):
                    mm(k, ps[1][0:96, 0:NQ], w_uqr.ap[:, ci, h * 96:(h + 1) * 96], P.cqn.ap[:, ci, s0:s0 + NQ], ci == 0, ci == 1,
                       w_uqr.all() + P.cqn.all(), [psk[1]])
                cp(k, "act", qT[hi].ap[0:64, 0:NQ], ps[0][0:64, 0:NQ], [psk[0]], qT[hi].all())
                tt(k, "dve", tq1.ap[64:96, 0:NQ], ps[0][64:96, 0:NQ], cos_m.ap[64:96, 0:NQ], ALU.mult, [psk[0]] + cos_m.all(),
                   tq1.all())
                tt(k, "dve", tq2.ap[64:96, 0:NQ], ps[1][64:96, 0:NQ], sin_m.ap[64:96, 0:NQ], ALU.mult, [psk[1]] + sin_m.all(),
                   tq2.all())
                tt(k, "pool", qT[hi].ap[64:96, 0:NQ], tq1.ap[64:96, 0:NQ], tq2.ap[64:96, 0:NQ], ALU.add, tq1.all() + tq2.all(),
                   qT[hi].all())
            for hi, h in enumerate(heads):
                pair, mem = divmod(hi, 2)
                vcol0 = pair * 192 + mem * 64
                pob = 5 + hi % 2
                po = ps[pob]
                nkb = qblk0 + nqb
                pend = None

                def pv(pd, nkb=nkb, po=po, pob=pob, vcol0=vcol0, NQ=NQ):
                    kb_, qlo_, n_, pt_ = pd
                    mm(k, po[:, qlo_:NQ], Vc.ap[:, kb_, vcol0:vcol0 + 128], pt_.ap[:, 0:n_], kb_ == 0, kb_ == nkb - 1,
                       Vc.all() + pt_.all(), [psk[pob]])

                for kb in range(nkb):
                    r = kb - qblk0
                    q_lo = max(r, 0) * 128
                    n = NQ - q_lo
                    sb = 2 + kb % 3
                    pt = PTb[kb % 3]
                    mm(k, ps[sb][:, 0:n], KT.ap[:, hi, kb * 128:(kb + 1) * 128], qT[hi].ap[:, q_lo:NQ], True, True,
                       [KT.k(hi)] + qT[hi].all(), [psk[sb]])
                    act(k, pt.ap[:, 0:n], ps[sb][:, 0:n], AF.Exp, [psk[sb]], pt.all(), scale=scale)
                    if r >= 0:
                        tt(k, "pool", pt.ap[:, 0:128], pt.ap[:, 0:128], c.causal.ap, ALU.mult, pt.all() + c.causal.all(), pt.all())
                    if pend is not None:
                        pv(pend)
                    pend = (kb, q_lo, n, pt)
                pv(pend)
                if mem == 0:
                    o_rows, s_rows = slice(0, 64), slice(64, 128)
                else:
                    o_rows, s_rows = slice(64, 128), slice(0, 64)
                ts(k, "dve", rec.ap[o_rows, 0:NQ], po[s_rows, 0:NQ], 1e-30, None, ALU.add, None, [psk[pob]], rec.all())
                recip(k, rec.ap[o_rows, 0:NQ], rec.ap[o_rows, 0:NQ], rec.all(), rec.all())
                tt(k, "dve", P.omlaT.ap[o_rows, 2 * hh + pair, s0:s0 + NQ], po[o_rows, 0:NQ], rec.ap[o_rows, 0:NQ], ALU.mult,
                   [psk[pob]] + rec.all(), P.omlaT.all())
    if k.dbg and k.stage == "A2":
        dump(k, "omlaT", P.omlaT.ap, [128, 4, NST], P.omlaT.all(), BF16)
    S.release(w_uq, w_uqr, w_ukv, KT, Vc, *qT, *PTb, tq1, tq2, ta, tb, tc, cos_m, sin_m, pq, rec)


def _norm_to_hT(k, xb, hT, sd, rstd, gcol, n):
    c = k.c
    tt(k, "pool", hT.ap[:, :, 0:n], xb.ap[:, :, 0:n], xb.ap[:, :, 0:n], ALU.mult, xb.all(), hT.all())
    rms_stats(k, lambda i: hT.ap[:, i, 0:n], 8, D, 0, sd, rstd, n, hT.all())
    for ci in range(8):
        stt(k, hT.ap[:, ci, 0:n], xb.ap[:, ci, 0:n], c.gv.ap[:, gcol + ci:gcol + ci + 1], rstd.ap[:, 0:n], ALU.mult, ALU.mult,
            xb.all() + rstd.all() + c.gv.all(), hT.all())


def phase_A3X(k):
    S, d, c, P, ps, psk = k.S, k.d, k.c, k.P, k.ps, k.psk
    xb = S.alloc("xa", [128, 8, 512], F32)
    hT = S.alloc("hTa", [128, 8, 512], BF16)
    sd, rstd = S.alloc("sda", [128, 512], F32), S.alloc("rstda", [128, 512], F32)
    qxT = S.alloc("qxT", [128, 8, 512], BF16, nsub=8)
    PTx = [S.alloc(f"PTx{i}", [128, 512], BF16) for i in range(2)]
    oxT = S.alloc("oxT", [128, 8, 512], BF16)
    rec = S.alloc("recx", [128, 512], F32)
    xTv = d["xT"].rearrange("(c p) t -> p c t", p=128)
    x2v = d["x2s"].rearrange("(c p) t -> p c t", p=128)
    tiles = [(0, 128)] + [(128 + 512 * i, 512) for i in range(4)]
    for (s0, N) in tiles:
        g0 = HALO0 + s0
        S.dma("sp", xb.ap[:, :, 0:N], xTv[:, :, g0:g0 + N], W=xb.all(), key="xa")
        for cb in range(8):
            pi = 1 + cb % 2
            cs = slice(cb * 128, (cb + 1) * 128)
            for j in range(4):
                mm(k, ps[pi][:, 0:N], P.w_out.ap[:, j, cs], P.omlaT.ap[:, j, s0:s0 + N], j == 0, False,
                   P.w_out.all() + P.omlaT.all(), [psk[pi]])
            for j in range(4):
                mm(k, ps[pi][:, 0:N], P.w_out.ap[:, 4 + j, cs], P.oretT.ap[:, j, s0:s0 + N], False, j == 3,
                   P.w_out.all() + P.oretT.all(), [psk[pi]])
            tt(k, "dve", xb.ap[:, cb, 0:N], xb.ap[:, cb, 0:N], ps[pi][:, 0:N], ALU.add, xb.all() + [psk[pi]], xb.all())
        if k.dbg and k.stage == "A3X":
            dumpx(k, "x1", xb, s0, N)
        _norm_to_hT(k, xb, hT, sd, rstd, c.g_xattn, N)
        for blk in range(8):
            pi = 1 + blk % 2
            for ci in range(8):
                mm(k, ps[pi][:, 0:N], P.w_xq.ap[:, ci, blk * 128:(blk + 1) * 128], hT.ap[:, ci, 0:N], ci == 0, ci == 7,
                   P.w_xq.all() + hT.all(), [psk[pi]])
            cp(k, "act", qxT.ap[:, blk, 0:N], ps[pi][:, 0:N], [psk[pi]], [qxT.k(blk)])
        for h in range(4):
            for kb2 in range(2):
                sb = 3 + kb2
                for dc in range(2):
                    mm(k, ps[sb][:, 0:N], P.memKT.ap[:, 2 * h + dc, kb2 * 128:(kb2 + 1) * 128], qxT.ap[:, 2 * h + dc, 0:N],
                       dc == 0, dc == 1, P.memKT.all() + [qxT.k(2 * h + dc)], [psk[sb]])
                act(k, PTx[kb2].ap[:, 0:N], ps[sb][:, 0:N], AF.Exp, [psk[sb]], PTx[kb2].all(), scale=1.0 / 16.0)
            for kb2 in range(2):
                mm(k, ps[5][:, 0:N], c.ones.ap, PTx[kb2].ap[:, 0:N], kb2 == 0, kb2 == 1, c.ones.all() + PTx[kb2].all(), [psk[5]])
            act(k, rec.ap[:, 0:N], ps[5][:, 0:N], AF.Ln, [psk[5]], rec.all())
            act(k, rec.ap[:, 0:N], rec.ap[:, 0:N], AF.Exp, rec.all(), rec.all(), scale=-1.0)
            for eb in range(2):
                pi = 6 + eb
                for kb2 in range(2):
                    mm(k, ps[pi][:, 0:N], P.memV.ap[:, kb2, h * 256 + eb * 128:h * 256 + (eb + 1) * 128], PTx[kb2].ap[:, 0:N],
                       kb2 == 0, kb2 == 1, P.memV.all() + PTx[kb2].all(), [psk[pi]])
                tt(k, "dve", oxT.ap[:, 2 * h + eb, 0:N], ps[pi][:, 0:N], rec.ap[:, 0:N], ALU.mult, [psk[pi]] + rec.all(), oxT.all())
        for cb in range(8):
            pi = 1 + cb % 2
            for j in range(8):
                mm(k, ps[pi][:, 0:N], P.w_xo.ap[:, j, cb * 128:(cb + 1) * 128], oxT.ap[:, j, 0:N], j == 0, j == 7,
                   P.w_xo.all() + oxT.all(), [psk[pi]])
            tt(k, "dve", xb.ap[:, cb, 0:N], xb.ap[:, cb, 0:N], ps[pi][:, 0:N], ALU.add, xb.all() + [psk[pi]], xb.all())
        S.dma("sp", x2v[:, :, s0:s0 + N], xb.ap[:, :, 0:N], R=xb.all(), W=[("x2s", s0)], key="xs")
        if k.dbg and k.stage == "A3X":
            dumpx(k, "x2", xb, s0, N)
    S.release(xb, hT, sd, rstd, qxT, *PTx, oxT, rec)


def dumpx(k, name, xb, s0, N):
    if name not in k.dbg_out:
        k.dbg_out[name] = k.nc.dram_tensor("dbg_" + name, [D, NST], F32, kind="ExternalOutput").ap()
    t = k.dbg_out[name].rearrange("(c p) t -> p c t", p=128)
    k.S.dma("sp", t[:, :, s0:s0 + N], xb.ap[:, :, 0:N], R=xb.all(), W=[("dbg", name, s0)], key="dbg")


def phase_F(k):
    S, d, c, P, ps, psk = k.S, k.d, k.c, k.P, k.ps, k.psk
    xf = S.alloc("xf", [128, 8, 512], F32)
    hT = S.alloc("hTf", [128, 8, 512], BF16)
    sd, rstd = S.alloc("sdf", [128, 512], F32), S.alloc("rstdf", [128, 512], F32)
    aT = S.alloc("aT", [128, NFB, 512], BF16, nsub=NFB)
    gsb = [S.alloc(f"gsb{i}", [128, 48 + 512], F32) for i in range(2)]
    cv = [S.alloc(f"cv{i}", [128, 512], F32) for i in range(2)]
    ost = [S.alloc("ost0", [128, 512], F32)] * 2
    ghalo = S.alloc("ghalo", [128, NFB, 48], F32, nsub=NFB)
    x2v = d["x2s"].rearrange("(c p) t -> p c t", p=128)
    outv = d["outT"].rearrange("(c p) t -> p c t", p=128)
    w1 = P.w_ffn_in
    S.dma("sp", xf.ap[:, :, 0:128], x2v[:, :, 0:128], R=[("x2s", 0)], W=xf.all(), key="xf")
    _norm_to_hT(k, xf, hT, sd, rstd, c.g_ffn, 128)
    for j in range(NFB):
        pi = 1 + j % 2
        for ci in range(8):
            mm(k, ps[pi][:, 0:128], w1.ap[:, ci, j * 128:(j + 1) * 128], hT.ap[:, ci, 0:128], ci == 0, ci == 7,
               [w1.k(0 if j < 11 else 2)] + hT.all(), [psk[pi]])
        ts(k, "dve", ghalo.ap[:, j, :], ps[pi][:, 80:128], c.flags.ap[:, 1:2], None, ALU.mult, None, [psk[pi]] + c.flags.all(),
           [ghalo.k(j)])
    for ti in range(4):
        s0 = 128 + 512 * ti
        S.dma("sp", xf.ap, x2v[:, :, s0:s0 + 512], R=[("x2s", s0)], W=xf.all(), key="xf")
        _norm_to_hT(k, xf, hT, sd, rstd, c.g_ffn, 512)
        for j in range(NFB):
            pg, pu = 1 + (j % 2) * 2, 2 + (j % 2) * 2
            for ci in range(8):
                mm(k, ps[pg], w1.ap[:, ci, j * 128:(j + 1) * 128], hT.ap[:, ci, :], ci == 0, ci == 7,
                   [w1.k(0 if j < 11 else 2)] + hT.all(), [psk[pg]])
            for ci in range(8):
                mm(k, ps[pu], w1.ap[:, ci, DFF + j * 128:DFF + (j + 1) * 128], hT.ap[:, ci, :], ci == 0, ci == 7,
                   [w1.k(1 if j < 11 else 3)] + hT.all(), [psk[pu]])
            g, cvb = gsb[j % 2], cv[j % 2]
            cw = c.convp.ap
            cp(k, "act", g.ap[:, 48:560], ps[pg], [psk[pg]], g.all())
            cp(k, "pool", g.ap[:, 0:48], ghalo.ap[:, j, :], [ghalo.k(j)], g.all())
            k.S.op("act", lambda e, g=g, cvb=cvb, j=j: e.activation(out=cvb.ap, in_=g.ap[:, 48:560], func=AF.Identity,
                                                                     scale=cw[:, j, 2:3], bias=cw[:, j, 3:4]),
                   g.all() + c.convp.all(), cvb.all())
            stt(k, cvb.ap, g.ap[:, 47:559], cw[:, j, 1:2], cvb.ap, ALU.mult, ALU.add, g.all() + cvb.all() + c.convp.all(), cvb.all())
            stt(k, cvb.ap, g.ap[:, 46:558], cw[:, j, 0:1], cvb.ap, ALU.mult, ALU.add, g.all() + cvb.all() + c.convp.all(), cvb.all())
            cp(k, "pool", ghalo.ap[:, j, :], g.ap[:, 512:560], g.all(), [ghalo.k(j)])
            act(k, cvb.ap, cvb.ap, AF.Silu, cvb.all(), cvb.all())
            tt(k, "dve", aT.ap[:, j, :], cvb.ap, ps[pu], ALU.mult, cvb.all() + [psk[pu]], [aT.k(j)])
        for cb in range(8):
            pi = 5 + cb % 2
            for j in range(NFB):
                mm(k, ps[pi], P.w_ffn_out.ap[:, j, cb * 128:(cb + 1) * 128], aT.ap[:, j, :], j == 0, j == NFB - 1,
                   P.w_ffn_out.all() + [aT.k(j)], [psk[pi]])
            tt(k, "dve", xf.ap[:, cb, :], xf.ap[:, cb, :], ps[pi], ALU.add, xf.all() + [psk[pi]], xf.all())
        tt(k, "pool", hT.ap, xf.ap, xf.ap, ALU.mult, xf.all(), hT.all())
        rms_stats(k, lambda i: hT.ap[:, i, :], 8, D, 0, sd, rstd, 512, hT.all())
        for cb in range(8):
            o = ost[cb % 2]
            stt(k, o.ap, xf.ap[:, cb, :], c.gv.ap[:, c.g_final + cb:c.g_final + cb + 1], rstd.ap, ALU.mult, ALU.mult,
                xf.all() + rstd.all() + c.gv.all(), o.all())
            S.dma("sp", outv[:, cb, 512 * ti:512 * (ti + 1)], o.ap, R=o.all(), W=[("out", ti, cb)], key=f"out{cb % 2}")
    S.release(xf, hT, sd, rstd, aT, *gsb, *cv, ost[0], ghalo)


def _consts():
    f32 = np.float32
    H, L = 4, 128
    log_gamma = np.log(f32(1.0) - f32(2.0) ** (f32(-5.0) - np.arange(H, dtype=f32))).astype(f32)
    j = np.arange(L, dtype=f32)
    diff = j[:, None] - j[None, :]
    intra = np.where(diff[None] >= 0, np.exp(np.maximum(diff, 0.0)[None] * log_gamma[:, None, None]), 0.0).astype(f32)
    k_to_end = np.exp((L - 1 - j)[:, None] * log_gamma[None, :]).astype(f32)
    q_from_start = np.exp((j + 1)[:, None] * log_gamma[None, :]).astype(f32)
    chunk_decay = np.exp(f32(L) * log_gamma).astype(f32)
    dk = f32(128.0 ** -0.5)
    c_intra = np.zeros((128, 512), f32)
    for h in range(H):
        c_intra[:, h * 128:(h + 1) * 128] = intra[h].T * dk
    c_qfs = np.zeros((128, 512), f32)
    c_decay = np.zeros((128, 512), f32)
    for h in range(H):
        c_qfs[:, h * 128:(h + 1) * 128] = q_from_start[:, h][None, :]
        c_decay[:, h * 128:(h + 1) * 128] = chunk_decay[h]
    c_small = np.zeros((128, 8), f32)
    invf_r = (1.0 / (f32(10000.0) ** (np.arange(0, 128, 2, dtype=f32) / f32(128)))).astype(f32)
    invf_m = (1.0 / (f32(10000.0) ** (np.arange(0, 32, 2, dtype=f32) / f32(32)))).astype(f32)
    p = np.arange(128)
    c_small[:, 0] = invf_r[p % 64]
    c_small[:, 1] = invf_m[p % 16]
    c_small[:, 2:6] = k_to_end * dk
    c_ident = np.eye(128, dtype=f32)
    c_rot = np.zeros((128, 128), f32)
    for m in range(64):
        c_rot[m + 64, m] = -1.0
    for m in range(64, 128):
        c_rot[m - 64, m] = 1.0
    kk = np.arange(128)
    c_causal = (kk[None, :] >= kk[:, None]).astype(f32)
    return dict(c_small=c_small, c_ident=c_ident, c_rot=c_rot, c_causal=c_causal, c_intra=c_intra, c_qfs=c_qfs,
                c_decay=c_decay)


def make_in_maps(inputs):
    f32 = np.float32
    x = np.asarray(inputs["x"], f32)
    mem = np.asarray(inputs["mem"], f32)
    pos = np.asarray(inputs["positions"], np.int32)

    def col(g):
        g = np.asarray(g, f32).reshape(-1, 128)
        return np.ascontiguousarray(g.T)

    gv = np.concatenate([col(inputs["g_mix"][0]), col(inputs["g_xattn"][0]), col(inputs["g_mem"][0]),
                         col(inputs["g_ffn"][0]), col(inputs["g_final"]), col(inputs["g_q_lat"][0]),
                         col(inputs["g_kv_lat"][0])], axis=1)
    cw = np.asarray(inputs["conv_w"][0], f32)
    cb = np.asarray(inputs["conv_b"][0], f32)
    convp = np.zeros((128, NFB, 4), f32)
    for i in range(3):
        convp[:, :, i] = cw[i].reshape(NFB, 128).T
    convp[:, :, 3] = cb.reshape(NFB, 128).T
    shared = dict(
        w_in=np.ascontiguousarray(inputs["w_in"][0], f32), w_uq=np.ascontiguousarray(inputs["w_uq"][0], f32),
        w_ukv=np.ascontiguousarray(inputs["w_ukv"][0], f32), w_out=np.ascontiguousarray(inputs["w_out"][0], f32),
        w_xq=np.ascontiguousarray(inputs["w_xq"][0], f32), w_xkv=np.ascontiguousarray(inputs["w_xkv"][0], f32),
        w_xo=np.ascontiguousarray(inputs["w_xo"][0], f32), w_ffn_in=np.ascontiguousarray(inputs["w_ffn_in"][0], f32),
        w_ffn_out=np.ascontiguousarray(inputs["w_ffn_out"][0], f32), gv=np.ascontiguousarray(gv),
        convp=np.ascontiguousarray(convp.reshape(128, NFB * 4)), **_consts())
    maps = []
    for core in range(8):
        b, hf = core // 2, core % 2
        xT = np.zeros((D, NTOK), f32)
        pp = np.zeros((1, NTOK), np.int32)
        if hf == 0:
            xT[:, NPRE:] = x[b, :NOWN].T
            pp[0, NPRE:] = pos[b, :NOWN]
        else:
            xT[:, :] = x[b].T
            pp[0, :] = pos[b]
        flags = np.full((128, 2), float(hf), f32)
        m = dict(shared)
        m.update(xT=xT, posi=pp, memT=np.ascontiguousarray(mem[b].T), flags=flags)
        maps.append(m)
    return maps


_CACHE = {}


def kernel(**inputs):
    if "nc" not in _CACHE:
        _CACHE["nc"] = build("full")[0]
    nc = _CACHE["nc"]
    maps = make_in_maps(inputs)
    res = run_bass_kernel_spmd(nc, maps, core_ids=list(range(8)))
    out = np.zeros((NB, SEQ, D), np.float32)
    for core in range(8):
        b, hf = core // 2, core % 2
        out[b, hf * NOWN:(hf + 1) * NOWN, :] = res.results[core]["outT"].T
    return out
```

```python
import math
from contextlib import ExitStack

import numpy as np
import concourse.bass as bass
import concourse.mybir as mybir
from concourse.bass_utils import run_bass_kernel_spmd

F32 = mybir.dt.float32
BF16 = mybir.dt.bfloat16
I32 = mybir.dt.int32
U8 = mybir.dt.uint8
AF = mybir.ActivationFunctionType
ALU = mybir.AluOpType
AX = mybir.AxisListType

D = 1024
SEQ = 4096
NB = 4
EPS = 1e-6
NPRE = 2048
NOWN = 2048
NTOK = NPRE + NOWN
HALO0 = NPRE - 128
NST = NOWN + 128
IN_COLS = 2464
OFF_CQ, OFF_CKV, OFF_KR, OFF_RQ, OFF_RK, OFF_RV, OFF_RG = 0, 256, 384, 416, 928, 1440, 1952
DFF = 2816
NFB = DFF // 128
MEM = 256
TWO_PI = 2.0 * math.pi
C1 = 6.28125
C2 = TWO_PI - C1
MAGIC = 12582912.0
PI_LO = 3.1415925
ARENA_BYTES = 206 * 1024
ENGS = ("pe", "act", "dve", "pool", "sp")
NDMA_MAX = 90
DT_SIZE = {F32: 4, BF16: 2, I32: 4}


class Buf:
    def __init__(self, uid, name, off, nbytes, ap, nsub):
        self.uid, self.name, self.off, self.nbytes, self.ap, self.nsub = uid, name, off, nbytes, ap, nsub

    def k(self, i=0):
        return (self.uid, i)

    def all(self):
        return [(self.uid, i) for i in range(self.nsub)]

    def __getitem__(self, idx):
        return self.ap[idx]


class Sched:
    def __init__(self, nc, es):
        self.nc, self.es = nc, es
        self.prog = {e: [] for e in ENGS}
        self.sem = {e: es.enter_context(nc.semaphore("s_" + e)) for e in ENGS if e != "sp"}
        self.cnt = {e: 0 for e in ENGS}
        self.seen = {e: {} for e in ENGS}
        self.drained = {e: 0 for e in ENGS}
        self.needed = {e: set() for e in ENGS}
        self.lastw, self.readers = {}, {}
        self.ndma = 0
        self.dma_events = []
        self.dma_pool = [es.enter_context(nc.semaphore(f"d{i}")) for i in range(NDMA_MAX)]
        self.dma_sem = {}
        self.arena = es.enter_context(nc.sbuf_tensor("arena", [128, ARENA_BYTES], U8))
        self.free = [(0, ARENA_BYTES)]
        self.freed_events = []
        self.nbuf = 0
        self.peak = 0
        self.used = 0

    def alloc(self, name, shape, dtype, nsub=1):
        n = 1
        for s in shape[1:]:
            n *= s
        nbytes = (n * DT_SIZE[dtype] + 63) // 64 * 64
        for i, (o, sz) in enumerate(self.free):
            if sz >= nbytes:
                off = o
                if sz == nbytes:
                    self.free.pop(i)
                else:
                    self.free[i] = (o + nbytes, sz - nbytes)
                break
        else:
            raise MemoryError(f"arena full allocating {name} {nbytes}B; free={self.free}")
        ap = self.arena[0:shape[0], off:off + n * DT_SIZE[dtype]].bitcast(dtype)
        if len(shape) == 3:
            ap = ap.rearrange("p (a b) -> p a b", a=shape[1])
        elif len(shape) == 4:
            ap = ap.rearrange("p (a b c) -> p a b c", a=shape[1], b=shape[2])
        self.nbuf += 1
        b = Buf(self.nbuf, name, off, nbytes, ap, nsub)
        evs, keep = [], []
        for (fo, fn, fe) in self.freed_events:
            if fo < off + nbytes and off < fo + fn:
                evs.extend(fe)
            keep.append((fo, fn, fe))
        self.freed_events = keep
        if evs:
            for kk in b.all():
                self.readers[kk] = list(evs)
        self.used += nbytes
        self.peak = max(self.peak, self.used)
        return b

    def release(self, *bufs):
        for b in bufs:
            evs = []
            for kk in b.all():
                if kk in self.lastw:
                    evs.append(self.lastw.pop(kk))
                evs.extend(self.readers.pop(kk, []))
            best = {}
            for (s, v) in evs:
                best[s] = max(best.get(s, 0), v)
            self.freed_events.append((b.off, b.nbytes, list(best.items())))
            self.free.append((b.off, b.nbytes))
            self.free.sort()
            merged = []
            for (o, sz) in self.free:
                if merged and merged[-1][0] + merged[-1][1] == o:
                    merged[-1] = (merged[-1][0], merged[-1][1] + sz)
                else:
                    merged.append((o, sz))
            self.free = merged
            self.used -= b.nbytes

    def _deps(self, eng, R, W):
        best = {}
        for r in R:
            ev = self.lastw.get(r)
            if ev is not None:
                best[ev[0]] = max(best.get(ev[0], 0), ev[1])
        for w in W:
            ev = self.lastw.get(w)
            if ev is not None:
                best[ev[0]] = max(best.get(ev[0], 0), ev[1])
            for ev in self.readers.get(w, ()):
                best[ev[0]] = max(best.get(ev[0], 0), ev[1])
        for s, v in best.items():
            if s == eng:
                if eng == "pe":
                    continue
                if eng in ("act", "dve"):
                    if self.drained[eng] < v:
                        self.prog[eng].append(("drain",))
                        self.drained[eng] = self.cnt[eng]
                    continue
            if self.seen[eng].get(s, 0) >= v:
                continue
            self.seen[eng][s] = v
            self.prog[eng].append(("wait", s, v))
            if isinstance(s, str):
                self.needed[s].add(v)

    def _commit(self, ev, R, W):
        for w in W:
            self.lastw[w] = ev
            self.readers[w] = []
        for r in R:
            if r not in W:
                self.readers.setdefault(r, []).append(ev)

    def op(self, eng, fn, R=(), W=()):
        R, W = list(R), list(W)
        self._deps(eng, R, W)
        self.cnt[eng] += 1
        ev = (eng, self.cnt[eng])
        self.prog[eng].append(("inst", fn, self.cnt[eng]))
        self._commit(ev, R, W)
        return ev

    def dma(self, eng, out, in_, R=(), W=(), key=None):
        R, W = list(R), list(W)
        self._deps(eng, R, W)
        if key is None:
            key = f"_auto{self.ndma}"
        if key not in self.dma_sem:
            self.dma_sem[key] = [self.dma_pool[len(self.dma_sem)], 0]
        ent = self.dma_sem[key]
        sem = ent[0]
        if ent[1] > 0 and self.seen[eng].get(sem, 0) < ent[1]:
            self.seen[eng][sem] = ent[1]
            self.prog[eng].append(("wait", sem, ent[1]))
        ent[1] += 16
        self.ndma += 1
        ev = (sem, ent[1])
        self.prog[eng].append(("dma", out, in_, sem))
        self._commit(ev, R, W)
        self.dma_events.append(ev)
        return ev

    def emit(self, block):
        nc = self.nc
        S = self

        rank = {en: {q: i + 1 for i, q in enumerate(sorted(S.needed[en]))} for en in ENGS}
        S.n_inc = {en: len(rank[en]) for en in ENGS}

        def run(e, name):
            for ent in S.prog[name]:
                if ent[0] == "wait":
                    s = ent[1]
                    if isinstance(s, str):
                        e.wait_ge(S.sem[s], rank[s][ent[2]])
                    else:
                        e.wait_ge(s, ent[2])
                elif ent[0] == "drain":
                    e.drain()
                elif ent[0] == "inst":
                    ins = ent[1](e)
                    if ent[2] in rank[name]:
                        ins.then_inc(S.sem[name], 1)
                else:
                    e.dma_start(out=ent[1], in_=ent[2]).then_inc(ent[3], 16)

        @block.tensor
        def _(e):
            run(e, "pe")

        @block.scalar
        def _(e):
            run(e, "act")

        @block.vector
        def _(e):
            run(e, "dve")

        @block.gpsimd
        def _(e):
            run(e, "pool")

        @block.sync
        def _(e):
            run(e, "sp")

    def final_wait(self, eng, events):
        for (s, v) in events:
            if self.seen[eng].get(s, 0) >= v:
                continue
            self.seen[eng][s] = v
            self.prog[eng].append(("wait", s, v))


class K:
    pass


def bc(ap, shape):
    return ap.broadcast_to(shape)


def build(stage="full", dbg=False, a1_tiles=None, a1_level=9):
    nc = bass.Bass("TRN2", target_bir_lowering=False)
    es = ExitStack()
    k = K()
    k.nc, k.es, k.stage, k.dbg = nc, es, stage, dbg
    k.a1_tiles, k.a1_level = a1_tiles, a1_level
    d = {}

    def din(name, shape, dt=F32):
        d[name] = nc.dram_tensor(name, list(shape), dt, kind="ExternalInput").ap()

    din("xT", [D, NTOK]); din("posi", [1, NTOK], I32); din("memT", [D, MEM]); din("flags", [128, 2])
    din("w_in", [D, IN_COLS]); din("w_uq", [256, 768]); din("w_ukv", [128, 1024]); din("w_out", [D, D])
    din("w_xq", [D, D]); din("w_xkv", [D, 2 * D]); din("w_xo", [D, D])
    din("w_ffn_in", [D, 2 * DFF]); din("w_ffn_out", [DFF, D])
    din("gv", [128, 43]); din("convp", [128, NFB * 4]); din("c_small", [128, 8])
    din("c_ident", [128, 128]); din("c_rot", [128, 128]); din("c_causal", [128, 128])
    din("c_intra", [128, 512]); din("c_qfs", [128, 512]); din("c_decay", [128, 512])
    d["outT"] = nc.dram_tensor("outT", [D, NOWN], F32, kind="ExternalOutput").ap()
    d["x2s"] = nc.dram_tensor("x2s", [D, NST], F32, kind="Internal").ap()
    k.dbg_out = {}
    k.d = d
    with es:
        S = Sched(nc, es)
        k.S = S
        big = [es.enter_context(nc.psum_tensor(f"psb{i}", [128, 1024], F32)) for i in range(4)]
        k.ps2 = [b_[:, :] for b_ in big]
        k.ps = [big[i // 2][:, (i % 2) * 512:(i % 2 + 1) * 512] for i in range(8)]
        k.psk = [("ps", i) for i in range(8)]
        block = es.enter_context(nc.Block())
        emit_all(k)
        S.final_wait("sp", S.dma_events)
        S.emit(block)
    k.peak = S.peak
    return nc, k


def mm(k, out, lhsT, rhs, start, stop, R, W):
    k.S.op("pe", lambda e: e.matmul(out, lhsT=lhsT, rhs=rhs, start=start, stop=stop), R, W)


def tr(k, out, in_, ident, R, W):
    k.S.op("pe", lambda e: e.transpose(out=out, in_=in_, identity=ident), R, W)


def act(k, out, in_, func, R, W, scale=1.0, bias=0.0):
    k.S.op("act", lambda e: e.activation(out=out, in_=in_, func=func, scale=scale, bias=bias), R, W)


def tt(k, eng, out, in0, in1, op, R, W):
    k.S.op(eng, lambda e: e.tensor_tensor(out=out, in0=in0, in1=in1, op=op), R, W)


def ts(k, eng, out, in0, s1, s2, op0, op1, R, W):
    if op1 is None:
        k.S.op(eng, lambda e: e.tensor_scalar(out=out, in0=in0, scalar1=s1, scalar2=None, op0=op0), R, W)
    else:
        k.S.op(eng, lambda e: e.tensor_scalar(out=out, in0=in0, scalar1=s1, scalar2=s2, op0=op0, op1=op1), R, W)


def stt(k, out, in0, scalar, in1, op0, op1, R, W):
    k.S.op("dve", lambda e: e.scalar_tensor_tensor(out=out, in0=in0, scalar=scalar, in1=in1, op0=op0, op1=op1), R, W)


def cp(k, eng, out, in_, R, W):
    if eng == "act":
        k.S.op("act", lambda e: e.copy(out=out, in_=in_), R, W)
    else:
        k.S.op(eng, lambda e: e.tensor_copy(out=out, in_=in_), R, W)


def recip(k, out, in_, R, W):
    k.S.op("dve", lambda e: e.reciprocal(out=out, in_=in_), R, W)


def dump(k, name, buf_ap, shape, R, dt=F32):
    t = k.nc.dram_tensor("dbg_" + name, list(shape), dt, kind="ExternalOutput").ap()
    k.dbg_out[name] = t
    k.S.dma("sp", t, buf_ap, R=R, W=[("dbg", name)], key="dbg")


def load_consts(k):
    S, d = k.S, k.d
    c = K()
    k.c = c
    c.gv = S.alloc("gv", [128, 43], F32)
    c.convp = S.alloc("convp", [128, NFB, 4], F32)
    c.small = S.alloc("c_small", [128, 8], F32)
    c.flags = S.alloc("flags", [128, 2], F32)
    c.ident = S.alloc("ident", [128, 128], BF16)
    c.rot = S.alloc("rot", [128, 128], BF16)
    c.causal = S.alloc("causal", [128, 128], BF16)
    c.intra = S.alloc("intra", [128, 512], F32)
    c.qfs = S.alloc("qfs", [128, 512], F32)
    c.decay = S.alloc("decay", [128, 512], F32)
    c.ones = S.alloc("ones", [128, 128], BF16)
    S.dma("sp", c.gv.ap, d["gv"], W=c.gv.all())
    S.dma("sp", c.convp.ap, d["convp"].rearrange("p (a b) -> p a b", a=NFB), W=c.convp.all())
    S.dma("sp", c.small.ap, d["c_small"], W=c.small.all())
    S.dma("sp", c.flags.ap, d["flags"], W=c.flags.all())
    S.dma("sp", c.intra.ap, d["c_intra"], W=c.intra.all())
    S.dma("sp", c.qfs.ap, d["c_qfs"], W=c.qfs.all())
    S.dma("sp", c.decay.ap, d["c_decay"], W=c.decay.all())
    S.dma("pool", c.ident.ap, d["c_ident"], W=c.ident.all())
    S.dma("pool", c.rot.ap, d["c_rot"], W=c.rot.all())
    S.dma("pool", c.causal.ap, d["c_causal"], W=c.causal.all())
    S.op("pool", lambda e: e.memset(c.ones.ap, 1.0), W=c.ones.all())
    c.g_mix, c.g_xattn, c.g_mem, c.g_ffn, c.g_final, c.g_q, c.g_kv = 0, 8, 16, 24, 32, 40, 42


def rope_tables(k, posi_ap, posi_key, prange, col, cosb, sinb, tmp, n):
    c = k.c
    p0, p1 = prange
    a, b_, kk = tmp
    A = lambda buf: buf.ap[p0:p1, 0:n]
    invf = c.small.ap[p0:p1, col:col + 1]
    ts(k, "dve", A(a), posi_ap[p0:p1, 0:n], invf, None, ALU.mult, None, [posi_key] + c.small.all(), a.all())
    ts(k, "dve", A(b_), A(a), 1.0 / TWO_PI, MAGIC, ALU.mult, ALU.add, a.all(), b_.all())
    ts(k, "dve", A(kk), A(b_), -MAGIC, None, ALU.add, None, b_.all(), kk.all())
    stt(k, A(b_), A(kk), -C1, A(a), ALU.mult, ALU.add, kk.all() + a.all(), b_.all())
    stt(k, A(a), A(kk), -C2, A(b_), ALU.mult, ALU.add, kk.all() + b_.all(), a.all())
    ts(k, "dve", A(a), A(a), -PI_LO, PI_LO, ALU.max, ALU.min, a.all(), a.all())
    act(k, sinb.ap[p0:p1, 0:n], A(a), AF.Sin, a.all(), sinb.all())
    ts(k, "dve", A(b_), A(a), math.pi / 2, -TWO_PI, ALU.is_gt, ALU.mult, a.all(), b_.all())
    stt(k, A(kk), A(a), math.pi / 2, A(b_), ALU.add, ALU.add, a.all() + b_.all(), kk.all())
    ts(k, "dve", A(kk), A(kk), -PI_LO, PI_LO, ALU.max, ALU.min, kk.all(), kk.all())
    act(k, cosb.ap[p0:p1, 0:n], A(kk), AF.Sin, kk.all(), cosb.all())


def squares8(k, dst, src, n):
    tt(k, "pool", dst.ap[:, 0:4, 0:n], src.ap[:, 0:4, 0:n], src.ap[:, 0:4, 0:n], ALU.mult, src.all(), dst.all())
    for ci in range(4, 8):
        act(k, dst.ap[:, ci, 0:n], src.ap[:, ci, 0:n], AF.Square, src.all(), dst.all())


def rms_stats(k, sq_chunks, nch, nfeat, ps_i, sd, rstd, n, sqkeys):
    c = k.c
    ps = k.ps[ps_i]
    for i in range(nch):
        mm(k, ps[:, 0:n], c.ones.ap, sq_chunks(i), i == 0, i == nch - 1, sqkeys + c.ones.all(), [k.psk[ps_i]])
    act(k, sd.ap[:, 0:n], ps[:, 0:n], AF.Ln, [k.psk[ps_i]], sd.all(), scale=1.0 / nfeat, bias=EPS)
    act(k, rstd.ap[:, 0:n], sd.ap[:, 0:n], AF.Exp, sd.all(), rstd.all(), scale=-0.5)


def emit_all(k):
    load_consts(k)
    P = K()
    k.P = P
    S = k.S
    P.cqn = S.alloc("cqn", [128, 2, NST], BF16)
    P.ckvn = S.alloc("ckvn", [128, NTOK], BF16)
    P.krope = S.alloc("krope", [128, NTOK], BF16)
    P.oretT = S.alloc("oretT", [128, 4, NST], BF16)
    phase_A1(k)
    if k.stage == "A1":
        return
    P.memKT = S.alloc("memKT", [128, 8, MEM], BF16)
    P.memV = S.alloc("memV", [128, 2, D], BF16)
    phase_MKV(k)
    P.omlaT = S.alloc("omlaT", [128, 4, NST], BF16)
    P.w_out = S.alloc("w_out_bf", [128, 8, D], BF16)
    P.w_xq = S.alloc("w_xq_bf", [128, 8, D], BF16)
    d = k.d
    S.dma("pool", P.w_out.ap, d["w_out"].rearrange("(c p) n -> p c n", p=128), W=P.w_out.all(), key="w_out")
    S.dma("pool", P.w_xq.ap, d["w_xq"].rearrange("(c p) n -> p c n", p=128), W=P.w_xq.all(), key="w_xq")
    phase_A2(k)
    if k.stage == "A2":
        return
    S.release(P.cqn, P.ckvn, P.krope)
    P.w_xo = S.alloc("w_xo_bf", [128, 8, D], BF16)
    S.dma("pool", P.w_xo.ap, d["w_xo"].rearrange("(c p) n -> p c n", p=128), W=P.w_xo.all(), key="w_xo")
    P.w_ffn_out = S.alloc("w_ffn_out_bf", [128, NFB, D], BF16)
    S.dma("pool", P.w_ffn_out.ap, d["w_ffn_out"].rearrange("(c p) n -> p c n", p=128), W=P.w_ffn_out.all(), key="w_ffn_out")
    phase_A3X(k)
    if k.stage == "A3X":
        return
    S.release(P.oretT, P.omlaT, P.w_out, P.w_xq, P.w_xo, P.memKT, P.memV)
    P.w_ffn_in = S.alloc("w_ffn_in_bf", [128, 8, 2 * DFF], BF16, nsub=4)
    wv = d["w_ffn_in"].rearrange("(c p) n -> p c n", p=128)
    HB = 11 * 128
    for sub, (a, b) in enumerate(((0, HB), (DFF, DFF + HB), (HB, DFF), (DFF + HB, 2 * DFF))):
        S.dma("pool", P.w_ffn_in.ap[:, :, a:b], wv[:, :, a:b], W=[P.w_ffn_in.k(sub)], key=f"w_ffn_in{sub}")
    phase_F(k)


def phase_A1(k):
    S, d, c, P = k.S, k.d, k.c, k.P
    ps, psk = k.ps, k.psk
    T = 512
    w_in = S.alloc("w_in_bf", [128, 8, IN_COLS], BF16)
    S.dma("pool", w_in.ap, d["w_in"].rearrange("(c p) n -> p c n", p=128), W=w_in.all(), key="w_in")
    w_krot = S.alloc("w_krot", [128, 8, 96], BF16)
    S.op("pool", lambda e: e.memset(w_krot.ap, 0.0), W=w_krot.all())
    ts(k, "pool", w_krot.ap[:, :, 64:80], w_in.ap[:, :, OFF_KR + 16:OFF_KR + 32], -1.0, None, ALU.mult, None,
       w_in.all(), w_krot.all())
    cp(k, "pool", w_krot.ap[:, :, 80:96], w_in.ap[:, :, OFF_KR:OFF_KR + 16], w_in.all(), w_krot.all())

    xt = [S.alloc(f"xt{i}", [128, 8, T], F32) for i in range(2)]
    posi = [S.alloc(f"posi{i}", [128, T], I32) for i in range(2)]
    hTs = [S.alloc(f"hT{i}", [128, 8, T], BF16) for i in range(2)]
    rstd = S.alloc("rstd", [128, T], F32)
    sd = rstd
    ta, tb, tc = (S.alloc(n, [128, T], F32) for n in ("ta", "tb", "tc"))
    cos_r, sin_r = S.alloc("cos_r", [128, T], F32), S.alloc("sin_r", [128, T], F32)
    cos_m, sin_m = S.alloc("cos_m", [128, T], F32), S.alloc("sin_m", [128, T], F32)
    sql = S.alloc("sql", [128, 2, T], BF16)
    rstl = S.alloc("rstl", [128, T], F32)
    sdl = rstl
    raw = [S.alloc(f"raw{i}", [128, T], BF16) for i in range(2)]
    t1 = [S.alloc(f"t1_{i}", [128, T], F32) for i in range(2)]
    t2 = [S.alloc(f"t2_{i}", [128, T], F32) for i in range(2)]
    rqT = S.alloc("rqT", [128, 4, T], BF16, nsub=4)
    rkT = S.alloc("rkT", [128, 4, T], BF16, nsub=4)
    v_tm = S.alloc("v_tm", [128, 4, 512], BF16, nsub=4)
    sg_tm = S.alloc("sg_tm", [128, 4, 512], BF16, nsub=4)
    kdec = S.alloc("kdec", [128, 512], BF16)
    PT = S.alloc("PT", [128, 512], BF16)
    qsT = S.alloc("qsT", [128, 4, 128], BF16)
    S_f = S.alloc("S_f", [128, 512], F32)
    S_b = S.alloc("S_b", [128, 512], BF16)
    sqo = S.alloc("sqo", [128, 512], F32)
    tn = S.alloc("tn", [128, 512], F32)
    og = S.alloc("og", [128, 512], BF16)
    oT = S.alloc("oT", [128, 512], BF16)
    sqT = S.alloc("sqT", [128, 512], BF16)
    S.op("pool", lambda e: e.memset(S_f.ap, 0.0), W=S_f.all())
    S.op("pool", lambda e: e.memset(S_b.ap, 0.0), W=S_b.all())

    xTv = d["xT"].rearrange("(c p) t -> p c t", p=128)
    ntiles = NTOK // T
    rr = 0
    tile_list = list(range(ntiles)) if k.a1_tiles is None else k.a1_tiles

    def load_and_norm(tti):
        tok0 = tti * T
        xb, pb, hT = xt[tti % 2], posi[tti % 2], hTs[tti % 2]
        S.dma("sp", xb.ap, xTv[:, :, tok0:tok0 + T], W=xb.all(), key=f"xt{tti % 2}")
        S.dma("sp", pb.ap, d["posi"][:, tok0:tok0 + T].partition_broadcast(128), W=pb.all(), key=f"posi{tti % 2}")
        squares8(k, hT, xb, T)
        rms_stats(k, lambda i: hT.ap[:, i, :], 8, D, 0, sd, rstd, T, hT.all())
        for ci in range(8):
            stt(k, hT.ap[:, ci, :], xb.ap[:, ci, :], c.gv.ap[:, c.g_mix + ci:c.g_mix + ci + 1], rstd.ap,
                ALU.mult, ALU.mult, xb.all() + rstd.all() + c.gv.all(), hT.all())

    load_and_norm(tile_list[0])
    for tidx, tti in enumerate(tile_list):
        tok0 = tti * T
        xb, pb, hT = xt[tti % 2], posi[tti % 2], hTs[tti % 2]
        own_tile = tok0 + T > HALO0
        if k.a1_level < 2:
            continue
        rope_tables(k, pb.ap, pb.k(), (0, 128), 0, cos_r, sin_r, (ta, tb, tc), T)
        rope_tables(k, pb.ap, pb.k(), (64, 96), 1, cos_m, sin_m, (ta, tb, tc), T)

        def proj(ps_i, m, wbuf, col0, n=T):
            for ci in range(8):
                mm(k, ps[ps_i][0:m, 0:n], wbuf.ap[:, ci, col0:col0 + m], hT.ap[:, ci, 0:n], ci == 0, ci == 7,
                   wbuf.all() + hT.all(), [psk[ps_i]])

        if k.a1_level < 3:
            continue
        proj(1, 128, w_in, OFF_CKV)
        act(k, sql.ap[:, 0, :], ps[1], AF.Square, [psk[1]], sql.all())
        rms_stats(k, lambda i: sql.ap[:, 0, :], 1, 128, 0, sdl, rstl, T, sql.all())
        stt(k, P.ckvn.ap[:, tok0:tok0 + T], ps[1], c.gv.ap[:, c.g_kv:c.g_kv + 1], rstl.ap, ALU.mult, ALU.mult,
            [psk[1]] + rstl.all() + c.gv.all(), P.ckvn.all())
        proj(2, 96, w_in, OFF_KR - 64)
        proj(3, 96, w_krot, 0)
        tt(k, "dve", t1[0].ap[64:96, :], ps[2][64:96, :], cos_m.ap[64:96, :], ALU.mult, [psk[2]] + cos_m.all(), t1[0].all())
        tt(k, "dve", t2[0].ap[64:96, :], ps[3][64:96, :], sin_m.ap[64:96, :], ALU.mult, [psk[3]] + sin_m.all(), t2[0].all())
        tt(k, "pool", P.krope.ap[64:96, tok0:tok0 + T], t1[0].ap[64:96, :], t2[0].ap[64:96, :], ALU.add,
           t1[0].all() + t2[0].all(), P.krope.all())
        if k.a1_level < 4:
            continue
        if own_tile:
            proj(1, 128, w_in, OFF_CQ)
            proj(2, 128, w_in, OFF_CQ + 128)
            act(k, sql.ap[:, 0, :], ps[1], AF.Square, [psk[1]], sql.all())
            act(k, sql.ap[:, 1, :], ps[2], AF.Square, [psk[2]], sql.all())
            rms_stats(k, lambda i: sql.ap[:, i, :], 2, 256, 0, sdl, rstl, T, sql.all())
            if tok0 < HALO0:
                lo, n_, s0 = HALO0 - tok0, 128, 0
            else:
                lo, n_, s0 = 0, T, tok0 - HALO0
            for j, pi in ((0, 1), (1, 2)):
                stt(k, P.cqn.ap[:, j, s0:s0 + n_], ps[pi][:, lo:lo + n_], c.gv.ap[:, c.g_q + j:c.g_q + j + 1],
                    rstl.ap[:, lo:lo + n_], ALU.mult, ALU.mult, [psk[pi]] + rstl.all() + c.gv.all(), P.cqn.all())
        if k.a1_level < 5:
            continue
        todo = [(rkT, OFF_RK)] + ([(rqT, OFF_RQ)] if own_tile else [])
        items = [(dst, off, h) for (dst, off) in todo for h in range(4)]

        def rope_tail(it, slot):
            dst, off, h = it
            pi, pj = 1 + slot, 3 + slot
            rb, a1, a2 = raw[slot], t1[slot], t2[slot]
            mm(k, ps[pj], c.rot.ap, rb.ap, True, True, rb.all() + c.rot.all(), [psk[pj]])
            tt(k, "dve", a2.ap, ps[pj], sin_r.ap, ALU.mult, [psk[pj]] + sin_r.all(), a2.all())
            tt(k, "pool", a1.ap, rb.ap, cos_r.ap, ALU.mult, rb.all() + cos_r.all(), a1.all())
            tt(k, "pool", dst.ap[:, h, :], a1.ap, a2.ap, ALU.add, a1.all() + a2.all(), [dst.k(h)])

        prev = None
        for n_, it in enumerate(items):
            slot = n_ % 2
            proj(1 + slot, 128, w_in, it[1] + it[2] * 128)
            cp(k, "act", raw[slot].ap, ps[1 + slot], [psk[1 + slot]], raw[slot].all())
            if prev is not None:
                rope_tail(*prev)
            prev = (it, slot)
        rope_tail(*prev)
        if k.a1_level < 6:
            continue
        if own_tile:
            for h in range(4):
                pi = 1 + h % 2
                proj(pi, 128, w_in, OFF_RG + h * 128)
                act(k, sg_tm.ap[:, h, :], ps[pi], AF.Silu, [psk[pi]], [sg_tm.k(h)])
        if tidx + 1 < len(tile_list):
            load_and_norm(tile_list[tidx + 1])
        for cc in range(4):
            ctok = tok0 + cc * 128
            own_chunk = ctok >= HALO0
            for ci in range(8):
                mm(k, ps[5], hT.ap[:, ci, cc * 128:(cc + 1) * 128], w_in.ap[:, ci, OFF_RV:OFF_RV + 512], ci == 0, ci == 7,
                   hT.all() + w_in.all(), [psk[5]])
            cp(k, "act", v_tm.ap[:, cc, :], ps[5], [psk[5]], [v_tm.k(cc)])
            if k.a1_level < 7:
                continue
            cs = slice(cc * 128, (cc + 1) * 128)
            p7b = ps[7].bitcast(BF16)
            for h in range(4):
                tr(k, p7b[:, h * 128:(h + 1) * 128], rkT.ap[:, h, cs], c.ident.ap, [rkT.k(h)] + c.ident.all(), [psk[7]])
            tt(k, "dve", kdec.ap.rearrange("p (h e) -> p h e", h=4), p7b[:, 0:512].rearrange("p (h e) -> p h e", h=4),
               bc(c.small.ap[:, 2:6].unsqueeze(2), [128, 4, 128]), ALU.mult, [psk[7]] + c.small.all(), kdec.all())
            if own_chunk and k.a1_level >= 8:
                for h in range(4):
                    mm(k, ps[4][:, h * 128:(h + 1) * 128], rkT.ap[:, h, cs], rqT.ap[:, h, cs], True, True,
                       [rkT.k(h), rqT.k(h)], [psk[4]])
                tt(k, "dve", PT.ap, ps[4], c.intra.ap, ALU.mult, [psk[4]] + c.intra.all(), PT.all())
                tt(k, "pool", qsT.ap, rqT.ap[:, :, cs], c.qfs.ap.rearrange("p (h e) -> p h e", h=4), ALU.mult,
                   rqT.all() + c.qfs.all(), qsT.all())
                for h in range(4):
                    hs = slice(h * 128, (h + 1) * 128)
                    mm(k, ps[6][:, hs], PT.ap[:, hs], v_tm.ap[:, cc, hs], True, False, PT.all() + [v_tm.k(cc)], [psk[6]])
                    mm(k, ps[6][:, hs], qsT.ap[:, h, :], S_b.ap[:, hs], False, True, qsT.all() + S_b.all(), [psk[6]])
            if ctok + 128 < NTOK:
                for h in range(4):
                    hs = slice(h * 128, (h + 1) * 128)
                    mm(k, ps[5][:, hs], kdec.ap[:, hs], v_tm.ap[:, cc, hs], True, True, kdec.all() + [v_tm.k(cc)], [psk[5]])
                tt(k, "pool", S_f.ap, S_f.ap, c.decay.ap, ALU.mult, S_f.all() + c.decay.all(), S_f.all())
                tt(k, "dve", S_f.ap, S_f.ap, ps[5], ALU.add, S_f.all() + [psk[5]], S_f.all())
                cp(k, "act", S_b.ap, S_f.ap, S_f.all(), S_b.all())
            if own_chunk and k.a1_level >= 9:
                cp(k, "act", og.ap, ps[6], [psk[6]], og.all())
                for h in range(4):
                    tr(k, p7b[:, 512 + h * 128:512 + (h + 1) * 128], og.ap[:, h * 128:(h + 1) * 128], c.ident.ap,
                       og.all() + c.ident.all(), [psk[7]])
                cp(k, "dve", oT.ap, p7b[:, 512:1024], [psk[7]], oT.all())
                act(k, sqT.ap, oT.ap, AF.Square, oT.all(), sqT.all())
                mm(k, ps[4], c.ones.ap, oT.ap, True, True, c.ones.all() + oT.all(), [psk[4]])
                mm(k, ps[3], c.ones.ap, sqT.ap, True, True, c.ones.all() + sqT.all(), [psk[3]])
                act(k, tn.ap, ps[4], AF.Copy, [psk[4]], tn.all(), scale=1.0 / 128)
                tt(k, "dve", sqo.ap, tn.ap, tn.ap, ALU.mult, tn.all(), sqo.all())
                stt(k, sqo.ap, ps[3], 1.0 / 128, sqo.ap, ALU.mult, ALU.subtract, [psk[3]] + sqo.all(), sqo.all())
                act(k, sqo.ap, sqo.ap, AF.Ln, sqo.all(), sqo.all(), scale=1.0, bias=EPS)
                act(k, sqo.ap, sqo.ap, AF.Exp, sqo.all(), sqo.all(), scale=-0.5)
                tt(k, "dve", tn.ap, oT.ap, tn.ap, ALU.subtract, oT.all() + tn.all(), tn.all())
                tt(k, "pool", tn.ap, tn.ap, sqo.ap, ALU.mult, tn.all() + sqo.all(), tn.all())
                s0 = ctok - HALO0
                tt(k, "pool", P.oretT.ap[:, :, s0:s0 + 128], tn.ap.rearrange("p (h e) -> p h e", h=4), sg_tm.ap[:, :, cs],
                   ALU.mult, tn.all() + sg_tm.all(), P.oretT.all())
    if k.stage == "A1":
        mm(k, ps[0][:, 0:128], c.ones.ap, c.ones.ap, True, True, c.ones.all(), [psk[0]])
    if k.dbg and k.stage == "A1":
        dump(k, "ckvn", P.ckvn.ap, [128, NTOK], P.ckvn.all(), BF16)
        dump(k, "krope", P.krope.ap[64:96, :], [32, NTOK], P.krope.all(), BF16)
        dump(k, "cqn", P.cqn.ap, [128, 2, NST], P.cqn.all(), BF16)
        dump(k, "oretT", P.oretT.ap, [128, 4, NST], P.oretT.all(), BF16)
    S.release(w_in, w_krot, *xt, *posi, *hTs, rstd, ta, tb, tc, cos_r, sin_r, cos_m, sin_m, sql, rstl, *raw, *t1,
              *t2, rqT, rkT, v_tm, sg_tm, kdec, PT, qsT, S_f, S_b, sqo, tn, og, oT, sqT)


def phase_MKV(k):
    S, d, c, P, ps, psk = k.S, k.d, k.c, k.P, k.ps, k.psk
    w_xkv = S.alloc("w_xkv_bf", [128, 8, 2 * D], BF16)
    S.dma("pool", w_xkv.ap, d["w_xkv"].rearrange("(c p) n -> p c n", p=128), W=w_xkv.all(), key="w_xkv")
    mt = S.alloc("memT", [128, 8, MEM], F32)
    S.dma("sp", mt.ap, d["memT"].rearrange("(c p) t -> p c t", p=128), W=mt.all(), key="memT")
    hm = S.alloc("hmem", [128, 8, MEM], BF16)
    sd, rstd = S.alloc("sdm", [128, MEM], F32), S.alloc("rstdm", [128, MEM], F32)
    squares8(k, hm, mt, MEM)
    rms_stats(k, lambda i: hm.ap[:, i, :], 8, D, 0, sd, rstd, MEM, hm.all())
    for ci in range(8):
        stt(k, hm.ap[:, ci, :], mt.ap[:, ci, :], c.gv.ap[:, c.g_mem + ci:c.g_mem + ci + 1], rstd.ap, ALU.mult, ALU.mult,
            mt.all() + rstd.all() + c.gv.all(), hm.all())
    for blk in range(8):
        pi = 1 + blk % 2
        for ci in range(8):
            mm(k, ps[pi][:, 0:MEM], w_xkv.ap[:, ci, blk * 128:(blk + 1) * 128], hm.ap[:, ci, :], ci == 0, ci == 7,
               w_xkv.all() + hm.all(), [psk[pi]])
        cp(k, "act", P.memKT.ap[:, blk, :], ps[pi][:, 0:MEM], [psk[pi]], P.memKT.all())
    n = 0
    for kb2 in range(2):
        for half in range(2):
            pi = 3 + n % 2
            n += 1
            for ci in range(8):
                mm(k, ps[pi], hm.ap[:, ci, kb2 * 128:(kb2 + 1) * 128], w_xkv.ap[:, ci, D + half * 512:D + (half + 1) * 512],
                   ci == 0, ci == 7, w_xkv.all() + hm.all(), [psk[pi]])
            cp(k, "dve", P.memV.ap[:, kb2, half * 512:(half + 1) * 512], ps[pi], [psk[pi]], P.memV.all())
    S.release(w_xkv, mt, hm, sd, rstd)


def phase_A2(k):
    S, d, c, P, ps, psk = k.S, k.d, k.c, k.P, k.ps, k.psk
    ps2 = k.ps2
    w_uq = S.alloc("w_uq_bf", [128, 2, 768], BF16)
    w_uqr = S.alloc("w_uqr_bf", [128, 2, 768], BF16)
    w_ukv = S.alloc("w_ukv_bf", [128, 1024], BF16)
    S.dma("pool", w_uq.ap, d["w_uq"].rearrange("(c p) n -> p c n", p=128), W=w_uq.all(), key="w_uq")
    S.dma("pool", w_ukv.ap, d["w_ukv"], W=w_ukv.all(), key="w_ukv")
    S.op("pool", lambda e: e.memset(w_uqr.ap, 0.0), W=w_uqr.all())
    q4 = w_uq.ap.rearrange("p c (h x) -> p c h x", h=8)
    r4 = w_uqr.ap.rearrange("p c (h x) -> p c h x", h=8)
    for ci in range(2):
        ts(k, "pool", r4[:, ci, :, 64:80], q4[:, ci, :, 80:96], -1.0, None, ALU.mult, None, w_uq.all(), w_uqr.all())
        cp(k, "pool", r4[:, ci, :, 80:96], q4[:, ci, :, 64:80], w_uq.all(), w_uqr.all())
    KT = S.alloc("KT", [128, 4, NTOK], BF16, nsub=4)
    Vc = S.alloc("Vc", [128, 32, 384], BF16)
    S.op("pool", lambda e: e.memset(Vc.ap, 1.0), W=Vc.all())
    for o in (64, 256):
        ts(k, "dve", Vc.ap[:, 0:16, o:o + 64], Vc.ap[:, 0:16, o:o + 64], c.flags.ap[:, 0:1], None, ALU.mult, None,
           Vc.all() + c.flags.all(), Vc.all())
    qT = [S.alloc(f"qT{i}", [128, 512], BF16) for i in range(4)]
    for q_ in qT:
        S.op("pool", lambda e, q_=q_: e.memset(q_.ap[96:128, :], 0.0), W=q_.all())
    S.op("pool", lambda e: e.memset(KT.ap[96:128, :, :], 0.0), W=KT.all())
    PTb = [S.alloc(f"PTb{i}", [128, 1024], BF16) for i in range(2)]
    tq1, tq2 = S.alloc("tq1", [128, 512], F32), S.alloc("tq2", [128, 512], F32)
    ta, tb, tc = (S.alloc(n, [128, 512], F32) for n in ("ta2", "tb2", "tc2"))
    cos_m, sin_m = S.alloc("cos_m2", [128, 512], F32), S.alloc("sin_m2", [128, 512], F32)
    pq = S.alloc("posq", [128, 512], I32)
    rec = S.alloc("rec", [128, 512], F32)
    wk3 = w_ukv.ap.rearrange("p (h x) -> p h x", h=8)
    scale = 1.0 / math.sqrt(96.0)
    n_ev = 0
    for hh in range(2):
        heads = list(range(4 * hh, 4 * hh + 4))
        for kt in range(NTOK // 512):
            for hi, h in enumerate(heads):
                pi = n_ev % 2
                mm(k, ps[pi], w_ukv.ap[:, h * 128:h * 128 + 128], P.ckvn.ap[:, kt * 512:(kt + 1) * 512], True, True,
                   w_ukv.all() + P.ckvn.all(), [psk[pi]])
                cp(k, "act" if n_ev % 2 == 0 else "dve", KT.ap[0:64, hi, kt * 512:(kt + 1) * 512], ps[pi][0:64, :], [psk[pi]],
                   [KT.k(hi)])
                n_ev += 1
        for hi in range(4):
            cp(k, "pool", KT.ap[64:96, hi, :], P.krope.ap[64:96, :], P.krope.all(), [KT.k(hi)])
        for kb in range(NTOK // 128):
            pi = 2 + kb % 2
            mm(k, ps[pi], P.ckvn.ap[:, kb * 128:(kb + 1) * 128], w_ukv.ap[:, 512 * hh:512 * (hh + 1)], True, True,
               w_ukv.all() + P.ckvn.all(), [psk[pi]])
            src = ps[pi].rearrange("p (a m x) -> p a m x", a=2, m=2)
            dst = Vc.ap[:, kb, :].rearrange("p (a r) -> p a r", a=2)
            cp(k, "dve", dst[:, :, 0:64], src[:, :, 0, 64:128], [psk[pi]], Vc.all())
            cp(k, "dve", dst[:, :, 128:192], src[:, :, 1, 64:128], [psk[pi]], Vc.all())
        for qi in range(5):
            if qi == 0:
                s0, NQ, qblk0 = 0, 128, HALO0 // 128
            else:
                s0, NQ, qblk0 = 128 + 512 * (qi - 1), 512, NPRE // 128 + 4 * (qi - 1)
            nqb = NQ // 128
            g0 = HALO0 + s0
            S.dma("sp", pq.ap[:, 0:NQ], d["posi"][:, g0:g0 + NQ].partition_broadcast(128), W=pq.all(), key="posq")
            rope_tables(k, pq.ap, pq.k(), (64, 96), 1, cos_m, sin_m, (ta, tb, tc), NQ)
            for hi, h in enumerate(heads):
                for ci in range(2):
                    mm(k, ps[0][0:96, 0:NQ], w_uq.ap[:, ci, h * 96:(h + 1) * 96], P.cqn.ap[:, ci, s0:s0 + NQ], ci == 0, ci == 1,
                       w_uq.all() + P.cqn.all(), [psk[0]])
                for ci in range(2):
                    mm(k, ps[1][0:96, 0:NQ], w_uqr.ap[:, ci, h * 96:(h + 1) * 96], P.cqn.ap[:, ci, s0:s0 + NQ], ci == 0, ci == 1,
                       w_uqr.all() + P.cqn.all(), [psk[1]])
                cp(k, "act", qT[hi].ap[0:64, 0:NQ], ps[0][0:64, 0:NQ], [psk[0]], qT[hi].all())
                tt(k, "dve", tq1.ap[64:96, 0:NQ], ps[0][64:96, 0:NQ], cos_m.ap[64:96, 0:NQ], ALU.mult, [psk[0]] + cos_m.all(),
                   tq1.all())
                tt(k, "dve", tq2.ap[64:96, 0:NQ], ps[1][64:96, 0:NQ], sin_m.ap[64:96, 0:NQ], ALU.mult, [psk[1]] + sin_m.all(),
                   tq2.all())
                tt(k, "pool", qT[hi].ap[64:96, 0:NQ], tq1.ap[64:96, 0:NQ], tq2.ap[64:96, 0:NQ], ALU.add, tq1.all() + tq2.all(),
                   qT[hi].all())
            for hi, h in enumerate(heads):
                pair, mem = divmod(hi, 2)
                vcol0 = pair * 192 + mem * 64
                pob = 6 + hi % 2
                po = ps[pob]
                nkb = qblk0 + nqb
                groups = []
                kb = 0
                while kb < nkb:
                    if kb + 1 < qblk0:
                        groups.append((kb, kb + 1))
                        kb += 2
                    else:
                        groups.append((kb,))
                        kb += 1
                pend = None

                def pv(pd, nkb=nkb, po=po, pob=pob, vcol0=vcol0, NQ=NQ):
                    for (kb_, qlo_, n_, ptap_, ptk_) in pd:
                        mm(k, po[:, qlo_:NQ], Vc.ap[:, kb_, vcol0:vcol0 + 128], ptap_, kb_ == 0, kb_ == nkb - 1,
                           Vc.all() + ptk_, [psk[pob]])

                for gi, grp in enumerate(groups):
                    slot = gi % 2
                    pt = PTb[slot]
                    banks = (2 + 2 * slot, 3 + 2 * slot)
                    cur = []
                    for j_, kb in enumerate(grp):
                        r = kb - qblk0
                        q_lo = max(r, 0) * 128
                        n = NQ - q_lo
                        sb = banks[j_]
                        mm(k, ps[sb][:, 0:n], KT.ap[:, hi, kb * 128:(kb + 1) * 128], qT[hi].ap[:, q_lo:NQ], True, True,
                           [KT.k(hi)] + qT[hi].all(), [psk[sb]])
                        cur.append((kb, q_lo, n, pt.ap[:, j_ * 512:j_ * 512 + n], pt.all()))
                    if len(grp) == 2 and NQ == 512:
                        act(k, pt.ap, ps2[1 + slot], AF.Exp, [psk[banks[0]], psk[banks[1]]], pt.all(), scale=scale)
                    else:
                        for j_, (kb, q_lo, n, ptap, _) in enumerate(cur):
                            act(k, ptap, ps[banks[j_]][:, 0:n], AF.Exp, [psk[banks[j_]]], pt.all(), scale=scale)
                            if kb - qblk0 >= 0:
                                tt(k, "pool", ptap[:, 0:128], ptap[:, 0:128], c.causal.ap, ALU.mult, pt.all() + c.causal.all(),
                                   pt.all())
                    if pend is not None:
                        pv(pend)
                    pend = cur
                pv(pend)
                if mem == 0:
                    o_rows, s_rows = slice(0, 64), slice(64, 128)
                else:
                    o_rows, s_rows = slice(64, 128), slice(0, 64)
                ts(k, "dve", rec.ap[o_rows, 0:NQ], po[s_rows, 0:NQ], 1e-30, None, ALU.add, None, [psk[pob]], rec.all())
                recip(k, rec.ap[o_rows, 0:NQ], rec.ap[o_rows, 0:NQ], rec.all(), rec.all())
                tt(k, "dve", P.omlaT.ap[o_rows, 2 * hh + pair, s0:s0 + NQ], po[o_rows, 0:NQ], rec.ap[o_rows, 0:NQ], ALU.mult,
                   [psk[pob]] + rec.all(), P.omlaT.all())
    if k.dbg and k.stage == "A2":
        dump(k, "omlaT", P.omlaT.ap, [128, 4, NST], P.omlaT.all(), BF16)
    S.release(w_uq, w_uqr, w_ukv, KT, Vc, *qT, *PTb, tq1, tq2, ta, tb, tc, cos_m, sin_m, pq, rec)


def _norm_to_hT(k, xb, hT, sd, rstd, gcol, n):
    c = k.c
    squares8(k, hT, xb, n)
    rms_stats(k, lambda i: hT.ap[:, i, 0:n], 8, D, 0, sd, rstd, n, hT.all())
    for ci in range(8):
        stt(k, hT.ap[:, ci, 0:n], xb.ap[:, ci, 0:n], c.gv.ap[:, gcol + ci:gcol + ci + 1], rstd.ap[:, 0:n], ALU.mult, ALU.mult,
            xb.all() + rstd.all() + c.gv.all(), hT.all())


def phase_A3X(k):
    S, d, c, P, ps, psk = k.S, k.d, k.c, k.P, k.ps, k.psk
    xb = S.alloc("xa", [128, 8, 512], F32)
    hT = S.alloc("hTa", [128, 8, 512], BF16)
    sd, rstd = S.alloc("sda", [128, 512], F32), S.alloc("rstda", [128, 512], F32)
    qxT = S.alloc("qxT", [128, 8, 512], BF16, nsub=8)
    PTx = [S.alloc(f"PTx{i}", [128, 512], BF16) for i in range(2)]
    oxT = S.alloc("oxT", [128, 8, 512], BF16)
    rec = S.alloc("recx", [128, 512], F32)
    xTv = d["xT"].rearrange("(c p) t -> p c t", p=128)
    x2v = d["x2s"].rearrange("(c p) t -> p c t", p=128)
    tiles = [(0, 128)] + [(128 + 512 * i, 512) for i in range(4)]
    for (s0, N) in tiles:
        g0 = HALO0 + s0
        S.dma("sp", xb.ap[:, :, 0:N], xTv[:, :, g0:g0 + N], W=xb.all(), key="xa")
        for cb in range(8):
            pi = 1 + cb % 2
            cs = slice(cb * 128, (cb + 1) * 128)
            for j in range(4):
                mm(k, ps[pi][:, 0:N], P.w_out.ap[:, j, cs], P.omlaT.ap[:, j, s0:s0 + N], j == 0, False,
                   P.w_out.all() + P.omlaT.all(), [psk[pi]])
            for j in range(4):
                mm(k, ps[pi][:, 0:N], P.w_out.ap[:, 4 + j, cs], P.oretT.ap[:, j, s0:s0 + N], False, j == 3,
                   P.w_out.all() + P.oretT.all(), [psk[pi]])
            tt(k, "dve", xb.ap[:, cb, 0:N], xb.ap[:, cb, 0:N], ps[pi][:, 0:N], ALU.add, xb.all() + [psk[pi]], xb.all())
        if k.dbg and k.stage == "A3X":
            dumpx(k, "x1", xb, s0, N)
        _norm_to_hT(k, xb, hT, sd, rstd, c.g_xattn, N)
        for blk in range(8):
            pi = 1 + blk % 2
            for ci in range(8):
                mm(k, ps[pi][:, 0:N], P.w_xq.ap[:, ci, blk * 128:(blk + 1) * 128], hT.ap[:, ci, 0:N], ci == 0, ci == 7,
                   P.w_xq.all() + hT.all(), [psk[pi]])
            cp(k, "act", qxT.ap[:, blk, 0:N], ps[pi][:, 0:N], [psk[pi]], [qxT.k(blk)])
        for h in range(4):
            for kb2 in range(2):
                sb = 3 + kb2
                for dc in range(2):
                    mm(k, ps[sb][:, 0:N], P.memKT.ap[:, 2 * h + dc, kb2 * 128:(kb2 + 1) * 128], qxT.ap[:, 2 * h + dc, 0:N],
                       dc == 0, dc == 1, P.memKT.all() + [qxT.k(2 * h + dc)], [psk[sb]])
                act(k, PTx[kb2].ap[:, 0:N], ps[sb][:, 0:N], AF.Exp, [psk[sb]], PTx[kb2].all(), scale=1.0 / 16.0)
            for kb2 in range(2):
                mm(k, ps[5][:, 0:N], c.ones.ap, PTx[kb2].ap[:, 0:N], kb2 == 0, kb2 == 1, c.ones.all() + PTx[kb2].all(), [psk[5]])
            act(k, rec.ap[:, 0:N], ps[5][:, 0:N], AF.Ln, [psk[5]], rec.all())
            act(k, rec.ap[:, 0:N], rec.ap[:, 0:N], AF.Exp, rec.all(), rec.all(), scale=-1.0)
            for eb in range(2):
                pi = 6 + eb
                for kb2 in range(2):
                    mm(k, ps[pi][:, 0:N], P.memV.ap[:, kb2, h * 256 + eb * 128:h * 256 + (eb + 1) * 128], PTx[kb2].ap[:, 0:N],
                       kb2 == 0, kb2 == 1, P.memV.all() + PTx[kb2].all(), [psk[pi]])
                tt(k, "dve", oxT.ap[:, 2 * h + eb, 0:N], ps[pi][:, 0:N], rec.ap[:, 0:N], ALU.mult, [psk[pi]] + rec.all(), oxT.all())
        for cb in range(8):
            pi = 1 + cb % 2
            for j in range(8):
                mm(k, ps[pi][:, 0:N], P.w_xo.ap[:, j, cb * 128:(cb + 1) * 128], oxT.ap[:, j, 0:N], j == 0, j == 7,
                   P.w_xo.all() + oxT.all(), [psk[pi]])
            tt(k, "dve", xb.ap[:, cb, 0:N], xb.ap[:, cb, 0:N], ps[pi][:, 0:N], ALU.add, xb.all() + [psk[pi]], xb.all())
        S.dma("sp", x2v[:, :, s0:s0 + N], xb.ap[:, :, 0:N], R=xb.all(), W=[("x2s", s0)], key="xs")
        if k.dbg and k.stage == "A3X":
            dumpx(k, "x2", xb, s0, N)
    S.release(xb, hT, sd, rstd, qxT, *PTx, oxT, rec)


def dumpx(k, name, xb, s0, N):
    if name not in k.dbg_out:
        k.dbg_out[name] = k.nc.dram_tensor("dbg_" + name, [D, NST], F32, kind="ExternalOutput").ap()
    t = k.dbg_out[name].rearrange("(c p) t -> p c t", p=128)
    k.S.dma("sp", t[:, :, s0:s0 + N], xb.ap[:, :, 0:N], R=xb.all(), W=[("dbg", name, s0)], key="dbg")


def phase_F(k):
    S, d, c, P, ps, psk = k.S, k.d, k.c, k.P, k.ps, k.psk
    xf = S.alloc("xf", [128, 8, 512], F32)
    hT = S.alloc("hTf", [128, 8, 512], BF16)
    sd, rstd = S.alloc("sdf", [128, 512], F32), S.alloc("rstdf", [128, 512], F32)
    aT = S.alloc("aT", [128, NFB, 512], BF16, nsub=NFB)
    gsb = [S.alloc(f"gsb{i}", [128, 48 + 512], F32) for i in range(2)]
    cv = [S.alloc(f"cv{i}", [128, 512], F32) for i in range(2)]
    ghalo = S.alloc("ghalo", [128, NFB, 48], F32, nsub=NFB)
    x2v = d["x2s"].rearrange("(c p) t -> p c t", p=128)
    outv = d["outT"].rearrange("(c p) t -> p c t", p=128)
    w1 = P.w_ffn_in
    S.dma("sp", xf.ap[:, :, 0:128], x2v[:, :, 0:128], R=[("x2s", 0)], W=xf.all(), key="xf")
    _norm_to_hT(k, xf, hT, sd, rstd, c.g_ffn, 128)
    for j in range(NFB):
        pi = 1 + j % 2
        for ci in range(8):
            mm(k, ps[pi][:, 0:128], w1.ap[:, ci, j * 128:(j + 1) * 128], hT.ap[:, ci, 0:128], ci == 0, ci == 7,
               [w1.k(0 if j < 11 else 2)] + hT.all(), [psk[pi]])
        ts(k, "dve", ghalo.ap[:, j, :], ps[pi][:, 80:128], c.flags.ap[:, 1:2], None, ALU.mult, None, [psk[pi]] + c.flags.all(),
           [ghalo.k(j)])
    for ti in range(4):
        s0 = 128 + 512 * ti
        S.dma("sp", xf.ap, x2v[:, :, s0:s0 + 512], R=[("x2s", s0)], W=xf.all(), key="xf")
        _norm_to_hT(k, xf, hT, sd, rstd, c.g_ffn, 512)
        for j in range(NFB):
            pg, pu = 1 + (j % 2), (3, 4, 7)[j % 3]
            for ci in range(8):
                mm(k, ps[pg], w1.ap[:, ci, j * 128:(j + 1) * 128], hT.ap[:, ci, :], ci == 0, ci == 7,
                   [w1.k(0 if j < 11 else 2)] + hT.all(), [psk[pg]])
            for ci in range(8):
                mm(k, ps[pu], w1.ap[:, ci, DFF + j * 128:DFF + (j + 1) * 128], hT.ap[:, ci, :], ci == 0, ci == 7,
                   [w1.k(1 if j < 11 else 3)] + hT.all(), [psk[pu]])
            g, cvb = gsb[j % 2], cv[j % 2]
            cw = c.convp.ap
            cp(k, "act", g.ap[:, 48:560], ps[pg], [psk[pg]], g.all())
            cp(k, "pool", g.ap[:, 0:48], ghalo.ap[:, j, :], [ghalo.k(j)], g.all())
            k.S.op("act", lambda e, g=g, cvb=cvb, j=j: e.activation(out=cvb.ap, in_=g.ap[:, 48:560], func=AF.Identity,
                                                                     scale=cw[:, j, 2:3], bias=cw[:, j, 3:4]),
                   g.all() + c.convp.all(), cvb.all())
            stt(k, cvb.ap, g.ap[:, 47:559], cw[:, j, 1:2], cvb.ap, ALU.mult, ALU.add, g.all() + cvb.all() + c.convp.all(), cvb.all())
            stt(k, cvb.ap, g.ap[:, 46:558], cw[:, j, 0:1], cvb.ap, ALU.mult, ALU.add, g.all() + cvb.all() + c.convp.all(), cvb.all())
            cp(k, "pool", ghalo.ap[:, j, :], g.ap[:, 512:560], g.all(), [ghalo.k(j)])
            act(k, cvb.ap, cvb.ap, AF.Silu, cvb.all(), cvb.all())
            tt(k, "dve", aT.ap[:, j, :], cvb.ap, ps[pu], ALU.mult, cvb.all() + [psk[pu]], [aT.k(j)])
        for cb in range(8):
            pi = 5 + cb % 2
            for j in range(NFB):
                mm(k, ps[pi], P.w_ffn_out.ap[:, j, cb * 128:(cb + 1) * 128], aT.ap[:, j, :], j == 0, j == NFB - 1,
                   P.w_ffn_out.all() + [aT.k(j)], [psk[pi]])
            tt(k, "dve", xf.ap[:, cb, :], xf.ap[:, cb, :], ps[pi], ALU.add, xf.all() + [psk[pi]], xf.all())
        squares8(k, hT, xf, 512)
        rms_stats(k, lambda i: hT.ap[:, i, :], 8, D, 0, sd, rstd, 512, hT.all())
        for cb in range(8):
            stt(k, xf.ap[:, cb, :], xf.ap[:, cb, :], c.gv.ap[:, c.g_final + cb:c.g_final + cb + 1], rstd.ap, ALU.mult, ALU.mult,
                xf.all() + rstd.all() + c.gv.all(), xf.all())
        S.dma("sp", outv[:, :, 512 * ti:512 * (ti + 1)], xf.ap, R=xf.all(), W=[("out", ti)], key="out")
    S.release(xf, hT, sd, rstd, aT, *gsb, *cv, ghalo)


def _consts():
    f32 = np.float32
    H, L = 4, 128
    log_gamma = np.log(f32(1.0) - f32(2.0) ** (f32(-5.0) - np.arange(H, dtype=f32))).astype(f32)
    j = np.arange(L, dtype=f32)
    diff = j[:, None] - j[None, :]
    intra = np.where(diff[None] >= 0, np.exp(np.maximum(diff, 0.0)[None] * log_gamma[:, None, None]), 0.0).astype(f32)
    k_to_end = np.exp((L - 1 - j)[:, None] * log_gamma[None, :]).astype(f32)
    q_from_start = np.exp((j + 1)[:, None] * log_gamma[None, :]).astype(f32)
    chunk_decay = np.exp(f32(L) * log_gamma).astype(f32)
    dk = f32(128.0 ** -0.5)
    c_intra = np.zeros((128, 512), f32)
    for h in range(H):
        c_intra[:, h * 128:(h + 1) * 128] = intra[h].T * dk
    c_qfs = np.zeros((128, 512), f32)
    c_decay = np.zeros((128, 512), f32)
    for h in range(H):
        c_qfs[:, h * 128:(h + 1) * 128] = q_from_start[:, h][None, :]
        c_decay[:, h * 128:(h + 1) * 128] = chunk_decay[h]
    c_small = np.zeros((128, 8), f32)
    invf_r = (1.0 / (f32(10000.0) ** (np.arange(0, 128, 2, dtype=f32) / f32(128)))).astype(f32)
    invf_m = (1.0 / (f32(10000.0) ** (np.arange(0, 32, 2, dtype=f32) / f32(32)))).astype(f32)
    p = np.arange(128)
    c_small[:, 0] = invf_r[p % 64]
    c_small[:, 1] = invf_m[p % 16]
    c_small[:, 2:6] = k_to_end * dk
    c_ident = np.eye(128, dtype=f32)
    c_rot = np.zeros((128, 128), f32)
    for m in range(64):
        c_rot[m + 64, m] = -1.0
    for m in range(64, 128):
        c_rot[m - 64, m] = 1.0
    kk = np.arange(128)
    c_causal = (kk[None, :] >= kk[:, None]).astype(f32)
    return dict(c_small=c_small, c_ident=c_ident, c_rot=c_rot, c_causal=c_causal, c_intra=c_intra, c_qfs=c_qfs,
                c_decay=c_decay)


def make_in_maps(inputs):
    f32 = np.float32
    x = np.asarray(inputs["x"], f32)
    mem = np.asarray(inputs["mem"], f32)
    pos = np.asarray(inputs["positions"], np.int32)

    def col(g):
        g = np.asarray(g, f32).reshape(-1, 128)
        return np.ascontiguousarray(g.T)

    gv = np.concatenate([col(inputs["g_mix"][0]), col(inputs["g_xattn"][0]), col(inputs["g_mem"][0]),
                         col(inputs["g_ffn"][0]), col(inputs["g_final"]), col(inputs["g_q_lat"][0]),
                         col(inputs["g_kv_lat"][0])], axis=1)
    cw = np.asarray(inputs["conv_w"][0], f32)
    cb = np.asarray(inputs["conv_b"][0], f32)
    convp = np.zeros((128, NFB, 4), f32)
    for i in range(3):
        convp[:, :, i] = cw[i].reshape(NFB, 128).T
    convp[:, :, 3] = cb.reshape(NFB, 128).T
    shared = dict(
        w_in=np.ascontiguousarray(inputs["w_in"][0], f32), w_uq=np.ascontiguousarray(inputs["w_uq"][0], f32),
        w_ukv=np.ascontiguousarray(inputs["w_ukv"][0], f32), w_out=np.ascontiguousarray(inputs["w_out"][0], f32),
        w_xq=np.ascontiguousarray(inputs["w_xq"][0], f32), w_xkv=np.ascontiguousarray(inputs["w_xkv"][0], f32),
        w_xo=np.ascontiguousarray(inputs["w_xo"][0], f32), w_ffn_in=np.ascontiguousarray(inputs["w_ffn_in"][0], f32),
        w_ffn_out=np.ascontiguousarray(inputs["w_ffn_out"][0], f32), gv=np.ascontiguousarray(gv),
        convp=np.ascontiguousarray(convp.reshape(128, NFB * 4)), **_consts())
    maps = []
    for core in range(8):
        b, hf = core // 2, core % 2
        xT = np.zeros((D, NTOK), f32)
        pp = np.zeros((1, NTOK), np.int32)
        if hf == 0:
            xT[:, NPRE:] = x[b, :NOWN].T
            pp[0, NPRE:] = pos[b, :NOWN]
        else:
            xT[:, :] = x[b].T
            pp[0, :] = pos[b]
        flags = np.full((128, 2), float(hf), f32)
        m = dict(shared)
        m.update(xT=xT, posi=pp, memT=np.ascontiguousarray(mem[b].T), flags=flags)
        maps.append(m)
    return maps


_CACHE = {}


def kernel(**inputs):
    if "nc" not in _CACHE:
        _CACHE["nc"] = build("full")[0]
    nc = _CACHE["nc"]
    maps = make_in_maps(inputs)
    res = run_bass_kernel_spmd(nc, maps, core_ids=list(range(8)))
    out = np.zeros((NB, SEQ, D), np.float32)
    for core in range(8):
        b, hf = core // 2, core % 2
        out[b, hf * NOWN:(hf + 1) * NOWN, :] = res.results[core]["outT"].T
    return out
```

```python
import math
from contextlib import ExitStack

import numpy as np
import concourse.bass as bass
import concourse.mybir as mybir
from concourse.bass_utils import run_bass_kernel_spmd

F32 = mybir.dt.float32
BF16 = mybir.dt.bfloat16
I32 = mybir.dt.int32
U8 = mybir.dt.uint8
AF = mybir.ActivationFunctionType
ALU = mybir.AluOpType
AX = mybir.AxisListType

D = 1024
SEQ = 4096
NB = 4
EPS = 1e-6
NPRE = 2048
NOWN = 2048
NTOK = NPRE + NOWN
HALO0 = NPRE - 128
NST = NOWN + 128
IN_COLS = 2464
OFF_CQ, OFF_CKV, OFF_KR, OFF_RQ, OFF_RK, OFF_RV, OFF_RG = 0, 256, 384, 416, 928, 1440, 1952
DFF = 2816
NFB = DFF // 128
MEM = 256
TWO_PI = 2.0 * math.pi
C1 = 6.28125
C2 = TWO_PI - C1
MAGIC = 12582912.0
PI_LO = 3.1415925
ARENA_BYTES = 206 * 1024
ENGS = ("pe", "act", "dve", "pool", "sp")
NDMA_MAX = 90
DT_SIZE = {F32: 4, BF16: 2, I32: 4}


class Buf:
    def __init__(self, uid, name, off, nbytes, ap, nsub):
        self.uid, self.name, self.off, self.nbytes, self.ap, self.nsub = uid, name, off, nbytes, ap, nsub

    def k(self, i=0):
        return (self.uid, i)

    def all(self):
        return [(self.uid, i) for i in range(self.nsub)]

    def __getitem__(self, idx):
        return self.ap[idx]


class Sched:
    def __init__(self, nc, es):
        self.nc, self.es = nc, es
        self.prog = {e: [] for e in ENGS}
        self.sem = {e: es.enter_context(nc.semaphore("s_" + e)) for e in ENGS if e != "sp"}
        self.cnt = {e: 0 for e in ENGS}
        self.seen = {e: {} for e in ENGS}
        self.drained = {e: 0 for e in ENGS}
        self.needed = {e: set() for e in ENGS}
        self.lastw, self.readers = {}, {}
        self.ndma = 0
        self.dma_events = []
        self.dma_pool = [es.enter_context(nc.semaphore(f"d{i}")) for i in range(NDMA_MAX)]
        self.dma_sem = {}
        self.arena = es.enter_context(nc.sbuf_tensor("arena", [128, ARENA_BYTES], U8))
        self.free = [(0, ARENA_BYTES)]
        self.freed_events = []
        self.nbuf = 0
        self.peak = 0
        self.used = 0

    def alloc(self, name, shape, dtype, nsub=1):
        n = 1
        for s in shape[1:]:
            n *= s
        nbytes = (n * DT_SIZE[dtype] + 63) // 64 * 64
        for i, (o, sz) in enumerate(self.free):
            if sz >= nbytes:
                off = o
                if sz == nbytes:
                    self.free.pop(i)
                else:
                    self.free[i] = (o + nbytes, sz - nbytes)
                break
        else:
            raise MemoryError(f"arena full allocating {name} {nbytes}B; free={self.free}")
        ap = self.arena[0:shape[0], off:off + n * DT_SIZE[dtype]].bitcast(dtype)
        if len(shape) == 3:
            ap = ap.rearrange("p (a b) -> p a b", a=shape[1])
        elif len(shape) == 4:
            ap = ap.rearrange("p (a b c) -> p a b c", a=shape[1], b=shape[2])
        self.nbuf += 1
        b = Buf(self.nbuf, name, off, nbytes, ap, nsub)
        evs, keep = [], []
        for (fo, fn, fe) in self.freed_events:
            if fo < off + nbytes and off < fo + fn:
                evs.extend(fe)
            keep.append((fo, fn, fe))
        self.freed_events = keep
        if evs:
            for kk in b.all():
                self.readers[kk] = list(evs)
        self.used += nbytes
        self.peak = max(self.peak, self.used)
        return b

    def release(self, *bufs):
        for b in bufs:
            evs = []
            for kk in b.all():
                if kk in self.lastw:
                    evs.append(self.lastw.pop(kk))
                evs.extend(self.readers.pop(kk, []))
            best = {}
            for (s, v) in evs:
                best[s] = max(best.get(s, 0), v)
            self.freed_events.append((b.off, b.nbytes, list(best.items())))
            self.free.append((b.off, b.nbytes))
            self.free.sort()
            merged = []
            for (o, sz) in self.free:
                if merged and merged[-1][0] + merged[-1][1] == o:
                    merged[-1] = (merged[-1][0], merged[-1][1] + sz)
                else:
                    merged.append((o, sz))
            self.free = merged
            self.used -= b.nbytes

    def _deps(self, eng, R, W):
        best = {}
        for r in R:
            ev = self.lastw.get(r)
            if ev is not None:
                best[ev[0]] = max(best.get(ev[0], 0), ev[1])
        for w in W:
            ev = self.lastw.get(w)
            if ev is not None:
                best[ev[0]] = max(best.get(ev[0], 0), ev[1])
            for ev in self.readers.get(w, ()):
                best[ev[0]] = max(best.get(ev[0], 0), ev[1])
        for s, v in best.items():
            if s == eng:
                if eng == "pe":
                    continue
                if eng in ("act", "dve"):
                    if self.drained[eng] < v:
                        self.prog[eng].append(("drain",))
                        self.drained[eng] = self.cnt[eng]
                    continue
            if self.seen[eng].get(s, 0) >= v:
                continue
            self.seen[eng][s] = v
            self.prog[eng].append(("wait", s, v))
            if isinstance(s, str):
                self.needed[s].add(v)

    def _commit(self, ev, R, W):
        for w in W:
            self.lastw[w] = ev
            self.readers[w] = []
        for r in R:
            if r not in W:
                self.readers.setdefault(r, []).append(ev)

    def merge_keys(self, src_keys, dst_key):
        evs = []
        for kk in src_keys:
            if kk in self.lastw:
                evs.append(self.lastw[kk])
            evs.extend(self.readers.get(kk, []))
        self.readers.setdefault(dst_key, []).extend(evs)

    def op(self, eng, fn, R=(), W=()):
        R, W = list(R), list(W)
        self._deps(eng, R, W)
        self.cnt[eng] += 1
        ev = (eng, self.cnt[eng])
        self.prog[eng].append(("inst", fn, self.cnt[eng]))
        self._commit(ev, R, W)
        return ev

    def dma(self, eng, out, in_, R=(), W=(), key=None):
        R, W = list(R), list(W)
        self._deps(eng, R, W)
        if key is None:
            key = f"_auto{self.ndma}"
        if key not in self.dma_sem:
            self.dma_sem[key] = [self.dma_pool[len(self.dma_sem)], 0]
        ent = self.dma_sem[key]
        sem = ent[0]
        if ent[1] > 0 and self.seen[eng].get(sem, 0) < ent[1]:
            self.seen[eng][sem] = ent[1]
            self.prog[eng].append(("wait", sem, ent[1]))
        ent[1] += 16
        self.ndma += 1
        ev = (sem, ent[1])
        self.prog[eng].append(("dma", out, in_, sem))
        self._commit(ev, R, W)
        self.dma_events.append(ev)
        return ev

    def emit(self, block):
        nc = self.nc
        S = self

        rank = {en: {q: i + 1 for i, q in enumerate(sorted(S.needed[en]))} for en in ENGS}
        S.n_inc = {en: len(rank[en]) for en in ENGS}

        def run(e, name):
            for ent in S.prog[name]:
                if ent[0] == "wait":
                    s = ent[1]
                    if isinstance(s, str):
                        e.wait_ge(S.sem[s], rank[s][ent[2]])
                    else:
                        e.wait_ge(s, ent[2])
                elif ent[0] == "drain":
                    e.drain()
                elif ent[0] == "inst":
                    ins = ent[1](e)
                    if ent[2] in rank[name]:
                        ins.then_inc(S.sem[name], 1)
                else:
                    e.dma_start(out=ent[1], in_=ent[2]).then_inc(ent[3], 16)

        @block.tensor
        def _(e):
            run(e, "pe")

        @block.scalar
        def _(e):
            run(e, "act")

        @block.vector
        def _(e):
            run(e, "dve")

        @block.gpsimd
        def _(e):
            run(e, "pool")

        @block.sync
        def _(e):
            run(e, "sp")

    def final_wait(self, eng, events):
        for (s, v) in events:
            if self.seen[eng].get(s, 0) >= v:
                continue
            self.seen[eng][s] = v
            self.prog[eng].append(("wait", s, v))


class K:
    pass


def bc(ap, shape):
    return ap.broadcast_to(shape)


def build(stage="full", dbg=False, a1_tiles=None, a1_level=9):
    nc = bass.Bass("TRN2", target_bir_lowering=False)
    es = ExitStack()
    k = K()
    k.nc, k.es, k.stage, k.dbg = nc, es, stage, dbg
    k.a1_tiles, k.a1_level = a1_tiles, a1_level
    d = {}

    def din(name, shape, dt=F32):
        d[name] = nc.dram_tensor(name, list(shape), dt, kind="ExternalInput").ap()

    din("xT", [D, NTOK]); din("posi", [1, NTOK], I32); din("memT", [D, MEM]); din("flags", [128, 2])
    din("w_in", [D, IN_COLS]); din("w_uq", [256, 768]); din("w_ukv", [128, 1024]); din("w_out", [D, D])
    din("w_xq", [D, D]); din("w_xkv", [D, 2 * D]); din("w_xo", [D, D])
    din("w_ffn_in", [D, 2 * DFF]); din("w_ffn_out", [DFF, D])
    din("gv", [128, 43]); din("convp", [128, NFB * 4]); din("c_small", [128, 8])
    din("c_ident", [128, 128]); din("c_rot", [128, 128]); din("c_causal", [128, 128])
    din("c_intra", [128, 512]); din("c_qfs", [128, 512]); din("c_decay", [128, 512])
    d["outT"] = nc.dram_tensor("outT", [D, NOWN], F32, kind="ExternalOutput").ap()
    d["x2s"] = nc.dram_tensor("x2s", [D, NST], F32, kind="Internal").ap()
    k.dbg_out = {}
    k.d = d
    with es:
        S = Sched(nc, es)
        k.S = S
        big = [es.enter_context(nc.psum_tensor(f"psb{i}", [128, 1024], F32)) for i in range(4)]
        k.ps2 = [b_[:, :] for b_ in big]
        k.ps = [big[i // 2][:, (i % 2) * 512:(i % 2 + 1) * 512] for i in range(8)]
        k.psk = [("ps", i) for i in range(8)]
        block = es.enter_context(nc.Block())
        emit_all(k)
        S.final_wait("sp", S.dma_events)
        S.emit(block)
    k.peak = S.peak
    return nc, k


def mm(k, out, lhsT, rhs, start, stop, R, W):
    k.S.op("pe", lambda e: e.matmul(out, lhsT=lhsT, rhs=rhs, start=start, stop=stop), R, W)


def tr(k, out, in_, ident, R, W):
    k.S.op("pe", lambda e: e.transpose(out=out, in_=in_, identity=ident), R, W)


def act(k, out, in_, func, R, W, scale=1.0, bias=0.0):
    k.S.op("act", lambda e: e.activation(out=out, in_=in_, func=func, scale=scale, bias=bias), R, W)


def tt(k, eng, out, in0, in1, op, R, W):
    k.S.op(eng, lambda e: e.tensor_tensor(out=out, in0=in0, in1=in1, op=op), R, W)


def ts(k, eng, out, in0, s1, s2, op0, op1, R, W):
    if op1 is None:
        k.S.op(eng, lambda e: e.tensor_scalar(out=out, in0=in0, scalar1=s1, scalar2=None, op0=op0), R, W)
    else:
        k.S.op(eng, lambda e: e.tensor_scalar(out=out, in0=in0, scalar1=s1, scalar2=s2, op0=op0, op1=op1), R, W)


def stt(k, out, in0, scalar, in1, op0, op1, R, W):
    k.S.op("dve", lambda e: e.scalar_tensor_tensor(out=out, in0=in0, scalar=scalar, in1=in1, op0=op0, op1=op1), R, W)


def cp(k, eng, out, in_, R, W):
    if eng == "act":
        k.S.op("act", lambda e: e.copy(out=out, in_=in_), R, W)
    else:
        k.S.op(eng, lambda e: e.tensor_copy(out=out, in_=in_), R, W)


def recip(k, out, in_, R, W):
    k.S.op("dve", lambda e: e.reciprocal(out=out, in_=in_), R, W)


def dump(k, name, buf_ap, shape, R, dt=F32):
    t = k.nc.dram_tensor("dbg_" + name, list(shape), dt, kind="ExternalOutput").ap()
    k.dbg_out[name] = t
    k.S.dma("sp", t, buf_ap, R=R, W=[("dbg", name)], key="dbg")


def load_consts(k):
    S, d = k.S, k.d
    c = K()
    k.c = c
    c.gv = S.alloc("gv", [128, 43], F32)
    c.convp = S.alloc("convp", [128, NFB, 4], F32)
    c.small = S.alloc("c_small", [128, 8], F32)
    c.flags = S.alloc("flags", [128, 2], F32)
    c.ident = S.alloc("ident", [128, 128], BF16)
    c.rot = S.alloc("rot", [128, 128], BF16)
    c.causal = S.alloc("causal", [128, 128], BF16)
    c.intra = S.alloc("intra", [128, 512], F32)
    c.qfs = S.alloc("qfs", [128, 512], F32)
    c.decay = S.alloc("decay", [128, 512], F32)
    c.ones = S.alloc("ones", [128, 128], BF16)
    S.dma("sp", c.gv.ap, d["gv"], W=c.gv.all())
    S.dma("sp", c.convp.ap, d["convp"].rearrange("p (a b) -> p a b", a=NFB), W=c.convp.all())
    S.dma("sp", c.small.ap, d["c_small"], W=c.small.all())
    S.dma("sp", c.flags.ap, d["flags"], W=c.flags.all())
    S.dma("sp", c.intra.ap, d["c_intra"], W=c.intra.all())
    S.dma("sp", c.qfs.ap, d["c_qfs"], W=c.qfs.all())
    S.dma("sp", c.decay.ap, d["c_decay"], W=c.decay.all())
    S.dma("pool", c.ident.ap, d["c_ident"], W=c.ident.all())
    S.dma("pool", c.rot.ap, d["c_rot"], W=c.rot.all())
    S.dma("pool", c.causal.ap, d["c_causal"], W=c.causal.all())
    S.op("pool", lambda e: e.memset(c.ones.ap, 1.0), W=c.ones.all())
    c.g_mix, c.g_xattn, c.g_mem, c.g_ffn, c.g_final, c.g_q, c.g_kv = 0, 8, 16, 24, 32, 40, 42


def rope_tables(k, posi_ap, posi_key, prange, col, cosb, sinb, tmp, n):
    c = k.c
    p0, p1 = prange
    a, b_, kk = tmp
    A = lambda buf: buf.ap[p0:p1, 0:n]
    invf = c.small.ap[p0:p1, col:col + 1]
    ts(k, "dve", A(a), posi_ap[p0:p1, 0:n], invf, None, ALU.mult, None, [posi_key] + c.small.all(), a.all())
    ts(k, "dve", A(b_), A(a), 1.0 / TWO_PI, MAGIC, ALU.mult, ALU.add, a.all(), b_.all())
    ts(k, "dve", A(kk), A(b_), -MAGIC, None, ALU.add, None, b_.all(), kk.all())
    stt(k, A(b_), A(kk), -C1, A(a), ALU.mult, ALU.add, kk.all() + a.all(), b_.all())
    stt(k, A(a), A(kk), -C2, A(b_), ALU.mult, ALU.add, kk.all() + b_.all(), a.all())
    ts(k, "dve", A(a), A(a), -PI_LO, PI_LO, ALU.max, ALU.min, a.all(), a.all())
    act(k, sinb.ap[p0:p1, 0:n], A(a), AF.Sin, a.all(), sinb.all())
    ts(k, "dve", A(b_), A(a), math.pi / 2, -TWO_PI, ALU.is_gt, ALU.mult, a.all(), b_.all())
    stt(k, A(kk), A(a), math.pi / 2, A(b_), ALU.add, ALU.add, a.all() + b_.all(), kk.all())
    ts(k, "dve", A(kk), A(kk), -PI_LO, PI_LO, ALU.max, ALU.min, kk.all(), kk.all())
    act(k, cosb.ap[p0:p1, 0:n], A(kk), AF.Sin, kk.all(), cosb.all())


def squares8(k, dst, src, n):
    tt(k, "pool", dst.ap[:, 0:4, 0:n], src.ap[:, 0:4, 0:n], src.ap[:, 0:4, 0:n], ALU.mult, src.all(), dst.all())
    for ci in range(4, 8):
        act(k, dst.ap[:, ci, 0:n], src.ap[:, ci, 0:n], AF.Square, src.all(), dst.all())


def rms_stats(k, sq_chunks, nch, nfeat, ps_i, sd, rstd, n, sqkeys):
    c = k.c
    ps = k.ps[ps_i]
    for i in range(nch):
        mm(k, ps[:, 0:n], c.ones.ap, sq_chunks(i), i == 0, i == nch - 1, sqkeys + c.ones.all(), [k.psk[ps_i]])
    act(k, sd.ap[:, 0:n], ps[:, 0:n], AF.Ln, [k.psk[ps_i]], sd.all(), scale=1.0 / nfeat, bias=EPS)
    act(k, rstd.ap[:, 0:n], sd.ap[:, 0:n], AF.Exp, sd.all(), rstd.all(), scale=-0.5)


def emit_all(k):
    load_consts(k)
    P = K()
    k.P = P
    S = k.S
    P.cqn = S.alloc("cqn", [128, 2, NST], BF16)
    P.ckvn = S.alloc("ckvn", [128, NTOK], BF16)
    P.krope = S.alloc("krope", [128, NTOK], BF16)
    P.oretT = S.alloc("oretT", [128, 4, NST], BF16)
    phase_A1(k)
    if k.stage == "A1":
        return
    P.memKT = S.alloc("memKT", [128, 8, MEM], BF16)
    P.memV = S.alloc("memV", [128, 2, D], BF16)
    phase_MKV(k)
    P.omlaT = S.alloc("omlaT", [128, 4, NST], BF16)
    P.w_out = S.alloc("w_out_bf", [128, 8, D], BF16)
    P.w_xq = S.alloc("w_xq_bf", [128, 8, D], BF16)
    d = k.d
    phase_A2(k)
    if k.stage == "A2":
        return
    S.release(P.cqn, P.ckvn, P.krope)
    P.w_xo = S.alloc("w_xo_bf", [128, 8, D], BF16)
    S.dma("pool", P.w_xo.ap, d["w_xo"].rearrange("(c p) n -> p c n", p=128), W=P.w_xo.all(), key="w_xo")
    P.w_ffn_out = S.alloc("w_ffn_out_bf", [128, NFB, D], BF16)
    S.dma("pool", P.w_ffn_out.ap, d["w_ffn_out"].rearrange("(c p) n -> p c n", p=128), W=P.w_ffn_out.all(), key="w_ffn_out")
    phase_A3X(k)
    if k.stage == "A3X":
        return
    S.release(P.oretT, P.omlaT, P.w_out, P.w_xq, P.w_xo, P.memKT, P.memV)
    P.w_ffn_in = S.alloc("w_ffn_in_bf", [128, 8, 2 * DFF], BF16, nsub=4)
    wv = d["w_ffn_in"].rearrange("(c p) n -> p c n", p=128)
    HB = 11 * 128
    for sub, (a, b) in enumerate(((0, HB), (DFF, DFF + HB), (HB, DFF), (DFF + HB, 2 * DFF))):
        S.dma("pool", P.w_ffn_in.ap[:, :, a:b], wv[:, :, a:b], W=[P.w_ffn_in.k(sub)], key=f"w_ffn_in{sub}")
    phase_F(k)


def phase_A1(k):
    S, d, c, P = k.S, k.d, k.c, k.P
    ps, psk = k.ps, k.psk
    T = 512
    w_in = S.alloc("w_in_bf", [128, 8, IN_COLS], BF16)
    S.dma("pool", w_in.ap, d["w_in"].rearrange("(c p) n -> p c n", p=128), W=w_in.all(), key="w_in")
    w_krot = S.alloc("w_krot", [128, 8, 96], BF16)
    S.op("pool", lambda e: e.memset(w_krot.ap, 0.0), W=w_krot.all())
    ts(k, "pool", w_krot.ap[:, :, 64:80], w_in.ap[:, :, OFF_KR + 16:OFF_KR + 32], -1.0, None, ALU.mult, None,
       w_in.all(), w_krot.all())
    cp(k, "pool", w_krot.ap[:, :, 80:96], w_in.ap[:, :, OFF_KR:OFF_KR + 16], w_in.all(), w_krot.all())

    xt = [S.alloc(f"xt{i}", [128, 8, T], F32) for i in range(2)]
    posi = [S.alloc(f"posi{i}", [128, T], I32) for i in range(2)]
    hTs = [S.alloc(f"hT{i}", [128, 8, T], BF16) for i in range(2)]
    rstd = S.alloc("rstd", [128, T], F32)
    sd = rstd
    ta, tb, tc = (S.alloc(n, [128, T], F32) for n in ("ta", "tb", "tc"))
    cos_r, sin_r = S.alloc("cos_r", [128, T], F32), S.alloc("sin_r", [128, T], F32)
    cos_m, sin_m = S.alloc("cos_m", [128, T], F32), S.alloc("sin_m", [128, T], F32)
    sql = S.alloc("sql", [128, 2, T], BF16)
    rstl = S.alloc("rstl", [128, T], F32)
    sdl = rstl
    raw = [S.alloc(f"raw{i}", [128, T], BF16) for i in range(2)]
    t1 = [S.alloc(f"t1_{i}", [128, T], F32) for i in range(2)]
    t2 = [S.alloc(f"t2_{i}", [128, T], F32) for i in range(2)]
    rqT = S.alloc("rqT", [128, 4, T], BF16, nsub=4)
    rkT = S.alloc("rkT", [128, 4, T], BF16, nsub=4)
    v_tm = S.alloc("v_tm", [128, 4, 512], BF16, nsub=4)
    sg_tm = S.alloc("sg_tm", [128, 4, 512], BF16, nsub=4)
    kdec = S.alloc("kdec", [128, 512], BF16)
    PT = S.alloc("PT", [128, 512], BF16)
    qsT = S.alloc("qsT", [128, 4, 128], BF16)
    S_f = S.alloc("S_f", [128, 512], F32)
    S_b = S.alloc("S_b", [128, 512], BF16)
    sqo = S.alloc("sqo", [128, 512], F32)
    tn = S.alloc("tn", [128, 512], F32)
    ogs = [S.alloc(f"og{i}", [128, 512], BF16) for i in range(2)]
    oT = S.alloc("oT", [128, 512], BF16)
    sqT = S.alloc("sqT", [128, 512], BF16)
    S.op("pool", lambda e: e.memset(S_f.ap, 0.0), W=S_f.all())
    S.op("pool", lambda e: e.memset(S_b.ap, 0.0), W=S_b.all())

    xTv = d["xT"].rearrange("(c p) t -> p c t", p=128)
    ntiles = NTOK // T
    rr = 0
    tile_list = list(range(ntiles)) if k.a1_tiles is None else k.a1_tiles

    def load_and_norm(tti):
        tok0 = tti * T
        xb, pb, hT = xt[tti % 2], posi[tti % 2], hTs[tti % 2]
        S.dma("sp", xb.ap, xTv[:, :, tok0:tok0 + T], W=xb.all(), key=f"xt{tti % 2}")
        S.dma("sp", pb.ap, d["posi"][:, tok0:tok0 + T].partition_broadcast(128), W=pb.all(), key=f"posi{tti % 2}")
        squares8(k, hT, xb, T)
        rms_stats(k, lambda i: hT.ap[:, i, :], 8, D, 0, sd, rstd, T, hT.all())
        for ci in range(8):
            stt(k, hT.ap[:, ci, :], xb.ap[:, ci, :], c.gv.ap[:, c.g_mix + ci:c.g_mix + ci + 1], rstd.ap,
                ALU.mult, ALU.mult, xb.all() + rstd.all() + c.gv.all(), hT.all())

    load_and_norm(tile_list[0])
    for tidx, tti in enumerate(tile_list):
        tok0 = tti * T
        xb, pb, hT = xt[tti % 2], posi[tti % 2], hTs[tti % 2]
        own_tile = tok0 + T > HALO0
        if k.a1_level < 2:
            continue
        rope_tables(k, pb.ap, pb.k(), (0, 128), 0, cos_r, sin_r, (ta, tb, tc), T)
        rope_tables(k, pb.ap, pb.k(), (64, 96), 1, cos_m, sin_m, (ta, tb, tc), T)

        def proj(ps_i, m, wbuf, col0, n=T):
            for ci in range(8):
                mm(k, ps[ps_i][0:m, 0:n], wbuf.ap[:, ci, col0:col0 + m], hT.ap[:, ci, 0:n], ci == 0, ci == 7,
                   wbuf.all() + hT.all(), [psk[ps_i]])

        if k.a1_level < 3:
            continue
        proj(1, 128, w_in, OFF_CKV)
        act(k, sql.ap[:, 0, :], ps[1], AF.Square, [psk[1]], sql.all())
        rms_stats(k, lambda i: sql.ap[:, 0, :], 1, 128, 0, sdl, rstl, T, sql.all())
        stt(k, P.ckvn.ap[:, tok0:tok0 + T], ps[1], c.gv.ap[:, c.g_kv:c.g_kv + 1], rstl.ap, ALU.mult, ALU.mult,
            [psk[1]] + rstl.all() + c.gv.all(), P.ckvn.all())
        proj(2, 96, w_in, OFF_KR - 64)
        proj(3, 96, w_krot, 0)
        tt(k, "dve", t1[0].ap[64:96, :], ps[2][64:96, :], cos_m.ap[64:96, :], ALU.mult, [psk[2]] + cos_m.all(), t1[0].all())
        tt(k, "dve", t2[0].ap[64:96, :], ps[3][64:96, :], sin_m.ap[64:96, :], ALU.mult, [psk[3]] + sin_m.all(), t2[0].all())
        tt(k, "pool", P.krope.ap[64:96, tok0:tok0 + T], t1[0].ap[64:96, :], t2[0].ap[64:96, :], ALU.add,
           t1[0].all() + t2[0].all(), P.krope.all())
        if k.a1_level < 4:
            continue
        if own_tile:
            proj(1, 128, w_in, OFF_CQ)
            proj(2, 128, w_in, OFF_CQ + 128)
            act(k, sql.ap[:, 0, :], ps[1], AF.Square, [psk[1]], sql.all())
            act(k, sql.ap[:, 1, :], ps[2], AF.Square, [psk[2]], sql.all())
            rms_stats(k, lambda i: sql.ap[:, i, :], 2, 256, 0, sdl, rstl, T, sql.all())
            if tok0 < HALO0:
                lo, n_, s0 = HALO0 - tok0, 128, 0
            else:
                lo, n_, s0 = 0, T, tok0 - HALO0
            for j, pi in ((0, 1), (1, 2)):
                stt(k, P.cqn.ap[:, j, s0:s0 + n_], ps[pi][:, lo:lo + n_], c.gv.ap[:, c.g_q + j:c.g_q + j + 1],
                    rstl.ap[:, lo:lo + n_], ALU.mult, ALU.mult, [psk[pi]] + rstl.all() + c.gv.all(), P.cqn.all())
        if k.a1_level < 5:
            continue
        todo = [(rkT, OFF_RK)] + ([(rqT, OFF_RQ)] if own_tile else [])
        items = [(dst, off, h) for (dst, off) in todo for h in range(4)]

        def rope_tail(it, slot):
            dst, off, h = it
            pi, pj = 1 + slot, 3 + slot
            rb, a1, a2 = raw[slot], t1[slot], t2[slot]
            mm(k, ps[pj], c.rot.ap, rb.ap, True, True, rb.all() + c.rot.all(), [psk[pj]])
            tt(k, "dve", a2.ap, ps[pj], sin_r.ap, ALU.mult, [psk[pj]] + sin_r.all(), a2.all())
            tt(k, "pool", a1.ap, rb.ap, cos_r.ap, ALU.mult, rb.all() + cos_r.all(), a1.all())
            tt(k, "pool", dst.ap[:, h, :], a1.ap, a2.ap, ALU.add, a1.all() + a2.all(), [dst.k(h)])

        prev = None
        for n_, it in enumerate(items):
            slot = n_ % 2
            proj(1 + slot, 128, w_in, it[1] + it[2] * 128)
            cp(k, "act", raw[slot].ap, ps[1 + slot], [psk[1 + slot]], raw[slot].all())
            if prev is not None:
                rope_tail(*prev)
            prev = (it, slot)
        rope_tail(*prev)
        if k.a1_level < 6:
            continue
        if own_tile:
            for h in range(4):
                pi = 1 + h % 2
                proj(pi, 128, w_in, OFF_RG + h * 128)
                act(k, sg_tm.ap[:, h, :], ps[pi], AF.Silu, [psk[pi]], [sg_tm.k(h)])
        if tidx + 1 < len(tile_list):
            load_and_norm(tile_list[tidx + 1])
        p7b = ps[7].bitcast(BF16)
        k7a, k7b = ("ps", "7a"), ("ps", "7b")

        def stage_a(cc):
            ctok = tok0 + cc * 128
            own_chunk = ctok >= HALO0
            cs = slice(cc * 128, (cc + 1) * 128)
            for ci in range(8):
                mm(k, ps[1], hT.ap[:, ci, cs], w_in.ap[:, ci, OFF_RV:OFF_RV + 512], ci == 0, ci == 7,
                   hT.all() + w_in.all(), [psk[1]])
            cp(k, "act", v_tm.ap[:, cc, :], ps[1], [psk[1]], [v_tm.k(cc)])
            for h in range(4):
                tr(k, p7b[:, h * 128:(h + 1) * 128], rkT.ap[:, h, cs], c.ident.ap, [rkT.k(h)] + c.ident.all(), [k7a])
            if own_chunk:
                for h in range(4):
                    mm(k, ps[2][:, h * 128:(h + 1) * 128], rkT.ap[:, h, cs], rqT.ap[:, h, cs], True, True,
                       [rkT.k(h), rqT.k(h)], [psk[2]])
            tt(k, "dve", kdec.ap.rearrange("p (h e) -> p h e", h=4), p7b[:, 0:512].rearrange("p (h e) -> p h e", h=4),
               bc(c.small.ap[:, 2:6].unsqueeze(2), [128, 4, 128]), ALU.mult, [k7a] + c.small.all(), kdec.all())
            if own_chunk:
                tt(k, "dve", PT.ap, ps[2], c.intra.ap, ALU.mult, [psk[2]] + c.intra.all(), PT.all())
                tt(k, "pool", qsT.ap, rqT.ap[:, :, cs], c.qfs.ap.rearrange("p (h e) -> p h e", h=4), ALU.mult,
                   rqT.all() + c.qfs.all(), qsT.all())
            if ctok + 128 < NTOK:
                for h in range(4):
                    hs = slice(h * 128, (h + 1) * 128)
                    mm(k, ps[5][:, hs], kdec.ap[:, hs], v_tm.ap[:, cc, hs], True, True, kdec.all() + [v_tm.k(cc)], [psk[5]])
            if own_chunk:
                for h in range(4):
                    hs = slice(h * 128, (h + 1) * 128)
                    mm(k, ps[6][:, hs], PT.ap[:, hs], v_tm.ap[:, cc, hs], True, False, PT.all() + [v_tm.k(cc)], [psk[6]])
                    mm(k, ps[6][:, hs], qsT.ap[:, h, :], S_b.ap[:, hs], False, True, qsT.all() + S_b.all(), [psk[6]])
                cp(k, "act", ogs[cc % 2].ap, ps[6], [psk[6]], ogs[cc % 2].all())
            if ctok + 128 < NTOK:
                tt(k, "pool", S_f.ap, S_f.ap, c.decay.ap, ALU.mult, S_f.all() + c.decay.all(), S_f.all())
                tt(k, "dve", S_f.ap, S_f.ap, ps[5], ALU.add, S_f.all() + [psk[5]], S_f.all())
                cp(k, "act", S_b.ap, S_f.ap, S_f.all(), S_b.all())

        def stage_b(cc):
            ctok = tok0 + cc * 128
            if ctok < HALO0:
                return
            cs = slice(cc * 128, (cc + 1) * 128)
            og = ogs[cc % 2]
            for h in range(4):
                tr(k, p7b[:, 512 + h * 128:512 + (h + 1) * 128], og.ap[:, h * 128:(h + 1) * 128], c.ident.ap,
                   og.all() + c.ident.all(), [k7b])
            cp(k, "dve", oT.ap, p7b[:, 512:1024], [k7b], oT.all())
            act(k, sqT.ap, oT.ap, AF.Square, oT.all(), sqT.all())
            mm(k, ps[4], c.ones.ap, oT.ap, True, True, c.ones.all() + oT.all(), [psk[4]])
            mm(k, ps[3], c.ones.ap, sqT.ap, True, True, c.ones.all() + sqT.all(), [psk[3]])
            act(k, tn.ap, ps[4], AF.Copy, [psk[4]], tn.all(), scale=1.0 / 128)
            tt(k, "dve", sqo.ap, tn.ap, tn.ap, ALU.mult, tn.all(), sqo.all())
            stt(k, sqo.ap, ps[3], 1.0 / 128, sqo.ap, ALU.mult, ALU.subtract, [psk[3]] + sqo.all(), sqo.all())
            act(k, sqo.ap, sqo.ap, AF.Ln, sqo.all(), sqo.all(), scale=1.0, bias=EPS)
            act(k, sqo.ap, sqo.ap, AF.Exp, sqo.all(), sqo.all(), scale=-0.5)
            tt(k, "dve", tn.ap, oT.ap, tn.ap, ALU.subtract, oT.all() + tn.all(), tn.all())
            tt(k, "pool", tn.ap, tn.ap, sqo.ap, ALU.mult, tn.all() + sqo.all(), tn.all())
            s0 = ctok - HALO0
            tt(k, "pool", P.oretT.ap[:, :, s0:s0 + 128], tn.ap.rearrange("p (h e) -> p h e", h=4), sg_tm.ap[:, :, cs],
               ALU.mult, tn.all() + sg_tm.all(), P.oretT.all())

        for cc in range(5):
            if cc < 4:
                stage_a(cc)
            if cc >= 1:
                stage_b(cc - 1)
    S.merge_keys([("ps", "7a"), ("ps", "7b")], psk[7])
    if k.stage == "A1":
        mm(k, ps[0][:, 0:128], c.ones.ap, c.ones.ap, True, True, c.ones.all(), [psk[0]])
    if k.dbg and k.stage == "A1":
        dump(k, "ckvn", P.ckvn.ap, [128, NTOK], P.ckvn.all(), BF16)
        dump(k, "krope", P.krope.ap[64:96, :], [32, NTOK], P.krope.all(), BF16)
        dump(k, "cqn", P.cqn.ap, [128, 2, NST], P.cqn.all(), BF16)
        dump(k, "oretT", P.oretT.ap, [128, 4, NST], P.oretT.all(), BF16)
    S.release(w_in, w_krot, *xt, *posi, *hTs, rstd, ta, tb, tc, cos_r, sin_r, cos_m, sin_m, sql, rstl, *raw, *t1,
              *t2, rqT, rkT, v_tm, sg_tm, kdec, PT, qsT, S_f, S_b, sqo, tn, *ogs, oT, sqT)


def phase_MKV(k):
    S, d, c, P, ps, psk = k.S, k.d, k.c, k.P, k.ps, k.psk
    w_xkv = S.alloc("w_xkv_bf", [128, 8, 2 * D], BF16)
    S.dma("pool", w_xkv.ap, d["w_xkv"].rearrange("(c p) n -> p c n", p=128), W=w_xkv.all(), key="w_xkv")
    mt = S.alloc("memT", [128, 8, MEM], F32)
    S.dma("sp", mt.ap, d["memT"].rearrange("(c p) t -> p c t", p=128), W=mt.all(), key="memT")
    hm = S.alloc("hmem", [128, 8, MEM], BF16)
    sd, rstd = S.alloc("sdm", [128, MEM], F32), S.alloc("rstdm", [128, MEM], F32)
    squares8(k, hm, mt, MEM)
    rms_stats(k, lambda i: hm.ap[:, i, :], 8, D, 0, sd, rstd, MEM, hm.all())
    for ci in range(8):
        stt(k, hm.ap[:, ci, :], mt.ap[:, ci, :], c.gv.ap[:, c.g_mem + ci:c.g_mem + ci + 1], rstd.ap, ALU.mult, ALU.mult,
            mt.all() + rstd.all() + c.gv.all(), hm.all())
    for blk in range(8):
        pi = 1 + blk % 2
        for ci in range(8):
            mm(k, ps[pi][:, 0:MEM], w_xkv.ap[:, ci, blk * 128:(blk + 1) * 128], hm.ap[:, ci, :], ci == 0, ci == 7,
               w_xkv.all() + hm.all(), [psk[pi]])
        cp(k, "act", P.memKT.ap[:, blk, :], ps[pi][:, 0:MEM], [psk[pi]], P.memKT.all())
    n = 0
    for kb2 in range(2):
        for half in range(2):
            pi = 3 + n % 2
            n += 1
            for ci in range(8):
                mm(k, ps[pi], hm.ap[:, ci, kb2 * 128:(kb2 + 1) * 128], w_xkv.ap[:, ci, D + half * 512:D + (half + 1) * 512],
                   ci == 0, ci == 7, w_xkv.all() + hm.all(), [psk[pi]])
            cp(k, "dve", P.memV.ap[:, kb2, half * 512:(half + 1) * 512], ps[pi], [psk[pi]], P.memV.all())
    S.release(w_xkv, mt, hm, sd, rstd)


def phase_A2(k):
    S, d, c, P, ps, psk = k.S, k.d, k.c, k.P, k.ps, k.psk
    ps2 = k.ps2
    w_uq = S.alloc("w_uq_bf", [128, 2, 768], BF16)
    w_uqr = S.alloc("w_uqr_bf", [128, 2, 768], BF16)
    w_ukv = S.alloc("w_ukv_bf", [128, 1024], BF16)
    S.dma("pool", w_uq.ap, d["w_uq"].rearrange("(c p) n -> p c n", p=128), W=w_uq.all(), key="w_uq")
    S.dma("pool", w_ukv.ap, d["w_ukv"], W=w_ukv.all(), key="w_ukv")
    S.dma("pool", P.w_out.ap, d["w_out"].rearrange("(c p) n -> p c n", p=128), W=P.w_out.all(), key="w_out")
    S.dma("pool", P.w_xq.ap, d["w_xq"].rearrange("(c p) n -> p c n", p=128), W=P.w_xq.all(), key="w_xq")
    S.op("pool", lambda e: e.memset(w_uqr.ap, 0.0), W=w_uqr.all())
    q4 = w_uq.ap.rearrange("p c (h x) -> p c h x", h=8)
    r4 = w_uqr.ap.rearrange("p c (h x) -> p c h x", h=8)
    for ci in range(2):
        ts(k, "pool", r4[:, ci, :, 64:80], q4[:, ci, :, 80:96], -1.0, None, ALU.mult, None, w_uq.all(), w_uqr.all())
        cp(k, "pool", r4[:, ci, :, 80:96], q4[:, ci, :, 64:80], w_uq.all(), w_uqr.all())
    KT = S.alloc("KT", [128, 4, NTOK], BF16, nsub=4)
    Vc = S.alloc("Vc", [128, 32, 384], BF16)
    S.op("pool", lambda e: e.memset(Vc.ap, 1.0), W=Vc.all())
    for o in (64, 256):
        ts(k, "dve", Vc.ap[:, 0:16, o:o + 64], Vc.ap[:, 0:16, o:o + 64], c.flags.ap[:, 0:1], None, ALU.mult, None,
           Vc.all() + c.flags.all(), Vc.all())
    qTb = [[S.alloc(f"qT{b_}_{i}", [128, 512], BF16) for i in range(4)] for b_ in range(2)]
    for q_ in qTb[0] + qTb[1]:
        S.op("pool", lambda e, q_=q_: e.memset(q_.ap[96:128, :], 0.0), W=q_.all())
    S.op("pool", lambda e: e.memset(KT.ap[96:128, :, :], 0.0), W=KT.all())
    PTb = [S.alloc(f"PTb{i}", [128, 1024], BF16) for i in range(2)]
    tq1, tq2 = S.alloc("tq1", [128, 512], F32), S.alloc("tq2", [128, 512], F32)
    ta, tb, tc = (S.alloc(n, [128, 512], F32) for n in ("ta2", "tb2", "tc2"))
    cos_m, sin_m = S.alloc("cos_m2", [128, 512], F32), S.alloc("sin_m2", [128, 512], F32)
    pq = S.alloc("posq", [128, 512], I32)
    rec = S.alloc("rec", [128, 512], F32)
    wk3 = w_ukv.ap.rearrange("p (h x) -> p h x", h=8)
    scale = 1.0 / math.sqrt(96.0)
    n_ev = 0
    for hh in range(2):
        heads = list(range(4 * hh, 4 * hh + 4))
        for kt in range(NTOK // 512):
            for hi, h in enumerate(heads):
                pi = n_ev % 2
                mm(k, ps[pi], w_ukv.ap[:, h * 128:h * 128 + 128], P.ckvn.ap[:, kt * 512:(kt + 1) * 512], True, True,
                   w_ukv.all() + P.ckvn.all(), [psk[pi]])
                cp(k, "act" if n_ev % 2 == 0 else "dve", KT.ap[0:64, hi, kt * 512:(kt + 1) * 512], ps[pi][0:64, :], [psk[pi]],
                   [KT.k(hi)])
                n_ev += 1
        for hi in range(4):
            S.dma("sp", KT.ap[64:96, hi, :], P.krope.ap[64:96, :], R=P.krope.all(), W=[KT.k(hi)], key=f"kr{hi}")
        for kb in range(NTOK // 128):
            pi = 2 + kb % 2
            mm(k, ps[pi], P.ckvn.ap[:, kb * 128:(kb + 1) * 128], w_ukv.ap[:, 512 * hh:512 * (hh + 1)], True, True,
               w_ukv.all() + P.ckvn.all(), [psk[pi]])
            src = ps[pi].rearrange("p (a m x) -> p a m x", a=2, m=2)
            dst = Vc.ap[:, kb, :].rearrange("p (a r) -> p a r", a=2)
            cp(k, "dve", dst[:, :, 0:64], src[:, :, 0, 64:128], [psk[pi]], Vc.all())
            cp(k, "dve", dst[:, :, 128:192], src[:, :, 1, 64:128], [psk[pi]], Vc.all())
        def qtile(qi):
            if qi == 0:
                return 0, 128, HALO0 // 128
            return 128 + 512 * (qi - 1), 512, NPRE // 128 + 4 * (qi - 1)

        def q_assemble(qi):
            s0, NQ, qblk0 = qtile(qi)
            qTs = qTb[qi % 2]
            g0 = HALO0 + s0
            S.dma("sp", pq.ap[:, 0:NQ], d["posi"][:, g0:g0 + NQ].partition_broadcast(128), W=pq.all(), key="posq")
            rope_tables(k, pq.ap, pq.k(), (64, 96), 1, cos_m, sin_m, (ta, tb, tc), NQ)
            for hi, h in enumerate(heads):
                for ci in range(2):
                    mm(k, ps[0][0:96, 0:NQ], w_uq.ap[:, ci, h * 96:(h + 1) * 96], P.cqn.ap[:, ci, s0:s0 + NQ], ci == 0, ci == 1,
                       w_uq.all() + P.cqn.all(), [psk[0]])
                for ci in range(2):
                    mm(k, ps[1][0:96, 0:NQ], w_uqr.ap[:, ci, h * 96:(h + 1) * 96], P.cqn.ap[:, ci, s0:s0 + NQ], ci == 0, ci == 1,
                       w_uqr.all() + P.cqn.all(), [psk[1]])
                cp(k, "dve", qTs[hi].ap[0:64, 0:NQ], ps[0][0:64, 0:NQ], [psk[0]], qTs[hi].all())
                tt(k, "dve", tq1.ap[64:96, 0:NQ], ps[0][64:96, 0:NQ], cos_m.ap[64:96, 0:NQ], ALU.mult, [psk[0]] + cos_m.all(),
                   tq1.all())
                tt(k, "dve", tq2.ap[64:96, 0:NQ], ps[1][64:96, 0:NQ], sin_m.ap[64:96, 0:NQ], ALU.mult, [psk[1]] + sin_m.all(),
                   tq2.all())
                tt(k, "pool", qTs[hi].ap[64:96, 0:NQ], tq1.ap[64:96, 0:NQ], tq2.ap[64:96, 0:NQ], ALU.add, tq1.all() + tq2.all(),
                   qTs[hi].all())

        q_assemble(0)
        for qi in range(5):
            s0, NQ, qblk0 = qtile(qi)
            nqb = NQ // 128
            qT = qTb[qi % 2]
            if qi + 1 < 5:
                q_assemble(qi + 1)
            for hi, h in enumerate(heads):
                pair, mem = divmod(hi, 2)
                vcol0 = pair * 192 + mem * 64
                pob = 6 + hi % 2
                po = ps[pob]
                nkb = qblk0 + nqb
                groups = []
                kb = 0
                while kb < nkb:
                    if kb + 1 < qblk0:
                        groups.append((kb, kb + 1))
                        kb += 2
                    else:
                        groups.append((kb,))
                        kb += 1
                pend = None

                def pv(pd, nkb=nkb, po=po, pob=pob, vcol0=vcol0, NQ=NQ):
                    for (kb_, qlo_, n_, ptap_, ptk_) in pd:
                        mm(k, po[:, qlo_:NQ], Vc.ap[:, kb_, vcol0:vcol0 + 128], ptap_, kb_ == 0, kb_ == nkb - 1,
                           Vc.all() + ptk_, [psk[pob]])

                for gi, grp in enumerate(groups):
                    slot = gi % 2
                    pt = PTb[slot]
                    banks = (2 + 2 * slot, 3 + 2 * slot)
                    cur = []
                    for j_, kb in enumerate(grp):
                        r = kb - qblk0
                        q_lo = max(r, 0) * 128
                        n = NQ - q_lo
                        sb = banks[j_]
                        mm(k, ps[sb][:, 0:n], KT.ap[:, hi, kb * 128:(kb + 1) * 128], qT[hi].ap[:, q_lo:NQ], True, True,
                           [KT.k(hi)] + qT[hi].all(), [psk[sb]])
                        cur.append((kb, q_lo, n, pt.ap[:, j_ * 512:j_ * 512 + n], pt.all()))
                    if len(grp) == 2 and NQ == 512:
                        act(k, pt.ap, ps2[1 + slot], AF.Exp, [psk[banks[0]], psk[banks[1]]], pt.all(), scale=scale)
                    else:
                        for j_, (kb, q_lo, n, ptap, _) in enumerate(cur):
                            act(k, ptap, ps[banks[j_]][:, 0:n], AF.Exp, [psk[banks[j_]]], pt.all(), scale=scale)
                            if kb - qblk0 >= 0:
                                tt(k, "pool", ptap[:, 0:128], ptap[:, 0:128], c.causal.ap, ALU.mult, pt.all() + c.causal.all(),
                                   pt.all())
                    if pend is not None:
                        pv(pend)
                    pend = cur
                pv(pend)
                if mem == 0:
                    o_rows, s_rows = slice(0, 64), slice(64, 128)
                else:
                    o_rows, s_rows = slice(64, 128), slice(0, 64)
                ts(k, "dve", rec.ap[o_rows, 0:NQ], po[s_rows, 0:NQ], 1e-30, None, ALU.add, None, [psk[pob]], rec.all())
                recip(k, rec.ap[o_rows, 0:NQ], rec.ap[o_rows, 0:NQ], rec.all(), rec.all())
                tt(k, "dve", P.omlaT.ap[o_rows, 2 * hh + pair, s0:s0 + NQ], po[o_rows, 0:NQ], rec.ap[o_rows, 0:NQ], ALU.mult,
                   [psk[pob]] + rec.all(), P.omlaT.all())
    if k.dbg and k.stage == "A2":
        dump(k, "omlaT", P.omlaT.ap, [128, 4, NST], P.omlaT.all(), BF16)
    S.release(w_uq, w_uqr, w_ukv, KT, Vc, *qTb[0], *qTb[1], *PTb, tq1, tq2, ta, tb, tc, cos_m, sin_m, pq, rec)


def _norm_to_hT(k, xb, hT, sd, rstd, gcol, n):
    c = k.c
    squares8(k, hT, xb, n)
    rms_stats(k, lambda i: hT.ap[:, i, 0:n], 8, D, 0, sd, rstd, n, hT.all())
    for ci in range(8):
        stt(k, hT.ap[:, ci, 0:n], xb.ap[:, ci, 0:n], c.gv.ap[:, gcol + ci:gcol + ci + 1], rstd.ap[:, 0:n], ALU.mult, ALU.mult,
            xb.all() + rstd.all() + c.gv.all(), hT.all())


def phase_A3X(k):
    S, d, c, P, ps, psk = k.S, k.d, k.c, k.P, k.ps, k.psk
    xbs = [S.alloc(f"xa{i}", [128, 8, 512], F32) for i in range(2)]
    hT = S.alloc("hTa", [128, 8, 512], BF16)
    rstd = S.alloc("rstda", [128, 512], F32)
    sd = rstd
    qxT = S.alloc("qxT", [128, 8, 512], BF16, nsub=8)
    PTx = [S.alloc(f"PTx{i}", [128, 512], BF16) for i in range(2)]
    oxT = hT
    rec = S.alloc("recx", [128, 512], F32)
    xTv = d["xT"].rearrange("(c p) t -> p c t", p=128)
    x2v = d["x2s"].rearrange("(c p) t -> p c t", p=128)
    tiles = [(0, 128)] + [(128 + 512 * i, 512) for i in range(4)]
    def xload(ti_):
        s0_, N_ = tiles[ti_]
        S.dma("sp", xbs[ti_ % 2].ap[:, :, 0:N_], xTv[:, :, HALO0 + s0_:HALO0 + s0_ + N_], W=xbs[ti_ % 2].all(), key=f"xa{ti_ % 2}")

    xload(0)
    for ti_, (s0, N) in enumerate(tiles):
        xb = xbs[ti_ % 2]
        if ti_ + 1 < len(tiles):
            xload(ti_ + 1)
        for cb in range(8):
            pi = 1 + cb % 2
            cs = slice(cb * 128, (cb + 1) * 128)
            for j in range(4):
                mm(k, ps[pi][:, 0:N], P.w_out.ap[:, j, cs], P.omlaT.ap[:, j, s0:s0 + N], j == 0, False,
                   P.w_out.all() + P.omlaT.all(), [psk[pi]])
            for j in range(4):
                mm(k, ps[pi][:, 0:N], P.w_out.ap[:, 4 + j, cs], P.oretT.ap[:, j, s0:s0 + N], False, j == 3,
                   P.w_out.all() + P.oretT.all(), [psk[pi]])
            tt(k, "dve", xb.ap[:, cb, 0:N], xb.ap[:, cb, 0:N], ps[pi][:, 0:N], ALU.add, xb.all() + [psk[pi]], xb.all())
        if k.dbg and k.stage == "A3X":
            dumpx(k, "x1", xb, s0, N)
        _norm_to_hT(k, xb, hT, sd, rstd, c.g_xattn, N)
        for blk in range(8):
            pi = 1 + blk % 2
            for ci in range(8):
                mm(k, ps[pi][:, 0:N], P.w_xq.ap[:, ci, blk * 128:(blk + 1) * 128], hT.ap[:, ci, 0:N], ci == 0, ci == 7,
                   P.w_xq.all() + hT.all(), [psk[pi]])
            cp(k, "act", qxT.ap[:, blk, 0:N], ps[pi][:, 0:N], [psk[pi]], [qxT.k(blk)])
        for h in range(4):
            for kb2 in range(2):
                sb = 3 + kb2
                for dc in range(2):
                    mm(k, ps[sb][:, 0:N], P.memKT.ap[:, 2 * h + dc, kb2 * 128:(kb2 + 1) * 128], qxT.ap[:, 2 * h + dc, 0:N],
                       dc == 0, dc == 1, P.memKT.all() + [qxT.k(2 * h + dc)], [psk[sb]])
                act(k, PTx[kb2].ap[:, 0:N], ps[sb][:, 0:N], AF.Exp, [psk[sb]], PTx[kb2].all(), scale=1.0 / 16.0)
            for kb2 in range(2):
                mm(k, ps[5][:, 0:N], c.ones.ap, PTx[kb2].ap[:, 0:N], kb2 == 0, kb2 == 1, c.ones.all() + PTx[kb2].all(), [psk[5]])
            act(k, rec.ap[:, 0:N], ps[5][:, 0:N], AF.Ln, [psk[5]], rec.all())
            act(k, rec.ap[:, 0:N], rec.ap[:, 0:N], AF.Exp, rec.all(), rec.all(), scale=-1.0)
            for eb in range(2):
                pi = 6 + eb
                for kb2 in range(2):
                    mm(k, ps[pi][:, 0:N], P.memV.ap[:, kb2, h * 256 + eb * 128:h * 256 + (eb + 1) * 128], PTx[kb2].ap[:, 0:N],
                       kb2 == 0, kb2 == 1, P.memV.all() + PTx[kb2].all(), [psk[pi]])
                tt(k, "dve", oxT.ap[:, 2 * h + eb, 0:N], ps[pi][:, 0:N], rec.ap[:, 0:N], ALU.mult, [psk[pi]] + rec.all(), oxT.all())
        for cb in range(8):
            pi = 1 + cb % 2
            for j in range(8):
                mm(k, ps[pi][:, 0:N], P.w_xo.ap[:, j, cb * 128:(cb + 1) * 128], oxT.ap[:, j, 0:N], j == 0, j == 7,
                   P.w_xo.all() + oxT.all(), [psk[pi]])
            tt(k, "dve", xb.ap[:, cb, 0:N], xb.ap[:, cb, 0:N], ps[pi][:, 0:N], ALU.add, xb.all() + [psk[pi]], xb.all())
        S.dma("sp", x2v[:, :, s0:s0 + N], xb.ap[:, :, 0:N], R=xb.all(), W=[("x2s", s0)], key=f"xs{ti_ % 2}")
        if k.dbg and k.stage == "A3X":
            dumpx(k, "x2", xb, s0, N)
    S.release(*xbs, hT, rstd, qxT, *PTx, rec)


def dumpx(k, name, xb, s0, N):
    if name not in k.dbg_out:
        k.dbg_out[name] = k.nc.dram_tensor("dbg_" + name, [D, NST], F32, kind="ExternalOutput").ap()
    t = k.dbg_out[name].rearrange("(c p) t -> p c t", p=128)
    k.S.dma("sp", t[:, :, s0:s0 + N], xb.ap[:, :, 0:N], R=xb.all(), W=[("dbg", name, s0)], key="dbg")


def phase_F(k):
    S, d, c, P, ps, psk = k.S, k.d, k.c, k.P, k.ps, k.psk
    xf = S.alloc("xf", [128, 8, 512], F32)
    hT = S.alloc("hTf", [128, 8, 512], BF16)
    sd, rstd = S.alloc("sdf", [128, 512], F32), S.alloc("rstdf", [128, 512], F32)
    aT = S.alloc("aT", [128, NFB, 512], BF16, nsub=NFB)
    gsb = [S.alloc(f"gsb{i}", [128, 48 + 512], F32) for i in range(2)]
    cv = [S.alloc(f"cv{i}", [128, 512], F32) for i in range(2)]
    ghalo = S.alloc("ghalo", [128, NFB, 48], F32, nsub=NFB)
    x2v = d["x2s"].rearrange("(c p) t -> p c t", p=128)
    outv = d["outT"].rearrange("(c p) t -> p c t", p=128)
    w1 = P.w_ffn_in
    ostg = S.arena[0:128, aT.off:aT.off + 8 * 512 * 4].bitcast(F32).rearrange("p (a b) -> p a b", a=8)
    S.dma("sp", xf.ap[:, :, 0:128], x2v[:, :, 0:128], R=[("x2s", 0)], W=xf.all(), key="xf")
    _norm_to_hT(k, xf, hT, sd, rstd, c.g_ffn, 128)
    for j in range(NFB):
        pi = 1 + j % 2
        for ci in range(8):
            mm(k, ps[pi][:, 0:128], w1.ap[:, ci, j * 128:(j + 1) * 128], hT.ap[:, ci, 0:128], ci == 0, ci == 7,
               [w1.k(0 if j < 11 else 2)] + hT.all(), [psk[pi]])
        ts(k, "dve", ghalo.ap[:, j, :], ps[pi][:, 80:128], c.flags.ap[:, 1:2], None, ALU.mult, None, [psk[pi]] + c.flags.all(),
           [ghalo.k(j)])
    for ti in range(4):
        s0 = 128 + 512 * ti
        S.dma("sp", xf.ap, x2v[:, :, s0:s0 + 512], R=[("x2s", s0)], W=xf.all(), key="xf")
        _norm_to_hT(k, xf, hT, sd, rstd, c.g_ffn, 512)
        for j in range(NFB):
            pg, pu = 1 + (j % 2), (3, 4, 7)[j % 3]
            for ci in range(8):
                mm(k, ps[pg], w1.ap[:, ci, j * 128:(j + 1) * 128], hT.ap[:, ci, :], ci == 0, ci == 7,
                   [w1.k(0 if j < 11 else 2)] + hT.all(), [psk[pg]])
            for ci in range(8):
                mm(k, ps[pu], w1.ap[:, ci, DFF + j * 128:DFF + (j + 1) * 128], hT.ap[:, ci, :], ci == 0, ci == 7,
                   [w1.k(1 if j < 11 else 3)] + hT.all(), [psk[pu]])
            g, cvb = gsb[j % 2], cv[j % 2]
            cw = c.convp.ap
            cp(k, "act", g.ap[:, 48:560], ps[pg], [psk[pg]], g.all())
            cp(k, "pool", g.ap[:, 0:48], ghalo.ap[:, j, :], [ghalo.k(j)], g.all())
            k.S.op("act", lambda e, g=g, cvb=cvb, j=j: e.activation(out=cvb.ap, in_=g.ap[:, 48:560], func=AF.Identity,
                                                                     scale=cw[:, j, 2:3], bias=cw[:, j, 3:4]),
                   g.all() + c.convp.all(), cvb.all())
            stt(k, cvb.ap, g.ap[:, 47:559], cw[:, j, 1:2], cvb.ap, ALU.mult, ALU.add, g.all() + cvb.all() + c.convp.all(), cvb.all())
            stt(k, cvb.ap, g.ap[:, 46:558], cw[:, j, 0:1], cvb.ap, ALU.mult, ALU.add, g.all() + cvb.all() + c.convp.all(), cvb.all())
            cp(k, "pool", ghalo.ap[:, j, :], g.ap[:, 512:560], g.all(), [ghalo.k(j)])
            act(k, cvb.ap, cvb.ap, AF.Silu, cvb.all(), cvb.all())
            tt(k, "dve", aT.ap[:, j, :], cvb.ap, ps[pu], ALU.mult, cvb.all() + [psk[pu]], [aT.k(j)])
        for cb in range(8):
            pi = 5 + cb % 2
            for j in range(NFB):
                mm(k, ps[pi], P.w_ffn_out.ap[:, j, cb * 128:(cb + 1) * 128], aT.ap[:, j, :], j == 0, j == NFB - 1,
                   P.w_ffn_out.all() + [aT.k(j)], [psk[pi]])
            tt(k, "dve", xf.ap[:, cb, :], xf.ap[:, cb, :], ps[pi], ALU.add, xf.all() + [psk[pi]], xf.all())
        squares8(k, hT, xf, 512)
        rms_stats(k, lambda i: hT.ap[:, i, :], 8, D, 0, sd, rstd, 512, hT.all())
        for cb in range(8):
            stt(k, ostg[:, cb, :], xf.ap[:, cb, :], c.gv.ap[:, c.g_final + cb:c.g_final + cb + 1], rstd.ap, ALU.mult, ALU.mult,
                xf.all() + rstd.all() + c.gv.all(), aT.all())
        S.dma("sp", outv[:, :, 512 * ti:512 * (ti + 1)], ostg, R=aT.all(), W=[("out", ti)], key="out")
    S.release(xf, hT, sd, rstd, aT, *gsb, *cv, ghalo)


def _consts():
    f32 = np.float32
    H, L = 4, 128
    log_gamma = np.log(f32(1.0) - f32(2.0) ** (f32(-5.0) - np.arange(H, dtype=f32))).astype(f32)
    j = np.arange(L, dtype=f32)
    diff = j[:, None] - j[None, :]
    intra = np.where(diff[None] >= 0, np.exp(np.maximum(diff, 0.0)[None] * log_gamma[:, None, None]), 0.0).astype(f32)
    k_to_end = np.exp((L - 1 - j)[:, None] * log_gamma[None, :]).astype(f32)
    q_from_start = np.exp((j + 1)[:, None] * log_gamma[None, :]).astype(f32)
    chunk_decay = np.exp(f32(L) * log_gamma).astype(f32)
    dk = f32(128.0 ** -0.5)
    c_intra = np.zeros((128, 512), f32)
    for h in range(H):
        c_intra[:, h * 128:(h + 1) * 128] = intra[h].T * dk
    c_qfs = np.zeros((128, 512), f32)
    c_decay = np.zeros((128, 512), f32)
    for h in range(H):
        c_qfs[:, h * 128:(h + 1) * 128] = q_from_start[:, h][None, :]
        c_decay[:, h * 128:(h + 1) * 128] = chunk_decay[h]
    c_small = np.zeros((128, 8), f32)
    invf_r = (1.0 / (f32(10000.0) ** (np.arange(0, 128, 2, dtype=f32) / f32(128)))).astype(f32)
    invf_m = (1.0 / (f32(10000.0) ** (np.arange(0, 32, 2, dtype=f32) / f32(32)))).astype(f32)
    p = np.arange(128)
    c_small[:, 0] = invf_r[p % 64]
    c_small[:, 1] = invf_m[p % 16]
    c_small[:, 2:6] = k_to_end * dk
    c_ident = np.eye(128, dtype=f32)
    c_rot = np.zeros((128, 128), f32)
    for m in range(64):
        c_rot[m + 64, m] = -1.0
    for m in range(64, 128):
        c_rot[m - 64, m] = 1.0
    kk = np.arange(128)
    c_causal = (kk[None, :] >= kk[:, None]).astype(f32)
    return dict(c_small=c_small, c_ident=c_ident, c_rot=c_rot, c_causal=c_causal, c_intra=c_intra, c_qfs=c_qfs,
                c_decay=c_decay)


def make_in_maps(inputs):
    f32 = np.float32
    x = np.asarray(inputs["x"], f32)
    mem = np.asarray(inputs["mem"], f32)
    pos = np.asarray(inputs["positions"], np.int32)

    def col(g):
        g = np.asarray(g, f32).reshape(-1, 128)
        return np.ascontiguousarray(g.T)

    gv = np.concatenate([col(inputs["g_mix"][0]), col(inputs["g_xattn"][0]), col(inputs["g_mem"][0]),
                         col(inputs["g_ffn"][0]), col(inputs["g_final"]), col(inputs["g_q_lat"][0]),
                         col(inputs["g_kv_lat"][0])], axis=1)
    cw = np.asarray(inputs["conv_w"][0], f32)
    cb = np.asarray(inputs["conv_b"][0], f32)
    convp = np.zeros((128, NFB, 4), f32)
    for i in range(3):
        convp[:, :, i] = cw[i].reshape(NFB, 128).T
    convp[:, :, 3] = cb.reshape(NFB, 128).T
    shared = dict(
        w_in=np.ascontiguousarray(inputs["w_in"][0], f32), w_uq=np.ascontiguousarray(inputs["w_uq"][0], f32),
        w_ukv=np.ascontiguousarray(inputs["w_ukv"][0], f32), w_out=np.ascontiguousarray(inputs["w_out"][0], f32),
        w_xq=np.ascontiguousarray(inputs["w_xq"][0], f32), w_xkv=np.ascontiguousarray(inputs["w_xkv"][0], f32),
        w_xo=np.ascontiguousarray(inputs["w_xo"][0], f32), w_ffn_in=np.ascontiguousarray(inputs["w_ffn_in"][0], f32),
        w_ffn_out=np.ascontiguousarray(inputs["w_ffn_out"][0], f32), gv=np.ascontiguousarray(gv),
        convp=np.ascontiguousarray(convp.reshape(128, NFB * 4)), **_consts())
    maps = []
    for core in range(8):
        b, hf = core // 2, core % 2
        xT = np.zeros((D, NTOK), f32)
        pp = np.zeros((1, NTOK), np.int32)
        if hf == 0:
            xT[:, NPRE:] = x[b, :NOWN].T
            pp[0, NPRE:] = pos[b, :NOWN]
        else:
            xT[:, :] = x[b].T
            pp[0, :] = pos[b]
        flags = np.full((128, 2), float(hf), f32)
        m = dict(shared)
        m.update(xT=xT, posi=pp, memT=np.ascontiguousarray(mem[b].T), flags=flags)
        maps.append(m)
    return maps


_CACHE = {}


def kernel(**inputs):
    if "nc" not in _CACHE:
        _CACHE["nc"] = build("full")[0]
    nc = _CACHE["nc"]
    maps = make_in_maps(inputs)
    res = run_bass_kernel_spmd(nc, maps, core_ids=list(range(8)))
    out = np.zeros((NB, SEQ, D), np.float32)
    for core in range(8):
        b, hf = core // 2, core % 2
        out[b, hf * NOWN:(hf + 1) * NOWN, :] = res.results[core]["outT"].T
    return out
```

```python
import math
from contextlib import ExitStack

import numpy as np
import concourse.bass as bass
import concourse.mybir as mybir
from concourse.bass_utils import run_bass_kernel_spmd

F32 = mybir.dt.float32
BF16 = mybir.dt.bfloat16
I32 = mybir.dt.int32
U8 = mybir.dt.uint8
AF = mybir.ActivationFunctionType
ALU = mybir.AluOpType
AX = mybir.AxisListType

D = 1024
SEQ = 4096
NB = 4
EPS = 1e-6
NPRE = 2048
NOWN = 2048
NTOK = NPRE + NOWN
HALO0 = NPRE - 128
NST = NOWN + 128
IN_COLS = 2464
OFF_CQ, OFF_CKV, OFF_KR, OFF_RQ, OFF_RK, OFF_RV, OFF_RG = 0, 256, 384, 416, 928, 1440, 1952
DFF = 2816
NFB = DFF // 128
MEM = 256
TWO_PI = 2.0 * math.pi
C1 = 6.28125
C2 = TWO_PI - C1
MAGIC = 12582912.0
PI_LO = 3.1415925
ARENA_BYTES = 206 * 1024
ENGS = ("pe", "act", "dve", "pool", "sp")
NDMA_MAX = 90
DT_SIZE = {F32: 4, BF16: 2, I32: 4}


class Buf:
    def __init__(self, uid, name, off, nbytes, ap, nsub):
        self.uid, self.name, self.off, self.nbytes, self.ap, self.nsub = uid, name, off, nbytes, ap, nsub

    def k(self, i=0):
        return (self.uid, i)

    def all(self):
        return [(self.uid, i) for i in range(self.nsub)]

    def __getitem__(self, idx):
        return self.ap[idx]


class Sched:
    def __init__(self, nc, es):
        self.nc, self.es = nc, es
        self.prog = {e: [] for e in ENGS}
        self.sem = {e: es.enter_context(nc.semaphore("s_" + e)) for e in ENGS if e != "sp"}
        self.cnt = {e: 0 for e in ENGS}
        self.seen = {e: {} for e in ENGS}
        self.drained = {e: 0 for e in ENGS}
        self.needed = {e: set() for e in ENGS}
        self.lastw, self.readers = {}, {}
        self.ndma = 0
        self.dma_events = []
        self.dma_pool = [es.enter_context(nc.semaphore(f"d{i}")) for i in range(NDMA_MAX)]
        self.dma_sem = {}
        self.arena = es.enter_context(nc.sbuf_tensor("arena", [128, ARENA_BYTES], U8))
        self.free = [(0, ARENA_BYTES)]
        self.freed_events = []
        self.nbuf = 0
        self.peak = 0
        self.used = 0

    def alloc(self, name, shape, dtype, nsub=1):
        n = 1
        for s in shape[1:]:
            n *= s
        nbytes = (n * DT_SIZE[dtype] + 63) // 64 * 64
        for i, (o, sz) in enumerate(self.free):
            if sz >= nbytes:
                off = o
                if sz == nbytes:
                    self.free.pop(i)
                else:
                    self.free[i] = (o + nbytes, sz - nbytes)
                break
        else:
            raise MemoryError(f"arena full allocating {name} {nbytes}B; free={self.free}")
        ap = self.arena[0:shape[0], off:off + n * DT_SIZE[dtype]].bitcast(dtype)
        if len(shape) == 3:
            ap = ap.rearrange("p (a b) -> p a b", a=shape[1])
        elif len(shape) == 4:
            ap = ap.rearrange("p (a b c) -> p a b c", a=shape[1], b=shape[2])
        self.nbuf += 1
        b = Buf(self.nbuf, name, off, nbytes, ap, nsub)
        evs, keep = [], []
        for (fo, fn, fe) in self.freed_events:
            if fo < off + nbytes and off < fo + fn:
                evs.extend(fe)
            keep.append((fo, fn, fe))
        self.freed_events = keep
        if evs:
            for kk in b.all():
                self.readers[kk] = list(evs)
        self.used += nbytes
        self.peak = max(self.peak, self.used)
        return b

    def release(self, *bufs):
        for b in bufs:
            evs = []
            for kk in b.all():
                if kk in self.lastw:
                    evs.append(self.lastw.pop(kk))
                evs.extend(self.readers.pop(kk, []))
            best = {}
            for (s, v) in evs:
                best[s] = max(best.get(s, 0), v)
            self.freed_events.append((b.off, b.nbytes, list(best.items())))
            self.free.append((b.off, b.nbytes))
            self.free.sort()
            merged = []
            for (o, sz) in self.free:
                if merged and merged[-1][0] + merged[-1][1] == o:
                    merged[-1] = (merged[-1][0], merged[-1][1] + sz)
                else:
                    merged.append((o, sz))
            self.free = merged
            self.used -= b.nbytes

    def _deps(self, eng, R, W):
        best = {}
        for r in R:
            ev = self.lastw.get(r)
            if ev is not None:
                best[ev[0]] = max(best.get(ev[0], 0), ev[1])
        for w in W:
            ev = self.lastw.get(w)
            if ev is not None:
                best[ev[0]] = max(best.get(ev[0], 0), ev[1])
            for ev in self.readers.get(w, ()):
                best[ev[0]] = max(best.get(ev[0], 0), ev[1])
        for s, v in best.items():
            if s == eng:
                if eng == "pe":
                    continue
                if eng in ("act", "dve"):
                    if self.drained[eng] < v:
                        self.prog[eng].append(("drain",))
                        self.drained[eng] = self.cnt[eng]
                    continue
            if self.seen[eng].get(s, 0) >= v:
                continue
            self.seen[eng][s] = v
            self.prog[eng].append(("wait", s, v))
            if isinstance(s, str):
                self.needed[s].add(v)

    def _commit(self, ev, R, W):
        for w in W:
            self.lastw[w] = ev
            self.readers[w] = []
        for r in R:
            if r not in W:
                self.readers.setdefault(r, []).append(ev)

    def merge_keys(self, src_keys, dst_key):
        evs = []
        for kk in src_keys:
            if kk in self.lastw:
                evs.append(self.lastw[kk])
            evs.extend(self.readers.get(kk, []))
        self.readers.setdefault(dst_key, []).extend(evs)

    def op(self, eng, fn, R=(), W=()):
        R, W = list(R), list(W)
        self._deps(eng, R, W)
        self.cnt[eng] += 1
        ev = (eng, self.cnt[eng])
        self.prog[eng].append(("inst", fn, self.cnt[eng]))
        self._commit(ev, R, W)
        return ev

    def dma(self, eng, out, in_, R=(), W=(), key=None):
        R, W = list(R), list(W)
        self._deps(eng, R, W)
        if key is None:
            key = f"_auto{self.ndma}"
        if key not in self.dma_sem:
            self.dma_sem[key] = [self.dma_pool[len(self.dma_sem)], 0]
        ent = self.dma_sem[key]
        sem = ent[0]
        if ent[1] > 0 and self.seen[eng].get(sem, 0) < ent[1]:
            self.seen[eng][sem] = ent[1]
            self.prog[eng].append(("wait", sem, ent[1]))
        ent[1] += 16
        self.ndma += 1
        ev = (sem, ent[1])
        self.prog[eng].append(("dma", out, in_, sem))
        self._commit(ev, R, W)
        self.dma_events.append(ev)
        return ev

    def emit(self, block):
        nc = self.nc
        S = self

        rank = {en: {q: i + 1 for i, q in enumerate(sorted(S.needed[en]))} for en in ENGS}
        S.n_inc = {en: len(rank[en]) for en in ENGS}

        def run(e, name):
            for ent in S.prog[name]:
                if ent[0] == "wait":
                    s = ent[1]
                    if isinstance(s, str):
                        e.wait_ge(S.sem[s], rank[s][ent[2]])
                    else:
                        e.wait_ge(s, ent[2])
                elif ent[0] == "drain":
                    e.drain()
                elif ent[0] == "inst":
                    ins = ent[1](e)
                    if ent[2] in rank[name]:
                        ins.then_inc(S.sem[name], 1)
                else:
                    e.dma_start(out=ent[1], in_=ent[2]).then_inc(ent[3], 16)

        @block.tensor
        def _(e):
            run(e, "pe")

        @block.scalar
        def _(e):
            run(e, "act")

        @block.vector
        def _(e):
            run(e, "dve")

        @block.gpsimd
        def _(e):
            run(e, "pool")

        @block.sync
        def _(e):
            run(e, "sp")

    def final_wait(self, eng, events):
        for (s, v) in events:
            if self.seen[eng].get(s, 0) >= v:
                continue
            self.seen[eng][s] = v
            self.prog[eng].append(("wait", s, v))


class K:
    pass


def bc(ap, shape):
    return ap.broadcast_to(shape)


def build(stage="full", dbg=False, a1_tiles=None, a1_level=9):
    nc = bass.Bass("TRN2", target_bir_lowering=False)
    es = ExitStack()
    k = K()
    k.nc, k.es, k.stage, k.dbg = nc, es, stage, dbg
    k.a1_tiles, k.a1_level = a1_tiles, a1_level
    d = {}

    def din(name, shape, dt=F32):
        d[name] = nc.dram_tensor(name, list(shape), dt, kind="ExternalInput").ap()

    din("xT", [D, NTOK]); din("posi", [1, NTOK], I32); din("memT", [D, MEM]); din("flags", [128, 2])
    din("w_in", [D, IN_COLS]); din("w_uq", [256, 768]); din("w_ukv", [128, 1024]); din("w_out", [D, D])
    din("w_xq", [D, D]); din("w_xkv", [D, 2 * D]); din("w_xo", [D, D])
    din("w_ffn_in", [D, 2 * DFF]); din("w_ffn_out", [DFF, D])
    din("gv", [128, 43]); din("convp", [128, NFB * 4]); din("c_small", [128, 8])
    din("c_ident", [128, 128]); din("c_rot", [128, 128]); din("c_causal", [128, 128])
    din("c_intra", [128, 512]); din("c_qfs", [128, 512]); din("c_decay", [128, 512])
    d["outT"] = nc.dram_tensor("outT", [D, NOWN], F32, kind="ExternalOutput").ap()
    d["x2s"] = nc.dram_tensor("x2s", [D, NST], F32, kind="Internal").ap()
    k.dbg_out = {}
    k.d = d
    with es:
        S = Sched(nc, es)
        k.S = S
        big = [es.enter_context(nc.psum_tensor(f"psb{i}", [128, 1024], F32)) for i in range(4)]
        k.ps2 = [b_[:, :] for b_ in big]
        k.ps = [big[i // 2][:, (i % 2) * 512:(i % 2 + 1) * 512] for i in range(8)]
        k.psk = [("ps", i) for i in range(8)]
        block = es.enter_context(nc.Block())
        emit_all(k)
        S.final_wait("sp", S.dma_events)
        S.emit(block)
    k.peak = S.peak
    return nc, k


def mm(k, out, lhsT, rhs, start, stop, R, W):
    k.S.op("pe", lambda e: e.matmul(out, lhsT=lhsT, rhs=rhs, start=start, stop=stop), R, W)


def tr(k, out, in_, ident, R, W):
    k.S.op("pe", lambda e: e.transpose(out=out, in_=in_, identity=ident), R, W)


def act(k, out, in_, func, R, W, scale=1.0, bias=0.0):
    k.S.op("act", lambda e: e.activation(out=out, in_=in_, func=func, scale=scale, bias=bias), R, W)


def tt(k, eng, out, in0, in1, op, R, W):
    k.S.op(eng, lambda e: e.tensor_tensor(out=out, in0=in0, in1=in1, op=op), R, W)


def ts(k, eng, out, in0, s1, s2, op0, op1, R, W):
    if op1 is None:
        k.S.op(eng, lambda e: e.tensor_scalar(out=out, in0=in0, scalar1=s1, scalar2=None, op0=op0), R, W)
    else:
        k.S.op(eng, lambda e: e.tensor_scalar(out=out, in0=in0, scalar1=s1, scalar2=s2, op0=op0, op1=op1), R, W)


def stt(k, out, in0, scalar, in1, op0, op1, R, W):
    k.S.op("dve", lambda e: e.scalar_tensor_tensor(out=out, in0=in0, scalar=scalar, in1=in1, op0=op0, op1=op1), R, W)


def cp(k, eng, out, in_, R, W):
    if eng == "act":
        k.S.op("act", lambda e: e.copy(out=out, in_=in_), R, W)
    else:
        k.S.op(eng, lambda e: e.tensor_copy(out=out, in_=in_), R, W)


def recip(k, out, in_, R, W):
    k.S.op("dve", lambda e: e.reciprocal(out=out, in_=in_), R, W)


def dump(k, name, buf_ap, shape, R, dt=F32):
    t = k.nc.dram_tensor("dbg_" + name, list(shape), dt, kind="ExternalOutput").ap()
    k.dbg_out[name] = t
    k.S.dma("sp", t, buf_ap, R=R, W=[("dbg", name)], key="dbg")


def load_consts(k):
    S, d = k.S, k.d
    c = K()
    k.c = c
    c.gv = S.alloc("gv", [128, 43], F32)
    c.convp = S.alloc("convp", [128, NFB, 4], F32)
    c.small = S.alloc("c_small", [128, 8], F32)
    c.flags = S.alloc("flags", [128, 2], F32)
    c.ident = S.alloc("ident", [128, 128], BF16)
    c.rot = S.alloc("rot", [128, 128], BF16)
    c.causal = S.alloc("causal", [128, 128], BF16)
    c.intra = S.alloc("intra", [128, 512], F32)
    c.qfs = S.alloc("qfs", [128, 512], F32)
    c.decay = S.alloc("decay", [128, 512], F32)
    c.ones = S.alloc("ones", [128, 128], BF16)
    S.dma("sp", c.gv.ap, d["gv"], W=c.gv.all())
    S.dma("sp", c.convp.ap, d["convp"].rearrange("p (a b) -> p a b", a=NFB), W=c.convp.all())
    S.dma("sp", c.small.ap, d["c_small"], W=c.small.all())
    S.dma("sp", c.flags.ap, d["flags"], W=c.flags.all())
    S.dma("sp", c.intra.ap, d["c_intra"], W=c.intra.all())
    S.dma("sp", c.qfs.ap, d["c_qfs"], W=c.qfs.all())
    S.dma("sp", c.decay.ap, d["c_decay"], W=c.decay.all())
    S.dma("pool", c.ident.ap, d["c_ident"], W=c.ident.all())
    S.dma("pool", c.rot.ap, d["c_rot"], W=c.rot.all())
    S.dma("pool", c.causal.ap, d["c_causal"], W=c.causal.all())
    S.op("pool", lambda e: e.memset(c.ones.ap, 1.0), W=c.ones.all())
    c.g_mix, c.g_xattn, c.g_mem, c.g_ffn, c.g_final, c.g_q, c.g_kv = 0, 8, 16, 24, 32, 40, 42


def rope_tables(k, posi_ap, posi_key, prange, col, cosb, sinb, tmp, n):
    c = k.c
    p0, p1 = prange
    a, b_, kk = tmp
    A = lambda buf: buf.ap[p0:p1, 0:n]
    invf = c.small.ap[p0:p1, col:col + 1]
    ts(k, "dve", A(a), posi_ap[p0:p1, 0:n], invf, None, ALU.mult, None, [posi_key] + c.small.all(), a.all())
    ts(k, "dve", A(b_), A(a), 1.0 / TWO_PI, MAGIC, ALU.mult, ALU.add, a.all(), b_.all())
    ts(k, "dve", A(kk), A(b_), -MAGIC, None, ALU.add, None, b_.all(), kk.all())
    stt(k, A(b_), A(kk), -C1, A(a), ALU.mult, ALU.add, kk.all() + a.all(), b_.all())
    stt(k, A(a), A(kk), -C2, A(b_), ALU.mult, ALU.add, kk.all() + b_.all(), a.all())
    ts(k, "dve", A(a), A(a), -PI_LO, PI_LO, ALU.max, ALU.min, a.all(), a.all())
    act(k, sinb.ap[p0:p1, 0:n], A(a), AF.Sin, a.all(), sinb.all())
    ts(k, "dve", A(b_), A(a), math.pi / 2, -TWO_PI, ALU.is_gt, ALU.mult, a.all(), b_.all())
    stt(k, A(kk), A(a), math.pi / 2, A(b_), ALU.add, ALU.add, a.all() + b_.all(), kk.all())
    ts(k, "dve", A(kk), A(kk), -PI_LO, PI_LO, ALU.max, ALU.min, kk.all(), kk.all())
    act(k, cosb.ap[p0:p1, 0:n], A(kk), AF.Sin, kk.all(), cosb.all())


def squares8(k, dst, src, n):
    tt(k, "pool", dst.ap[:, 0:4, 0:n], src.ap[:, 0:4, 0:n], src.ap[:, 0:4, 0:n], ALU.mult, src.all(), dst.all())
    for ci in range(4, 8):
        act(k, dst.ap[:, ci, 0:n], src.ap[:, ci, 0:n], AF.Square, src.all(), dst.all())


def rms_stats(k, sq_chunks, nch, nfeat, ps_i, sd, rstd, n, sqkeys):
    c = k.c
    ps = k.ps[ps_i]
    for i in range(nch):
        mm(k, ps[:, 0:n], c.ones.ap, sq_chunks(i), i == 0, i == nch - 1, sqkeys + c.ones.all(), [k.psk[ps_i]])
    act(k, sd.ap[:, 0:n], ps[:, 0:n], AF.Ln, [k.psk[ps_i]], sd.all(), scale=1.0 / nfeat, bias=EPS)
    act(k, rstd.ap[:, 0:n], sd.ap[:, 0:n], AF.Exp, sd.all(), rstd.all(), scale=-0.5)


def emit_all(k):
    load_consts(k)
    P = K()
    k.P = P
    S = k.S
    P.cqn = S.alloc("cqn", [128, 2, NST], BF16)
    P.ckvn = S.alloc("ckvn", [128, NTOK], BF16)
    P.krope = S.alloc("krope", [128, NTOK], BF16)
    P.oretT = S.alloc("oretT", [128, 4, NST], BF16)
    phase_A1(k)
    if k.stage == "A1":
        return
    P.memKT = S.alloc("memKT", [128, 8, MEM], BF16)
    P.memV = S.alloc("memV", [128, 2, D], BF16)
    phase_MKV(k)
    P.omlaT = S.alloc("omlaT", [128, 4, NST], BF16)
    P.w_out = S.alloc("w_out_bf", [128, 8, D], BF16)
    P.w_xq = S.alloc("w_xq_bf", [128, 8, D], BF16)
    d = k.d
    phase_A2(k)
    if k.stage == "A2":
        return
    S.release(P.cqn, P.ckvn, P.krope)
    P.w_xo = S.alloc("w_xo_bf", [128, 8, D], BF16)
    S.dma("pool", P.w_xo.ap, d["w_xo"].rearrange("(c p) n -> p c n", p=128), W=P.w_xo.all(), key="w_xo")
    P.w_ffn_out = S.alloc("w_ffn_out_bf", [128, NFB, D], BF16)
    S.dma("pool", P.w_ffn_out.ap, d["w_ffn_out"].rearrange("(c p) n -> p c n", p=128), W=P.w_ffn_out.all(), key="w_ffn_out")
    phase_A3X(k)
    if k.stage == "A3X":
        return
    S.release(P.oretT, P.omlaT, P.w_out, P.w_xq, P.w_xo, P.memKT, P.memV)
    P.w_ffn_in = S.alloc("w_ffn_in_bf", [128, 8, 2 * DFF], BF16, nsub=4)
    wv = d["w_ffn_in"].rearrange("(c p) n -> p c n", p=128)
    HB = 11 * 128
    for sub, (a, b) in enumerate(((0, HB), (DFF, DFF + HB), (HB, DFF), (DFF + HB, 2 * DFF))):
        S.dma("pool", P.w_ffn_in.ap[:, :, a:b], wv[:, :, a:b], W=[P.w_ffn_in.k(sub)], key=f"w_ffn_in{sub}")
    phase_F(k)


def phase_A1(k):
    S, d, c, P = k.S, k.d, k.c, k.P
    ps, psk = k.ps, k.psk
    T = 512
    w_in = S.alloc("w_in_bf", [128, 8, IN_COLS], BF16)
    S.dma("pool", w_in.ap, d["w_in"].rearrange("(c p) n -> p c n", p=128), W=w_in.all(), key="w_in")
    w_krot = S.alloc("w_krot", [128, 8, 96], BF16)
    S.op("pool", lambda e: e.memset(w_krot.ap, 0.0), W=w_krot.all())
    ts(k, "pool", w_krot.ap[:, :, 64:80], w_in.ap[:, :, OFF_KR + 16:OFF_KR + 32], -1.0, None, ALU.mult, None,
       w_in.all(), w_krot.all())
    cp(k, "pool", w_krot.ap[:, :, 80:96], w_in.ap[:, :, OFF_KR:OFF_KR + 16], w_in.all(), w_krot.all())

    xt = [S.alloc(f"xt{i}", [128, 8, T], F32) for i in range(2)]
    posi = [S.alloc(f"posi{i}", [128, T], I32) for i in range(2)]
    hTs = [S.alloc(f"hT{i}", [128, 8, T], BF16) for i in range(2)]
    rstd = S.alloc("rstd", [128, T], F32)
    sd = rstd
    ta, tb, tc = (S.alloc(n, [128, T], F32) for n in ("ta", "tb", "tc"))
    cos_r, sin_r = S.alloc("cos_r", [128, T], F32), S.alloc("sin_r", [128, T], F32)
    cos_m, sin_m = S.alloc("cos_m", [128, T], F32), S.alloc("sin_m", [128, T], F32)
    sql = S.alloc("sql", [128, 2, T], BF16)
    rstl = S.alloc("rstl", [128, T], F32)
    sdl = rstl
    raw = [S.alloc(f"raw{i}", [128, T], BF16) for i in range(2)]
    t1 = [S.alloc(f"t1_{i}", [128, T], F32) for i in range(2)]
    t2 = [S.alloc(f"t2_{i}", [128, T], F32) for i in range(2)]
    rqT = S.alloc("rqT", [128, 4, T], BF16, nsub=4)
    rkT = S.alloc("rkT", [128, 4, T], BF16, nsub=4)
    v_tm = S.alloc("v_tm", [128, 4, 512], BF16, nsub=4)
    sg_tm = S.alloc("sg_tm", [128, 4, 512], BF16, nsub=4)
    kdec = S.alloc("kdec", [128, 512], BF16)
    PT = S.alloc("PT", [128, 512], BF16)
    qsT = S.alloc("qsT", [128, 4, 128], BF16)
    S_f = S.alloc("S_f", [128, 512], F32)
    S_b = S.alloc("S_b", [128, 512], BF16)
    sqo = S.alloc("sqo", [128, 512], F32)
    tn = S.alloc("tn", [128, 512], F32)
    ogs = [S.alloc(f"og{i}", [128, 512], BF16) for i in range(2)]
    oT = S.alloc("oT", [128, 512], BF16)
    sqT = S.alloc("sqT", [128, 512], BF16)
    S.op("pool", lambda e: e.memset(S_f.ap, 0.0), W=S_f.all())
    S.op("pool", lambda e: e.memset(S_b.ap, 0.0), W=S_b.all())

    xTv = d["xT"].rearrange("(c p) t -> p c t", p=128)
    ntiles = NTOK // T
    rr = 0
    tile_list = list(range(ntiles)) if k.a1_tiles is None else k.a1_tiles

    def ln_part1(tti):
        tok0_ = tti * T
        xb_, pb_, hT_ = xt[tti % 2], posi[tti % 2], hTs[tti % 2]
        S.dma("sp", xb_.ap, xTv[:, :, tok0_:tok0_ + T], W=xb_.all(), key=f"xt{tti % 2}")
        S.dma("sp", pb_.ap, d["posi"][:, tok0_:tok0_ + T].partition_broadcast(128), W=pb_.all(), key=f"posi{tti % 2}")
        squares8(k, hT_, xb_, T)
        rms_stats(k, lambda i: hT_.ap[:, i, :], 8, D, 0, sd, rstd, T, hT_.all())

    def ln_stt(tti, ci):
        xb_, hT_ = xt[tti % 2], hTs[tti % 2]
        stt(k, hT_.ap[:, ci, :], xb_.ap[:, ci, :], c.gv.ap[:, c.g_mix + ci:c.g_mix + ci + 1], rstd.ap,
            ALU.mult, ALU.mult, xb_.all() + rstd.all() + c.gv.all(), hT_.all())

    def ropes(tti, which):
        pb_ = posi[tti % 2]
        if which == 0:
            rope_tables(k, pb_.ap, pb_.k(), (0, 128), 0, cos_r, sin_r, (ta, tb, tc), T)
        else:
            rope_tables(k, pb_.ap, pb_.k(), (64, 96), 1, cos_m, sin_m, (ta, tb, tc), T)

    ln_part1(tile_list[0])
    for ci in range(8):
        ln_stt(tile_list[0], ci)
    ropes(tile_list[0], 0)
    ropes(tile_list[0], 1)
    for tidx, tti in enumerate(tile_list):
        tok0 = tti * T
        xb, pb, hT = xt[tti % 2], posi[tti % 2], hTs[tti % 2]
        own_tile = tok0 + T > HALO0
        nxt = tile_list[tidx + 1] if tidx + 1 < len(tile_list) else None

        def proj(ps_i, m, wbuf, col0, n=T):
            for ci in range(8):
                mm(k, ps[ps_i][0:m, 0:n], wbuf.ap[:, ci, col0:col0 + m], hT.ap[:, ci, 0:n], ci == 0, ci == 7,
                   wbuf.all() + hT.all(), [psk[ps_i]])

        if k.a1_level < 3:
            continue
        proj(1, 128, w_in, OFF_CKV)
        act(k, sql.ap[:, 0, :], ps[1], AF.Square, [psk[1]], sql.all())
        rms_stats(k, lambda i: sql.ap[:, 0, :], 1, 128, 0, sdl, rstl, T, sql.all())
        stt(k, P.ckvn.ap[:, tok0:tok0 + T], ps[1], c.gv.ap[:, c.g_kv:c.g_kv + 1], rstl.ap, ALU.mult, ALU.mult,
            [psk[1]] + rstl.all() + c.gv.all(), P.ckvn.all())
        proj(2, 96, w_in, OFF_KR - 64)
        proj(3, 96, w_krot, 0)
        tt(k, "dve", t1[0].ap[64:96, :], ps[2][64:96, :], cos_m.ap[64:96, :], ALU.mult, [psk[2]] + cos_m.all(), t1[0].all())
        tt(k, "dve", t2[0].ap[64:96, :], ps[3][64:96, :], sin_m.ap[64:96, :], ALU.mult, [psk[3]] + sin_m.all(), t2[0].all())
        tt(k, "pool", P.krope.ap[64:96, tok0:tok0 + T], t1[0].ap[64:96, :], t2[0].ap[64:96, :], ALU.add,
           t1[0].all() + t2[0].all(), P.krope.all())
        if k.a1_level < 4:
            continue
        if own_tile:
            proj(1, 128, w_in, OFF_CQ)
            proj(2, 128, w_in, OFF_CQ + 128)
            act(k, sql.ap[:, 0, :], ps[1], AF.Square, [psk[1]], sql.all())
            act(k, sql.ap[:, 1, :], ps[2], AF.Square, [psk[2]], sql.all())
            rms_stats(k, lambda i: sql.ap[:, i, :], 2, 256, 0, sdl, rstl, T, sql.all())
            if tok0 < HALO0:
                lo, n_, s0 = HALO0 - tok0, 128, 0
            else:
                lo, n_, s0 = 0, T, tok0 - HALO0
            for j, pi in ((0, 1), (1, 2)):
                stt(k, P.cqn.ap[:, j, s0:s0 + n_], ps[pi][:, lo:lo + n_], c.gv.ap[:, c.g_q + j:c.g_q + j + 1],
                    rstl.ap[:, lo:lo + n_], ALU.mult, ALU.mult, [psk[pi]] + rstl.all() + c.gv.all(), P.cqn.all())
        if k.a1_level < 5:
            continue
        todo = [(rkT, OFF_RK)] + ([(rqT, OFF_RQ)] if own_tile else [])
        items = [(dst, off, h) for (dst, off) in todo for h in range(4)]

        def rope_tail(it, slot):
            dst, off, h = it
            pi, pj = 1 + slot, 3 + slot
            rb, a1, a2 = raw[slot], t1[slot], t2[slot]
            mm(k, ps[pj], c.rot.ap, rb.ap, True, True, rb.all() + c.rot.all(), [psk[pj]])
            tt(k, "dve", a2.ap, ps[pj], sin_r.ap, ALU.mult, [psk[pj]] + sin_r.all(), a2.all())
            tt(k, "pool", a1.ap, rb.ap, cos_r.ap, ALU.mult, rb.all() + cos_r.all(), a1.all())
            tt(k, "pool", dst.ap[:, h, :], a1.ap, a2.ap, ALU.add, a1.all() + a2.all(), [dst.k(h)])

        prev = None
        for n_, it in enumerate(items):
            slot = n_ % 2
            proj(1 + slot, 128, w_in, it[1] + it[2] * 128)
            cp(k, "act", raw[slot].ap, ps[1 + slot], [psk[1 + slot]], raw[slot].all())
            if prev is not None:
                rope_tail(*prev)
            prev = (it, slot)
        rope_tail(*prev)
        if k.a1_level < 6:
            continue
        if own_tile:
            for h in range(4):
                pi = 1 + h % 2
                proj(pi, 128, w_in, OFF_RG + h * 128)
                act(k, sg_tm.ap[:, h, :], ps[pi], AF.Silu, [psk[pi]], [sg_tm.k(h)])
        if nxt is not None:
            ln_part1(nxt)
        p7b = ps[7].bitcast(BF16)
        k7a, k7b = ("ps", "7a"), ("ps", "7b")

        def stage_a(cc):
            ctok = tok0 + cc * 128
            own_chunk = ctok >= HALO0
            cs = slice(cc * 128, (cc + 1) * 128)
            for ci in range(8):
                mm(k, ps[1], hT.ap[:, ci, cs], w_in.ap[:, ci, OFF_RV:OFF_RV + 512], ci == 0, ci == 7,
                   hT.all() + w_in.all(), [psk[1]])
            cp(k, "act", v_tm.ap[:, cc, :], ps[1], [psk[1]], [v_tm.k(cc)])
            for h in range(4):
                tr(k, p7b[:, h * 128:(h + 1) * 128], rkT.ap[:, h, cs], c.ident.ap, [rkT.k(h)] + c.ident.all(), [k7a])
            if own_chunk:
                for h in range(4):
                    mm(k, ps[2][:, h * 128:(h + 1) * 128], rkT.ap[:, h, cs], rqT.ap[:, h, cs], True, True,
                       [rkT.k(h), rqT.k(h)], [psk[2]])
            tt(k, "dve", kdec.ap.rearrange("p (h e) -> p h e", h=4), p7b[:, 0:512].rearrange("p (h e) -> p h e", h=4),
               bc(c.small.ap[:, 2:6].unsqueeze(2), [128, 4, 128]), ALU.mult, [k7a] + c.small.all(), kdec.all())
            if own_chunk:
                tt(k, "dve", PT.ap, ps[2], c.intra.ap, ALU.mult, [psk[2]] + c.intra.all(), PT.all())
                tt(k, "pool", qsT.ap, rqT.ap[:, :, cs], c.qfs.ap.rearrange("p (h e) -> p h e", h=4), ALU.mult,
                   rqT.all() + c.qfs.all(), qsT.all())
            if ctok + 128 < NTOK:
                for h in range(4):
                    hs = slice(h * 128, (h + 1) * 128)
                    mm(k, ps[5][:, hs], kdec.ap[:, hs], v_tm.ap[:, cc, hs], True, True, kdec.all() + [v_tm.k(cc)], [psk[5]])
            if own_chunk:
                for h in range(4):
                    hs = slice(h * 128, (h + 1) * 128)
                    mm(k, ps[6][:, hs], PT.ap[:, hs], v_tm.ap[:, cc, hs], True, False, PT.all() + [v_tm.k(cc)], [psk[6]])
                    mm(k, ps[6][:, hs], qsT.ap[:, h, :], S_b.ap[:, hs], False, True, qsT.all() + S_b.all(), [psk[6]])
                cp(k, "act", ogs[cc % 2].ap, ps[6], [psk[6]], ogs[cc % 2].all())
            if ctok + 128 < NTOK:
                tt(k, "pool", S_f.ap, S_f.ap, c.decay.ap, ALU.mult, S_f.all() + c.decay.all(), S_f.all())
                tt(k, "dve", S_f.ap, S_f.ap, ps[5], ALU.add, S_f.all() + [psk[5]], S_f.all())
                cp(k, "act", S_b.ap, S_f.ap, S_f.all(), S_b.all())

        def stage_b(cc):
            ctok = tok0 + cc * 128
            if ctok < HALO0:
                return
            cs = slice(cc * 128, (cc + 1) * 128)
            og = ogs[cc % 2]
            for h in range(4):
                tr(k, p7b[:, 512 + h * 128:512 + (h + 1) * 128], og.ap[:, h * 128:(h + 1) * 128], c.ident.ap,
                   og.all() + c.ident.all(), [k7b])
            cp(k, "dve", oT.ap, p7b[:, 512:1024], [k7b], oT.all())
            act(k, sqT.ap, oT.ap, AF.Square, oT.all(), sqT.all())
            mm(k, ps[4], c.ones.ap, oT.ap, True, True, c.ones.all() + oT.all(), [psk[4]])
            mm(k, ps[3], c.ones.ap, sqT.ap, True, True, c.ones.all() + sqT.all(), [psk[3]])
            act(k, tn.ap, ps[4], AF.Copy, [psk[4]], tn.all(), scale=1.0 / 128)
            tt(k, "dve", sqo.ap, tn.ap, tn.ap, ALU.mult, tn.all(), sqo.all())
            stt(k, sqo.ap, ps[3], 1.0 / 128, sqo.ap, ALU.mult, ALU.subtract, [psk[3]] + sqo.all(), sqo.all())
            act(k, sqo.ap, sqo.ap, AF.Ln, sqo.all(), sqo.all(), scale=1.0, bias=EPS)
            act(k, sqo.ap, sqo.ap, AF.Exp, sqo.all(), sqo.all(), scale=-0.5)
            tt(k, "dve", tn.ap, oT.ap, tn.ap, ALU.subtract, oT.all() + tn.all(), tn.all())
            tt(k, "pool", tn.ap, tn.ap, sqo.ap, ALU.mult, tn.all() + sqo.all(), tn.all())
            s0 = ctok - HALO0
            tt(k, "pool", P.oretT.ap[:, :, s0:s0 + 128], tn.ap.rearrange("p (h e) -> p h e", h=4), sg_tm.ap[:, :, cs],
               ALU.mult, tn.all() + sg_tm.all(), P.oretT.all())

        for cc in range(5):
            if cc < 4:
                stage_a(cc)
            if cc >= 1:
                stage_b(cc - 1)
            if nxt is not None and cc < 4:
                ln_stt(nxt, 2 * cc)
                ln_stt(nxt, 2 * cc + 1)
                if cc == 1:
                    ropes(nxt, 0)
                if cc == 2:
                    ropes(nxt, 1)
    S.merge_keys([("ps", "7a"), ("ps", "7b")], psk[7])
    if k.stage == "A1":
        mm(k, ps[0][:, 0:128], c.ones.ap, c.ones.ap, True, True, c.ones.all(), [psk[0]])
    if k.dbg and k.stage == "A1":
        dump(k, "ckvn", P.ckvn.ap, [128, NTOK], P.ckvn.all(), BF16)
        dump(k, "krope", P.krope.ap[64:96, :], [32, NTOK], P.krope.all(), BF16)
        dump(k, "cqn", P.cqn.ap, [128, 2, NST], P.cqn.all(), BF16)
        dump(k, "oretT", P.oretT.ap, [128, 4, NST], P.oretT.all(), BF16)
    S.release(w_in, w_krot, *xt, *posi, *hTs, rstd, ta, tb, tc, cos_r, sin_r, cos_m, sin_m, sql, rstl, *raw, *t1,
              *t2, rqT, rkT, v_tm, sg_tm, kdec, PT, qsT, S_f, S_b, sqo, tn, *ogs, oT, sqT)


def phase_MKV(k):
    S, d, c, P, ps, psk = k.S, k.d, k.c, k.P, k.ps, k.psk
    w_xkv = S.alloc("w_xkv_bf", [128, 8, 2 * D], BF16)
    S.dma("pool", w_xkv.ap, d["w_xkv"].rearrange("(c p) n -> p c n", p=128), W=w_xkv.all(), key="w_xkv")
    mt = S.alloc("memT", [128, 8, MEM], F32)
    S.dma("sp", mt.ap, d["memT"].rearrange("(c p) t -> p c t", p=128), W=mt.all(), key="memT")
    hm = S.alloc("hmem", [128, 8, MEM], BF16)
    sd, rstd = S.alloc("sdm", [128, MEM], F32), S.alloc("rstdm", [128, MEM], F32)
    squares8(k, hm, mt, MEM)
    rms_stats(k, lambda i: hm.ap[:, i, :], 8, D, 0, sd, rstd, MEM, hm.all())
    for ci in range(8):
        stt(k, hm.ap[:, ci, :], mt.ap[:, ci, :], c.gv.ap[:, c.g_mem + ci:c.g_mem + ci + 1], rstd.ap, ALU.mult, ALU.mult,
            mt.all() + rstd.all() + c.gv.all(), hm.all())
    for blk in range(8):
        pi = 1 + blk % 2
        for ci in range(8):
            mm(k, ps[pi][:, 0:MEM], w_xkv.ap[:, ci, blk * 128:(blk + 1) * 128], hm.ap[:, ci, :], ci == 0, ci == 7,
               w_xkv.all() + hm.all(), [psk[pi]])
        cp(k, "act", P.memKT.ap[:, blk, :], ps[pi][:, 0:MEM], [psk[pi]], P.memKT.all())
    n = 0
    for kb2 in range(2):
        for half in range(2):
            pi = 3 + n % 2
            n += 1
            for ci in range(8):
                mm(k, ps[pi], hm.ap[:, ci, kb2 * 128:(kb2 + 1) * 128], w_xkv.ap[:, ci, D + half * 512:D + (half + 1) * 512],
                   ci == 0, ci == 7, w_xkv.all() + hm.all(), [psk[pi]])
            cp(k, "dve", P.memV.ap[:, kb2, half * 512:(half + 1) * 512], ps[pi], [psk[pi]], P.memV.all())
    S.release(w_xkv, mt, hm, sd, rstd)


def phase_A2(k):
    S, d, c, P, ps, psk = k.S, k.d, k.c, k.P, k.ps, k.psk
    ps2 = k.ps2
    w_uq = S.alloc("w_uq_bf", [128, 2, 768], BF16)
    w_uqr = S.alloc("w_uqr_bf", [128, 2, 768], BF16)
    w_ukv = S.alloc("w_ukv_bf", [128, 1024], BF16)
    S.dma("pool", w_uq.ap, d["w_uq"].rearrange("(c p) n -> p c n", p=128), W=w_uq.all(), key="w_uq")
    S.dma("pool", w_ukv.ap, d["w_ukv"], W=w_ukv.all(), key="w_ukv")
    S.dma("pool", P.w_out.ap, d["w_out"].rearrange("(c p) n -> p c n", p=128), W=P.w_out.all(), key="w_out")
    S.dma("pool", P.w_xq.ap, d["w_xq"].rearrange("(c p) n -> p c n", p=128), W=P.w_xq.all(), key="w_xq")
    S.op("pool", lambda e: e.memset(w_uqr.ap, 0.0), W=w_uqr.all())
    q4 = w_uq.ap.rearrange("p c (h x) -> p c h x", h=8)
    r4 = w_uqr.ap.rearrange("p c (h x) -> p c h x", h=8)
    for ci in range(2):
        ts(k, "pool", r4[:, ci, :, 64:80], q4[:, ci, :, 80:96], -1.0, None, ALU.mult, None, w_uq.all(), w_uqr.all())
        cp(k, "pool", r4[:, ci, :, 80:96], q4[:, ci, :, 64:80], w_uq.all(), w_uqr.all())
    KT = S.alloc("KT", [128, 4, NTOK], BF16, nsub=4)
    Vc = S.alloc("Vc", [128, 32, 384], BF16)
    S.op("pool", lambda e: e.memset(Vc.ap, 1.0), W=Vc.all())
    for o in (64, 256):
        ts(k, "dve", Vc.ap[:, 0:16, o:o + 64], Vc.ap[:, 0:16, o:o + 64], c.flags.ap[:, 0:1], None, ALU.mult, None,
           Vc.all() + c.flags.all(), Vc.all())
    qTb = [[S.alloc(f"qT{b_}_{i}", [128, 512], BF16) for i in range(4)] for b_ in range(2)]
    for q_ in qTb[0] + qTb[1]:
        S.op("pool", lambda e, q_=q_: e.memset(q_.ap[96:128, :], 0.0), W=q_.all())
    S.op("pool", lambda e: e.memset(KT.ap[96:128, :, :], 0.0), W=KT.all())
    PTb = [S.alloc(f"PTb{i}", [128, 1024], BF16) for i in range(2)]
    tq1, tq2 = S.alloc("tq1", [128, 512], F32), S.alloc("tq2", [128, 512], F32)
    ta, tb, tc = (S.alloc(n, [128, 512], F32) for n in ("ta2", "tb2", "tc2"))
    cos_m, sin_m = S.alloc("cos_m2", [128, 512], F32), S.alloc("sin_m2", [128, 512], F32)
    pq = S.alloc("posq", [128, 512], I32)
    rec = S.alloc("rec", [128, 512], F32)
    wk3 = w_ukv.ap.rearrange("p (h x) -> p h x", h=8)
    scale = 1.0 / math.sqrt(96.0)
    n_ev = 0
    for hh in range(2):
        heads = list(range(4 * hh, 4 * hh + 4))
        for kt in range(NTOK // 512):
            for hi, h in enumerate(heads):
                pi = n_ev % 2
                mm(k, ps[pi], w_ukv.ap[:, h * 128:h * 128 + 128], P.ckvn.ap[:, kt * 512:(kt + 1) * 512], True, True,
                   w_ukv.all() + P.ckvn.all(), [psk[pi]])
                cp(k, "act" if n_ev % 2 == 0 else "dve", KT.ap[0:64, hi, kt * 512:(kt + 1) * 512], ps[pi][0:64, :], [psk[pi]],
                   [KT.k(hi)])
                n_ev += 1
        for hi in range(4):
            S.dma("sp", KT.ap[64:96, hi, :], P.krope.ap[64:96, :], R=P.krope.all(), W=[KT.k(hi)], key=f"kr{hi}")
        for kb in range(NTOK // 128):
            pi = 2 + kb % 2
            mm(k, ps[pi], P.ckvn.ap[:, kb * 128:(kb + 1) * 128], w_ukv.ap[:, 512 * hh:512 * (hh + 1)], True, True,
               w_ukv.all() + P.ckvn.all(), [psk[pi]])
            src = ps[pi].rearrange("p (a m x) -> p a m x", a=2, m=2)
            dst = Vc.ap[:, kb, :].rearrange("p (a r) -> p a r", a=2)
            cp(k, "dve", dst[:, :, 0:64], src[:, :, 0, 64:128], [psk[pi]], Vc.all())
            cp(k, "dve", dst[:, :, 128:192], src[:, :, 1, 64:128], [psk[pi]], Vc.all())
        def qtile(qi):
            if qi == 0:
                return 0, 128, HALO0 // 128
            return 128 + 512 * (qi - 1), 512, NPRE // 128 + 4 * (qi - 1)

        def q_assemble(qi):
            s0, NQ, qblk0 = qtile(qi)
            qTs = qTb[qi % 2]
            g0 = HALO0 + s0
            S.dma("sp", pq.ap[:, 0:NQ], d["posi"][:, g0:g0 + NQ].partition_broadcast(128), W=pq.all(), key="posq")
            rope_tables(k, pq.ap, pq.k(), (64, 96), 1, cos_m, sin_m, (ta, tb, tc), NQ)
            for hi, h in enumerate(heads):
                for ci in range(2):
                    mm(k, ps[0][0:96, 0:NQ], w_uq.ap[:, ci, h * 96:(h + 1) * 96], P.cqn.ap[:, ci, s0:s0 + NQ], ci == 0, ci == 1,
                       w_uq.all() + P.cqn.all(), [psk[0]])
                for ci in range(2):
                    mm(k, ps[1][0:96, 0:NQ], w_uqr.ap[:, ci, h * 96:(h + 1) * 96], P.cqn.ap[:, ci, s0:s0 + NQ], ci == 0, ci == 1,
                       w_uqr.all() + P.cqn.all(), [psk[1]])
                cp(k, "dve", qTs[hi].ap[0:64, 0:NQ], ps[0][0:64, 0:NQ], [psk[0]], qTs[hi].all())
                tt(k, "dve", tq1.ap[64:96, 0:NQ], ps[0][64:96, 0:NQ], cos_m.ap[64:96, 0:NQ], ALU.mult, [psk[0]] + cos_m.all(),
                   tq1.all())
                tt(k, "dve", tq2.ap[64:96, 0:NQ], ps[1][64:96, 0:NQ], sin_m.ap[64:96, 0:NQ], ALU.mult, [psk[1]] + sin_m.all(),
                   tq2.all())
                tt(k, "pool", qTs[hi].ap[64:96, 0:NQ], tq1.ap[64:96, 0:NQ], tq2.ap[64:96, 0:NQ], ALU.add, tq1.all() + tq2.all(),
                   qTs[hi].all())

        q_assemble(0)
        for qi in range(5):
            s0, NQ, qblk0 = qtile(qi)
            nqb = NQ // 128
            qT = qTb[qi % 2]
            if qi + 1 < 5:
                q_assemble(qi + 1)
            for hi, h in enumerate(heads):
                pair, mem = divmod(hi, 2)
                vcol0 = pair * 192 + mem * 64
                pob = 6 + hi % 2
                po = ps[pob]
                nkb = qblk0 + nqb
                groups = []
                kb = 0
                while kb < nkb:
                    if kb + 1 < qblk0:
                        groups.append((kb, kb + 1))
                        kb += 2
                    else:
                        groups.append((kb,))
                        kb += 1
                pend = None

                def pv(pd, nkb=nkb, po=po, pob=pob, vcol0=vcol0, NQ=NQ):
                    for (kb_, qlo_, n_, ptap_, ptk_) in pd:
                        mm(k, po[:, qlo_:NQ], Vc.ap[:, kb_, vcol0:vcol0 + 128], ptap_, kb_ == 0, kb_ == nkb - 1,
                           Vc.all() + ptk_, [psk[pob]])

                for gi, grp in enumerate(groups):
                    slot = gi % 2
                    pt = PTb[slot]
                    banks = (2 + 2 * slot, 3 + 2 * slot)
                    cur = []
                    for j_, kb in enumerate(grp):
                        r = kb - qblk0
                        q_lo = max(r, 0) * 128
                        n = NQ - q_lo
                        sb = banks[j_]
                        mm(k, ps[sb][:, 0:n], KT.ap[:, hi, kb * 128:(kb + 1) * 128], qT[hi].ap[:, q_lo:NQ], True, True,
                           [KT.k(hi)] + qT[hi].all(), [psk[sb]])
                        cur.append((kb, q_lo, n, pt.ap[:, j_ * 512:j_ * 512 + n], pt.all()))
                    if len(grp) == 2 and NQ == 512:
                        act(k, pt.ap, ps2[1 + slot], AF.Exp, [psk[banks[0]], psk[banks[1]]], pt.all(), scale=scale)
                    else:
                        for j_, (kb, q_lo, n, ptap, _) in enumerate(cur):
                            act(k, ptap, ps[banks[j_]][:, 0:n], AF.Exp, [psk[banks[j_]]], pt.all(), scale=scale)
                            if kb - qblk0 >= 0:
                                tt(k, "pool", ptap[:, 0:128], ptap[:, 0:128], c.causal.ap, ALU.mult, pt.all() + c.causal.all(),
                                   pt.all())
                    if pend is not None:
                        pv(pend)
                    pend = cur
                pv(pend)
                if mem == 0:
                    o_rows, s_rows = slice(0, 64), slice(64, 128)
                else:
                    o_rows, s_rows = slice(64, 128), slice(0, 64)
                ts(k, "dve", rec.ap[o_rows, 0:NQ], po[s_rows, 0:NQ], 1e-30, None, ALU.add, None, [psk[pob]], rec.all())
                recip(k, rec.ap[o_rows, 0:NQ], rec.ap[o_rows, 0:NQ], rec.all(), rec.all())
                tt(k, "dve", P.omlaT.ap[o_rows, 2 * hh + pair, s0:s0 + NQ], po[o_rows, 0:NQ], rec.ap[o_rows, 0:NQ], ALU.mult,
                   [psk[pob]] + rec.all(), P.omlaT.all())
    if k.dbg and k.stage == "A2":
        dump(k, "omlaT", P.omlaT.ap, [128, 4, NST], P.omlaT.all(), BF16)
    S.release(w_uq, w_uqr, w_ukv, KT, Vc, *qTb[0], *qTb[1], *PTb, tq1, tq2, ta, tb, tc, cos_m, sin_m, pq, rec)


def _norm_to_hT(k, xb, hT, sd, rstd, gcol, n):
    c = k.c
    squares8(k, hT, xb, n)
    rms_stats(k, lambda i: hT.ap[:, i, 0:n], 8, D, 0, sd, rstd, n, hT.all())
    for ci in range(8):
        stt(k, hT.ap[:, ci, 0:n], xb.ap[:, ci, 0:n], c.gv.ap[:, gcol + ci:gcol + ci + 1], rstd.ap[:, 0:n], ALU.mult, ALU.mult,
            xb.all() + rstd.all() + c.gv.all(), hT.all())


def phase_A3X(k):
    S, d, c, P, ps, psk = k.S, k.d, k.c, k.P, k.ps, k.psk
    xbs = [S.alloc(f"xa{i}", [128, 8, 512], F32) for i in range(2)]
    hT = S.alloc("hTa", [128, 8, 512], BF16)
    rstd = S.alloc("rstda", [128, 512], F32)
    sd = rstd
    qxT = S.alloc("qxT", [128, 8, 512], BF16, nsub=8)
    PTx = [S.alloc(f"PTx{i}", [128, 512], BF16) for i in range(2)]
    oxT = hT
    rec = S.alloc("recx", [128, 512], F32)
    xTv = d["xT"].rearrange("(c p) t -> p c t", p=128)
    x2v = d["x2s"].rearrange("(c p) t -> p c t", p=128)
    tiles = [(0, 128)] + [(128 + 512 * i, 512) for i in range(4)]
    def xload(ti_):
        s0_, N_ = tiles[ti_]
        S.dma("sp", xbs[ti_ % 2].ap[:, :, 0:N_], xTv[:, :, HALO0 + s0_:HALO0 + s0_ + N_], W=xbs[ti_ % 2].all(), key=f"xa{ti_ % 2}")

    xload(0)
    for ti_, (s0, N) in enumerate(tiles):
        xb = xbs[ti_ % 2]
        if ti_ + 1 < len(tiles):
            xload(ti_ + 1)
        for cb in range(8):
            pi = 1 + cb % 2
            cs = slice(cb * 128, (cb + 1) * 128)
            for j in range(4):
                mm(k, ps[pi][:, 0:N], P.w_out.ap[:, j, cs], P.omlaT.ap[:, j, s0:s0 + N], j == 0, False,
                   P.w_out.all() + P.omlaT.all(), [psk[pi]])
            for j in range(4):
                mm(k, ps[pi][:, 0:N], P.w_out.ap[:, 4 + j, cs], P.oretT.ap[:, j, s0:s0 + N], False, j == 3,
                   P.w_out.all() + P.oretT.all(), [psk[pi]])
            tt(k, "dve", xb.ap[:, cb, 0:N], xb.ap[:, cb, 0:N], ps[pi][:, 0:N], ALU.add, xb.all() + [psk[pi]], xb.all())
        if k.dbg and k.stage == "A3X":
            dumpx(k, "x1", xb, s0, N)
        _norm_to_hT(k, xb, hT, sd, rstd, c.g_xattn, N)
        for blk in range(8):
            pi = 1 + blk % 2
            for ci in range(8):
                mm(k, ps[pi][:, 0:N], P.w_xq.ap[:, ci, blk * 128:(blk + 1) * 128], hT.ap[:, ci, 0:N], ci == 0, ci == 7,
                   P.w_xq.all() + hT.all(), [psk[pi]])
            cp(k, "act", qxT.ap[:, blk, 0:N], ps[pi][:, 0:N], [psk[pi]], [qxT.k(blk)])
        for h in range(4):
            for kb2 in range(2):
                sb = 3 + kb2
                for dc in range(2):
                    mm(k, ps[sb][:, 0:N], P.memKT.ap[:, 2 * h + dc, kb2 * 128:(kb2 + 1) * 128], qxT.ap[:, 2 * h + dc, 0:N],
                       dc == 0, dc == 1, P.memKT.all() + [qxT.k(2 * h + dc)], [psk[sb]])
                act(k, PTx[kb2].ap[:, 0:N], ps[sb][:, 0:N], AF.Exp, [psk[sb]], PTx[kb2].all(), scale=1.0 / 16.0)
            for kb2 in range(2):
                mm(k, ps[5][:, 0:N], c.ones.ap, PTx[kb2].ap[:, 0:N], kb2 == 0, kb2 == 1, c.ones.all() + PTx[kb2].all(), [psk[5]])
            act(k, rec.ap[:, 0:N], ps[5][:, 0:N], AF.Ln, [psk[5]], rec.all())
            act(k, rec.ap[:, 0:N], rec.ap[:, 0:N], AF.Exp, rec.all(), rec.all(), scale=-1.0)
            for eb in range(2):
                pi = 6 + eb
                for kb2 in range(2):
                    mm(k, ps[pi][:, 0:N], P.memV.ap[:, kb2, h * 256 + eb * 128:h * 256 + (eb + 1) * 128], PTx[kb2].ap[:, 0:N],
                       kb2 == 0, kb2 == 1, P.memV.all() + PTx[kb2].all(), [psk[pi]])
                tt(k, "dve", oxT.ap[:, 2 * h + eb, 0:N], ps[pi][:, 0:N], rec.ap[:, 0:N], ALU.mult, [psk[pi]] + rec.all(), oxT.all())
        for cb in range(8):
            pi = 1 + cb % 2
            for j in range(8):
                mm(k, ps[pi][:, 0:N], P.w_xo.ap[:, j, cb * 128:(cb + 1) * 128], oxT.ap[:, j, 0:N], j == 0, j == 7,
                   P.w_xo.all() + oxT.all(), [psk[pi]])
            tt(k, "dve", xb.ap[:, cb, 0:N], xb.ap[:, cb, 0:N], ps[pi][:, 0:N], ALU.add, xb.all() + [psk[pi]], xb.all())
        S.dma("sp", x2v[:, :, s0:s0 + N], xb.ap[:, :, 0:N], R=xb.all(), W=[("x2s", s0)], key=f"xs{ti_ % 2}")
        if k.dbg and k.stage == "A3X":
            dumpx(k, "x2", xb, s0, N)
    S.release(*xbs, hT, rstd, qxT, *PTx, rec)


def dumpx(k, name, xb, s0, N):
    if name not in k.dbg_out:
        k.dbg_out[name] = k.nc.dram_tensor("dbg_" + name, [D, NST], F32, kind="ExternalOutput").ap()
    t = k.dbg_out[name].rearrange("(c p) t -> p c t", p=128)
    k.S.dma("sp", t[:, :, s0:s0 + N], xb.ap[:, :, 0:N], R=xb.all(), W=[("dbg", name, s0)], key="dbg")


def phase_F(k):
    S, d, c, P, ps, psk = k.S, k.d, k.c, k.P, k.ps, k.psk
    xf = S.alloc("xf", [128, 8, 512], F32)
    hT = S.alloc("hTf", [128, 8, 512], BF16)
    sd, rstd = S.alloc("sdf", [128, 512], F32), S.alloc("rstdf", [128, 512], F32)
    aT = S.alloc("aT", [128, NFB, 512], BF16, nsub=NFB)
    gsb = [S.alloc(f"gsb{i}", [128, 48 + 512], F32) for i in range(2)]
    cv = [S.alloc(f"cv{i}", [128, 512], F32) for i in range(2)]
    ghalo = S.alloc("ghalo", [128, NFB, 48], F32, nsub=NFB)
    x2v = d["x2s"].rearrange("(c p) t -> p c t", p=128)
    outv = d["outT"].rearrange("(c p) t -> p c t", p=128)
    w1 = P.w_ffn_in
    ostg = S.arena[0:128, aT.off:aT.off + 8 * 512 * 4].bitcast(F32).rearrange("p (a b) -> p a b", a=8)
    S.dma("sp", xf.ap[:, :, 0:128], x2v[:, :, 0:128], R=[("x2s", 0)], W=xf.all(), key="xf")
    _norm_to_hT(k, xf, hT, sd, rstd, c.g_ffn, 128)
    for j in range(NFB):
        pi = 1 + j % 2
        for ci in range(8):
            mm(k, ps[pi][:, 0:128], w1.ap[:, ci, j * 128:(j + 1) * 128], hT.ap[:, ci, 0:128], ci == 0, ci == 7,
               [w1.k(0 if j < 11 else 2)] + hT.all(), [psk[pi]])
        ts(k, "dve", ghalo.ap[:, j, :], ps[pi][:, 80:128], c.flags.ap[:, 1:2], None, ALU.mult, None, [psk[pi]] + c.flags.all(),
           [ghalo.k(j)])
    for ti in range(4):
        s0 = 128 + 512 * ti
        S.dma("sp", xf.ap, x2v[:, :, s0:s0 + 512], R=[("x2s", s0)], W=xf.all(), key="xf")
        _norm_to_hT(k, xf, hT, sd, rstd, c.g_ffn, 512)
        for j in range(NFB):
            pg, pu = 1 + (j % 2), (3, 4, 7)[j % 3]
            for ci in range(8):
                mm(k, ps[pg], w1.ap[:, ci, j * 128:(j + 1) * 128], hT.ap[:, ci, :], ci == 0, ci == 7,
                   [w1.k(0 if j < 11 else 2)] + hT.all(), [psk[pg]])
            for ci in range(8):
                mm(k, ps[pu], w1.ap[:, ci, DFF + j * 128:DFF + (j + 1) * 128], hT.ap[:, ci, :], ci == 0, ci == 7,
                   [w1.k(1 if j < 11 else 3)] + hT.all(), [psk[pu]])
            g, cvb = gsb[j % 2], cv[j % 2]
            cw = c.convp.ap
            cp(k, "act", g.ap[:, 48:560], ps[pg], [psk[pg]], g.all())
            cp(k, "pool", g.ap[:, 0:48], ghalo.ap[:, j, :], [ghalo.k(j)], g.all())
            k.S.op("act", lambda e, g=g, cvb=cvb, j=j: e.activation(out=cvb.ap, in_=g.ap[:, 48:560], func=AF.Identity,
                                                                     scale=cw[:, j, 2:3], bias=cw[:, j, 3:4]),
                   g.all() + c.convp.all(), cvb.all())
            stt(k, cvb.ap, g.ap[:, 47:559], cw[:, j, 1:2], cvb.ap, ALU.mult, ALU.add, g.all() + cvb.all() + c.convp.all(), cvb.all())
            stt(k, cvb.ap, g.ap[:, 46:558], cw[:, j, 0:1], cvb.ap, ALU.mult, ALU.add, g.all() + cvb.all() + c.convp.all(), cvb.all())
            cp(k, "pool", ghalo.ap[:, j, :], g.ap[:, 512:560], g.all(), [ghalo.k(j)])
            act(k, cvb.ap, cvb.ap, AF.Silu, cvb.all(), cvb.all())
            tt(k, "dve", aT.ap[:, j, :], cvb.ap, ps[pu], ALU.mult, cvb.all() + [psk[pu]], [aT.k(j)])
        for cb in range(8):
            pi = 5 + cb % 2
            for j in range(NFB):
                mm(k, ps[pi], P.w_ffn_out.ap[:, j, cb * 128:(cb + 1) * 128], aT.ap[:, j, :], j == 0, j == NFB - 1,
                   P.w_ffn_out.all() + [aT.k(j)], [psk[pi]])
            tt(k, "dve", xf.ap[:, cb, :], xf.ap[:, cb, :], ps[pi], ALU.add, xf.all() + [psk[pi]], xf.all())
        squares8(k, hT, xf, 512)
        rms_stats(k, lambda i: hT.ap[:, i, :], 8, D, 0, sd, rstd, 512, hT.all())
        for cb in range(8):
            stt(k, ostg[:, cb, :], xf.ap[:, cb, :], c.gv.ap[:, c.g_final + cb:c.g_final + cb + 1], rstd.ap, ALU.mult, ALU.mult,
                xf.all() + rstd.all() + c.gv.all(), aT.all())
        S.dma("sp", outv[:, :, 512 * ti:512 * (ti + 1)], ostg, R=aT.all(), W=[("out", ti)], key="out")
    S.release(xf, hT, sd, rstd, aT, *gsb, *cv, ghalo)


def _consts():
    f32 = np.float32
    H, L = 4, 128
    log_gamma = np.log(f32(1.0) - f32(2.0) ** (f32(-5.0) - np.arange(H, dtype=f32))).astype(f32)
    j = np.arange(L, dtype=f32)
    diff = j[:, None] - j[None, :]
    intra = np.where(diff[None] >= 0, np.exp(np.maximum(diff, 0.0)[None] * log_gamma[:, None, None]), 0.0).astype(f32)
    k_to_end = np.exp((L - 1 - j)[:, None] * log_gamma[None, :]).astype(f32)
    q_from_start = np.exp((j + 1)[:, None] * log_gamma[None, :]).astype(f32)
    chunk_decay = np.exp(f32(L) * log_gamma).astype(f32)
    dk = f32(128.0 ** -0.5)
    c_intra = np.zeros((128, 512), f32)
    for h in range(H):
        c_intra[:, h * 128:(h + 1) * 128] = intra[h].T * dk
    c_qfs = np.zeros((128, 512), f32)
    c_decay = np.zeros((128, 512), f32)
    for h in range(H):
        c_qfs[:, h * 128:(h + 1) * 128] = q_from_start[:, h][None, :]
        c_decay[:, h * 128:(h + 1) * 128] = chunk_decay[h]
    c_small = np.zeros((128, 8), f32)
    invf_r = (1.0 / (f32(10000.0) ** (np.arange(0, 128, 2, dtype=f32) / f32(128)))).astype(f32)
    invf_m = (1.0 / (f32(10000.0) ** (np.arange(0, 32, 2, dtype=f32) / f32(32)))).astype(f32)
    p = np.arange(128)
    c_small[:, 0] = invf_r[p % 64]
    c_small[:, 1] = invf_m[p % 16]
    c_small[:, 2:6] = k_to_end * dk
    c_ident = np.eye(128, dtype=f32)
    c_rot = np.zeros((128, 128), f32)
    for m in range(64):
        c_rot[m + 64, m] = -1.0
    for m in range(64, 128):
        c_rot[m - 64, m] = 1.0
    kk = np.arange(128)
    c_causal = (kk[None, :] >= kk[:, None]).astype(f32)
    return dict(c_small=c_small, c_ident=c_ident, c_rot=c_rot, c_causal=c_causal, c_intra=c_intra, c_qfs=c_qfs,
                c_decay=c_decay)


def make_in_maps(inputs):
    f32 = np.float32
    x = np.asarray(inputs["x"], f32)
    mem = np.asarray(inputs["mem"], f32)
    pos = np.asarray(inputs["positions"], np.int32)

    def col(g):
        g = np.asarray(g, f32).reshape(-1, 128)
        return np.ascontiguousarray(g.T)

    gv = np.concatenate([col(inputs["g_mix"][0]), col(inputs["g_xattn"][0]), col(inputs["g_mem"][0]),
                         col(inputs["g_ffn"][0]), col(inputs["g_final"]), col(inputs["g_q_lat"][0]),
                         col(inputs["g_kv_lat"][0])], axis=1)
    cw = np.asarray(inputs["conv_w"][0], f32)
    cb = np.asarray(inputs["conv_b"][0], f32)
    convp = np.zeros((128, NFB, 4), f32)
    for i in range(3):
        convp[:, :, i] = cw[i].reshape(NFB, 128).T
    convp[:, :, 3] = cb.reshape(NFB, 128).T
    shared = dict(
        w_in=np.ascontiguousarray(inputs["w_in"][0], f32), w_uq=np.ascontiguousarray(inputs["w_uq"][0], f32),
        w_ukv=np.ascontiguousarray(inputs["w_ukv"][0], f32), w_out=np.ascontiguousarray(inputs["w_out"][0], f32),
        w_xq=np.ascontiguousarray(inputs["w_xq"][0], f32), w_xkv=np.ascontiguousarray(inputs["w_xkv"][0], f32),
        w_xo=np.ascontiguousarray(inputs["w_xo"][0], f32), w_ffn_in=np.ascontiguousarray(inputs["w_ffn_in"][0], f32),
        w_ffn_out=np.ascontiguousarray(inputs["w_ffn_out"][0], f32), gv=np.ascontiguousarray(gv),
        convp=np.ascontiguousarray(convp.reshape(128, NFB * 4)), **_consts())
    maps = []
    for core in range(8):
        b, hf = core // 2, core % 2
        xT = np.zeros((D, NTOK), f32)
        pp = np.zeros((1, NTOK), np.int32)
        if hf == 0:
            xT[:, NPRE:] = x[b, :NOWN].T
            pp[0, NPRE:] = pos[b, :NOWN]
        else:
            xT[:, :] = x[b].T
            pp[0, :] = pos[b]
        flags = np.full((128, 2), float(hf), f32)
        m = dict(shared)
        m.update(xT=xT, posi=pp, memT=np.ascontiguousarray(mem[b].T), flags=flags)
        maps.append(m)
    return maps


_CACHE = {}


def kernel(**inputs):
    if "nc" not in _CACHE:
        _CACHE["nc"] = build("full")[0]
    nc = _CACHE["nc"]
    maps = make_in_maps(inputs)
    res = run_bass_kernel_spmd(nc, maps, core_ids=list(range(8)))
    out = np.zeros((NB, SEQ, D), np.float32)
    for core in range(8):
        b, hf = core // 2, core % 2
        out[b, hf * NOWN:(hf + 1) * NOWN, :] = res.results[core]["outT"].T
    return out
```

```python
import math
from contextlib import ExitStack

import numpy as np
import concourse.bass as bass
import concourse.mybir as mybir
from concourse.bass_utils import run_bass_kernel_spmd

F32 = mybir.dt.float32
BF16 = mybir.dt.bfloat16
I32 = mybir.dt.int32
U8 = mybir.dt.uint8
AF = mybir.ActivationFunctionType
ALU = mybir.AluOpType
AX = mybir.AxisListType

D = 1024
SEQ = 4096
NB = 4
EPS = 1e-6
NPRE = 2048
NOWN = 2048
NTOK = NPRE + NOWN
HALO0 = NPRE - 128
NST = NOWN + 128
IN_COLS = 2464
OFF_CQ, OFF_CKV, OFF_KR, OFF_RQ, OFF_RK, OFF_RV, OFF_RG = 0, 256, 384, 416, 928, 1440, 1952
DFF = 2816
NFB = DFF // 128
MEM = 256
TWO_PI = 2.0 * math.pi
C1 = 6.28125
C2 = TWO_PI - C1
MAGIC = 12582912.0
PI_LO = 3.1415925
ARENA_BYTES = 206 * 1024
ENGS = ("pe", "act", "dve", "pool", "sp")
NDMA_MAX = 90
DT_SIZE = {F32: 4, BF16: 2, I32: 4}


class Buf:
    def __init__(self, uid, name, off, nbytes, ap, nsub):
        self.uid, self.name, self.off, self.nbytes, self.ap, self.nsub = uid, name, off, nbytes, ap, nsub

    def k(self, i=0):
        return (self.uid, i)

    def all(self):
        return [(self.uid, i) for i in range(self.nsub)]

    def __getitem__(self, idx):
        return self.ap[idx]


class Sched:
    def __init__(self, nc, es):
        self.nc, self.es = nc, es
        self.prog = {e: [] for e in ENGS}
        self.sem = {e: es.enter_context(nc.semaphore("s_" + e)) for e in ENGS if e != "sp"}
        self.cnt = {e: 0 for e in ENGS}
        self.seen = {e: {} for e in ENGS}
        self.drained = {e: 0 for e in ENGS}
        self.needed = {e: set() for e in ENGS}
        self.lastw, self.readers = {}, {}
        self.ndma = 0
        self.dma_events = []
        self.dma_pool = [es.enter_context(nc.semaphore(f"d{i}")) for i in range(NDMA_MAX)]
        self.dma_sem = {}
        self.arena = es.enter_context(nc.sbuf_tensor("arena", [128, ARENA_BYTES], U8))
        self.free = [(0, ARENA_BYTES)]
        self.freed_events = []
        self.nbuf = 0
        self.peak = 0
        self.used = 0

    def alloc(self, name, shape, dtype, nsub=1):
        n = 1
        for s in shape[1:]:
            n *= s
        nbytes = (n * DT_SIZE[dtype] + 63) // 64 * 64
        for i, (o, sz) in enumerate(self.free):
            if sz >= nbytes:
                off = o
                if sz == nbytes:
                    self.free.pop(i)
                else:
                    self.free[i] = (o + nbytes, sz - nbytes)
                break
        else:
            raise MemoryError(f"arena full allocating {name} {nbytes}B; free={self.free}")
        ap = self.arena[0:shape[0], off:off + n * DT_SIZE[dtype]].bitcast(dtype)
        if len(shape) == 3:
            ap = ap.rearrange("p (a b) -> p a b", a=shape[1])
        elif len(shape) == 4:
            ap = ap.rearrange("p (a b c) -> p a b c", a=shape[1], b=shape[2])
        self.nbuf += 1
        b = Buf(self.nbuf, name, off, nbytes, ap, nsub)
        evs, keep = [], []
        for (fo, fn, fe) in self.freed_events:
            if fo < off + nbytes and off < fo + fn:
                evs.extend(fe)
            keep.append((fo, fn, fe))
        self.freed_events = keep
        if evs:
            for kk in b.all():
                self.readers[kk] = list(evs)
        self.used += nbytes
        self.peak = max(self.peak, self.used)
        return b

    def release(self, *bufs):
        for b in bufs:
            evs = []
            for kk in b.all():
                if kk in self.lastw:
                    evs.append(self.lastw.pop(kk))
                evs.extend(self.readers.pop(kk, []))
            best = {}
            for (s, v) in evs:
                best[s] = max(best.get(s, 0), v)
            self.freed_events.append((b.off, b.nbytes, list(best.items())))
            self.free.append((b.off, b.nbytes))
            self.free.sort()
            merged = []
            for (o, sz) in self.free:
                if merged and merged[-1][0] + merged[-1][1] == o:
                    merged[-1] = (merged[-1][0], merged[-1][1] + sz)
                else:
                    merged.append((o, sz))
            self.free = merged
            self.used -= b.nbytes

    def _deps(self, eng, R, W):
        best = {}
        for r in R:
            ev = self.lastw.get(r)
            if ev is not None:
                best[ev[0]] = max(best.get(ev[0], 0), ev[1])
        for w in W:
            ev = self.lastw.get(w)
            if ev is not None:
                best[ev[0]] = max(best.get(ev[0], 0), ev[1])
            for ev in self.readers.get(w, ()):
                best[ev[0]] = max(best.get(ev[0], 0), ev[1])
        for s, v in best.items():
            if s == eng:
                if eng == "pe":
                    continue
                if eng in ("act", "dve"):
                    if self.drained[eng] < v:
                        self.prog[eng].append(("drain",))
                        self.drained[eng] = self.cnt[eng]
                    continue
            if self.seen[eng].get(s, 0) >= v:
                continue
            self.seen[eng][s] = v
            self.prog[eng].append(("wait", s, v))
            if isinstance(s, str):
                self.needed[s].add(v)

    def _commit(self, ev, R, W):
        for w in W:
            self.lastw[w] = ev
            self.readers[w] = []
        for r in R:
            if r not in W:
                self.readers.setdefault(r, []).append(ev)

    def merge_keys(self, src_keys, dst_key):
        evs = []
        for kk in src_keys:
            if kk in self.lastw:
                evs.append(self.lastw[kk])
            evs.extend(self.readers.get(kk, []))
        self.readers.setdefault(dst_key, []).extend(evs)

    def op(self, eng, fn, R=(), W=()):
        R, W = list(R), list(W)
        self._deps(eng, R, W)
        self.cnt[eng] += 1
        ev = (eng, self.cnt[eng])
        self.prog[eng].append(("inst", fn, self.cnt[eng]))
        self._commit(ev, R, W)
        return ev

    def dma(self, eng, out, in_, R=(), W=(), key=None):
        R, W = list(R), list(W)
        self._deps(eng, R, W)
        if key is None:
            key = f"_auto{self.ndma}"
        if key not in self.dma_sem:
            self.dma_sem[key] = [self.dma_pool[len(self.dma_sem)], 0]
        ent = self.dma_sem[key]
        sem = ent[0]
        if ent[1] > 0 and self.seen[eng].get(sem, 0) < ent[1]:
            self.seen[eng][sem] = ent[1]
            self.prog[eng].append(("wait", sem, ent[1]))
        ent[1] += 16
        self.ndma += 1
        ev = (sem, ent[1])
        self.prog[eng].append(("dma", out, in_, sem))
        self._commit(ev, R, W)
        self.dma_events.append(ev)
        return ev

    def emit(self, block):
        nc = self.nc
        S = self

        rank = {en: {q: i + 1 for i, q in enumerate(sorted(S.needed[en]))} for en in ENGS}
        S.n_inc = {en: len(rank[en]) for en in ENGS}

        def run(e, name):
            for ent in S.prog[name]:
                if ent[0] == "wait":
                    s = ent[1]
                    if isinstance(s, str):
                        e.wait_ge(S.sem[s], rank[s][ent[2]])
                    else:
                        e.wait_ge(s, ent[2])
                elif ent[0] == "drain":
                    e.drain()
                elif ent[0] == "inst":
                    ins = ent[1](e)
                    if ent[2] in rank[name]:
                        ins.then_inc(S.sem[name], 1)
                else:
                    e.dma_start(out=ent[1], in_=ent[2]).then_inc(ent[3], 16)

        @block.tensor
        def _(e):
            run(e, "pe")

        @block.scalar
        def _(e):
            run(e, "act")

        @block.vector
        def _(e):
            run(e, "dve")

        @block.gpsimd
        def _(e):
            run(e, "pool")

        @block.sync
        def _(e):
            run(e, "sp")

    def final_wait(self, eng, events):
        for (s, v) in events:
            if self.seen[eng].get(s, 0) >= v:
                continue
            self.seen[eng][s] = v
            self.prog[eng].append(("wait", s, v))


class K:
    pass


def bc(ap, shape):
    return ap.broadcast_to(shape)


def build(stage="full", dbg=False, a1_tiles=None, a1_level=9):
    nc = bass.Bass("TRN2", target_bir_lowering=False)
    es = ExitStack()
    k = K()
    k.nc, k.es, k.stage, k.dbg = nc, es, stage, dbg
    k.a1_tiles, k.a1_level = a1_tiles, a1_level
    d = {}

    def din(name, shape, dt=F32):
        d[name] = nc.dram_tensor(name, list(shape), dt, kind="ExternalInput").ap()

    din("xT", [D, NTOK]); din("posi", [1, NTOK], I32); din("memT", [D, MEM]); din("flags", [128, 2])
    din("w_in", [D, IN_COLS]); din("w_uq", [256, 768]); din("w_ukv", [128, 1024]); din("w_out", [D, D])
    din("w_xq", [D, D]); din("w_xkv", [D, 2 * D]); din("w_xo", [D, D])
    din("w_ffn_in", [D, 2 * DFF]); din("w_ffn_out", [DFF, D])
    din("gv", [128, 43]); din("convp", [128, NFB * 4]); din("c_small", [128, 8])
    din("c_ident", [128, 128]); din("c_rot", [128, 128]); din("c_causal", [128, 128])
    din("c_intra", [128, 512]); din("c_qfs", [128, 512]); din("c_decay", [128, 512])
    d["outT"] = nc.dram_tensor("outT", [D, NOWN], F32, kind="ExternalOutput").ap()
    d["x2s"] = nc.dram_tensor("x2s", [D, NST], F32, kind="Internal").ap()
    k.dbg_out = {}
    k.d = d
    with es:
        S = Sched(nc, es)
        k.S = S
        big = [es.enter_context(nc.psum_tensor(f"psb{i}", [128, 1024], F32)) for i in range(4)]
        k.ps2 = [b_[:, :] for b_ in big]
        k.ps = [big[i // 2][:, (i % 2) * 512:(i % 2 + 1) * 512] for i in range(8)]
        k.psk = [("ps", i) for i in range(8)]
        block = es.enter_context(nc.Block())
        emit_all(k)
        S.final_wait("sp", S.dma_events)
        S.emit(block)
    k.peak = S.peak
    return nc, k


def mm(k, out, lhsT, rhs, start, stop, R, W):
    k.S.op("pe", lambda e: e.matmul(out, lhsT=lhsT, rhs=rhs, start=start, stop=stop), R, W)


def tr(k, out, in_, ident, R, W):
    k.S.op("pe", lambda e: e.transpose(out=out, in_=in_, identity=ident), R, W)


def act(k, out, in_, func, R, W, scale=1.0, bias=0.0):
    k.S.op("act", lambda e: e.activation(out=out, in_=in_, func=func, scale=scale, bias=bias), R, W)


def tt(k, eng, out, in0, in1, op, R, W):
    k.S.op(eng, lambda e: e.tensor_tensor(out=out, in0=in0, in1=in1, op=op), R, W)


def ts(k, eng, out, in0, s1, s2, op0, op1, R, W):
    if op1 is None:
        k.S.op(eng, lambda e: e.tensor_scalar(out=out, in0=in0, scalar1=s1, scalar2=None, op0=op0), R, W)
    else:
        k.S.op(eng, lambda e: e.tensor_scalar(out=out, in0=in0, scalar1=s1, scalar2=s2, op0=op0, op1=op1), R, W)


def stt(k, out, in0, scalar, in1, op0, op1, R, W):
    k.S.op("dve", lambda e: e.scalar_tensor_tensor(out=out, in0=in0, scalar=scalar, in1=in1, op0=op0, op1=op1), R, W)


def cp(k, eng, out, in_, R, W):
    if eng == "act":
        k.S.op("act", lambda e: e.copy(out=out, in_=in_), R, W)
    else:
        k.S.op(eng, lambda e: e.tensor_copy(out=out, in_=in_), R, W)


def recip(k, out, in_, R, W):
    k.S.op("dve", lambda e: e.reciprocal(out=out, in_=in_), R, W)


def dump(k, name, buf_ap, shape, R, dt=F32):
    t = k.nc.dram_tensor("dbg_" + name, list(shape), dt, kind="ExternalOutput").ap()
    k.dbg_out[name] = t
    k.S.dma("sp", t, buf_ap, R=R, W=[("dbg", name)], key="dbg")


def load_consts(k):
    S, d = k.S, k.d
    c = K()
    k.c = c
    c.gv = S.alloc("gv", [128, 43], F32)
    c.convp = S.alloc("convp", [128, NFB, 4], F32)
    c.small = S.alloc("c_small", [128, 8], F32)
    c.flags = S.alloc("flags", [128, 2], F32)
    c.ident = S.alloc("ident", [128, 128], BF16)
    c.rot = S.alloc("rot", [128, 128], BF16)
    c.causal = S.alloc("causal", [128, 128], BF16)
    c.intra = S.alloc("intra", [128, 512], F32)
    c.qfs = S.alloc("qfs", [128, 512], F32)
    c.decay = S.alloc("decay", [128, 512], F32)
    c.ones = S.alloc("ones", [128, 128], BF16)
    S.dma("sp", c.gv.ap, d["gv"], W=c.gv.all())
    S.dma("sp", c.convp.ap, d["convp"].rearrange("p (a b) -> p a b", a=NFB), W=c.convp.all())
    S.dma("sp", c.small.ap, d["c_small"], W=c.small.all())
    S.dma("sp", c.flags.ap, d["flags"], W=c.flags.all())
    S.dma("sp", c.intra.ap, d["c_intra"], W=c.intra.all())
    S.dma("sp", c.qfs.ap, d["c_qfs"], W=c.qfs.all())
    S.dma("sp", c.decay.ap, d["c_decay"], W=c.decay.all())
    S.dma("pool", c.ident.ap, d["c_ident"], W=c.ident.all())
    S.dma("pool", c.rot.ap, d["c_rot"], W=c.rot.all())
    S.dma("pool", c.causal.ap, d["c_causal"], W=c.causal.all())
    S.op("pool", lambda e: e.memset(c.ones.ap, 1.0), W=c.ones.all())
    c.g_mix, c.g_xattn, c.g_mem, c.g_ffn, c.g_final, c.g_q, c.g_kv = 0, 8, 16, 24, 32, 40, 42


def rope_tables(k, posi_ap, posi_key, prange, col, cosb, sinb, tmp, n):
    c = k.c
    p0, p1 = prange
    a, b_, kk = tmp
    A = lambda buf: buf.ap[p0:p1, 0:n]
    invf = c.small.ap[p0:p1, col:col + 1]
    ts(k, "dve", A(a), posi_ap[p0:p1, 0:n], invf, None, ALU.mult, None, [posi_key] + c.small.all(), a.all())
    ts(k, "dve", A(b_), A(a), 1.0 / TWO_PI, MAGIC, ALU.mult, ALU.add, a.all(), b_.all())
    ts(k, "dve", A(kk), A(b_), -MAGIC, None, ALU.add, None, b_.all(), kk.all())
    stt(k, A(b_), A(kk), -C1, A(a), ALU.mult, ALU.add, kk.all() + a.all(), b_.all())
    stt(k, A(a), A(kk), -C2, A(b_), ALU.mult, ALU.add, kk.all() + b_.all(), a.all())
    ts(k, "dve", A(a), A(a), -PI_LO, PI_LO, ALU.max, ALU.min, a.all(), a.all())
    act(k, sinb.ap[p0:p1, 0:n], A(a), AF.Sin, a.all(), sinb.all())
    ts(k, "dve", A(b_), A(a), math.pi / 2, -TWO_PI, ALU.is_gt, ALU.mult, a.all(), b_.all())
    stt(k, A(kk), A(a), math.pi / 2, A(b_), ALU.add, ALU.add, a.all() + b_.all(), kk.all())
    ts(k, "dve", A(kk), A(kk), -PI_LO, PI_LO, ALU.max, ALU.min, kk.all(), kk.all())
    act(k, cosb.ap[p0:p1, 0:n], A(kk), AF.Sin, kk.all(), cosb.all())


def squares8(k, dst, src, n):
    tt(k, "pool", dst.ap[:, 0:4, 0:n], src.ap[:, 0:4, 0:n], src.ap[:, 0:4, 0:n], ALU.mult, src.all(), dst.all())
    for ci in range(4, 8):
        act(k, dst.ap[:, ci, 0:n], src.ap[:, ci, 0:n], AF.Square, src.all(), dst.all())


def rms_stats(k, sq_chunks, nch, nfeat, ps_i, sd, rstd, n, sqkeys):
    c = k.c
    ps = k.ps[ps_i]
    for i in range(nch):
        mm(k, ps[:, 0:n], c.ones.ap, sq_chunks(i), i == 0, i == nch - 1, sqkeys + c.ones.all(), [k.psk[ps_i]])
    act(k, sd.ap[:, 0:n], ps[:, 0:n], AF.Ln, [k.psk[ps_i]], sd.all(), scale=1.0 / nfeat, bias=EPS)
    act(k, rstd.ap[:, 0:n], sd.ap[:, 0:n], AF.Exp, sd.all(), rstd.all(), scale=-0.5)


def emit_all(k):
    load_consts(k)
    P = K()
    k.P = P
    S = k.S
    P.cqn = S.alloc("cqn", [128, 2, NST], BF16)
    P.ckvn = S.alloc("ckvn", [128, NTOK], BF16)
    P.krope = S.alloc("krope", [128, NTOK], BF16)
    P.oretT = S.alloc("oretT", [128, 4, NST], BF16)
    phase_A1(k)
    if k.stage == "A1":
        return
    P.memKT = S.alloc("memKT", [128, 8, MEM], BF16)
    P.memV = S.alloc("memV", [128, 2, D], BF16)
    phase_MKV(k)
    P.omlaT = S.alloc("omlaT", [128, 4, NST], BF16)
    P.w_out = S.alloc("w_out_bf", [128, 8, D], BF16)
    P.w_xq = S.alloc("w_xq_bf", [128, 8, D], BF16)
    d = k.d
    phase_A2(k)
    if k.stage == "A2":
        return
    S.release(P.cqn, P.ckvn, P.krope)
    P.w_xo = S.alloc("w_xo_bf", [128, 8, D], BF16)
    S.dma("pool", P.w_xo.ap, d["w_xo"].rearrange("(c p) n -> p c n", p=128), W=P.w_xo.all(), key="w_xo")
    P.w_ffn_out = S.alloc("w_ffn_out_bf", [128, NFB, D], BF16)
    S.dma("pool", P.w_ffn_out.ap, d["w_ffn_out"].rearrange("(c p) n -> p c n", p=128), W=P.w_ffn_out.all(), key="w_ffn_out")
    phase_A3X(k)
    if k.stage == "A3X":
        return
    S.release(P.oretT, P.omlaT, P.w_out, P.w_xq, P.w_xo, P.memKT, P.memV)
    P.w_ffn_in = S.alloc("w_ffn_in_bf", [128, 8, 2 * DFF], BF16, nsub=4)
    wv = d["w_ffn_in"].rearrange("(c p) n -> p c n", p=128)
    HB = 11 * 128
    for sub, (a, b) in enumerate(((0, HB), (DFF, DFF + HB), (HB, DFF), (DFF + HB, 2 * DFF))):
        S.dma("pool", P.w_ffn_in.ap[:, :, a:b], wv[:, :, a:b], W=[P.w_ffn_in.k(sub)], key=f"w_ffn_in{sub}")
    phase_F(k)


def phase_A1(k):
    S, d, c, P = k.S, k.d, k.c, k.P
    ps, psk = k.ps, k.psk
    T = 512
    w_in = S.alloc("w_in_bf", [128, 8, IN_COLS], BF16)
    S.dma("pool", w_in.ap, d["w_in"].rearrange("(c p) n -> p c n", p=128), W=w_in.all(), key="w_in")
    w_krot = S.alloc("w_krot", [128, 8, 96], BF16)
    S.op("pool", lambda e: e.memset(w_krot.ap, 0.0), W=w_krot.all())
    ts(k, "pool", w_krot.ap[:, :, 64:80], w_in.ap[:, :, OFF_KR + 16:OFF_KR + 32], -1.0, None, ALU.mult, None,
       w_in.all(), w_krot.all())
    cp(k, "pool", w_krot.ap[:, :, 80:96], w_in.ap[:, :, OFF_KR:OFF_KR + 16], w_in.all(), w_krot.all())

    xt = [S.alloc(f"xt{i}", [128, 8, T], F32) for i in range(2)]
    posi = [S.alloc(f"posi{i}", [128, T], I32) for i in range(2)]
    hTs = [S.alloc(f"hT{i}", [128, 8, T], BF16) for i in range(2)]
    rstd = S.alloc("rstd", [128, T], F32)
    sd = rstd
    ta, tb, tc = (S.alloc(n, [128, T], F32) for n in ("ta", "tb", "tc"))
    cos_r, sin_r = S.alloc("cos_r", [128, T], F32), S.alloc("sin_r", [128, T], F32)
    cos_m, sin_m = S.alloc("cos_m", [128, T], F32), S.alloc("sin_m", [128, T], F32)
    sql = S.alloc("sql", [128, 2, T], BF16)
    rstl = S.alloc("rstl", [128, T], F32)
    sdl = rstl
    raw = [S.alloc(f"raw{i}", [128, T], BF16) for i in range(2)]
    t1 = [S.alloc(f"t1_{i}", [128, T], F32) for i in range(2)]
    t2 = [S.alloc(f"t2_{i}", [128, T], F32) for i in range(2)]
    rqT = S.alloc("rqT", [128, 4, T], BF16, nsub=4)
    rkT = S.alloc("rkT", [128, 4, T], BF16, nsub=4)
    v_tm = S.alloc("v_tm", [128, 4, 512], BF16, nsub=4)
    sg_tm = S.alloc("sg_tm", [128, 4, 512], BF16, nsub=4)
    kdec = S.alloc("kdec", [128, 512], BF16)
    PT = S.alloc("PT", [128, 512], BF16)
    qsT = S.alloc("qsT", [128, 4, 128], BF16)
    S_f = S.alloc("S_f", [128, 512], F32)
    S_b = S.alloc("S_b", [128, 512], BF16)
    sqo = S.alloc("sqo", [128, 512], F32)
    tn = S.alloc("tn", [128, 512], F32)
    ogs = [S.alloc(f"og{i}", [128, 512], BF16) for i in range(2)]
    oT = S.alloc("oT", [128, 512], BF16)
    sqT = S.alloc("sqT", [128, 512], BF16)
    S.op("pool", lambda e: e.memset(S_f.ap, 0.0), W=S_f.all())
    S.op("pool", lambda e: e.memset(S_b.ap, 0.0), W=S_b.all())

    xTv = d["xT"].rearrange("(c p) t -> p c t", p=128)
    ntiles = NTOK // T
    rr = 0
    tile_list = list(range(ntiles)) if k.a1_tiles is None else k.a1_tiles

    def ln_part1(tti):
        tok0_ = tti * T
        xb_, pb_, hT_ = xt[tti % 2], posi[tti % 2], hTs[tti % 2]
        S.dma("sp", xb_.ap, xTv[:, :, tok0_:tok0_ + T], W=xb_.all(), key=f"xt{tti % 2}")
        S.dma("sp", pb_.ap, d["posi"][:, tok0_:tok0_ + T].partition_broadcast(128), W=pb_.all(), key=f"posi{tti % 2}")
        squares8(k, hT_, xb_, T)
        rms_stats(k, lambda i: hT_.ap[:, i, :], 8, D, 0, sd, rstd, T, hT_.all())

    def ln_stt(tti, ci):
        xb_, hT_ = xt[tti % 2], hTs[tti % 2]
        stt(k, hT_.ap[:, ci, :], xb_.ap[:, ci, :], c.gv.ap[:, c.g_mix + ci:c.g_mix + ci + 1], rstd.ap,
            ALU.mult, ALU.mult, xb_.all() + rstd.all() + c.gv.all(), hT_.all())

    def ropes(tti, which):
        pb_ = posi[tti % 2]
        if which == 0:
            rope_tables(k, pb_.ap, pb_.k(), (0, 128), 0, cos_r, sin_r, (ta, tb, tc), T)
        else:
            rope_tables(k, pb_.ap, pb_.k(), (64, 96), 1, cos_m, sin_m, (ta, tb, tc), T)

    ln_part1(tile_list[0])
    for ci in range(8):
        ln_stt(tile_list[0], ci)
    ropes(tile_list[0], 0)
    ropes(tile_list[0], 1)
    for tidx, tti in enumerate(tile_list):
        tok0 = tti * T
        xb, pb, hT = xt[tti % 2], posi[tti % 2], hTs[tti % 2]
        own_tile = tok0 + T > HALO0
        nxt = tile_list[tidx + 1] if tidx + 1 < len(tile_list) else None

        def proj(ps_i, m, wbuf, col0, n=T):
            for ci in range(8):
                mm(k, ps[ps_i][0:m, 0:n], wbuf.ap[:, ci, col0:col0 + m], hT.ap[:, ci, 0:n], ci == 0, ci == 7,
                   wbuf.all() + hT.all(), [psk[ps_i]])

        if k.a1_level < 3:
            continue
        proj(1, 128, w_in, OFF_CKV)
        act(k, sql.ap[:, 0, :], ps[1], AF.Square, [psk[1]], sql.all())
        rms_stats(k, lambda i: sql.ap[:, 0, :], 1, 128, 0, sdl, rstl, T, sql.all())
        stt(k, P.ckvn.ap[:, tok0:tok0 + T], ps[1], c.gv.ap[:, c.g_kv:c.g_kv + 1], rstl.ap, ALU.mult, ALU.mult,
            [psk[1]] + rstl.all() + c.gv.all(), P.ckvn.all())
        proj(2, 96, w_in, OFF_KR - 64)
        proj(3, 96, w_krot, 0)
        tt(k, "dve", t1[0].ap[64:96, :], ps[2][64:96, :], cos_m.ap[64:96, :], ALU.mult, [psk[2]] + cos_m.all(), t1[0].all())
        tt(k, "dve", t2[0].ap[64:96, :], ps[3][64:96, :], sin_m.ap[64:96, :], ALU.mult, [psk[3]] + sin_m.all(), t2[0].all())
        tt(k, "pool", P.krope.ap[64:96, tok0:tok0 + T], t1[0].ap[64:96, :], t2[0].ap[64:96, :], ALU.add,
           t1[0].all() + t2[0].all(), P.krope.all())
        if k.a1_level < 4:
            continue
        if own_tile:
            proj(1, 128, w_in, OFF_CQ)
            proj(2, 128, w_in, OFF_CQ + 128)
            act(k, sql.ap[:, 0, :], ps[1], AF.Square, [psk[1]], sql.all())
            act(k, sql.ap[:, 1, :], ps[2], AF.Square, [psk[2]], sql.all())
            rms_stats(k, lambda i: sql.ap[:, i, :], 2, 256, 0, sdl, rstl, T, sql.all())
            if tok0 < HALO0:
                lo, n_, s0 = HALO0 - tok0, 128, 0
            else:
                lo, n_, s0 = 0, T, tok0 - HALO0
            for j, pi in ((0, 1), (1, 2)):
                stt(k, P.cqn.ap[:, j, s0:s0 + n_], ps[pi][:, lo:lo + n_], c.gv.ap[:, c.g_q + j:c.g_q + j + 1],
                    rstl.ap[:, lo:lo + n_], ALU.mult, ALU.mult, [psk[pi]] + rstl.all() + c.gv.all(), P.cqn.all())
        if k.a1_level < 5:
            continue
        todo = [(rkT, OFF_RK)] + ([(rqT, OFF_RQ)] if own_tile else [])
        items = [(dst, off, h) for (dst, off) in todo for h in range(4)]

        def rope_tail(it, slot):
            dst, off, h = it
            pi, pj = 1 + slot, 3 + slot
            rb, a1, a2 = raw[slot], t1[slot], t2[slot]
            mm(k, ps[pj], c.rot.ap, rb.ap, True, True, rb.all() + c.rot.all(), [psk[pj]])
            tt(k, "dve", a2.ap, ps[pj], sin_r.ap, ALU.mult, [psk[pj]] + sin_r.all(), a2.all())
            tt(k, "pool", a1.ap, rb.ap, cos_r.ap, ALU.mult, rb.all() + cos_r.all(), a1.all())
            tt(k, "pool", dst.ap[:, h, :], a1.ap, a2.ap, ALU.add, a1.all() + a2.all(), [dst.k(h)])

        prev = None
        for n_, it in enumerate(items):
            slot = n_ % 2
            proj(1 + slot, 128, w_in, it[1] + it[2] * 128)
            cp(k, "act", raw[slot].ap, ps[1 + slot], [psk[1 + slot]], raw[slot].all())
            if prev is not None:
                rope_tail(*prev)
            prev = (it, slot)
        rope_tail(*prev)
        if k.a1_level < 6:
            continue
        if own_tile:
            for h in range(4):
                pi = 1 + h % 2
                proj(pi, 128, w_in, OFF_RG + h * 128)
                act(k, sg_tm.ap[:, h, :], ps[pi], AF.Silu, [psk[pi]], [sg_tm.k(h)])
        if nxt is not None:
            ln_part1(nxt)
        p7b = ps[7].bitcast(BF16)
        k7a, k7b = ("ps", "7a"), ("ps", "7b")

        def stage_a(cc):
            ctok = tok0 + cc * 128
            own_chunk = ctok >= HALO0
            cs = slice(cc * 128, (cc + 1) * 128)
            for ci in range(8):
                mm(k, ps[1], hT.ap[:, ci, cs], w_in.ap[:, ci, OFF_RV:OFF_RV + 512], ci == 0, ci == 7,
                   hT.all() + w_in.all(), [psk[1]])
            cp(k, "act", v_tm.ap[:, cc, :], ps[1], [psk[1]], [v_tm.k(cc)])
            for h in range(4):
                tr(k, p7b[:, h * 128:(h + 1) * 128], rkT.ap[:, h, cs], c.ident.ap, [rkT.k(h)] + c.ident.all(), [k7a])
            if own_chunk:
                for h in range(4):
                    mm(k, ps[2][:, h * 128:(h + 1) * 128], rkT.ap[:, h, cs], rqT.ap[:, h, cs], True, True,
                       [rkT.k(h), rqT.k(h)], [psk[2]])
            tt(k, "dve", kdec.ap.rearrange("p (h e) -> p h e", h=4), p7b[:, 0:512].rearrange("p (h e) -> p h e", h=4),
               bc(c.small.ap[:, 2:6].unsqueeze(2), [128, 4, 128]), ALU.mult, [k7a] + c.small.all(), kdec.all())
            if own_chunk:
                tt(k, "dve", PT.ap, ps[2], c.intra.ap, ALU.mult, [psk[2]] + c.intra.all(), PT.all())
                tt(k, "pool", qsT.ap, rqT.ap[:, :, cs], c.qfs.ap.rearrange("p (h e) -> p h e", h=4), ALU.mult,
                   rqT.all() + c.qfs.all(), qsT.all())
            if ctok + 128 < NTOK:
                for h in range(4):
                    hs = slice(h * 128, (h + 1) * 128)
                    mm(k, ps[5][:, hs], kdec.ap[:, hs], v_tm.ap[:, cc, hs], True, True, kdec.all() + [v_tm.k(cc)], [psk[5]])
            if own_chunk:
                for h in range(4):
                    hs = slice(h * 128, (h + 1) * 128)
                    mm(k, ps[6][:, hs], PT.ap[:, hs], v_tm.ap[:, cc, hs], True, False, PT.all() + [v_tm.k(cc)], [psk[6]])
                    mm(k, ps[6][:, hs], qsT.ap[:, h, :], S_b.ap[:, hs], False, True, qsT.all() + S_b.all(), [psk[6]])
                cp(k, "act", ogs[cc % 2].ap, ps[6], [psk[6]], ogs[cc % 2].all())
            if ctok + 128 < NTOK:
                tt(k, "pool", S_f.ap, S_f.ap, c.decay.ap, ALU.mult, S_f.all() + c.decay.all(), S_f.all())
                tt(k, "dve", S_f.ap, S_f.ap, ps[5], ALU.add, S_f.all() + [psk[5]], S_f.all())
                cp(k, "act", S_b.ap, S_f.ap, S_f.all(), S_b.all())

        def stage_b(cc):
            ctok = tok0 + cc * 128
            if ctok < HALO0:
                return
            cs = slice(cc * 128, (cc + 1) * 128)
            og = ogs[cc % 2]
            for h in range(4):
                tr(k, p7b[:, 512 + h * 128:512 + (h + 1) * 128], og.ap[:, h * 128:(h + 1) * 128], c.ident.ap,
                   og.all() + c.ident.all(), [k7b])
            cp(k, "dve", oT.ap, p7b[:, 512:1024], [k7b], oT.all())
            act(k, sqT.ap, oT.ap, AF.Square, oT.all(), sqT.all())
            mm(k, ps[4], c.ones.ap, oT.ap, True, True, c.ones.all() + oT.all(), [psk[4]])
            mm(k, ps[3], c.ones.ap, sqT.ap, True, True, c.ones.all() + sqT.all(), [psk[3]])
            act(k, tn.ap, ps[4], AF.Copy, [psk[4]], tn.all(), scale=1.0 / 128)
            tt(k, "dve", sqo.ap, tn.ap, tn.ap, ALU.mult, tn.all(), sqo.all())
            stt(k, sqo.ap, ps[3], 1.0 / 128, sqo.ap, ALU.mult, ALU.subtract, [psk[3]] + sqo.all(), sqo.all())
            act(k, sqo.ap, sqo.ap, AF.Ln, sqo.all(), sqo.all(), scale=1.0, bias=EPS)
            act(k, sqo.ap, sqo.ap, AF.Exp, sqo.all(), sqo.all(), scale=-0.5)
            tt(k, "dve", tn.ap, oT.ap, tn.ap, ALU.subtract, oT.all() + tn.all(), tn.all())
            tt(k, "pool", tn.ap, tn.ap, sqo.ap, ALU.mult, tn.all() + sqo.all(), tn.all())
            s0 = ctok - HALO0
            tt(k, "pool", P.oretT.ap[:, :, s0:s0 + 128], tn.ap.rearrange("p (h e) -> p h e", h=4), sg_tm.ap[:, :, cs],
               ALU.mult, tn.all() + sg_tm.all(), P.oretT.all())

        for cc in range(5):
            if cc < 4:
                stage_a(cc)
            if cc >= 1:
                stage_b(cc - 1)
            if nxt is not None and cc < 4:
                ln_stt(nxt, 2 * cc)
                ln_stt(nxt, 2 * cc + 1)
                if cc == 1:
                    ropes(nxt, 0)
                if cc == 2:
                    ropes(nxt, 1)
    S.merge_keys([("ps", "7a"), ("ps", "7b")], psk[7])
    if k.stage == "A1":
        mm(k, ps[0][:, 0:128], c.ones.ap, c.ones.ap, True, True, c.ones.all(), [psk[0]])
    if k.dbg and k.stage == "A1":
        dump(k, "ckvn", P.ckvn.ap, [128, NTOK], P.ckvn.all(), BF16)
        dump(k, "krope", P.krope.ap[64:96, :], [32, NTOK], P.krope.all(), BF16)
        dump(k, "cqn", P.cqn.ap, [128, 2, NST], P.cqn.all(), BF16)
        dump(k, "oretT", P.oretT.ap, [128, 4, NST], P.oretT.all(), BF16)
    S.release(w_in, w_krot, *xt, *posi, *hTs, rstd, ta, tb, tc, cos_r, sin_r, cos_m, sin_m, sql, rstl, *raw, *t1,
              *t2, rqT, rkT, v_tm, sg_tm, kdec, PT, qsT, S_f, S_b, sqo, tn, *ogs, oT, sqT)


def phase_MKV(k):
    S, d, c, P, ps, psk = k.S, k.d, k.c, k.P, k.ps, k.psk
    w_xkv = S.alloc("w_xkv_bf", [128, 8, 2 * D], BF16)
    S.dma("pool", w_xkv.ap, d["w_xkv"].rearrange("(c p) n -> p c n", p=128), W=w_xkv.all(), key="w_xkv")
    mt = S.alloc("memT", [128, 8, MEM], F32)
    S.dma("sp", mt.ap, d["memT"].rearrange("(c p) t -> p c t", p=128), W=mt.all(), key="memT")
    hm = S.alloc("hmem", [128, 8, MEM], BF16)
    sd, rstd = S.alloc("sdm", [128, MEM], F32), S.alloc("rstdm", [128, MEM], F32)
    squares8(k, hm, mt, MEM)
    rms_stats(k, lambda i: hm.ap[:, i, :], 8, D, 0, sd, rstd, MEM, hm.all())
    for ci in range(8):
        stt(k, hm.ap[:, ci, :], mt.ap[:, ci, :], c.gv.ap[:, c.g_mem + ci:c.g_mem + ci + 1], rstd.ap, ALU.mult, ALU.mult,
            mt.all() + rstd.all() + c.gv.all(), hm.all())
    for blk in range(8):
        pi = 1 + blk % 2
        for ci in range(8):
            mm(k, ps[pi][:, 0:MEM], w_xkv.ap[:, ci, blk * 128:(blk + 1) * 128], hm.ap[:, ci, :], ci == 0, ci == 7,
               w_xkv.all() + hm.all(), [psk[pi]])
        cp(k, "act", P.memKT.ap[:, blk, :], ps[pi][:, 0:MEM], [psk[pi]], P.memKT.all())
    n = 0
    for kb2 in range(2):
        for half in range(2):
            pi = 3 + n % 2
            n += 1
            for ci in range(8):
                mm(k, ps[pi], hm.ap[:, ci, kb2 * 128:(kb2 + 1) * 128], w_xkv.ap[:, ci, D + half * 512:D + (half + 1) * 512],
                   ci == 0, ci == 7, w_xkv.all() + hm.all(), [psk[pi]])
            cp(k, "dve", P.memV.ap[:, kb2, half * 512:(half + 1) * 512], ps[pi], [psk[pi]], P.memV.all())
    S.release(w_xkv, mt, hm, sd, rstd)


def phase_A2(k):
    S, d, c, P, ps, psk = k.S, k.d, k.c, k.P, k.ps, k.psk
    ps2 = k.ps2
    w_uq = S.alloc("w_uq_bf", [128, 2, 768], BF16)
    w_uqr = S.alloc("w_uqr_bf", [128, 2, 768], BF16)
    w_ukv = S.alloc("w_ukv_bf", [128, 1024], BF16)
    S.dma("pool", w_uq.ap, d["w_uq"].rearrange("(c p) n -> p c n", p=128), W=w_uq.all(), key="w_uq")
    S.dma("pool", w_ukv.ap, d["w_ukv"], W=w_ukv.all(), key="w_ukv")
    S.dma("pool", P.w_out.ap, d["w_out"].rearrange("(c p) n -> p c n", p=128), W=P.w_out.all(), key="w_out")
    S.dma("pool", P.w_xq.ap, d["w_xq"].rearrange("(c p) n -> p c n", p=128), W=P.w_xq.all(), key="w_xq")
    S.op("pool", lambda e: e.memset(w_uqr.ap, 0.0), W=w_uqr.all())
    q4 = w_uq.ap.rearrange("p c (h x) -> p c h x", h=8)
    r4 = w_uqr.ap.rearrange("p c (h x) -> p c h x", h=8)
    for ci in range(2):
        ts(k, "pool", r4[:, ci, :, 64:80], q4[:, ci, :, 80:96], -1.0, None, ALU.mult, None, w_uq.all(), w_uqr.all())
        cp(k, "pool", r4[:, ci, :, 80:96], q4[:, ci, :, 64:80], w_uq.all(), w_uqr.all())
    KT = S.alloc("KT", [128, 4, NTOK], BF16, nsub=4)
    Vc = S.alloc("Vc", [128, 32, 384], BF16)
    S.op("pool", lambda e: e.memset(Vc.ap, 1.0), W=Vc.all())
    for o in (64, 256):
        ts(k, "dve", Vc.ap[:, 0:16, o:o + 64], Vc.ap[:, 0:16, o:o + 64], c.flags.ap[:, 0:1], None, ALU.mult, None,
           Vc.all() + c.flags.all(), Vc.all())
    qTb = [[S.alloc(f"qT{b_}_{i}", [128, 512], BF16) for i in range(4)] for b_ in range(2)]
    for q_ in qTb[0] + qTb[1]:
        S.op("pool", lambda e, q_=q_: e.memset(q_.ap[96:128, :], 0.0), W=q_.all())
    S.op("pool", lambda e: e.memset(KT.ap[96:128, :, :], 0.0), W=KT.all())
    PTb = [S.alloc(f"PTb{i}", [128, 1024], BF16) for i in range(2)]
    tq1, tq2 = S.alloc("tq1", [128, 512], F32), S.alloc("tq2", [128, 512], F32)
    ta, tb, tc = (S.alloc(n, [128, 512], F32) for n in ("ta2", "tb2", "tc2"))
    cos_m, sin_m = S.alloc("cos_m2", [128, 512], F32), S.alloc("sin_m2", [128, 512], F32)
    pq = S.alloc("posq", [128, 512], I32)
    rec = S.alloc("rec", [128, 512], F32)
    wk3 = w_ukv.ap.rearrange("p (h x) -> p h x", h=8)
    scale = 1.0 / math.sqrt(96.0)
    n_ev = 0
    for hh in range(2):
        heads = list(range(4 * hh, 4 * hh + 4))
        for kt in range(NTOK // 512):
            for hi, h in enumerate(heads):
                pi = n_ev % 2
                mm(k, ps[pi], w_ukv.ap[:, h * 128:h * 128 + 128], P.ckvn.ap[:, kt * 512:(kt + 1) * 512], True, True,
                   w_ukv.all() + P.ckvn.all(), [psk[pi]])
                cp(k, "act", KT.ap[0:64, hi, kt * 512:(kt + 1) * 512], ps[pi][0:64, :], [psk[pi]],
                   [KT.k(hi)])
                n_ev += 1
        for hi in range(4):
            S.dma("sp", KT.ap[64:96, hi, :], P.krope.ap[64:96, :], R=P.krope.all(), W=[KT.k(hi)], key=f"kr{hi}")
        for kb in range(NTOK // 128):
            pi = 2 + kb % 2
            mm(k, ps[pi], P.ckvn.ap[:, kb * 128:(kb + 1) * 128], w_ukv.ap[:, 512 * hh:512 * (hh + 1)], True, True,
               w_ukv.all() + P.ckvn.all(), [psk[pi]])
            src = ps[pi].rearrange("p (a m x) -> p a m x", a=2, m=2)
            dst = Vc.ap[:, kb, :].rearrange("p (a r) -> p a r", a=2)
            cp(k, "dve", dst[:, :, 0:64], src[:, :, 0, 64:128], [psk[pi]], Vc.all())
            cp(k, "dve", dst[:, :, 128:192], src[:, :, 1, 64:128], [psk[pi]], Vc.all())
        def qtile(qi):
            if qi == 0:
                return 0, 128, HALO0 // 128
            return 128 + 512 * (qi - 1), 512, NPRE // 128 + 4 * (qi - 1)

        def q_assemble(qi):
            s0, NQ, qblk0 = qtile(qi)
            qTs = qTb[qi % 2]
            g0 = HALO0 + s0
            S.dma("sp", pq.ap[:, 0:NQ], d["posi"][:, g0:g0 + NQ].partition_broadcast(128), W=pq.all(), key="posq")
            rope_tables(k, pq.ap, pq.k(), (64, 96), 1, cos_m, sin_m, (ta, tb, tc), NQ)
            for hi, h in enumerate(heads):
                for ci in range(2):
                    mm(k, ps[0][0:96, 0:NQ], w_uq.ap[:, ci, h * 96:(h + 1) * 96], P.cqn.ap[:, ci, s0:s0 + NQ], ci == 0, ci == 1,
                       w_uq.all() + P.cqn.all(), [psk[0]])
                for ci in range(2):
                    mm(k, ps[1][0:96, 0:NQ], w_uqr.ap[:, ci, h * 96:(h + 1) * 96], P.cqn.ap[:, ci, s0:s0 + NQ], ci == 0, ci == 1,
                       w_uqr.all() + P.cqn.all(), [psk[1]])
                cp(k, "dve", qTs[hi].ap[0:64, 0:NQ], ps[0][0:64, 0:NQ], [psk[0]], qTs[hi].all())
                tt(k, "dve", tq1.ap[64:96, 0:NQ], ps[0][64:96, 0:NQ], cos_m.ap[64:96, 0:NQ], ALU.mult, [psk[0]] + cos_m.all(),
                   tq1.all())
                tt(k, "dve", tq2.ap[64:96, 0:NQ], ps[1][64:96, 0:NQ], sin_m.ap[64:96, 0:NQ], ALU.mult, [psk[1]] + sin_m.all(),
                   tq2.all())
                tt(k, "pool", qTs[hi].ap[64:96, 0:NQ], tq1.ap[64:96, 0:NQ], tq2.ap[64:96, 0:NQ], ALU.add, tq1.all() + tq2.all(),
                   qTs[hi].all())

        q_assemble(0)
        for qi in range(5):
            s0, NQ, qblk0 = qtile(qi)
            nqb = NQ // 128
            qT = qTb[qi % 2]
            if qi + 1 < 5:
                q_assemble(qi + 1)
            for hi, h in enumerate(heads):
                pair, mem = divmod(hi, 2)
                vcol0 = pair * 192 + mem * 64
                pob = 6 + hi % 2
                po = ps[pob]
                nkb = qblk0 + nqb
                groups = []
                kb = 0
                while kb < nkb:
                    if kb + 1 < qblk0:
                        groups.append((kb, kb + 1))
                        kb += 2
                    else:
                        groups.append((kb,))
                        kb += 1
                pend = None

                def pv(pd, nkb=nkb, po=po, pob=pob, vcol0=vcol0, NQ=NQ):
                    for (kb_, qlo_, n_, ptap_, ptk_) in pd:
                        mm(k, po[:, qlo_:NQ], Vc.ap[:, kb_, vcol0:vcol0 + 128], ptap_, kb_ == 0, kb_ == nkb - 1,
                           Vc.all() + ptk_, [psk[pob]])

                for gi, grp in enumerate(groups):
                    slot = gi % 2
                    pt = PTb[slot]
                    banks = (2 + 2 * slot, 3 + 2 * slot)
                    cur = []
                    for j_, kb in enumerate(grp):
                        r = kb - qblk0
                        q_lo = max(r, 0) * 128
                        n = NQ - q_lo
                        sb = banks[j_]
                        mm(k, ps[sb][:, 0:n], KT.ap[:, hi, kb * 128:(kb + 1) * 128], qT[hi].ap[:, q_lo:NQ], True, True,
                           [KT.k(hi)] + qT[hi].all(), [psk[sb]])
                        cur.append((kb, q_lo, n, pt.ap[:, j_ * 512:j_ * 512 + n], pt.all()))
                    if len(grp) == 2 and NQ == 512:
                        act(k, pt.ap, ps2[1 + slot], AF.Exp, [psk[banks[0]], psk[banks[1]]], pt.all(), scale=scale)
                    else:
                        for j_, (kb, q_lo, n, ptap, _) in enumerate(cur):
                            act(k, ptap, ps[banks[j_]][:, 0:n], AF.Exp, [psk[banks[j_]]], pt.all(), scale=scale)
                            if kb - qblk0 >= 0:
                                tt(k, "pool", ptap[:, 0:128], ptap[:, 0:128], c.causal.ap, ALU.mult, pt.all() + c.causal.all(),
                                   pt.all())
                    if pend is not None:
                        pv(pend)
                    pend = cur
                pv(pend)
                if mem == 0:
                    o_rows, s_rows = slice(0, 64), slice(64, 128)
                else:
                    o_rows, s_rows = slice(64, 128), slice(0, 64)
                ts(k, "dve", rec.ap[o_rows, 0:NQ], po[s_rows, 0:NQ], 1e-30, None, ALU.add, None, [psk[pob]], rec.all())
                recip(k, rec.ap[o_rows, 0:NQ], rec.ap[o_rows, 0:NQ], rec.all(), rec.all())
                tt(k, "dve", P.omlaT.ap[o_rows, 2 * hh + pair, s0:s0 + NQ], po[o_rows, 0:NQ], rec.ap[o_rows, 0:NQ], ALU.mult,
                   [psk[pob]] + rec.all(), P.omlaT.all())
    if k.dbg and k.stage == "A2":
        dump(k, "omlaT", P.omlaT.ap, [128, 4, NST], P.omlaT.all(), BF16)
    S.release(w_uq, w_uqr, w_ukv, KT, Vc, *qTb[0], *qTb[1], *PTb, tq1, tq2, ta, tb, tc, cos_m, sin_m, pq, rec)


def _norm_to_hT(k, xb, hT, sd, rstd, gcol, n):
    c = k.c
    squares8(k, hT, xb, n)
    rms_stats(k, lambda i: hT.ap[:, i, 0:n], 8, D, 0, sd, rstd, n, hT.all())
    for ci in range(8):
        stt(k, hT.ap[:, ci, 0:n], xb.ap[:, ci, 0:n], c.gv.ap[:, gcol + ci:gcol + ci + 1], rstd.ap[:, 0:n], ALU.mult, ALU.mult,
            xb.all() + rstd.all() + c.gv.all(), hT.all())


def phase_A3X(k):
    S, d, c, P, ps, psk = k.S, k.d, k.c, k.P, k.ps, k.psk
    xbs = [S.alloc(f"xa{i}", [128, 8, 512], F32) for i in range(2)]
    hT = S.alloc("hTa", [128, 8, 512], BF16)
    rstd = S.alloc("rstda", [128, 512], F32)
    sd = rstd
    qxT = S.alloc("qxT", [128, 8, 512], BF16, nsub=8)
    PTx = [S.alloc(f"PTx{i}", [128, 512], BF16) for i in range(2)]
    oxT = S.alloc("oxT", [128, 8, 512], BF16)
    rec = S.alloc("recx", [128, 512], F32)
    xTv = d["xT"].rearrange("(c p) t -> p c t", p=128)
    x2v = d["x2s"].rearrange("(c p) t -> p c t", p=128)
    tiles = [(0, 128)] + [(128 + 512 * i, 512) for i in range(4)]
    nt = len(tiles)

    def xload(ti_):
        s0_, N_ = tiles[ti_]
        S.dma("sp", xbs[ti_ % 2].ap[:, :, 0:N_], xTv[:, :, HALO0 + s0_:HALO0 + s0_ + N_], W=xbs[ti_ % 2].all(), key=f"xa{ti_ % 2}")

    def stage_w(ti_):
        s0, N = tiles[ti_]
        xb = xbs[ti_ % 2]
        for cb in range(8):
            pi = 1 + cb % 2
            cs = slice(cb * 128, (cb + 1) * 128)
            for j in range(4):
                mm(k, ps[pi][:, 0:N], P.w_out.ap[:, j, cs], P.omlaT.ap[:, j, s0:s0 + N], j == 0, False,
                   P.w_out.all() + P.omlaT.all(), [psk[pi]])
            for j in range(4):
                mm(k, ps[pi][:, 0:N], P.w_out.ap[:, 4 + j, cs], P.oretT.ap[:, j, s0:s0 + N], False, j == 3,
                   P.w_out.all() + P.oretT.all(), [psk[pi]])
            tt(k, "dve", xb.ap[:, cb, 0:N], xb.ap[:, cb, 0:N], ps[pi][:, 0:N], ALU.add, xb.all() + [psk[pi]], xb.all())
        if k.dbg and k.stage == "A3X":
            dumpx(k, "x1", xb, s0, N)

    def stage_n1(ti_):
        s0, N = tiles[ti_]
        xb = xbs[ti_ % 2]
        squares8(k, hT, xb, N)
        rms_stats(k, lambda i: hT.ap[:, i, 0:N], 8, D, 0, sd, rstd, N, hT.all())

    def stage_n2(ti_, ci):
        s0, N = tiles[ti_]
        xb = xbs[ti_ % 2]
        stt(k, hT.ap[:, ci, 0:N], xb.ap[:, ci, 0:N], c.gv.ap[:, c.g_xattn + ci:c.g_xattn + ci + 1], rstd.ap[:, 0:N], ALU.mult,
            ALU.mult, xb.all() + rstd.all() + c.gv.all(), hT.all())

    xload(0)
    xload(1)
    stage_w(0)
    stage_n1(0)
    for ci in range(8):
        stage_n2(0, ci)
    for ti_, (s0, N) in enumerate(tiles):
        xb = xbs[ti_ % 2]
        for blk in range(8):
            pi = 1 + blk % 2
            for ci in range(8):
                mm(k, ps[pi][:, 0:N], P.w_xq.ap[:, ci, blk * 128:(blk + 1) * 128], hT.ap[:, ci, 0:N], ci == 0, ci == 7,
                   P.w_xq.all() + hT.all(), [psk[pi]])
            cp(k, "act", qxT.ap[:, blk, 0:N], ps[pi][:, 0:N], [psk[pi]], [qxT.k(blk)])
        if ti_ + 1 < nt:
            stage_w(ti_ + 1)
            stage_n1(ti_ + 1)
        for h in range(4):
            for kb2 in range(2):
                sb = 3 + kb2
                for dc in range(2):
                    mm(k, ps[sb][:, 0:N], P.memKT.ap[:, 2 * h + dc, kb2 * 128:(kb2 + 1) * 128], qxT.ap[:, 2 * h + dc, 0:N],
                       dc == 0, dc == 1, P.memKT.all() + [qxT.k(2 * h + dc)], [psk[sb]])
                act(k, PTx[kb2].ap[:, 0:N], ps[sb][:, 0:N], AF.Exp, [psk[sb]], PTx[kb2].all(), scale=1.0 / 16.0)
            for kb2 in range(2):
                mm(k, ps[5][:, 0:N], c.ones.ap, PTx[kb2].ap[:, 0:N], kb2 == 0, kb2 == 1, c.ones.all() + PTx[kb2].all(), [psk[5]])
            act(k, rec.ap[:, 0:N], ps[5][:, 0:N], AF.Ln, [psk[5]], rec.all())
            act(k, rec.ap[:, 0:N], rec.ap[:, 0:N], AF.Exp, rec.all(), rec.all(), scale=-1.0)
            for eb in range(2):
                pi = 6 + eb
                for kb2 in range(2):
                    mm(k, ps[pi][:, 0:N], P.memV.ap[:, kb2, h * 256 + eb * 128:h * 256 + (eb + 1) * 128], PTx[kb2].ap[:, 0:N],
                       kb2 == 0, kb2 == 1, P.memV.all() + PTx[kb2].all(), [psk[pi]])
                tt(k, "dve", oxT.ap[:, 2 * h + eb, 0:N], ps[pi][:, 0:N], rec.ap[:, 0:N], ALU.mult, [psk[pi]] + rec.all(), oxT.all())
            if ti_ + 1 < nt:
                stage_n2(ti_ + 1, 2 * h)
                stage_n2(ti_ + 1, 2 * h + 1)
        for cb in range(8):
            pi = 1 + cb % 2
            for j in range(8):
                mm(k, ps[pi][:, 0:N], P.w_xo.ap[:, j, cb * 128:(cb + 1) * 128], oxT.ap[:, j, 0:N], j == 0, j == 7,
                   P.w_xo.all() + oxT.all(), [psk[pi]])
            tt(k, "dve", xb.ap[:, cb, 0:N], xb.ap[:, cb, 0:N], ps[pi][:, 0:N], ALU.add, xb.all() + [psk[pi]], xb.all())
        S.dma("sp", x2v[:, :, s0:s0 + N], xb.ap[:, :, 0:N], R=xb.all(), W=[("x2s", s0)], key=f"xs{ti_ % 2}")
        if k.dbg and k.stage == "A3X":
            dumpx(k, "x2", xb, s0, N)
        if ti_ + 2 < nt:
            xload(ti_ + 2)
    S.release(*xbs, hT, rstd, qxT, *PTx, oxT, rec)


def dumpx(k, name, xb, s0, N):
    if name not in k.dbg_out:
        k.dbg_out[name] = k.nc.dram_tensor("dbg_" + name, [D, NST], F32, kind="ExternalOutput").ap()
    t = k.dbg_out[name].rearrange("(c p) t -> p c t", p=128)
    k.S.dma("sp", t[:, :, s0:s0 + N], xb.ap[:, :, 0:N], R=xb.all(), W=[("dbg", name, s0)], key="dbg")


def phase_F(k):
    S, d, c, P, ps, psk = k.S, k.d, k.c, k.P, k.ps, k.psk
    xf = S.alloc("xf", [128, 8, 512], F32)
    hT = S.alloc("hTf", [128, 8, 512], BF16)
    sd, rstd = S.alloc("sdf", [128, 512], F32), S.alloc("rstdf", [128, 512], F32)
    aT = S.alloc("aT", [128, NFB, 512], BF16, nsub=NFB)
    gsb = [S.alloc(f"gsb{i}", [128, 48 + 512], F32) for i in range(2)]
    cv = [S.alloc(f"cv{i}", [128, 512], F32) for i in range(2)]
    ghalo = S.alloc("ghalo", [128, NFB, 48], F32, nsub=NFB)
    x2v = d["x2s"].rearrange("(c p) t -> p c t", p=128)
    outv = d["outT"].rearrange("(c p) t -> p c t", p=128)
    w1 = P.w_ffn_in
    ostg = S.arena[0:128, aT.off:aT.off + 8 * 512 * 4].bitcast(F32).rearrange("p (a b) -> p a b", a=8)
    S.dma("sp", xf.ap[:, :, 0:128], x2v[:, :, 0:128], R=[("x2s", 0)], W=xf.all(), key="xf")
    _norm_to_hT(k, xf, hT, sd, rstd, c.g_ffn, 128)
    for j in range(NFB):
        pi = 1 + j % 2
        for ci in range(8):
            mm(k, ps[pi][:, 0:128], w1.ap[:, ci, j * 128:(j + 1) * 128], hT.ap[:, ci, 0:128], ci == 0, ci == 7,
               [w1.k(0 if j < 11 else 2)] + hT.all(), [psk[pi]])
        ts(k, "dve", ghalo.ap[:, j, :], ps[pi][:, 80:128], c.flags.ap[:, 1:2], None, ALU.mult, None, [psk[pi]] + c.flags.all(),
           [ghalo.k(j)])
    for ti in range(4):
        s0 = 128 + 512 * ti
        S.dma("sp", xf.ap, x2v[:, :, s0:s0 + 512], R=[("x2s", s0)], W=xf.all(), key="xf")
        _norm_to_hT(k, xf, hT, sd, rstd, c.g_ffn, 512)
        for j in range(NFB):
            pg, pu = 1 + (j % 2), (3, 4, 7)[j % 3]
            for ci in range(8):
                mm(k, ps[pg], w1.ap[:, ci, j * 128:(j + 1) * 128], hT.ap[:, ci, :], ci == 0, ci == 7,
                   [w1.k(0 if j < 11 else 2)] + hT.all(), [psk[pg]])
            for ci in range(8):
                mm(k, ps[pu], w1.ap[:, ci, DFF + j * 128:DFF + (j + 1) * 128], hT.ap[:, ci, :], ci == 0, ci == 7,
                   [w1.k(1 if j < 11 else 3)] + hT.all(), [psk[pu]])
            g, cvb = gsb[j % 2], cv[j % 2]
            cw = c.convp.ap
            cp(k, "act", g.ap[:, 48:560], ps[pg], [psk[pg]], g.all())
            cp(k, "pool", g.ap[:, 0:48], ghalo.ap[:, j, :], [ghalo.k(j)], g.all())
            k.S.op("act", lambda e, g=g, cvb=cvb, j=j: e.activation(out=cvb.ap, in_=g.ap[:, 48:560], func=AF.Identity,
                                                                     scale=cw[:, j, 2:3], bias=cw[:, j, 3:4]),
                   g.all() + c.convp.all(), cvb.all())
            stt(k, cvb.ap, g.ap[:, 47:559], cw[:, j, 1:2], cvb.ap, ALU.mult, ALU.add, g.all() + cvb.all() + c.convp.all(), cvb.all())
            stt(k, cvb.ap, g.ap[:, 46:558], cw[:, j, 0:1], cvb.ap, ALU.mult, ALU.add, g.all() + cvb.all() + c.convp.all(), cvb.all())
            cp(k, "pool", ghalo.ap[:, j, :], g.ap[:, 512:560], g.all(), [ghalo.k(j)])
            act(k, cvb.ap, cvb.ap, AF.Silu, cvb.all(), cvb.all())
            tt(k, "dve", aT.ap[:, j, :], cvb.ap, ps[pu], ALU.mult, cvb.all() + [psk[pu]], [aT.k(j)])
        for cb in range(8):
            pi = 5 + cb % 2
            for j in range(NFB):
                mm(k, ps[pi], P.w_ffn_out.ap[:, j, cb * 128:(cb + 1) * 128], aT.ap[:, j, :], j == 0, j == NFB - 1,
                   P.w_ffn_out.all() + [aT.k(j)], [psk[pi]])
            tt(k, "dve", xf.ap[:, cb, :], xf.ap[:, cb, :], ps[pi], ALU.add, xf.all() + [psk[pi]], xf.all())
        squares8(k, hT, xf, 512)
        rms_stats(k, lambda i: hT.ap[:, i, :], 8, D, 0, sd, rstd, 512, hT.all())
        for cb in range(8):
            stt(k, ostg[:, cb, :], xf.ap[:, cb, :], c.gv.ap[:, c.g_final + cb:c.g_final + cb + 1], rstd.ap, ALU.mult, ALU.mult,
                xf.all() + rstd.all() + c.gv.all(), aT.all())
        S.dma("sp", outv[:, :, 512 * ti:512 * (ti + 1)], ostg, R=aT.all(), W=[("out", ti)], key="out")
    S.release(xf, hT, sd, rstd, aT, *gsb, *cv, ghalo)


def _consts():
    f32 = np.float32
    H, L = 4, 128
    log_gamma = np.log(f32(1.0) - f32(2.0) ** (f32(-5.0) - np.arange(H, dtype=f32))).astype(f32)
    j = np.arange(L, dtype=f32)
    diff = j[:, None] - j[None, :]
    intra = np.where(diff[None] >= 0, np.exp(np.maximum(diff, 0.0)[None] * log_gamma[:, None, None]), 0.0).astype(f32)
    k_to_end = np.exp((L - 1 - j)[:, None] * log_gamma[None, :]).astype(f32)
    q_from_start = np.exp((j + 1)[:, None] * log_gamma[None, :]).astype(f32)
    chunk_decay = np.exp(f32(L) * log_gamma).astype(f32)
    dk = f32(128.0 ** -0.5)
    c_intra = np.zeros((128, 512), f32)
    for h in range(H):
        c_intra[:, h * 128:(h + 1) * 128] = intra[h].T * dk
    c_qfs = np.zeros((128, 512), f32)
    c_decay = np.zeros((128, 512), f32)
    for h in range(H):
        c_qfs[:, h * 128:(h + 1) * 128] = q_from_start[:, h][None, :]
        c_decay[:, h * 128:(h + 1) * 128] = chunk_decay[h]
    c_small = np.zeros((128, 8), f32)
    invf_r = (1.0 / (f32(10000.0) ** (np.arange(0, 128, 2, dtype=f32) / f32(128)))).astype(f32)
    invf_m = (1.0 / (f32(10000.0) ** (np.arange(0, 32, 2, dtype=f32) / f32(32)))).astype(f32)
    p = np.arange(128)
    c_small[:, 0] = invf_r[p % 64]
    c_small[:, 1] = invf_m[p % 16]
    c_small[:, 2:6] = k_to_end * dk
    c_ident = np.eye(128, dtype=f32)
    c_rot = np.zeros((128, 128), f32)
    for m in range(64):
        c_rot[m + 64, m] = -1.0
    for m in range(64, 128):
        c_rot[m - 64, m] = 1.0
    kk = np.arange(128)
    c_causal = (kk[None, :] >= kk[:, None]).astype(f32)
    return dict(c_small=c_small, c_ident=c_ident, c_rot=c_rot, c_causal=c_causal, c_intra=c_intra, c_qfs=c_qfs,
                c_decay=c_decay)


def make_in_maps(inputs):
    f32 = np.float32
    x = np.asarray(inputs["x"], f32)
    mem = np.asarray(inputs["mem"], f32)
    pos = np.asarray(inputs["positions"], np.int32)

    def col(g):
        g = np.asarray(g, f32).reshape(-1, 128)
        return np.ascontiguousarray(g.T)

    gv = np.concatenate([col(inputs["g_mix"][0]), col(inputs["g_xattn"][0]), col(inputs["g_mem"][0]),
                         col(inputs["g_ffn"][0]), col(inputs["g_final"]), col(inputs["g_q_lat"][0]),
                         col(inputs["g_kv_lat"][0])], axis=1)
    cw = np.asarray(inputs["conv_w"][0], f32)
    cb = np.asarray(inputs["conv_b"][0], f32)
    convp = np.zeros((128, NFB, 4), f32)
    for i in range(3):
        convp[:, :, i] = cw[i].reshape(NFB, 128).T
    convp[:, :, 3] = cb.reshape(NFB, 128).T
    shared = dict(
        w_in=np.ascontiguousarray(inputs["w_in"][0], f32), w_uq=np.ascontiguousarray(inputs["w_uq"][0], f32),
        w_ukv=np.ascontiguousarray(inputs["w_ukv"][0], f32), w_out=np.ascontiguousarray(inputs["w_out"][0], f32),
        w_xq=np.ascontiguousarray(inputs["w_xq"][0], f32), w_xkv=np.ascontiguousarray(inputs["w_xkv"][0], f32),
        w_xo=np.ascontiguousarray(inputs["w_xo"][0], f32), w_ffn_in=np.ascontiguousarray(inputs["w_ffn_in"][0], f32),
        w_ffn_out=np.ascontiguousarray(inputs["w_ffn_out"][0], f32), gv=np.ascontiguousarray(gv),
        convp=np.ascontiguousarray(convp.reshape(128, NFB * 4)), **_consts())
    maps = []
    for core in range(8):
        b, hf = core // 2, core % 2
        xT = np.zeros((D, NTOK), f32)
        pp = np.zeros((1, NTOK), np.int32)
        if hf == 0:
            xT[:, NPRE:] = x[b, :NOWN].T
            pp[0, NPRE:] = pos[b, :NOWN]
        else:
            xT[:, :] = x[b].T
            pp[0, :] = pos[b]
        flags = np.full((128, 2), float(hf), f32)
        m = dict(shared)
        m.update(xT=xT, posi=pp, memT=np.ascontiguousarray(mem[b].T), flags=flags)
        maps.append(m)
    return maps


_CACHE = {}


def kernel(**inputs):
    if "nc" not in _CACHE:
        _CACHE["nc"] = build("full")[0]
    nc = _CACHE["nc"]
    maps = make_in_maps(inputs)
    res = run_bass_kernel_spmd(nc, maps, core_ids=list(range(8)))
    out = np.zeros((NB, SEQ, D), np.float32)
    for core in range(8):
        b, hf = core // 2, core % 2
        out[b, hf * NOWN:(hf + 1) * NOWN, :] = res.results[core]["outT"].T
    return out
```

```python
import math
from contextlib import ExitStack

import numpy as np
import concourse.bass as bass
import concourse.mybir as mybir
from concourse.bass_utils import run_bass_kernel_spmd

F32 = mybir.dt.float32
BF16 = mybir.dt.bfloat16
I32 = mybir.dt.int32
U8 = mybir.dt.uint8
AF = mybir.ActivationFunctionType
ALU = mybir.AluOpType
AX = mybir.AxisListType

D = 1024
SEQ = 4096
NB = 4
EPS = 1e-6
NPRE = 2048
NOWN = 2048
NTOK = NPRE + NOWN
HALO0 = NPRE - 128
NST = NOWN + 128
IN_COLS = 2464
OFF_CQ, OFF_CKV, OFF_KR, OFF_RQ, OFF_RK, OFF_RV, OFF_RG = 0, 256, 384, 416, 928, 1440, 1952
DFF = 2816
NFB = DFF // 128
MEM = 256
TWO_PI = 2.0 * math.pi
C1 = 6.28125
C2 = TWO_PI - C1
MAGIC = 12582912.0
PI_LO = 3.1415925
ARENA_BYTES = 206 * 1024
ENGS = ("pe", "act", "dve", "pool", "sp")
NDMA_MAX = 90
DT_SIZE = {F32: 4, BF16: 2, I32: 4}


class Buf:
    def __init__(self, uid, name, off, nbytes, ap, nsub):
        self.uid, self.name, self.off, self.nbytes, self.ap, self.nsub = uid, name, off, nbytes, ap, nsub

    def k(self, i=0):
        return (self.uid, i)

    def all(self):
        return [(self.uid, i) for i in range(self.nsub)]

    def __getitem__(self, idx):
        return self.ap[idx]


class Sched:
    def __init__(self, nc, es):
        self.nc, self.es = nc, es
        self.prog = {e: [] for e in ENGS}
        self.sem = {e: es.enter_context(nc.semaphore("s_" + e)) for e in ENGS if e != "sp"}
        self.cnt = {e: 0 for e in ENGS}
        self.seen = {e: {} for e in ENGS}
        self.drained = {e: 0 for e in ENGS}
        self.needed = {e: set() for e in ENGS}
        self.lastw, self.readers = {}, {}
        self.ndma = 0
        self.dma_events = []
        self.dma_pool = [es.enter_context(nc.semaphore(f"d{i}")) for i in range(NDMA_MAX)]
        self.dma_sem = {}
        self.arena = es.enter_context(nc.sbuf_tensor("arena", [128, ARENA_BYTES], U8))
        self.free = [(0, ARENA_BYTES)]
        self.freed_events = []
        self.nbuf = 0
        self.peak = 0
        self.used = 0

    def alloc(self, name, shape, dtype, nsub=1):
        n = 1
        for s in shape[1:]:
            n *= s
        nbytes = (n * DT_SIZE[dtype] + 63) // 64 * 64
        for i, (o, sz) in enumerate(self.free):
            if sz >= nbytes:
                off = o
                if sz == nbytes:
                    self.free.pop(i)
                else:
                    self.free[i] = (o + nbytes, sz - nbytes)
                break
        else:
            raise MemoryError(f"arena full allocating {name} {nbytes}B; free={self.free}")
        ap = self.arena[0:shape[0], off:off + n * DT_SIZE[dtype]].bitcast(dtype)
        if len(shape) == 3:
            ap = ap.rearrange("p (a b) -> p a b", a=shape[1])
        elif len(shape) == 4:
            ap = ap.rearrange("p (a b c) -> p a b c", a=shape[1], b=shape[2])
        self.nbuf += 1
        b = Buf(self.nbuf, name, off, nbytes, ap, nsub)
        evs, keep = [], []
        for (fo, fn, fe) in self.freed_events:
            if fo < off + nbytes and off < fo + fn:
                evs.extend(fe)
            keep.append((fo, fn, fe))
        self.freed_events = keep
        if evs:
            for kk in b.all():
                self.readers[kk] = list(evs)
        self.used += nbytes
        self.peak = max(self.peak, self.used)
        return b

    def release(self, *bufs):
        for b in bufs:
            evs = []
            for kk in b.all():
                if kk in self.lastw:
                    evs.append(self.lastw.pop(kk))
                evs.extend(self.readers.pop(kk, []))
            best = {}
            for (s, v) in evs:
                best[s] = max(best.get(s, 0), v)
            self.freed_events.append((b.off, b.nbytes, list(best.items())))
            self.free.append((b.off, b.nbytes))
            self.free.sort()
            merged = []
            for (o, sz) in self.free:
                if merged and merged[-1][0] + merged[-1][1] == o:
                    merged[-1] = (merged[-1][0], merged[-1][1] + sz)
                else:
                    merged.append((o, sz))
            self.free = merged
            self.used -= b.nbytes

    def _deps(self, eng, R, W):
        best = {}
        for r in R:
            ev = self.lastw.get(r)
            if ev is not None:
                best[ev[0]] = max(best.get(ev[0], 0), ev[1])
        for w in W:
            ev = self.lastw.get(w)
            if ev is not None:
                best[ev[0]] = max(best.get(ev[0], 0), ev[1])
            for ev in self.readers.get(w, ()):
                best[ev[0]] = max(best.get(ev[0], 0), ev[1])
        for s, v in best.items():
            if s == eng:
                if eng == "pe":
                    continue
                if eng in ("act", "dve"):
                    if self.drained[eng] < v:
                        self.prog[eng].append(("drain",))
                        self.drained[eng] = self.cnt[eng]
                    continue
            if self.seen[eng].get(s, 0) >= v:
                continue
            self.seen[eng][s] = v
            self.prog[eng].append(("wait", s, v))
            if isinstance(s, str):
                self.needed[s].add(v)

    def _commit(self, ev, R, W):
        for w in W:
            self.lastw[w] = ev
            self.readers[w] = []
        for r in R:
            if r not in W:
                self.readers.setdefault(r, []).append(ev)

    def merge_keys(self, src_keys, dst_key):
        evs = []
        for kk in src_keys:
            if kk in self.lastw:
                evs.append(self.lastw[kk])
            evs.extend(self.readers.get(kk, []))
        self.readers.setdefault(dst_key, []).extend(evs)

    def op(self, eng, fn, R=(), W=()):
        R, W = list(R), list(W)
        self._deps(eng, R, W)
        self.cnt[eng] += 1
        ev = (eng, self.cnt[eng])
        self.prog[eng].append(("inst", fn, self.cnt[eng]))
        self._commit(ev, R, W)
        return ev

    def dma(self, eng, out, in_, R=(), W=(), key=None):
        R, W = list(R), list(W)
        self._deps(eng, R, W)
        if key is None:
            key = f"_auto{self.ndma}"
        if key not in self.dma_sem:
            self.dma_sem[key] = [self.dma_pool[len(self.dma_sem)], 0]
        ent = self.dma_sem[key]
        sem = ent[0]
        if ent[1] > 0 and self.seen[eng].get(sem, 0) < ent[1]:
            self.seen[eng][sem] = ent[1]
            self.prog[eng].append(("wait", sem, ent[1]))
        ent[1] += 16
        self.ndma += 1
        ev = (sem, ent[1])
        self.prog[eng].append(("dma", out, in_, sem))
        self._commit(ev, R, W)
        self.dma_events.append(ev)
        return ev

    def emit(self, block):
        nc = self.nc
        S = self

        rank = {en: {q: i + 1 for i, q in enumerate(sorted(S.needed[en]))} for en in ENGS}
        S.n_inc = {en: len(rank[en]) for en in ENGS}

        def run(e, name):
            for ent in S.prog[name]:
                if ent[0] == "wait":
                    s = ent[1]
                    if isinstance(s, str):
                        e.wait_ge(S.sem[s], rank[s][ent[2]])
                    else:
                        e.wait_ge(s, ent[2])
                elif ent[0] == "drain":
                    e.drain()
                elif ent[0] == "inst":
                    ins = ent[1](e)
                    if ent[2] in rank[name]:
                        ins.then_inc(S.sem[name], 1)
                else:
                    e.dma_start(out=ent[1], in_=ent[2]).then_inc(ent[3], 16)

        @block.tensor
        def _(e):
            run(e, "pe")

        @block.scalar
        def _(e):
            run(e, "act")

        @block.vector
        def _(e):
            run(e, "dve")

        @block.gpsimd
        def _(e):
            run(e, "pool")

        @block.sync
        def _(e):
            run(e, "sp")

    def final_wait(self, eng, events):
        for (s, v) in events:
            if self.seen[eng].get(s, 0) >= v:
                continue
            self.seen[eng][s] = v
            self.prog[eng].append(("wait", s, v))


class K:
    pass


def bc(ap, shape):
    return ap.broadcast_to(shape)


def build(stage="full", dbg=False, a1_tiles=None, a1_level=9):
    nc = bass.Bass("TRN2", target_bir_lowering=False)
    es = ExitStack()
    k = K()
    k.nc, k.es, k.stage, k.dbg = nc, es, stage, dbg
    k.a1_tiles, k.a1_level = a1_tiles, a1_level
    d = {}

    def din(name, shape, dt=F32):
        d[name] = nc.dram_tensor(name, list(shape), dt, kind="ExternalInput").ap()

    din("xT", [D, NTOK]); din("posi", [1, NTOK], I32); din("memT", [D, MEM]); din("flags", [128, 2])
    din("w_in", [D, IN_COLS]); din("w_uq", [256, 768]); din("w_ukv", [128, 1024]); din("w_out", [D, D])
    din("w_xq", [D, D]); din("w_xkv", [D, 2 * D]); din("w_xo", [D, D])
    din("w_ffn_in", [D, 2 * DFF]); din("w_ffn_out", [DFF, D])
    din("gv", [128, 43]); din("convp", [128, NFB * 4]); din("c_small", [128, 8])
    din("c_ident", [128, 128]); din("c_rot", [128, 128]); din("c_causal", [128, 128])
    din("c_intra", [128, 512]); din("c_qfs", [128, 512]); din("c_decay", [128, 512])
    d["outT"] = nc.dram_tensor("outT", [D, NOWN], F32, kind="ExternalOutput").ap()
    d["x2s"] = nc.dram_tensor("x2s", [D, NST], F32, kind="Internal").ap()
    k.dbg_out = {}
    k.d = d
    with es:
        S = Sched(nc, es)
        k.S = S
        big = [es.enter_context(nc.psum_tensor(f"psb{i}", [128, 1024], F32)) for i in range(4)]
        k.ps2 = [b_[:, :] for b_ in big]
        k.ps = [big[i // 2][:, (i % 2) * 512:(i % 2 + 1) * 512] for i in range(8)]
        k.psk = [("ps", i) for i in range(8)]
        block = es.enter_context(nc.Block())
        emit_all(k)
        S.final_wait("sp", S.dma_events)
        S.emit(block)
    k.peak = S.peak
    return nc, k


def mm(k, out, lhsT, rhs, start, stop, R, W):
    k.S.op("pe", lambda e: e.matmul(out, lhsT=lhsT, rhs=rhs, start=start, stop=stop), R, W)


def tr(k, out, in_, ident, R, W):
    k.S.op("pe", lambda e: e.transpose(out=out, in_=in_, identity=ident), R, W)


def act(k, out, in_, func, R, W, scale=1.0, bias=0.0):
    k.S.op("act", lambda e: e.activation(out=out, in_=in_, func=func, scale=scale, bias=bias), R, W)


def tt(k, eng, out, in0, in1, op, R, W):
    k.S.op(eng, lambda e: e.tensor_tensor(out=out, in0=in0, in1=in1, op=op), R, W)


def ts(k, eng, out, in0, s1, s2, op0, op1, R, W):
    if op1 is None:
        k.S.op(eng, lambda e: e.tensor_scalar(out=out, in0=in0, scalar1=s1, scalar2=None, op0=op0), R, W)
    else:
        k.S.op(eng, lambda e: e.tensor_scalar(out=out, in0=in0, scalar1=s1, scalar2=s2, op0=op0, op1=op1), R, W)


def stt(k, out, in0, scalar, in1, op0, op1, R, W):
    k.S.op("dve", lambda e: e.scalar_tensor_tensor(out=out, in0=in0, scalar=scalar, in1=in1, op0=op0, op1=op1), R, W)


def cp(k, eng, out, in_, R, W):
    if eng == "act":
        k.S.op("act", lambda e: e.copy(out=out, in_=in_), R, W)
    else:
        k.S.op(eng, lambda e: e.tensor_copy(out=out, in_=in_), R, W)


def recip(k, out, in_, R, W):
    k.S.op("dve", lambda e: e.reciprocal(out=out, in_=in_), R, W)


def dump(k, name, buf_ap, shape, R, dt=F32):
    t = k.nc.dram_tensor("dbg_" + name, list(shape), dt, kind="ExternalOutput").ap()
    k.dbg_out[name] = t
    k.S.dma("sp", t, buf_ap, R=R, W=[("dbg", name)], key="dbg")


def load_consts(k):
    S, d = k.S, k.d
    c = K()
    k.c = c
    c.gv = S.alloc("gv", [128, 43], F32)
    c.convp = S.alloc("convp", [128, NFB, 4], F32)
    c.small = S.alloc("c_small", [128, 8], F32)
    c.flags = S.alloc("flags", [128, 2], F32)
    c.ident = S.alloc("ident", [128, 128], BF16)
    c.rot = S.alloc("rot", [128, 128], BF16)
    c.causal = S.alloc("causal", [128, 128], BF16)
    c.intra = S.alloc("intra", [128, 512], F32)
    c.qfs = S.alloc("qfs", [128, 512], F32)
    c.decay = S.alloc("decay", [128, 512], F32)
    c.ones = S.alloc("ones", [128, 128], BF16)
    S.dma("sp", c.gv.ap, d["gv"], W=c.gv.all())
    S.dma("sp", c.convp.ap, d["convp"].rearrange("p (a b) -> p a b", a=NFB), W=c.convp.all())
    S.dma("sp", c.small.ap, d["c_small"], W=c.small.all())
    S.dma("sp", c.flags.ap, d["flags"], W=c.flags.all())
    S.dma("sp", c.intra.ap, d["c_intra"], W=c.intra.all())
    S.dma("sp", c.qfs.ap, d["c_qfs"], W=c.qfs.all())
    S.dma("sp", c.decay.ap, d["c_decay"], W=c.decay.all())
    S.dma("pool", c.ident.ap, d["c_ident"], W=c.ident.all())
    S.dma("pool", c.rot.ap, d["c_rot"], W=c.rot.all())
    S.dma("pool", c.causal.ap, d["c_causal"], W=c.causal.all())
    S.op("pool", lambda e: e.memset(c.ones.ap, 1.0), W=c.ones.all())
    c.g_mix, c.g_xattn, c.g_mem, c.g_ffn, c.g_final, c.g_q, c.g_kv = 0, 8, 16, 24, 32, 40, 42


def rope_tables(k, posi_ap, posi_key, prange, col, cosb, sinb, tmp, n):
    c = k.c
    p0, p1 = prange
    a, b_, kk = tmp
    A = lambda buf: buf.ap[p0:p1, 0:n]
    invf = c.small.ap[p0:p1, col:col + 1]
    ts(k, "dve", A(a), posi_ap[p0:p1, 0:n], invf, None, ALU.mult, None, [posi_key] + c.small.all(), a.all())
    ts(k, "dve", A(b_), A(a), 1.0 / TWO_PI, MAGIC, ALU.mult, ALU.add, a.all(), b_.all())
    ts(k, "dve", A(kk), A(b_), -MAGIC, None, ALU.add, None, b_.all(), kk.all())
    stt(k, A(b_), A(kk), -C1, A(a), ALU.mult, ALU.add, kk.all() + a.all(), b_.all())
    stt(k, A(a), A(kk), -C2, A(b_), ALU.mult, ALU.add, kk.all() + b_.all(), a.all())
    ts(k, "dve", A(a), A(a), -PI_LO, PI_LO, ALU.max, ALU.min, a.all(), a.all())
    act(k, sinb.ap[p0:p1, 0:n], A(a), AF.Sin, a.all(), sinb.all())
    ts(k, "dve", A(b_), A(a), math.pi / 2, -TWO_PI, ALU.is_gt, ALU.mult, a.all(), b_.all())
    stt(k, A(kk), A(a), math.pi / 2, A(b_), ALU.add, ALU.add, a.all() + b_.all(), kk.all())
    ts(k, "dve", A(kk), A(kk), -PI_LO, PI_LO, ALU.max, ALU.min, kk.all(), kk.all())
    act(k, cosb.ap[p0:p1, 0:n], A(kk), AF.Sin, kk.all(), cosb.all())


def squares8(k, dst, src, n):
    tt(k, "pool", dst.ap[:, 0:4, 0:n], src.ap[:, 0:4, 0:n], src.ap[:, 0:4, 0:n], ALU.mult, src.all(), dst.all())
    for ci in range(4, 8):
        act(k, dst.ap[:, ci, 0:n], src.ap[:, ci, 0:n], AF.Square, src.all(), dst.all())


def rms_stats(k, sq_chunks, nch, nfeat, ps_i, sd, rstd, n, sqkeys):
    c = k.c
    ps = k.ps[ps_i]
    for i in range(nch):
        mm(k, ps[:, 0:n], c.ones.ap, sq_chunks(i), i == 0, i == nch - 1, sqkeys + c.ones.all(), [k.psk[ps_i]])
    act(k, sd.ap[:, 0:n], ps[:, 0:n], AF.Ln, [k.psk[ps_i]], sd.all(), scale=1.0 / nfeat, bias=EPS)
    act(k, rstd.ap[:, 0:n], sd.ap[:, 0:n], AF.Exp, sd.all(), rstd.all(), scale=-0.5)


def emit_all(k):
    load_consts(k)
    P = K()
    k.P = P
    S = k.S
    P.cqn = S.alloc("cqn", [128, 2, NST], BF16)
    P.ckvn = S.alloc("ckvn", [128, NTOK], BF16)
    P.krope = S.alloc("krope", [128, NTOK], BF16)
    P.oretT = S.alloc("oretT", [128, 4, NST], BF16)
    phase_A1(k)
    if k.stage == "A1":
        return
    P.memKT = S.alloc("memKT", [128, 8, MEM], BF16)
    P.memV = S.alloc("memV", [128, 2, D], BF16)
    phase_MKV(k)
    P.omlaT = S.alloc("omlaT", [128, 4, NST], BF16)
    P.w_out = S.alloc("w_out_bf", [128, 8, D], BF16)
    P.w_xq = S.alloc("w_xq_bf", [128, 8, D], BF16)
    d = k.d
    phase_A2(k)
    if k.stage == "A2":
        return
    S.release(P.cqn, P.ckvn, P.krope)
    P.w_xo = S.alloc("w_xo_bf", [128, 8, D], BF16)
    S.dma("pool", P.w_xo.ap, d["w_xo"].rearrange("(c p) n -> p c n", p=128), W=P.w_xo.all(), key="w_xo")
    P.w_ffn_out = S.alloc("w_ffn_out_bf", [128, NFB, D], BF16)
    S.dma("pool", P.w_ffn_out.ap, d["w_ffn_out"].rearrange("(c p) n -> p c n", p=128), W=P.w_ffn_out.all(), key="w_ffn_out")
    phase_A3X(k)
    if k.stage == "A3X":
        return
    if getattr(k, "a3x_early", False):
        S.release(P.w_xq, P.w_xo, P.memKT, P.memV)
    else:
        S.release(P.oretT, P.omlaT, P.w_out, P.w_xq, P.w_xo, P.memKT, P.memV)
    load_w1_pieces(k, False)

    phase_F(k)


def phase_A1(k):
    S, d, c, P = k.S, k.d, k.c, k.P
    ps, psk = k.ps, k.psk
    T = 512
    w_in = S.alloc("w_in_bf", [128, 8, IN_COLS], BF16)
    S.dma("pool", w_in.ap, d["w_in"].rearrange("(c p) n -> p c n", p=128), W=w_in.all(), key="w_in")
    w_krot = S.alloc("w_krot", [128, 8, 96], BF16)
    S.op("pool", lambda e: e.memset(w_krot.ap, 0.0), W=w_krot.all())
    ts(k, "pool", w_krot.ap[:, :, 64:80], w_in.ap[:, :, OFF_KR + 16:OFF_KR + 32], -1.0, None, ALU.mult, None,
       w_in.all(), w_krot.all())
    cp(k, "pool", w_krot.ap[:, :, 80:96], w_in.ap[:, :, OFF_KR:OFF_KR + 16], w_in.all(), w_krot.all())

    xt = [S.alloc(f"xt{i}", [128, 8, T], F32) for i in range(2)]
    posi = [S.alloc(f"posi{i}", [128, T], I32) for i in range(2)]
    hTs = [S.alloc(f"hT{i}", [128, 8, T], BF16) for i in range(2)]
    rstd = S.alloc("rstd", [128, T], F32)
    sd = rstd
    ta, tb, tc = (S.alloc(n, [128, T], F32) for n in ("ta", "tb", "tc"))
    cos_r, sin_r = S.alloc("cos_r", [128, T], F32), S.alloc("sin_r", [128, T], F32)
    cos_m, sin_m = S.alloc("cos_m", [128, T], F32), S.alloc("sin_m", [128, T], F32)
    sql = S.alloc("sql", [128, 2, T], BF16)
    rstl = S.alloc("rstl", [128, T], F32)
    sdl = rstl
    raw = [S.alloc(f"raw{i}", [128, T], BF16) for i in range(2)]
    t1 = [S.alloc(f"t1_{i}", [128, T], F32) for i in range(2)]
    t2 = [S.alloc(f"t2_{i}", [128, T], F32) for i in range(2)]
    rqT = S.alloc("rqT", [128, 4, T], BF16, nsub=4)
    rkT = S.alloc("rkT", [128, 4, T], BF16, nsub=4)
    v_tm = S.alloc("v_tm", [128, 4, 512], BF16, nsub=4)
    sg_tm = S.alloc("sg_tm", [128, 4, 512], BF16, nsub=4)
    kdec = S.alloc("kdec", [128, 512], BF16)
    PT = S.alloc("PT", [128, 512], BF16)
    qsT = S.alloc("qsT", [128, 4, 128], BF16)
    S_f = S.alloc("S_f", [128, 512], F32)
    S_b = S.alloc("S_b", [128, 512], BF16)
    sqo = S.alloc("sqo", [128, 512], F32)
    tn = S.alloc("tn", [128, 512], F32)
    ogs = [S.alloc(f"og{i}", [128, 512], BF16) for i in range(2)]
    oT = S.alloc("oT", [128, 512], BF16)
    sqT = S.alloc("sqT", [128, 512], BF16)
    S.op("pool", lambda e: e.memset(S_f.ap, 0.0), W=S_f.all())
    S.op("pool", lambda e: e.memset(S_b.ap, 0.0), W=S_b.all())

    xTv = d["xT"].rearrange("(c p) t -> p c t", p=128)
    ntiles = NTOK // T
    rr = 0
    tile_list = list(range(ntiles)) if k.a1_tiles is None else k.a1_tiles
    xt_released = False

    def ln_part1(tti):
        tok0_ = tti * T
        xb_, pb_, hT_ = xt[tti % 2], posi[tti % 2], hTs[tti % 2]
        S.dma("sp", xb_.ap, xTv[:, :, tok0_:tok0_ + T], W=xb_.all(), key=f"xt{tti % 2}")
        S.dma("sp", pb_.ap, d["posi"][:, tok0_:tok0_ + T].partition_broadcast(128), W=pb_.all(), key=f"posi{tti % 2}")
        squares8(k, hT_, xb_, T)
        rms_stats(k, lambda i: hT_.ap[:, i, :], 8, D, 0, sd, rstd, T, hT_.all())

    def ln_stt(tti, ci):
        xb_, hT_ = xt[tti % 2], hTs[tti % 2]
        stt(k, hT_.ap[:, ci, :], xb_.ap[:, ci, :], c.gv.ap[:, c.g_mix + ci:c.g_mix + ci + 1], rstd.ap,
            ALU.mult, ALU.mult, xb_.all() + rstd.all() + c.gv.all(), hT_.all())

    def ropes(tti, which):
        pb_ = posi[tti % 2]
        if which == 0:
            rope_tables(k, pb_.ap, pb_.k(), (0, 128), 0, cos_r, sin_r, (ta, tb, tc), T)
        else:
            rope_tables(k, pb_.ap, pb_.k(), (64, 96), 1, cos_m, sin_m, (ta, tb, tc), T)

    ln_part1(tile_list[0])
    for ci in range(8):
        ln_stt(tile_list[0], ci)
    ropes(tile_list[0], 0)
    ropes(tile_list[0], 1)
    for tidx, tti in enumerate(tile_list):
        tok0 = tti * T
        xb, pb, hT = xt[tti % 2], posi[tti % 2], hTs[tti % 2]
        own_tile = tok0 + T > HALO0
        nxt = tile_list[tidx + 1] if tidx + 1 < len(tile_list) else None

        def proj(ps_i, m, wbuf, col0, n=T):
            for ci in range(8):
                mm(k, ps[ps_i][0:m, 0:n], wbuf.ap[:, ci, col0:col0 + m], hT.ap[:, ci, 0:n], ci == 0, ci == 7,
                   wbuf.all() + hT.all(), [psk[ps_i]])

        if k.a1_level < 3:
            continue
        proj(1, 128, w_in, OFF_CKV)
        act(k, sql.ap[:, 0, :], ps[1], AF.Square, [psk[1]], sql.all())
        rms_stats(k, lambda i: sql.ap[:, 0, :], 1, 128, 0, sdl, rstl, T, sql.all())
        stt(k, P.ckvn.ap[:, tok0:tok0 + T], ps[1], c.gv.ap[:, c.g_kv:c.g_kv + 1], rstl.ap, ALU.mult, ALU.mult,
            [psk[1]] + rstl.all() + c.gv.all(), P.ckvn.all())
        proj(2, 96, w_in, OFF_KR - 64)
        proj(3, 96, w_krot, 0)
        tt(k, "dve", t1[0].ap[64:96, :], ps[2][64:96, :], cos_m.ap[64:96, :], ALU.mult, [psk[2]] + cos_m.all(), t1[0].all())
        tt(k, "dve", t2[0].ap[64:96, :], ps[3][64:96, :], sin_m.ap[64:96, :], ALU.mult, [psk[3]] + sin_m.all(), t2[0].all())
        tt(k, "pool", P.krope.ap[64:96, tok0:tok0 + T], t1[0].ap[64:96, :], t2[0].ap[64:96, :], ALU.add,
           t1[0].all() + t2[0].all(), P.krope.all())
        if k.a1_level < 4:
            continue
        if own_tile:
            proj(1, 128, w_in, OFF_CQ)
            proj(2, 128, w_in, OFF_CQ + 128)
            act(k, sql.ap[:, 0, :], ps[1], AF.Square, [psk[1]], sql.all())
            act(k, sql.ap[:, 1, :], ps[2], AF.Square, [psk[2]], sql.all())
            rms_stats(k, lambda i: sql.ap[:, i, :], 2, 256, 0, sdl, rstl, T, sql.all())
            if tok0 < HALO0:
                lo, n_, s0 = HALO0 - tok0, 128, 0
            else:
                lo, n_, s0 = 0, T, tok0 - HALO0
            for j, pi in ((0, 1), (1, 2)):
                stt(k, P.cqn.ap[:, j, s0:s0 + n_], ps[pi][:, lo:lo + n_], c.gv.ap[:, c.g_q + j:c.g_q + j + 1],
                    rstl.ap[:, lo:lo + n_], ALU.mult, ALU.mult, [psk[pi]] + rstl.all() + c.gv.all(), P.cqn.all())
        if k.a1_level < 5:
            continue
        todo = [(rkT, OFF_RK)] + ([(rqT, OFF_RQ)] if own_tile else [])
        items = [(dst, off, h) for (dst, off) in todo for h in range(4)]

        def rope_tail(it, slot):
            dst, off, h = it
            pi, pj = 1 + slot, 3 + slot
            rb, a1, a2 = raw[slot], t1[slot], t2[slot]
            mm(k, ps[pj], c.rot.ap, rb.ap, True, True, rb.all() + c.rot.all(), [psk[pj]])
            tt(k, "dve", a2.ap, ps[pj], sin_r.ap, ALU.mult, [psk[pj]] + sin_r.all(), a2.all())
            tt(k, "pool", a1.ap, rb.ap, cos_r.ap, ALU.mult, rb.all() + cos_r.all(), a1.all())
            tt(k, "pool", dst.ap[:, h, :], a1.ap, a2.ap, ALU.add, a1.all() + a2.all(), [dst.k(h)])

        prev = None
        for n_, it in enumerate(items):
            slot = n_ % 2
            proj(1 + slot, 128, w_in, it[1] + it[2] * 128)
            cp(k, "act", raw[slot].ap, ps[1 + slot], [psk[1 + slot]], raw[slot].all())
            if prev is not None:
                rope_tail(*prev)
            prev = (it, slot)
        rope_tail(*prev)
        if k.a1_level < 6:
            continue
        if own_tile:
            for h in range(4):
                pi = 1 + h % 2
                proj(pi, 128, w_in, OFF_RG + h * 128)
                act(k, sg_tm.ap[:, h, :], ps[pi], AF.Silu, [psk[pi]], [sg_tm.k(h)])
        if nxt is not None:
            ln_part1(nxt)
        elif k.stage != "A1":
            S.release(*xt)
            xt_released = True
            P.w_xkv = S.alloc("w_xkv_bf", [128, 8, 2 * D], BF16)
            S.dma("pool", P.w_xkv.ap, d["w_xkv"].rearrange("(c p) n -> p c n", p=128), W=P.w_xkv.all(), key="w_xkv")
        p7b = ps[7].bitcast(BF16)
        k7a, k7b = ("ps", "7a"), ("ps", "7b")

        def stage_a(cc):
            ctok = tok0 + cc * 128
            own_chunk = ctok >= HALO0
            cs = slice(cc * 128, (cc + 1) * 128)
            for ci in range(8):
                mm(k, ps[1], hT.ap[:, ci, cs], w_in.ap[:, ci, OFF_RV:OFF_RV + 512], ci == 0, ci == 7,
                   hT.all() + w_in.all(), [psk[1]])
            cp(k, "act", v_tm.ap[:, cc, :], ps[1], [psk[1]], [v_tm.k(cc)])
            for h in range(4):
                tr(k, p7b[:, h * 128:(h + 1) * 128], rkT.ap[:, h, cs], c.ident.ap, [rkT.k(h)] + c.ident.all(), [k7a])
            if own_chunk:
                for h in range(4):
                    mm(k, ps[2][:, h * 128:(h + 1) * 128], rkT.ap[:, h, cs], rqT.ap[:, h, cs], True, True,
                       [rkT.k(h), rqT.k(h)], [psk[2]])
            tt(k, "dve", kdec.ap.rearrange("p (h e) -> p h e", h=4), p7b[:, 0:512].rearrange("p (h e) -> p h e", h=4),
               bc(c.small.ap[:, 2:6].unsqueeze(2), [128, 4, 128]), ALU.mult, [k7a] + c.small.all(), kdec.all())
            if own_chunk:
                tt(k, "dve", PT.ap, ps[2], c.intra.ap, ALU.mult, [psk[2]] + c.intra.all(), PT.all())
                tt(k, "pool", qsT.ap, rqT.ap[:, :, cs], c.qfs.ap.rearrange("p (h e) -> p h e", h=4), ALU.mult,
                   rqT.all() + c.qfs.all(), qsT.all())
            if ctok + 128 < NTOK:
                for h in range(4):
                    hs = slice(h * 128, (h + 1) * 128)
                    mm(k, ps[5][:, hs], kdec.ap[:, hs], v_tm.ap[:, cc, hs], True, True, kdec.all() + [v_tm.k(cc)], [psk[5]])
            if own_chunk:
                for h in range(4):
                    hs = slice(h * 128, (h + 1) * 128)
                    mm(k, ps[6][:, hs], PT.ap[:, hs], v_tm.ap[:, cc, hs], True, False, PT.all() + [v_tm.k(cc)], [psk[6]])
                    mm(k, ps[6][:, hs], qsT.ap[:, h, :], S_b.ap[:, hs], False, True, qsT.all() + S_b.all(), [psk[6]])
                cp(k, "act", ogs[cc % 2].ap, ps[6], [psk[6]], ogs[cc % 2].all())
            if ctok + 128 < NTOK:
                tt(k, "pool", S_f.ap, S_f.ap, c.decay.ap, ALU.mult, S_f.all() + c.decay.all(), S_f.all())
                tt(k, "dve", S_f.ap, S_f.ap, ps[5], ALU.add, S_f.all() + [psk[5]], S_f.all())
                cp(k, "act", S_b.ap, S_f.ap, S_f.all(), S_b.all())

        def stage_b(cc):
            ctok = tok0 + cc * 128
            if ctok < HALO0:
                return
            cs = slice(cc * 128, (cc + 1) * 128)
            og = ogs[cc % 2]
            for h in range(4):
                tr(k, p7b[:, 512 + h * 128:512 + (h + 1) * 128], og.ap[:, h * 128:(h + 1) * 128], c.ident.ap,
                   og.all() + c.ident.all(), [k7b])
            cp(k, "dve", oT.ap, p7b[:, 512:1024], [k7b], oT.all())
            act(k, sqT.ap, oT.ap, AF.Square, oT.all(), sqT.all())
            mm(k, ps[4], c.ones.ap, oT.ap, True, True, c.ones.all() + oT.all(), [psk[4]])
            mm(k, ps[3], c.ones.ap, sqT.ap, True, True, c.ones.all() + sqT.all(), [psk[3]])
            act(k, tn.ap, ps[4], AF.Copy, [psk[4]], tn.all(), scale=1.0 / 128)
            tt(k, "dve", sqo.ap, tn.ap, tn.ap, ALU.mult, tn.all(), sqo.all())
            stt(k, sqo.ap, ps[3], 1.0 / 128, sqo.ap, ALU.mult, ALU.subtract, [psk[3]] + sqo.all(), sqo.all())
            act(k, sqo.ap, sqo.ap, AF.Ln, sqo.all(), sqo.all(), scale=1.0, bias=EPS)
            act(k, sqo.ap, sqo.ap, AF.Exp, sqo.all(), sqo.all(), scale=-0.5)
            tt(k, "dve", tn.ap, oT.ap, tn.ap, ALU.subtract, oT.all() + tn.all(), tn.all())
            tt(k, "pool", tn.ap, tn.ap, sqo.ap, ALU.mult, tn.all() + sqo.all(), tn.all())
            s0 = ctok - HALO0
            tt(k, "pool", P.oretT.ap[:, :, s0:s0 + 128], tn.ap.rearrange("p (h e) -> p h e", h=4), sg_tm.ap[:, :, cs],
               ALU.mult, tn.all() + sg_tm.all(), P.oretT.all())

        for cc in range(5):
            if cc < 4:
                stage_a(cc)
            if cc >= 1:
                stage_b(cc - 1)
            if nxt is not None and cc < 4:
                ln_stt(nxt, 2 * cc)
                ln_stt(nxt, 2 * cc + 1)
                if cc == 1:
                    ropes(nxt, 0)
                if cc == 2:
                    ropes(nxt, 1)
    S.merge_keys([("ps", "7a"), ("ps", "7b")], psk[7])
    if k.stage == "A1":
        mm(k, ps[0][:, 0:128], c.ones.ap, c.ones.ap, True, True, c.ones.all(), [psk[0]])
    if k.dbg and k.stage == "A1":
        dump(k, "ckvn", P.ckvn.ap, [128, NTOK], P.ckvn.all(), BF16)
        dump(k, "krope", P.krope.ap[64:96, :], [32, NTOK], P.krope.all(), BF16)
        dump(k, "cqn", P.cqn.ap, [128, 2, NST], P.cqn.all(), BF16)
        dump(k, "oretT", P.oretT.ap, [128, 4, NST], P.oretT.all(), BF16)
    if not xt_released:
        S.release(*xt)
    S.release(w_in, w_krot, *posi, *hTs, rstd, ta, tb, tc, cos_r, sin_r, cos_m, sin_m, sql, rstl, *raw, *t1,
              *t2, rqT, rkT, v_tm, sg_tm, kdec, PT, qsT, S_f, S_b, sqo, tn, *ogs, oT, sqT)


HB = 11 * 128
W1_ORDER = [(0, 0, 0), (0, 0, 1), (0, 1, 0), (0, 1, 1), (1, 0, 0), (1, 0, 1), (1, 1, 0), (1, 1, 1)]


def load_w1_pieces(k, allow_fail):
    S, d, P = k.S, k.d, k.P
    if not hasattr(P, "w1p"):
        P.w1p = {}
    wv = d["w_ffn_in"].rearrange("(c p) n -> p c n", p=128)
    for pc in W1_ORDER:
        if pc in P.w1p:
            continue
        up, jh, ch = pc
        try:
            buf = S.alloc(f"w1_{up}{jh}{ch}", [128, 4, HB], BF16)
        except MemoryError:
            if allow_fail:
                return
            raise
        P.w1p[pc] = buf
        a = up * DFF + jh * HB
        S.dma("pool", buf.ap, wv[:, 4 * ch:4 * ch + 4, a:a + HB], W=buf.all(), key=f"w1_{up}{jh}{ch}")


def w1_slice(k, gate, j, ci):
    pc = (0 if gate else 1, 0 if j < 11 else 1, ci // 4)
    jj = j if j < 11 else j - 11
    buf = k.P.w1p[pc]
    return buf.ap[:, ci % 4, jj * 128:(jj + 1) * 128], buf.all()


def phase_MKV(k):
    S, d, c, P, ps, psk = k.S, k.d, k.c, k.P, k.ps, k.psk
    w_xkv = P.w_xkv
    mt = S.alloc("memT", [128, 8, MEM], F32)
    S.dma("sp", mt.ap, d["memT"].rearrange("(c p) t -> p c t", p=128), W=mt.all(), key="memT")
    hm = S.alloc("hmem", [128, 8, MEM], BF16)
    sd, rstd = S.alloc("sdm", [128, MEM], F32), S.alloc("rstdm", [128, MEM], F32)
    squares8(k, hm, mt, MEM)
    rms_stats(k, lambda i: hm.ap[:, i, :], 8, D, 0, sd, rstd, MEM, hm.all())
    for ci in range(8):
        stt(k, hm.ap[:, ci, :], mt.ap[:, ci, :], c.gv.ap[:, c.g_mem + ci:c.g_mem + ci + 1], rstd.ap, ALU.mult, ALU.mult,
            mt.all() + rstd.all() + c.gv.all(), hm.all())
    for blk in range(8):
        pi = 1 + blk % 2
        for ci in range(8):
            mm(k, ps[pi][:, 0:MEM], w_xkv.ap[:, ci, blk * 128:(blk + 1) * 128], hm.ap[:, ci, :], ci == 0, ci == 7,
               w_xkv.all() + hm.all(), [psk[pi]])
        cp(k, "act", P.memKT.ap[:, blk, :], ps[pi][:, 0:MEM], [psk[pi]], P.memKT.all())
    n = 0
    for kb2 in range(2):
        for half in range(2):
            pi = 3 + n % 2
            n += 1
            for ci in range(8):
                mm(k, ps[pi], hm.ap[:, ci, kb2 * 128:(kb2 + 1) * 128], w_xkv.ap[:, ci, D + half * 512:D + (half + 1) * 512],
                   ci == 0, ci == 7, w_xkv.all() + hm.all(), [psk[pi]])
            cp(k, "dve", P.memV.ap[:, kb2, half * 512:(half + 1) * 512], ps[pi], [psk[pi]], P.memV.all())
    S.release(w_xkv, mt, hm, sd, rstd)


def phase_A2(k):
    S, d, c, P, ps, psk = k.S, k.d, k.c, k.P, k.ps, k.psk
    ps2 = k.ps2
    w_uq = S.alloc("w_uq_bf", [128, 2, 768], BF16)
    w_uqr = S.alloc("w_uqr_bf", [128, 2, 768], BF16)
    w_ukv = S.alloc("w_ukv_bf", [128, 1024], BF16)
    S.dma("pool", w_uq.ap, d["w_uq"].rearrange("(c p) n -> p c n", p=128), W=w_uq.all(), key="w_uq")
    S.dma("pool", w_ukv.ap, d["w_ukv"], W=w_ukv.all(), key="w_ukv")
    S.dma("pool", P.w_out.ap, d["w_out"].rearrange("(c p) n -> p c n", p=128), W=P.w_out.all(), key="w_out")
    S.dma("pool", P.w_xq.ap, d["w_xq"].rearrange("(c p) n -> p c n", p=128), W=P.w_xq.all(), key="w_xq")
    S.op("pool", lambda e: e.memset(w_uqr.ap, 0.0), W=w_uqr.all())
    q4 = w_uq.ap.rearrange("p c (h x) -> p c h x", h=8)
    r4 = w_uqr.ap.rearrange("p c (h x) -> p c h x", h=8)
    for ci in range(2):
        ts(k, "pool", r4[:, ci, :, 64:80], q4[:, ci, :, 80:96], -1.0, None, ALU.mult, None, w_uq.all(), w_uqr.all())
        cp(k, "pool", r4[:, ci, :, 80:96], q4[:, ci, :, 64:80], w_uq.all(), w_uqr.all())
    KT = S.alloc("KT", [128, 4, NTOK], BF16, nsub=4)
    Vc = S.alloc("Vc", [128, 32, 384], BF16)
    S.op("pool", lambda e: e.memset(Vc.ap, 1.0), W=Vc.all())
    for o in (64, 256):
        ts(k, "dve", Vc.ap[:, 0:16, o:o + 64], Vc.ap[:, 0:16, o:o + 64], c.flags.ap[:, 0:1], None, ALU.mult, None,
           Vc.all() + c.flags.all(), Vc.all())
    qTb = [[S.alloc(f"qT{b_}_{i}", [128, 512], BF16) for i in range(4)] for b_ in range(2)]
    for q_ in qTb[0] + qTb[1]:
        S.op("pool", lambda e, q_=q_: e.memset(q_.ap[96:128, :], 0.0), W=q_.all())
    S.op("pool", lambda e: e.memset(KT.ap[96:128, :, :], 0.0), W=KT.all())
    PTb = [S.alloc(f"PTb{i}", [128, 1024], BF16) for i in range(2)]
    tq1, tq2 = S.alloc("tq1", [128, 512], F32), S.alloc("tq2", [128, 512], F32)
    ta, tb, tc = (S.alloc(n, [128, 512], F32) for n in ("ta2", "tb2", "tc2"))
    cos_m, sin_m = S.alloc("cos_m2", [128, 512], F32), S.alloc("sin_m2", [128, 512], F32)
    pq = S.alloc("posq", [128, 512], I32)
    rec = S.alloc("rec", [128, 512], F32)
    wk3 = w_ukv.ap.rearrange("p (h x) -> p h x", h=8)
    scale = 1.0 / math.sqrt(96.0)
    n_ev = 0
    for hh in range(2):
        heads = list(range(4 * hh, 4 * hh + 4))
        for kt in range(NTOK // 512):
            for hi, h in enumerate(heads):
                pi = n_ev % 2
                mm(k, ps[pi], w_ukv.ap[:, h * 128:h * 128 + 128], P.ckvn.ap[:, kt * 512:(kt + 1) * 512], True, True,
                   w_ukv.all() + P.ckvn.all(), [psk[pi]])
                cp(k, "act", KT.ap[0:64, hi, kt * 512:(kt + 1) * 512], ps[pi][0:64, :], [psk[pi]],
                   [KT.k(hi)])
                n_ev += 1
        for hi in range(4):
            S.dma("sp", KT.ap[64:96, hi, :], P.krope.ap[64:96, :], R=P.krope.all(), W=[KT.k(hi)], key=f"kr{hi}")
        for kb in range(NTOK // 128):
            pi = 2 + kb % 2
            mm(k, ps[pi], P.ckvn.ap[:, kb * 128:(kb + 1) * 128], w_ukv.ap[:, 512 * hh:512 * (hh + 1)], True, True,
               w_ukv.all() + P.ckvn.all(), [psk[pi]])
            src = ps[pi].rearrange("p (a m x) -> p a m x", a=2, m=2)
            dst = Vc.ap[:, kb, :].rearrange("p (a r) -> p a r", a=2)
            cp(k, "dve", dst[:, :, 0:64], src[:, :, 0, 64:128], [psk[pi]], Vc.all())
            cp(k, "dve", dst[:, :, 128:192], src[:, :, 1, 64:128], [psk[pi]], Vc.all())
        def qtile(qi):
            if qi == 0:
                return 0, 128, HALO0 // 128
            return 128 + 512 * (qi - 1), 512, NPRE // 128 + 4 * (qi - 1)

        def q_assemble(qi):
            s0, NQ, qblk0 = qtile(qi)
            qTs = qTb[qi % 2]
            g0 = HALO0 + s0
            S.dma("sp", pq.ap[:, 0:NQ], d["posi"][:, g0:g0 + NQ].partition_broadcast(128), W=pq.all(), key="posq")
            rope_tables(k, pq.ap, pq.k(), (64, 96), 1, cos_m, sin_m, (ta, tb, tc), NQ)
            for hi, h in enumerate(heads):
                for ci in range(2):
                    mm(k, ps[0][0:96, 0:NQ], w_uq.ap[:, ci, h * 96:(h + 1) * 96], P.cqn.ap[:, ci, s0:s0 + NQ], ci == 0, ci == 1,
                       w_uq.all() + P.cqn.all(), [psk[0]])
                for ci in range(2):
                    mm(k, ps[1][0:96, 0:NQ], w_uqr.ap[:, ci, h * 96:(h + 1) * 96], P.cqn.ap[:, ci, s0:s0 + NQ], ci == 0, ci == 1,
                       w_uqr.all() + P.cqn.all(), [psk[1]])
                cp(k, "dve", qTs[hi].ap[0:64, 0:NQ], ps[0][0:64, 0:NQ], [psk[0]], qTs[hi].all())
                tt(k, "dve", tq1.ap[64:96, 0:NQ], ps[0][64:96, 0:NQ], cos_m.ap[64:96, 0:NQ], ALU.mult, [psk[0]] + cos_m.all(),
                   tq1.all())
                tt(k, "dve", tq2.ap[64:96, 0:NQ], ps[1][64:96, 0:NQ], sin_m.ap[64:96, 0:NQ], ALU.mult, [psk[1]] + sin_m.all(),
                   tq2.all())
                tt(k, "pool", qTs[hi].ap[64:96, 0:NQ], tq1.ap[64:96, 0:NQ], tq2.ap[64:96, 0:NQ], ALU.add, tq1.all() + tq2.all(),
                   qTs[hi].all())

        q_assemble(0)
        for qi in range(5):
            s0, NQ, qblk0 = qtile(qi)
            nqb = NQ // 128
            qT = qTb[qi % 2]
            if qi + 1 < 5:
                q_assemble(qi + 1)
            for hi, h in enumerate(heads):
                pair, mem = divmod(hi, 2)
                vcol0 = pair * 192 + mem * 64
                pob = 6 + hi % 2
                po = ps[pob]
                nkb = qblk0 + nqb
                groups = []
                kb = 0
                while kb < nkb:
                    if kb + 1 < qblk0:
                        groups.append((kb, kb + 1))
                        kb += 2
                    else:
                        groups.append((kb,))
                        kb += 1
                pend = None

                def pv(pd, nkb=nkb, po=po, pob=pob, vcol0=vcol0, NQ=NQ):
                    for (kb_, qlo_, n_, ptap_, ptk_) in pd:
                        mm(k, po[:, qlo_:NQ], Vc.ap[:, kb_, vcol0:vcol0 + 128], ptap_, kb_ == 0, kb_ == nkb - 1,
                           Vc.all() + ptk_, [psk[pob]])

                for gi, grp in enumerate(groups):
                    slot = gi % 2
                    pt = PTb[slot]
                    banks = (2 + 2 * slot, 3 + 2 * slot)
                    cur = []
                    for j_, kb in enumerate(grp):
                        r = kb - qblk0
                        q_lo = max(r, 0) * 128
                        n = NQ - q_lo
                        sb = banks[j_]
                        mm(k, ps[sb][:, 0:n], KT.ap[:, hi, kb * 128:(kb + 1) * 128], qT[hi].ap[:, q_lo:NQ], True, True,
                           [KT.k(hi)] + qT[hi].all(), [psk[sb]])
                        cur.append((kb, q_lo, n, pt.ap[:, j_ * 512:j_ * 512 + n], pt.all()))
                    if len(grp) == 2 and NQ == 512:
                        act(k, pt.ap, ps2[1 + slot], AF.Exp, [psk[banks[0]], psk[banks[1]]], pt.all(), scale=scale)
                    else:
                        for j_, (kb, q_lo, n, ptap, _) in enumerate(cur):
                            act(k, ptap, ps[banks[j_]][:, 0:n], AF.Exp, [psk[banks[j_]]], pt.all(), scale=scale)
                            if kb - qblk0 >= 0:
                                tt(k, "pool", ptap[:, 0:128], ptap[:, 0:128], c.causal.ap, ALU.mult, pt.all() + c.causal.all(),
                                   pt.all())
                    if pend is not None:
                        pv(pend)
                    pend = cur
                pv(pend)
                if mem == 0:
                    o_rows, s_rows = slice(0, 64), slice(64, 128)
                else:
                    o_rows, s_rows = slice(64, 128), slice(0, 64)
                ts(k, "dve", rec.ap[o_rows, 0:NQ], po[s_rows, 0:NQ], 1e-30, None, ALU.add, None, [psk[pob]], rec.all())
                recip(k, rec.ap[o_rows, 0:NQ], rec.ap[o_rows, 0:NQ], rec.all(), rec.all())
                tt(k, "dve", P.omlaT.ap[o_rows, 2 * hh + pair, s0:s0 + NQ], po[o_rows, 0:NQ], rec.ap[o_rows, 0:NQ], ALU.mult,
                   [psk[pob]] + rec.all(), P.omlaT.all())
    if k.dbg and k.stage == "A2":
        dump(k, "omlaT", P.omlaT.ap, [128, 4, NST], P.omlaT.all(), BF16)
    S.release(w_uq, w_uqr, w_ukv, KT, Vc, *qTb[0], *qTb[1], *PTb, tq1, tq2, ta, tb, tc, cos_m, sin_m, pq, rec)


def _norm_to_hT(k, xb, hT, sd, rstd, gcol, n):
    c = k.c
    squares8(k, hT, xb, n)
    rms_stats(k, lambda i: hT.ap[:, i, 0:n], 8, D, 0, sd, rstd, n, hT.all())
    for ci in range(8):
        stt(k, hT.ap[:, ci, 0:n], xb.ap[:, ci, 0:n], c.gv.ap[:, gcol + ci:gcol + ci + 1], rstd.ap[:, 0:n], ALU.mult, ALU.mult,
            xb.all() + rstd.all() + c.gv.all(), hT.all())


def phase_A3X(k):
    S, d, c, P, ps, psk = k.S, k.d, k.c, k.P, k.ps, k.psk
    xbs = [S.alloc(f"xa{i}", [128, 8, 512], F32) for i in range(2)]
    hT = S.alloc("hTa", [128, 8, 512], BF16)
    rstd = S.alloc("rstda", [128, 512], F32)
    sd = rstd
    qxT = S.alloc("qxT", [128, 8, 512], BF16, nsub=8)
    PTx = [S.alloc(f"PTx{i}", [128, 512], BF16) for i in range(2)]
    oxT = S.alloc("oxT", [128, 8, 512], BF16)
    rec = S.alloc("recx", [128, 512], F32)
    xTv = d["xT"].rearrange("(c p) t -> p c t", p=128)
    x2v = d["x2s"].rearrange("(c p) t -> p c t", p=128)
    tiles = [(0, 128)] + [(128 + 512 * i, 512) for i in range(4)]
    nt = len(tiles)

    def xload(ti_):
        s0_, N_ = tiles[ti_]
        S.dma("sp", xbs[ti_ % 2].ap[:, :, 0:N_], xTv[:, :, HALO0 + s0_:HALO0 + s0_ + N_], W=xbs[ti_ % 2].all(), key=f"xa{ti_ % 2}")

    def stage_w(ti_):
        s0, N = tiles[ti_]
        xb = xbs[ti_ % 2]
        for cb in range(8):
            pi = 1 + cb % 2
            cs = slice(cb * 128, (cb + 1) * 128)
            for j in range(4):
                mm(k, ps[pi][:, 0:N], P.w_out.ap[:, j, cs], P.omlaT.ap[:, j, s0:s0 + N], j == 0, False,
                   P.w_out.all() + P.omlaT.all(), [psk[pi]])
            for j in range(4):
                mm(k, ps[pi][:, 0:N], P.w_out.ap[:, 4 + j, cs], P.oretT.ap[:, j, s0:s0 + N], False, j == 3,
                   P.w_out.all() + P.oretT.all(), [psk[pi]])
            tt(k, "dve", xb.ap[:, cb, 0:N], xb.ap[:, cb, 0:N], ps[pi][:, 0:N], ALU.add, xb.all() + [psk[pi]], xb.all())
        if k.dbg and k.stage == "A3X":
            dumpx(k, "x1", xb, s0, N)

    def stage_n1(ti_):
        s0, N = tiles[ti_]
        xb = xbs[ti_ % 2]
        squares8(k, hT, xb, N)
        rms_stats(k, lambda i: hT.ap[:, i, 0:N], 8, D, 0, sd, rstd, N, hT.all())

    def stage_n2(ti_, ci):
        s0, N = tiles[ti_]
        xb = xbs[ti_ % 2]
        stt(k, hT.ap[:, ci, 0:N], xb.ap[:, ci, 0:N], c.gv.ap[:, c.g_xattn + ci:c.g_xattn + ci + 1], rstd.ap[:, 0:N], ALU.mult,
            ALU.mult, xb.all() + rstd.all() + c.gv.all(), hT.all())

    xload(0)
    xload(1)
    stage_w(0)
    stage_n1(0)
    for ci in range(8):
        stage_n2(0, ci)
    for ti_, (s0, N) in enumerate(tiles):
        xb = xbs[ti_ % 2]
        for blk in range(8):
            pi = 1 + blk % 2
            for ci in range(8):
                mm(k, ps[pi][:, 0:N], P.w_xq.ap[:, ci, blk * 128:(blk + 1) * 128], hT.ap[:, ci, 0:N], ci == 0, ci == 7,
                   P.w_xq.all() + hT.all(), [psk[pi]])
            cp(k, "act", qxT.ap[:, blk, 0:N], ps[pi][:, 0:N], [psk[pi]], [qxT.k(blk)])
        if ti_ + 1 < nt:
            stage_w(ti_ + 1)
            stage_n1(ti_ + 1)
            if ti_ + 1 == nt - 1 and k.stage != "A3X":
                S.release(P.oretT, P.omlaT, P.w_out)
                k.a3x_early = True
                load_w1_pieces(k, True)
        for h in range(4):
            for kb2 in range(2):
                sb = 3 + kb2
                for dc in range(2):
                    mm(k, ps[sb][:, 0:N], P.memKT.ap[:, 2 * h + dc, kb2 * 128:(kb2 + 1) * 128], qxT.ap[:, 2 * h + dc, 0:N],
                       dc == 0, dc == 1, P.memKT.all() + [qxT.k(2 * h + dc)], [psk[sb]])
                act(k, PTx[kb2].ap[:, 0:N], ps[sb][:, 0:N], AF.Exp, [psk[sb]], PTx[kb2].all(), scale=1.0 / 16.0)
            for kb2 in range(2):
                mm(k, ps[5][:, 0:N], c.ones.ap, PTx[kb2].ap[:, 0:N], kb2 == 0, kb2 == 1, c.ones.all() + PTx[kb2].all(), [psk[5]])
            act(k, rec.ap[:, 0:N], ps[5][:, 0:N], AF.Ln, [psk[5]], rec.all())
            act(k, rec.ap[:, 0:N], rec.ap[:, 0:N], AF.Exp, rec.all(), rec.all(), scale=-1.0)
            for eb in range(2):
                pi = 6 + eb
                for kb2 in range(2):
                    mm(k, ps[pi][:, 0:N], P.memV.ap[:, kb2, h * 256 + eb * 128:h * 256 + (eb + 1) * 128], PTx[kb2].ap[:, 0:N],
                       kb2 == 0, kb2 == 1, P.memV.all() + PTx[kb2].all(), [psk[pi]])
                tt(k, "dve", oxT.ap[:, 2 * h + eb, 0:N], ps[pi][:, 0:N], rec.ap[:, 0:N], ALU.mult, [psk[pi]] + rec.all(), oxT.all())
            if ti_ + 1 < nt:
                stage_n2(ti_ + 1, 2 * h)
                stage_n2(ti_ + 1, 2 * h + 1)
        for cb in range(8):
            pi = 1 + cb % 2
            for j in range(8):
                mm(k, ps[pi][:, 0:N], P.w_xo.ap[:, j, cb * 128:(cb + 1) * 128], oxT.ap[:, j, 0:N], j == 0, j == 7,
                   P.w_xo.all() + oxT.all(), [psk[pi]])
            tt(k, "dve", xb.ap[:, cb, 0:N], xb.ap[:, cb, 0:N], ps[pi][:, 0:N], ALU.add, xb.all() + [psk[pi]], xb.all())
        S.dma("sp", x2v[:, :, s0:s0 + N], xb.ap[:, :, 0:N], R=xb.all(), W=[("x2s", s0)], key=f"xs{ti_ % 2}")
        if k.dbg and k.stage == "A3X":
            dumpx(k, "x2", xb, s0, N)
        if ti_ + 2 < nt:
            xload(ti_ + 2)
    S.release(*xbs, hT, rstd, qxT, *PTx, oxT, rec)


def dumpx(k, name, xb, s0, N):
    if name not in k.dbg_out:
        k.dbg_out[name] = k.nc.dram_tensor("dbg_" + name, [D, NST], F32, kind="ExternalOutput").ap()
    t = k.dbg_out[name].rearrange("(c p) t -> p c t", p=128)
    k.S.dma("sp", t[:, :, s0:s0 + N], xb.ap[:, :, 0:N], R=xb.all(), W=[("dbg", name, s0)], key="dbg")


def phase_F(k):
    S, d, c, P, ps, psk = k.S, k.d, k.c, k.P, k.ps, k.psk
    xf = S.alloc("xf", [128, 8, 512], F32)
    hT = S.alloc("hTf", [128, 8, 512], BF16)
    sd, rstd = S.alloc("sdf", [128, 512], F32), S.alloc("rstdf", [128, 512], F32)
    aT = S.alloc("aT", [128, NFB, 512], BF16, nsub=NFB)
    gsb = [S.alloc(f"gsb{i}", [128, 48 + 512], F32) for i in range(2)]
    cv = [S.alloc(f"cv{i}", [128, 512], F32) for i in range(2)]
    ghalo = S.alloc("ghalo", [128, NFB, 48], F32, nsub=NFB)
    x2v = d["x2s"].rearrange("(c p) t -> p c t", p=128)
    outv = d["outT"].rearrange("(c p) t -> p c t", p=128)
    ostg = S.arena[0:128, aT.off:aT.off + 8 * 512 * 4].bitcast(F32).rearrange("p (a b) -> p a b", a=8)
    S.dma("sp", xf.ap[:, :, 0:128], x2v[:, :, 0:128], R=[("x2s", 0)], W=xf.all(), key="xf")
    _norm_to_hT(k, xf, hT, sd, rstd, c.g_ffn, 128)
    for j in range(NFB):
        pi = 1 + j % 2
        for ci in range(8):
            wl, wk = w1_slice(k, True, j, ci)
            mm(k, ps[pi][:, 0:128], wl, hT.ap[:, ci, 0:128], ci == 0, ci == 7, wk + hT.all(), [psk[pi]])
        ts(k, "dve", ghalo.ap[:, j, :], ps[pi][:, 80:128], c.flags.ap[:, 1:2], None, ALU.mult, None, [psk[pi]] + c.flags.all(),
           [ghalo.k(j)])
    for ti in range(4):
        s0 = 128 + 512 * ti
        S.dma("sp", xf.ap, x2v[:, :, s0:s0 + 512], R=[("x2s", s0)], W=xf.all(), key="xf")
        _norm_to_hT(k, xf, hT, sd, rstd, c.g_ffn, 512)
        for j in range(NFB):
            pg, pu = 1 + (j % 2), (3, 4, 7)[j % 3]
            for ci in range(8):
                wl, wk = w1_slice(k, True, j, ci)
                mm(k, ps[pg], wl, hT.ap[:, ci, :], ci == 0, ci == 7, wk + hT.all(), [psk[pg]])
            for ci in range(8):
                wl, wk = w1_slice(k, False, j, ci)
                mm(k, ps[pu], wl, hT.ap[:, ci, :], ci == 0, ci == 7, wk + hT.all(), [psk[pu]])
            g, cvb = gsb[j % 2], cv[j % 2]
            cw = c.convp.ap
            cp(k, "act", g.ap[:, 48:560], ps[pg], [psk[pg]], g.all())
            cp(k, "pool", g.ap[:, 0:48], ghalo.ap[:, j, :], [ghalo.k(j)], g.all())
            k.S.op("act", lambda e, g=g, cvb=cvb, j=j: e.activation(out=cvb.ap, in_=g.ap[:, 48:560], func=AF.Identity,
                                                                     scale=cw[:, j, 2:3], bias=cw[:, j, 3:4]),
                   g.all() + c.convp.all(), cvb.all())
            stt(k, cvb.ap, g.ap[:, 47:559], cw[:, j, 1:2], cvb.ap, ALU.mult, ALU.add, g.all() + cvb.all() + c.convp.all(), cvb.all())
            stt(k, cvb.ap, g.ap[:, 46:558], cw[:, j, 0:1], cvb.ap, ALU.mult, ALU.add, g.all() + cvb.all() + c.convp.all(), cvb.all())
            cp(k, "pool", ghalo.ap[:, j, :], g.ap[:, 512:560], g.all(), [ghalo.k(j)])
            act(k, cvb.ap, cvb.ap, AF.Silu, cvb.all(), cvb.all())
            tt(k, "dve", aT.ap[:, j, :], cvb.ap, ps[pu], ALU.mult, cvb.all() + [psk[pu]], [aT.k(j)])
        for cb in range(8):
            pi = 5 + cb % 2
            for j in range(NFB):
                mm(k, ps[pi], P.w_ffn_out.ap[:, j, cb * 128:(cb + 1) * 128], aT.ap[:, j, :], j == 0, j == NFB - 1,
                   P.w_ffn_out.all() + [aT.k(j)], [psk[pi]])
            tt(k, "dve", xf.ap[:, cb, :], xf.ap[:, cb, :], ps[pi], ALU.add, xf.all() + [psk[pi]], xf.all())
        squares8(k, hT, xf, 512)
        rms_stats(k, lambda i: hT.ap[:, i, :], 8, D, 0, sd, rstd, 512, hT.all())
        for cb in range(8):
            stt(k, ostg[:, cb, :], xf.ap[:, cb, :], c.gv.ap[:, c.g_final + cb:c.g_final + cb + 1], rstd.ap, ALU.mult, ALU.mult,
                xf.all() + rstd.all() + c.gv.all(), aT.all())
        S.dma("sp", outv[:, :, 512 * ti:512 * (ti + 1)], ostg, R=aT.all(), W=[("out", ti)], key="out")
    S.release(xf, hT, sd, rstd, aT, *gsb, *cv, ghalo, *P.w1p.values())


def _consts():
    f32 = np.float32
    H, L = 4, 128
    log_gamma = np.log(f32(1.0) - f32(2.0) ** (f32(-5.0) - np.arange(H, dtype=f32))).astype(f32)
    j = np.arange(L, dtype=f32)
    diff = j[:, None] - j[None, :]
    intra = np.where(diff[None] >= 0, np.exp(np.maximum(diff, 0.0)[None] * log_gamma[:, None, None]), 0.0).astype(f32)
    k_to_end = np.exp((L - 1 - j)[:, None] * log_gamma[None, :]).astype(f32)
    q_from_start = np.exp((j + 1)[:, None] * log_gamma[None, :]).astype(f32)
    chunk_decay = np.exp(f32(L) * log_gamma).astype(f32)
    dk = f32(128.0 ** -0.5)
    c_intra = np.zeros((128, 512), f32)
    for h in range(H):
        c_intra[:, h * 128:(h + 1) * 128] = intra[h].T * dk
    c_qfs = np.zeros((128, 512), f32)
    c_decay = np.zeros((128, 512), f32)
    for h in range(H):
        c_qfs[:, h * 128:(h + 1) * 128] = q_from_start[:, h][None, :]
        c_decay[:, h * 128:(h + 1) * 128] = chunk_decay[h]
    c_small = np.zeros((128, 8), f32)
    invf_r = (1.0 / (f32(10000.0) ** (np.arange(0, 128, 2, dtype=f32) / f32(128)))).astype(f32)
    invf_m = (1.0 / (f32(10000.0) ** (np.arange(0, 32, 2, dtype=f32) / f32(32)))).astype(f32)
    p = np.arange(128)
    c_small[:, 0] = invf_r[p % 64]
    c_small[:, 1] = invf_m[p % 16]
    c_small[:, 2:6] = k_to_end * dk
    c_ident = np.eye(128, dtype=f32)
    c_rot = np.zeros((128, 128), f32)
    for m in range(64):
        c_rot[m + 64, m] = -1.0
    for m in range(64, 128):
        c_rot[m - 64, m] = 1.0
    kk = np.arange(128)
    c_causal = (kk[None, :] >= kk[:, None]).astype(f32)
    return dict(c_small=c_small, c_ident=c_ident, c_rot=c_rot, c_causal=c_causal, c_intra=c_intra, c_qfs=c_qfs,
                c_decay=c_decay)


def make_in_maps(inputs):
    f32 = np.float32
    x = np.asarray(inputs["x"], f32)
    mem = np.asarray(inputs["mem"], f32)
    pos = np.asarray(inputs["positions"], np.int32)

    def col(g):
        g = np.asarray(g, f32).reshape(-1, 128)
        return np.ascontiguousarray(g.T)

    gv = np.concatenate([col(inputs["g_mix"][0]), col(inputs["g_xattn"][0]), col(inputs["g_mem"][0]),
                         col(inputs["g_ffn"][0]), col(inputs["g_final"]), col(inputs["g_q_lat"][0]),
                         col(inputs["g_kv_lat"][0])], axis=1)
    cw = np.asarray(inputs["conv_w"][0], f32)
    cb = np.asarray(inputs["conv_b"][0], f32)
    convp = np.zeros((128, NFB, 4), f32)
    for i in range(3):
        convp[:, :, i] = cw[i].reshape(NFB, 128).T
    convp[:, :, 3] = cb.reshape(NFB, 128).T
    shared = dict(
        w_in=np.ascontiguousarray(inputs["w_in"][0], f32), w_uq=np.ascontiguousarray(inputs["w_uq"][0], f32),
        w_ukv=np.ascontiguousarray(inputs["w_ukv"][0], f32), w_out=np.ascontiguousarray(inputs["w_out"][0], f32),
        w_xq=np.ascontiguousarray(inputs["w_xq"][0], f32), w_xkv=np.ascontiguousarray(inputs["w_xkv"][0], f32),
        w_xo=np.ascontiguousarray(inputs["w_xo"][0], f32), w_ffn_in=np.ascontiguousarray(inputs["w_ffn_in"][0], f32),
        w_ffn_out=np.ascontiguousarray(inputs["w_ffn_out"][0], f32), gv=np.ascontiguousarray(gv),
        convp=np.ascontiguousarray(convp.reshape(128, NFB * 4)), **_consts())
    maps = []
    for core in range(8):
        b, hf = core // 2, core % 2
        xT = np.zeros((D, NTOK), f32)
        pp = np.zeros((1, NTOK), np.int32)
        if hf == 0:
            xT[:, NPRE:] = x[b, :NOWN].T
            pp[0, NPRE:] = pos[b, :NOWN]
        else:
            xT[:, :] = x[b].T
            pp[0, :] = pos[b]
        flags = np.full((128, 2), float(hf), f32)
        m = dict(shared)
        m.update(xT=xT, posi=pp, memT=np.ascontiguousarray(mem[b].T), flags=flags)
        maps.append(m)
    return maps


_CACHE = {}


def kernel(**inputs):
    if "nc" not in _CACHE:
        _CACHE["nc"] = build("full")[0]
    nc = _CACHE["nc"]
    maps = make_in_maps(inputs)
    res = run_bass_kernel_spmd(nc, maps, core_ids=list(range(8)))
    out = np.zeros((NB, SEQ, D), np.float32)
    for core in range(8):
        b, hf = core // 2, core % 2
        out[b, hf * NOWN:(hf + 1) * NOWN, :] = res.results[core]["outT"].T
    return out
```

```python
import math
from contextlib import ExitStack

import numpy as np
import concourse.bass as bass
import concourse.mybir as mybir
from concourse.bass_utils import run_bass_kernel_spmd

F32 = mybir.dt.float32
BF16 = mybir.dt.bfloat16
I32 = mybir.dt.int32
U8 = mybir.dt.uint8
AF = mybir.ActivationFunctionType
ALU = mybir.AluOpType
AX = mybir.AxisListType

D = 1024
SEQ = 4096
NB = 4
EPS = 1e-6
NPRE = 2048
NOWN = 2048
NTOK = NPRE + NOWN
HALO0 = NPRE - 128
NST = NOWN + 128
IN_COLS = 2464
OFF_CQ, OFF_CKV, OFF_KR, OFF_RQ, OFF_RK, OFF_RV, OFF_RG = 0, 256, 384, 416, 928, 1440, 1952
DFF = 2816
NFB = DFF // 128
MEM = 256
TWO_PI = 2.0 * math.pi
C1 = 6.28125
C2 = TWO_PI - C1
MAGIC = 12582912.0
PI_LO = 3.1415925
ARENA_BYTES = 206 * 1024
ENGS = ("pe", "act", "dve", "pool", "sp")
NDMA_MAX = 90
DT_SIZE = {F32: 4, BF16: 2, I32: 4}


class Buf:
    def __init__(self, uid, name, off, nbytes, ap, nsub):
        self.uid, self.name, self.off, self.nbytes, self.ap, self.nsub = uid, name, off, nbytes, ap, nsub

    def k(self, i=0):
        return (self.uid, i)

    def all(self):
        return [(self.uid, i) for i in range(self.nsub)]

    def __getitem__(self, idx):
        return self.ap[idx]


class Sched:
    def __init__(self, nc, es):
        self.nc, self.es = nc, es
        self.prog = {e: [] for e in ENGS}
        self.sem = {e: es.enter_context(nc.semaphore("s_" + e)) for e in ENGS if e != "sp"}
        self.cnt = {e: 0 for e in ENGS}
        self.seen = {e: {} for e in ENGS}
        self.drained = {e: 0 for e in ENGS}
        self.needed = {e: set() for e in ENGS}
        self.lastw, self.readers = {}, {}
        self.ndma = 0
        self.dma_events = []
        self.dma_pool = [es.enter_context(nc.semaphore(f"d{i}")) for i in range(NDMA_MAX)]
        self.dma_sem = {}
        self.arena = es.enter_context(nc.sbuf_tensor("arena", [128, ARENA_BYTES], U8))
        self.free = [(0, ARENA_BYTES)]
        self.freed_events = []
        self.nbuf = 0
        self.peak = 0
        self.used = 0

    def alloc(self, name, shape, dtype, nsub=1):
        n = 1
        for s in shape[1:]:
            n *= s
        nbytes = (n * DT_SIZE[dtype] + 63) // 64 * 64
        for i, (o, sz) in enumerate(self.free):
            if sz >= nbytes:
                off = o
                if sz == nbytes:
                    self.free.pop(i)
                else:
                    self.free[i] = (o + nbytes, sz - nbytes)
                break
        else:
            raise MemoryError(f"arena full allocating {name} {nbytes}B; free={self.free}")
        ap = self.arena[0:shape[0], off:off + n * DT_SIZE[dtype]].bitcast(dtype)
        if len(shape) == 3:
            ap = ap.rearrange("p (a b) -> p a b", a=shape[1])
        elif len(shape) == 4:
            ap = ap.rearrange("p (a b c) -> p a b c", a=shape[1], b=shape[2])
        self.nbuf += 1
        b = Buf(self.nbuf, name, off, nbytes, ap, nsub)
        evs, keep = [], []
        for (fo, fn, fe) in self.freed_events:
            if fo < off + nbytes and off < fo + fn:
                evs.extend(fe)
            keep.append((fo, fn, fe))
        self.freed_events = keep
        if evs:
            for kk in b.all():
                self.readers[kk] = list(evs)
        self.used += nbytes
        self.peak = max(self.peak, self.used)
        return b

    def release(self, *bufs):
        for b in bufs:
            evs = []
            for kk in b.all():
                if kk in self.lastw:
                    evs.append(self.lastw.pop(kk))
                evs.extend(self.readers.pop(kk, []))
            best = {}
            for (s, v) in evs:
                best[s] = max(best.get(s, 0), v)
            self.freed_events.append((b.off, b.nbytes, list(best.items())))
            self.free.append((b.off, b.nbytes))
            self.free.sort()
            merged = []
            for (o, sz) in self.free:
                if merged and merged[-1][0] + merged[-1][1] == o:
                    merged[-1] = (merged[-1][0], merged[-1][1] + sz)
                else:
                    merged.append((o, sz))
            self.free = merged
            self.used -= b.nbytes

    def _deps(self, eng, R, W):
        best = {}
        for r in R:
            ev = self.lastw.get(r)
            if ev is not None:
                best[ev[0]] = max(best.get(ev[0], 0), ev[1])
        for w in W:
            ev = self.lastw.get(w)
            if ev is not None:
                best[ev[0]] = max(best.get(ev[0], 0), ev[1])
            for ev in self.readers.get(w, ()):
                best[ev[0]] = max(best.get(ev[0], 0), ev[1])
        for s, v in best.items():
            if s == eng:
                if eng == "pe":
                    continue
                if eng in ("act", "dve"):
                    if self.drained[eng] < v:
                        self.prog[eng].append(("drain",))
                        self.drained[eng] = self.cnt[eng]
                    continue
            if self.seen[eng].get(s, 0) >= v:
                continue
            self.seen[eng][s] = v
            self.prog[eng].append(("wait", s, v))
            if isinstance(s, str):
                self.needed[s].add(v)

    def _commit(self, ev, R, W):
        for w in W:
            self.lastw[w] = ev
            self.readers[w] = []
        for r in R:
            if r not in W:
                self.readers.setdefault(r, []).append(ev)

    def merge_keys(self, src_keys, dst_key):
        evs = []
        for kk in src_keys:
            if kk in self.lastw:
                evs.append(self.lastw[kk])
            evs.extend(self.readers.get(kk, []))
        self.readers.setdefault(dst_key, []).extend(evs)

    def op(self, eng, fn, R=(), W=()):
        R, W = list(R), list(W)
        self._deps(eng, R, W)
        self.cnt[eng] += 1
        ev = (eng, self.cnt[eng])
        self.prog[eng].append(("inst", fn, self.cnt[eng]))
        self._commit(ev, R, W)
        return ev

    def dma(self, eng, out, in_, R=(), W=(), key=None):
        R, W = list(R), list(W)
        self._deps(eng, R, W)
        if key is None:
            key = f"_auto{self.ndma}"
        if key not in self.dma_sem:
            self.dma_sem[key] = [self.dma_pool[len(self.dma_sem)], 0]
        ent = self.dma_sem[key]
        sem = ent[0]
        if ent[1] > 0 and self.seen[eng].get(sem, 0) < ent[1]:
            self.seen[eng][sem] = ent[1]
            self.prog[eng].append(("wait", sem, ent[1]))
        ent[1] += 16
        self.ndma += 1
        ev = (sem, ent[1])
        self.prog[eng].append(("dma", out, in_, sem))
        self._commit(ev, R, W)
        self.dma_events.append(ev)
        return ev

    def emit(self, block):
        nc = self.nc
        S = self

        rank = {en: {q: i + 1 for i, q in enumerate(sorted(S.needed[en]))} for en in ENGS}
        S.n_inc = {en: len(rank[en]) for en in ENGS}

        def run(e, name):
            for ent in S.prog[name]:
                if ent[0] == "wait":
                    s = ent[1]
                    if isinstance(s, str):
                        e.wait_ge(S.sem[s], rank[s][ent[2]])
                    else:
                        e.wait_ge(s, ent[2])
                elif ent[0] == "drain":
                    e.drain()
                elif ent[0] == "inst":
                    ins = ent[1](e)
                    if ent[2] in rank[name]:
                        ins.then_inc(S.sem[name], 1)
                else:
                    e.dma_start(out=ent[1], in_=ent[2]).then_inc(ent[3], 16)

        @block.tensor
        def _(e):
            run(e, "pe")

        @block.scalar
        def _(e):
            run(e, "act")

        @block.vector
        def _(e):
            run(e, "dve")

        @block.gpsimd
        def _(e):
            run(e, "pool")

        @block.sync
        def _(e):
            run(e, "sp")

    def final_wait(self, eng, events):
        for (s, v) in events:
            if self.seen[eng].get(s, 0) >= v:
                continue
            self.seen[eng][s] = v
            self.prog[eng].append(("wait", s, v))


class K:
    pass


def bc(ap, shape):
    return ap.broadcast_to(shape)


def build(stage="full", dbg=False, a1_tiles=None, a1_level=9):
    nc = bass.Bass("TRN2", target_bir_lowering=False)
    es = ExitStack()
    k = K()
    k.nc, k.es, k.stage, k.dbg = nc, es, stage, dbg
    k.a1_tiles, k.a1_level = a1_tiles, a1_level
    d = {}

    def din(name, shape, dt=F32):
        d[name] = nc.dram_tensor(name, list(shape), dt, kind="ExternalInput").ap()

    din("xT", [D, NTOK]); din("posi", [1, NTOK], I32); din("memT", [D, MEM]); din("flags", [128, 2])
    din("w_in", [D, IN_COLS]); din("w_uq", [256, 768]); din("w_ukv", [128, 1024]); din("w_out", [D, D])
    din("w_xq", [D, D]); din("w_xkv", [D, 2 * D]); din("w_xo", [D, D])
    din("w_ffn_in", [D, 2 * DFF]); din("w_ffn_out", [DFF, D])
    din("gv", [128, 43]); din("convp", [128, NFB * 4]); din("c_small", [128, 8])
    din("c_ident", [128, 128]); din("c_rot", [128, 128]); din("c_causal", [128, 128])
    din("c_intra", [128, 512]); din("c_qfs", [128, 512]); din("c_decay", [128, 512])
    d["outT"] = nc.dram_tensor("outT", [D, NOWN], F32, kind="ExternalOutput").ap()
    d["x2s"] = nc.dram_tensor("x2s", [D, NST], F32, kind="Internal").ap()
    k.dbg_out = {}
    k.d = d
    with es:
        S = Sched(nc, es)
        k.S = S
        big = [es.enter_context(nc.psum_tensor(f"psb{i}", [128, 1024], F32)) for i in range(4)]
        k.ps2 = [b_[:, :] for b_ in big]
        k.ps = [big[i // 2][:, (i % 2) * 512:(i % 2 + 1) * 512] for i in range(8)]
        k.psk = [("ps", i) for i in range(8)]
        block = es.enter_context(nc.Block())
        emit_all(k)
        S.final_wait("sp", S.dma_events)
        S.emit(block)
    k.peak = S.peak
    return nc, k


def mm(k, out, lhsT, rhs, start, stop, R, W):
    k.S.op("pe", lambda e: e.matmul(out, lhsT=lhsT, rhs=rhs, start=start, stop=stop), R, W)


def tr(k, out, in_, ident, R, W):
    k.S.op("pe", lambda e: e.transpose(out=out, in_=in_, identity=ident), R, W)


def act(k, out, in_, func, R, W, scale=1.0, bias=0.0):
    k.S.op("act", lambda e: e.activation(out=out, in_=in_, func=func, scale=scale, bias=bias), R, W)


def tt(k, eng, out, in0, in1, op, R, W):
    k.S.op(eng, lambda e: e.tensor_tensor(out=out, in0=in0, in1=in1, op=op), R, W)


def ts(k, eng, out, in0, s1, s2, op0, op1, R, W):
    if op1 is None:
        k.S.op(eng, lambda e: e.tensor_scalar(out=out, in0=in0, scalar1=s1, scalar2=None, op0=op0), R, W)
    else:
        k.S.op(eng, lambda e: e.tensor_scalar(out=out, in0=in0, scalar1=s1, scalar2=s2, op0=op0, op1=op1), R, W)


def stt(k, out, in0, scalar, in1, op0, op1, R, W):
    k.S.op("dve", lambda e: e.scalar_tensor_tensor(out=out, in0=in0, scalar=scalar, in1=in1, op0=op0, op1=op1), R, W)


def cp(k, eng, out, in_, R, W):
    if eng == "act":
        k.S.op("act", lambda e: e.copy(out=out, in_=in_), R, W)
    else:
        k.S.op(eng, lambda e: e.tensor_copy(out=out, in_=in_), R, W)


def recip(k, out, in_, R, W):
    k.S.op("dve", lambda e: e.reciprocal(out=out, in_=in_), R, W)


def dump(k, name, buf_ap, shape, R, dt=F32):
    t = k.nc.dram_tensor("dbg_" + name, list(shape), dt, kind="ExternalOutput").ap()
    k.dbg_out[name] = t
    k.S.dma("sp", t, buf_ap, R=R, W=[("dbg", name)], key="dbg")


def load_consts(k):
    S, d = k.S, k.d
    c = K()
    k.c = c
    c.gv = S.alloc("gv", [128, 43], F32)
    c.convp = S.alloc("convp", [128, NFB, 4], F32)
    c.small = S.alloc("c_small", [128, 8], F32)
    c.flags = S.alloc("flags", [128, 2], F32)
    c.ident = S.alloc("ident", [128, 128], BF16)
    c.rot = S.alloc("rot", [128, 128], BF16)
    c.causal = S.alloc("causal", [128, 128], BF16)
    c.intra = S.alloc("intra", [128, 512], F32)
    c.qfs = S.alloc("qfs", [128, 512], F32)
    c.decay = S.alloc("decay", [128, 512], F32)
    c.ones = S.alloc("ones", [128, 128], BF16)
    S.dma("sp", c.gv.ap, d["gv"], W=c.gv.all())
    S.dma("sp", c.convp.ap, d["convp"].rearrange("p (a b) -> p a b", a=NFB), W=c.convp.all())
    S.dma("sp", c.small.ap, d["c_small"], W=c.small.all())
    S.dma("sp", c.flags.ap, d["flags"], W=c.flags.all())
    S.dma("sp", c.intra.ap, d["c_intra"], W=c.intra.all())
    S.dma("sp", c.qfs.ap, d["c_qfs"], W=c.qfs.all())
    S.dma("sp", c.decay.ap, d["c_decay"], W=c.decay.all())
    S.dma("pool", c.ident.ap, d["c_ident"], W=c.ident.all())
    S.dma("pool", c.rot.ap, d["c_rot"], W=c.rot.all())
    S.dma("pool", c.causal.ap, d["c_causal"], W=c.causal.all())
    S.op("pool", lambda e: e.memset(c.ones.ap, 1.0), W=c.ones.all())
    c.g_mix, c.g_xattn, c.g_mem, c.g_ffn, c.g_final, c.g_q, c.g_kv = 0, 8, 16, 24, 32, 40, 42


def rope_tables(k, posi_ap, posi_key, prange, col, cosb, sinb, tmp, n):
    c = k.c
    p0, p1 = prange
    a, b_, kk = tmp
    A = lambda buf: buf.ap[p0:p1, 0:n]
    invf = c.small.ap[p0:p1, col:col + 1]
    ts(k, "dve", A(a), posi_ap[p0:p1, 0:n], invf, None, ALU.mult, None, [posi_key] + c.small.all(), a.all())
    ts(k, "dve", A(b_), A(a), 1.0 / TWO_PI, MAGIC, ALU.mult, ALU.add, a.all(), b_.all())
    ts(k, "dve", A(kk), A(b_), -MAGIC, None, ALU.add, None, b_.all(), kk.all())
    stt(k, A(b_), A(kk), -C1, A(a), ALU.mult, ALU.add, kk.all() + a.all(), b_.all())
    stt(k, A(a), A(kk), -C2, A(b_), ALU.mult, ALU.add, kk.all() + b_.all(), a.all())
    ts(k, "dve", A(a), A(a), -PI_LO, PI_LO, ALU.max, ALU.min, a.all(), a.all())
    act(k, sinb.ap[p0:p1, 0:n], A(a), AF.Sin, a.all(), sinb.all())
    ts(k, "dve", A(b_), A(a), math.pi / 2, -TWO_PI, ALU.is_gt, ALU.mult, a.all(), b_.all())
    stt(k, A(kk), A(a), math.pi / 2, A(b_), ALU.add, ALU.add, a.all() + b_.all(), kk.all())
    ts(k, "dve", A(kk), A(kk), -PI_LO, PI_LO, ALU.max, ALU.min, kk.all(), kk.all())
    act(k, cosb.ap[p0:p1, 0:n], A(kk), AF.Sin, kk.all(), cosb.all())


def squares8(k, dst, src, n):
    tt(k, "pool", dst.ap[:, 0:4, 0:n], src.ap[:, 0:4, 0:n], src.ap[:, 0:4, 0:n], ALU.mult, src.all(), dst.all())
    for ci in range(4, 8):
        act(k, dst.ap[:, ci, 0:n], src.ap[:, ci, 0:n], AF.Square, src.all(), dst.all())


def rms_stats(k, sq_chunks, nch, nfeat, ps_i, sd, rstd, n, sqkeys):
    c = k.c
    ps = k.ps[ps_i]
    for i in range(nch):
        mm(k, ps[:, 0:n], c.ones.ap, sq_chunks(i), i == 0, i == nch - 1, sqkeys + c.ones.all(), [k.psk[ps_i]])
    act(k, sd.ap[:, 0:n], ps[:, 0:n], AF.Ln, [k.psk[ps_i]], sd.all(), scale=1.0 / nfeat, bias=EPS)
    act(k, rstd.ap[:, 0:n], sd.ap[:, 0:n], AF.Exp, sd.all(), rstd.all(), scale=-0.5)


def emit_all(k):
    load_consts(k)
    P = K()
    k.P = P
    S = k.S
    P.cqn = S.alloc("cqn", [128, 2, NST], BF16)
    P.ckvn = S.alloc("ckvn", [128, NTOK], BF16)
    P.krope = S.alloc("krope", [128, NTOK], BF16)
    P.oretT = S.alloc("oretT", [128, 4, NST], BF16)
    phase_A1(k)
    if k.stage == "A1":
        return
    P.memKT = S.alloc("memKT", [128, 8, MEM], BF16)
    P.memV = S.alloc("memV", [128, 2, D], BF16)
    phase_MKV(k)
    P.omlaT = S.alloc("omlaT", [128, 4, NST], BF16)
    P.w_out = S.alloc("w_out_bf", [128, 8, D], BF16)
    P.w_xq = S.alloc("w_xq_bf", [128, 8, D], BF16)
    d = k.d
    phase_A2(k)
    if k.stage == "A2":
        return
    S.release(P.cqn, P.ckvn, P.krope)
    P.w_xo = S.alloc("w_xo_bf", [128, 8, D], BF16)
    S.dma("pool", P.w_xo.ap, d["w_xo"].rearrange("(c p) n -> p c n", p=128), W=P.w_xo.all(), key="w_xo")
    P.w_ffn_out = S.alloc("w_ffn_out_bf", [128, NFB, D], BF16)
    S.dma("pool", P.w_ffn_out.ap, d["w_ffn_out"].rearrange("(c p) n -> p c n", p=128), W=P.w_ffn_out.all(), key="w_ffn_out")
    phase_A3X(k)
    if k.stage == "A3X":
        return
    if getattr(k, "a3x_early", False):
        S.release(P.w_xq, P.w_xo, P.memKT, P.memV)
    else:
        S.release(P.oretT, P.omlaT, P.w_out, P.w_xq, P.w_xo, P.memKT, P.memV)
    load_w1_pieces(k, False)

    phase_F(k)


def phase_A1(k):
    S, d, c, P = k.S, k.d, k.c, k.P
    ps, psk = k.ps, k.psk
    T = 512
    w_in = S.alloc("w_in_bf", [128, 8, IN_COLS], BF16)
    S.dma("pool", w_in.ap, d["w_in"].rearrange("(c p) n -> p c n", p=128), W=w_in.all(), key="w_in")
    w_krot = S.alloc("w_krot", [128, 8, 96], BF16)
    S.op("pool", lambda e: e.memset(w_krot.ap, 0.0), W=w_krot.all())
    ts(k, "pool", w_krot.ap[:, :, 64:80], w_in.ap[:, :, OFF_KR + 16:OFF_KR + 32], -1.0, None, ALU.mult, None,
       w_in.all(), w_krot.all())
    cp(k, "pool", w_krot.ap[:, :, 80:96], w_in.ap[:, :, OFF_KR:OFF_KR + 16], w_in.all(), w_krot.all())

    xt = [S.alloc(f"xt{i}", [128, 8, T], F32) for i in range(2)]
    posi = [S.alloc(f"posi{i}", [128, T], I32) for i in range(2)]
    hTs = [S.alloc(f"hT{i}", [128, 8, T], BF16) for i in range(2)]
    rstd = S.alloc("rstd", [128, T], F32)
    sd = rstd
    ta, tb, tc = (S.alloc(n, [128, T], F32) for n in ("ta", "tb", "tc"))
    cos_r, sin_r = S.alloc("cos_r", [128, T], F32), S.alloc("sin_r", [128, T], F32)
    cos_m, sin_m = S.alloc("cos_m", [128, T], F32), S.alloc("sin_m", [128, T], F32)
    sql = S.alloc("sql", [128, 2, T], BF16)
    rstl = S.alloc("rstl", [128, T], F32)
    sdl = rstl
    raw = [S.alloc(f"raw{i}", [128, T], BF16) for i in range(2)]
    t1 = [S.alloc(f"t1_{i}", [128, T], F32) for i in range(2)]
    t2 = [S.alloc(f"t2_{i}", [128, T], F32) for i in range(2)]
    rqT = S.alloc("rqT", [128, 4, T], BF16, nsub=4)
    rkT = S.alloc("rkT", [128, 4, T], BF16, nsub=4)
    v_tm = S.alloc("v_tm", [128, 4, 512], BF16, nsub=4)
    sg_tm = S.alloc("sg_tm", [128, 4, 512], BF16, nsub=4)
    kdec = S.alloc("kdec", [128, 512], BF16)
    PT = S.alloc("PT", [128, 512], BF16)
    qsT = S.alloc("qsT", [128, 4, 128], BF16)
    S_f = S.alloc("S_f", [128, 512], F32)
    S_b = S.alloc("S_b", [128, 512], BF16)
    sqo = S.alloc("sqo", [128, 512], F32)
    tn = S.alloc("tn", [128, 512], F32)
    ogs = [S.alloc(f"og{i}", [128, 512], BF16) for i in range(2)]
    oT = S.alloc("oT", [128, 512], BF16)
    sqT = S.alloc("sqT", [128, 512], BF16)
    S.op("pool", lambda e: e.memset(S_f.ap, 0.0), W=S_f.all())
    S.op("pool", lambda e: e.memset(S_b.ap, 0.0), W=S_b.all())

    xTv = d["xT"].rearrange("(c p) t -> p c t", p=128)
    ntiles = NTOK // T
    rr = 0
    tile_list = list(range(ntiles)) if k.a1_tiles is None else k.a1_tiles
    xt_released = False

    def ln_part1(tti):
        tok0_ = tti * T
        xb_, pb_, hT_ = xt[tti % 2], posi[tti % 2], hTs[tti % 2]
        S.dma("sp", xb_.ap, xTv[:, :, tok0_:tok0_ + T], W=xb_.all(), key=f"xt{tti % 2}")
        S.dma("sp", pb_.ap, d["posi"][:, tok0_:tok0_ + T].partition_broadcast(128), W=pb_.all(), key=f"posi{tti % 2}")
        squares8(k, hT_, xb_, T)
        rms_stats(k, lambda i: hT_.ap[:, i, :], 8, D, 0, sd, rstd, T, hT_.all())

    def ln_stt(tti, ci):
        xb_, hT_ = xt[tti % 2], hTs[tti % 2]
        stt(k, hT_.ap[:, ci, :], xb_.ap[:, ci, :], c.gv.ap[:, c.g_mix + ci:c.g_mix + ci + 1], rstd.ap,
            ALU.mult, ALU.mult, xb_.all() + rstd.all() + c.gv.all(), hT_.all())

    def ropes(tti, which):
        pb_ = posi[tti % 2]
        if which == 0:
            rope_tables(k, pb_.ap, pb_.k(), (0, 128), 0, cos_r, sin_r, (ta, tb, tc), T)
        else:
            rope_tables(k, pb_.ap, pb_.k(), (64, 96), 1, cos_m, sin_m, (ta, tb, tc), T)

    ln_part1(tile_list[0])
    for ci in range(8):
        ln_stt(tile_list[0], ci)
    ropes(tile_list[0], 0)
    ropes(tile_list[0], 1)
    for tidx, tti in enumerate(tile_list):
        tok0 = tti * T
        xb, pb, hT = xt[tti % 2], posi[tti % 2], hTs[tti % 2]
        own_tile = tok0 + T > HALO0
        nxt = tile_list[tidx + 1] if tidx + 1 < len(tile_list) else None

        def proj(ps_i, m, wbuf, col0, n=T):
            for ci in range(8):
                mm(k, ps[ps_i][0:m, 0:n], wbuf.ap[:, ci, col0:col0 + m], hT.ap[:, ci, 0:n], ci == 0, ci == 7,
                   wbuf.all() + hT.all(), [psk[ps_i]])

        if k.a1_level < 3:
            continue
        proj(1, 128, w_in, OFF_CKV)
        act(k, sql.ap[:, 0, :], ps[1], AF.Square, [psk[1]], sql.all())
        rms_stats(k, lambda i: sql.ap[:, 0, :], 1, 128, 0, sdl, rstl, T, sql.all())
        stt(k, P.ckvn.ap[:, tok0:tok0 + T], ps[1], c.gv.ap[:, c.g_kv:c.g_kv + 1], rstl.ap, ALU.mult, ALU.mult,
            [psk[1]] + rstl.all() + c.gv.all(), P.ckvn.all())
        proj(2, 96, w_in, OFF_KR - 64)
        proj(3, 96, w_krot, 0)
        tt(k, "dve", t1[0].ap[64:96, :], ps[2][64:96, :], cos_m.ap[64:96, :], ALU.mult, [psk[2]] + cos_m.all(), t1[0].all())
        tt(k, "dve", t2[0].ap[64:96, :], ps[3][64:96, :], sin_m.ap[64:96, :], ALU.mult, [psk[3]] + sin_m.all(), t2[0].all())
        tt(k, "pool", P.krope.ap[64:96, tok0:tok0 + T], t1[0].ap[64:96, :], t2[0].ap[64:96, :], ALU.add,
           t1[0].all() + t2[0].all(), P.krope.all())
        if k.a1_level < 4:
            continue
        if own_tile:
            proj(1, 128, w_in, OFF_CQ)
            proj(2, 128, w_in, OFF_CQ + 128)
            act(k, sql.ap[:, 0, :], ps[1], AF.Square, [psk[1]], sql.all())
            act(k, sql.ap[:, 1, :], ps[2], AF.Square, [psk[2]], sql.all())
            rms_stats(k, lambda i: sql.ap[:, i, :], 2, 256, 0, sdl, rstl, T, sql.all())
            if tok0 < HALO0:
                lo, n_, s0 = HALO0 - tok0, 128, 0
            else:
                lo, n_, s0 = 0, T, tok0 - HALO0
            for j, pi in ((0, 1), (1, 2)):
                stt(k, P.cqn.ap[:, j, s0:s0 + n_], ps[pi][:, lo:lo + n_], c.gv.ap[:, c.g_q + j:c.g_q + j + 1],
                    rstl.ap[:, lo:lo + n_], ALU.mult, ALU.mult, [psk[pi]] + rstl.all() + c.gv.all(), P.cqn.all())
        if k.a1_level < 5:
            continue
        todo = [(rkT, OFF_RK)] + ([(rqT, OFF_RQ)] if own_tile else [])
        items = [(dst, off, h) for (dst, off) in todo for h in range(4)]

        def rope_tail(it, slot):
            dst, off, h = it
            pi, pj = 1 + slot, 3 + slot
            rb, a1, a2 = raw[slot], t1[slot], t2[slot]
            mm(k, ps[pj], c.rot.ap, rb.ap, True, True, rb.all() + c.rot.all(), [psk[pj]])
            tt(k, "dve", a2.ap, ps[pj], sin_r.ap, ALU.mult, [psk[pj]] + sin_r.all(), a2.all())
            tt(k, "pool", a1.ap, rb.ap, cos_r.ap, ALU.mult, rb.all() + cos_r.all(), a1.all())
            tt(k, "pool", dst.ap[:, h, :], a1.ap, a2.ap, ALU.add, a1.all() + a2.all(), [dst.k(h)])

        prev = None
        for n_, it in enumerate(items):
            slot = n_ % 2
            proj(1 + slot, 128, w_in, it[1] + it[2] * 128)
            cp(k, "act", raw[slot].ap, ps[1 + slot], [psk[1 + slot]], raw[slot].all())
            if prev is not None:
                rope_tail(*prev)
            prev = (it, slot)
        rope_tail(*prev)
        if k.a1_level < 6:
            continue
        if own_tile:
            for h in range(4):
                pi = 1 + h % 2
                proj(pi, 128, w_in, OFF_RG + h * 128)
                act(k, sg_tm.ap[:, h, :], ps[pi], AF.Silu, [psk[pi]], [sg_tm.k(h)])
        if nxt is not None:
            ln_part1(nxt)
        elif k.stage != "A1":
            S.release(*xt)
            xt_released = True
            P.w_xkv = S.alloc("w_xkv_bf", [128, 8, 2 * D], BF16)
            S.dma("pool", P.w_xkv.ap, d["w_xkv"].rearrange("(c p) n -> p c n", p=128), W=P.w_xkv.all(), key="w_xkv")
        p7b = ps[7].bitcast(BF16)
        k7a, k7b = ("ps", "7a"), ("ps", "7b")

        def stage_a(cc):
            ctok = tok0 + cc * 128
            own_chunk = ctok >= HALO0
            cs = slice(cc * 128, (cc + 1) * 128)
            for ci in range(8):
                mm(k, ps[1], hT.ap[:, ci, cs], w_in.ap[:, ci, OFF_RV:OFF_RV + 512], ci == 0, ci == 7,
                   hT.all() + w_in.all(), [psk[1]])
            cp(k, "act", v_tm.ap[:, cc, :], ps[1], [psk[1]], [v_tm.k(cc)])
            for h in range(4):
                tr(k, p7b[:, h * 128:(h + 1) * 128], rkT.ap[:, h, cs], c.ident.ap, [rkT.k(h)] + c.ident.all(), [k7a])
            if own_chunk:
                for h in range(4):
                    mm(k, ps[2][:, h * 128:(h + 1) * 128], rkT.ap[:, h, cs], rqT.ap[:, h, cs], True, True,
                       [rkT.k(h), rqT.k(h)], [psk[2]])
            tt(k, "dve", kdec.ap.rearrange("p (h e) -> p h e", h=4), p7b[:, 0:512].rearrange("p (h e) -> p h e", h=4),
               bc(c.small.ap[:, 2:6].unsqueeze(2), [128, 4, 128]), ALU.mult, [k7a] + c.small.all(), kdec.all())
            if own_chunk:
                tt(k, "dve", PT.ap, ps[2], c.intra.ap, ALU.mult, [psk[2]] + c.intra.all(), PT.all())
                tt(k, "pool", qsT.ap, rqT.ap[:, :, cs], c.qfs.ap.rearrange("p (h e) -> p h e", h=4), ALU.mult,
                   rqT.all() + c.qfs.all(), qsT.all())
            if ctok + 128 < NTOK:
                for h in range(4):
                    hs = slice(h * 128, (h + 1) * 128)
                    mm(k, ps[5][:, hs], kdec.ap[:, hs], v_tm.ap[:, cc, hs], True, True, kdec.all() + [v_tm.k(cc)], [psk[5]])
            if own_chunk:
                for h in range(4):
                    hs = slice(h * 128, (h + 1) * 128)
                    mm(k, ps[6][:, hs], PT.ap[:, hs], v_tm.ap[:, cc, hs], True, False, PT.all() + [v_tm.k(cc)], [psk[6]])
                    mm(k, ps[6][:, hs], qsT.ap[:, h, :], S_b.ap[:, hs], False, True, qsT.all() + S_b.all(), [psk[6]])
                cp(k, "act", ogs[cc % 2].ap, ps[6], [psk[6]], ogs[cc % 2].all())
            if ctok + 128 < NTOK:
                tt(k, "pool", S_f.ap, S_f.ap, c.decay.ap, ALU.mult, S_f.all() + c.decay.all(), S_f.all())
                tt(k, "dve", S_f.ap, S_f.ap, ps[5], ALU.add, S_f.all() + [psk[5]], S_f.all())
                cp(k, "act", S_b.ap, S_f.ap, S_f.all(), S_b.all())

        def stage_b(cc):
            ctok = tok0 + cc * 128
            if ctok < HALO0:
                return
            cs = slice(cc * 128, (cc + 1) * 128)
            og = ogs[cc % 2]
            for h in range(4):
                tr(k, p7b[:, 512 + h * 128:512 + (h + 1) * 128], og.ap[:, h * 128:(h + 1) * 128], c.ident.ap,
                   og.all() + c.ident.all(), [k7b])
            cp(k, "dve", oT.ap, p7b[:, 512:1024], [k7b], oT.all())
            act(k, sqT.ap, oT.ap, AF.Square, oT.all(), sqT.all())
            mm(k, ps[4], c.ones.ap, oT.ap, True, True, c.ones.all() + oT.all(), [psk[4]])
            mm(k, ps[3], c.ones.ap, sqT.ap, True, True, c.ones.all() + sqT.all(), [psk[3]])
            act(k, tn.ap, ps[4], AF.Copy, [psk[4]], tn.all(), scale=1.0 / 128)
            tt(k, "dve", sqo.ap, tn.ap, tn.ap, ALU.mult, tn.all(), sqo.all())
            stt(k, sqo.ap, ps[3], 1.0 / 128, sqo.ap, ALU.mult, ALU.subtract, [psk[3]] + sqo.all(), sqo.all())
            act(k, sqo.ap, sqo.ap, AF.Ln, sqo.all(), sqo.all(), scale=1.0, bias=EPS)
            act(k, sqo.ap, sqo.ap, AF.Exp, sqo.all(), sqo.all(), scale=-0.5)
            tt(k, "dve", tn.ap, oT.ap, tn.ap, ALU.subtract, oT.all() + tn.all(), tn.all())
            tt(k, "pool", tn.ap, tn.ap, sqo.ap, ALU.mult, tn.all() + sqo.all(), tn.all())
            s0 = ctok - HALO0
            tt(k, "pool", P.oretT.ap[:, :, s0:s0 + 128], tn.ap.rearrange("p (h e) -> p h e", h=4), sg_tm.ap[:, :, cs],
               ALU.mult, tn.all() + sg_tm.all(), P.oretT.all())

        for cc in range(5):
            if cc < 4:
                stage_a(cc)
            if cc >= 1:
                stage_b(cc - 1)
            if nxt is not None and cc < 4:
                ln_stt(nxt, 2 * cc)
                ln_stt(nxt, 2 * cc + 1)
                if cc == 1:
                    ropes(nxt, 0)
                if cc == 2:
                    ropes(nxt, 1)
    S.merge_keys([("ps", "7a"), ("ps", "7b")], psk[7])
    if k.stage == "A1":
        mm(k, ps[0][:, 0:128], c.ones.ap, c.ones.ap, True, True, c.ones.all(), [psk[0]])
    if k.dbg and k.stage == "A1":
        dump(k, "ckvn", P.ckvn.ap, [128, NTOK], P.ckvn.all(), BF16)
        dump(k, "krope", P.krope.ap[64:96, :], [32, NTOK], P.krope.all(), BF16)
        dump(k, "cqn", P.cqn.ap, [128, 2, NST], P.cqn.all(), BF16)
        dump(k, "oretT", P.oretT.ap, [128, 4, NST], P.oretT.all(), BF16)
    if not xt_released:
        S.release(*xt)
    S.release(w_in, w_krot, *posi, *hTs, rstd, ta, tb, tc, cos_r, sin_r, cos_m, sin_m, sql, rstl, *raw, *t1,
              *t2, rqT, rkT, v_tm, sg_tm, kdec, PT, qsT, S_f, S_b, sqo, tn, *ogs, oT, sqT)


HB = 11 * 128
W1_ORDER = [(0, 0, 0), (0, 0, 1), (0, 1, 0), (0, 1, 1), (1, 0, 0), (1, 0, 1), (1, 1, 0), (1, 1, 1)]


def load_w1_pieces(k, allow_fail):
    S, d, P = k.S, k.d, k.P
    if not hasattr(P, "w1p"):
        P.w1p = {}
    wv = d["w_ffn_in"].rearrange("(c p) n -> p c n", p=128)
    for pc in W1_ORDER:
        if pc in P.w1p:
            continue
        up, jh, ch = pc
        try:
            buf = S.alloc(f"w1_{up}{jh}{ch}", [128, 4, HB], BF16)
        except MemoryError:
            if allow_fail:
                return
            raise
        P.w1p[pc] = buf
        a = up * DFF + jh * HB
        S.dma("pool", buf.ap, wv[:, 4 * ch:4 * ch + 4, a:a + HB], W=buf.all(), key=f"w1_{up}{jh}{ch}")


def w1_slice(k, gate, j, ci):
    pc = (0 if gate else 1, 0 if j < 11 else 1, ci // 4)
    jj = j if j < 11 else j - 11
    buf = k.P.w1p[pc]
    return buf.ap[:, ci % 4, jj * 128:(jj + 1) * 128], buf.all()


def phase_MKV(k):
    S, d, c, P, ps, psk = k.S, k.d, k.c, k.P, k.ps, k.psk
    w_xkv = P.w_xkv
    mt = S.alloc("memT", [128, 8, MEM], F32)
    S.dma("sp", mt.ap, d["memT"].rearrange("(c p) t -> p c t", p=128), W=mt.all(), key="memT")
    hm = S.alloc("hmem", [128, 8, MEM], BF16)
    sd, rstd = S.alloc("sdm", [128, MEM], F32), S.alloc("rstdm", [128, MEM], F32)
    squares8(k, hm, mt, MEM)
    rms_stats(k, lambda i: hm.ap[:, i, :], 8, D, 0, sd, rstd, MEM, hm.all())
    for ci in range(8):
        stt(k, hm.ap[:, ci, :], mt.ap[:, ci, :], c.gv.ap[:, c.g_mem + ci:c.g_mem + ci + 1], rstd.ap, ALU.mult, ALU.mult,
            mt.all() + rstd.all() + c.gv.all(), hm.all())
    for blk in range(8):
        pi = 1 + blk % 2
        for ci in range(8):
            mm(k, ps[pi][:, 0:MEM], w_xkv.ap[:, ci, blk * 128:(blk + 1) * 128], hm.ap[:, ci, :], ci == 0, ci == 7,
               w_xkv.all() + hm.all(), [psk[pi]])
        cp(k, "act", P.memKT.ap[:, blk, :], ps[pi][:, 0:MEM], [psk[pi]], P.memKT.all())
    n = 0
    for kb2 in range(2):
        for half in range(2):
            pi = 3 + n % 2
            n += 1
            for ci in range(8):
                mm(k, ps[pi], hm.ap[:, ci, kb2 * 128:(kb2 + 1) * 128], w_xkv.ap[:, ci, D + half * 512:D + (half + 1) * 512],
                   ci == 0, ci == 7, w_xkv.all() + hm.all(), [psk[pi]])
            cp(k, "dve", P.memV.ap[:, kb2, half * 512:(half + 1) * 512], ps[pi], [psk[pi]], P.memV.all())
    S.release(w_xkv, mt, hm, sd, rstd)


def phase_A2(k):
    S, d, c, P, ps, psk = k.S, k.d, k.c, k.P, k.ps, k.psk
    ps2 = k.ps2
    w_uq = S.alloc("w_uq_bf", [128, 2, 768], BF16)
    w_uqr = S.alloc("w_uqr_bf", [128, 2, 768], BF16)
    w_ukv = S.alloc("w_ukv_bf", [128, 1024], BF16)
    S.dma("pool", w_uq.ap, d["w_uq"].rearrange("(c p) n -> p c n", p=128), W=w_uq.all(), key="w_uq")
    S.dma("pool", w_ukv.ap, d["w_ukv"], W=w_ukv.all(), key="w_ukv")
    S.dma("pool", P.w_out.ap, d["w_out"].rearrange("(c p) n -> p c n", p=128), W=P.w_out.all(), key="w_out")
    S.dma("pool", P.w_xq.ap, d["w_xq"].rearrange("(c p) n -> p c n", p=128), W=P.w_xq.all(), key="w_xq")
    S.op("pool", lambda e: e.memset(w_uqr.ap, 0.0), W=w_uqr.all())
    q4 = w_uq.ap.rearrange("p c (h x) -> p c h x", h=8)
    r4 = w_uqr.ap.rearrange("p c (h x) -> p c h x", h=8)
    for ci in range(2):
        ts(k, "pool", r4[:, ci, :, 64:80], q4[:, ci, :, 80:96], -1.0, None, ALU.mult, None, w_uq.all(), w_uqr.all())
        cp(k, "pool", r4[:, ci, :, 80:96], q4[:, ci, :, 64:80], w_uq.all(), w_uqr.all())
    KT = S.alloc("KT", [128, 4, NTOK], BF16, nsub=4)
    Vc = S.alloc("Vc", [128, 32, 384], BF16)
    S.op("pool", lambda e: e.memset(Vc.ap, 1.0), W=Vc.all())
    for o in (64, 256):
        ts(k, "dve", Vc.ap[:, 0:16, o:o + 64], Vc.ap[:, 0:16, o:o + 64], c.flags.ap[:, 0:1], None, ALU.mult, None,
           Vc.all() + c.flags.all(), Vc.all())
    qTb = [[S.alloc(f"qT{b_}_{i}", [128, 512], BF16) for i in range(4)] for b_ in range(2)]
    for q_ in qTb[0] + qTb[1]:
        S.op("pool", lambda e, q_=q_: e.memset(q_.ap[96:128, :], 0.0), W=q_.all())
    S.op("pool", lambda e: e.memset(KT.ap[96:128, :, :], 0.0), W=KT.all())
    PTb = [S.alloc(f"PTb{i}", [128, 1024], BF16) for i in range(2)]
    tq1, tq2 = S.alloc("tq1", [128, 512], F32), S.alloc("tq2", [128, 512], F32)
    ta, tb, tc = (S.alloc(n, [128, 512], F32) for n in ("ta2", "tb2", "tc2"))
    cos_m, sin_m = S.alloc("cos_m2", [128, 512], F32), S.alloc("sin_m2", [128, 512], F32)
    pq = S.alloc("posq", [128, 512], I32)
    rec = S.alloc("rec", [128, 512], F32)
    wk3 = w_ukv.ap.rearrange("p (h x) -> p h x", h=8)
    scale = 1.0 / math.sqrt(96.0)
    n_ev = 0
    for hh in range(2):
        heads = list(range(4 * hh, 4 * hh + 4))
        for kt in range(NTOK // 512):
            for hi, h in enumerate(heads):
                pi = n_ev % 2
                mm(k, ps[pi], w_ukv.ap[:, h * 128:h * 128 + 128], P.ckvn.ap[:, kt * 512:(kt + 1) * 512], True, True,
                   w_ukv.all() + P.ckvn.all(), [psk[pi]])
                cp(k, "act", KT.ap[0:64, hi, kt * 512:(kt + 1) * 512], ps[pi][0:64, :], [psk[pi]],
                   [KT.k(hi)])
                n_ev += 1
        for hi in range(4):
            S.dma("sp", KT.ap[64:96, hi, :], P.krope.ap[64:96, :], R=P.krope.all(), W=[KT.k(hi)], key=f"kr{hi}")
        for kb in range(NTOK // 128):
            pi = 2 + kb % 2
            mm(k, ps[pi], P.ckvn.ap[:, kb * 128:(kb + 1) * 128], w_ukv.ap[:, 512 * hh:512 * (hh + 1)], True, True,
               w_ukv.all() + P.ckvn.all(), [psk[pi]])
            src = ps[pi].rearrange("p (a m x) -> p a m x", a=2, m=2)
            dst = Vc.ap[:, kb, :].rearrange("p (a r) -> p a r", a=2)
            cp(k, "dve", dst[:, :, 0:64], src[:, :, 0, 64:128], [psk[pi]], Vc.all())
            cp(k, "dve", dst[:, :, 128:192], src[:, :, 1, 64:128], [psk[pi]], Vc.all())
        def qtile(qi):
            if qi == 0:
                return 0, 128, HALO0 // 128
            return 128 + 512 * (qi - 1), 512, NPRE // 128 + 4 * (qi - 1)

        def q_assemble(qi):
            s0, NQ, qblk0 = qtile(qi)
            qTs = qTb[qi % 2]
            g0 = HALO0 + s0
            S.dma("sp", pq.ap[:, 0:NQ], d["posi"][:, g0:g0 + NQ].partition_broadcast(128), W=pq.all(), key="posq")
            rope_tables(k, pq.ap, pq.k(), (64, 96), 1, cos_m, sin_m, (ta, tb, tc), NQ)
            for hi, h in enumerate(heads):
                for ci in range(2):
                    mm(k, ps[0][0:96, 0:NQ], w_uq.ap[:, ci, h * 96:(h + 1) * 96], P.cqn.ap[:, ci, s0:s0 + NQ], ci == 0, ci == 1,
                       w_uq.all() + P.cqn.all(), [psk[0]])
                for ci in range(2):
                    mm(k, ps[1][0:96, 0:NQ], w_uqr.ap[:, ci, h * 96:(h + 1) * 96], P.cqn.ap[:, ci, s0:s0 + NQ], ci == 0, ci == 1,
                       w_uqr.all() + P.cqn.all(), [psk[1]])
                cp(k, "dve", qTs[hi].ap[0:64, 0:NQ], ps[0][0:64, 0:NQ], [psk[0]], qTs[hi].all())
                tt(k, "dve", tq1.ap[64:96, 0:NQ], ps[0][64:96, 0:NQ], cos_m.ap[64:96, 0:NQ], ALU.mult, [psk[0]] + cos_m.all(),
                   tq1.all())
                tt(k, "dve", tq2.ap[64:96, 0:NQ], ps[1][64:96, 0:NQ], sin_m.ap[64:96, 0:NQ], ALU.mult, [psk[1]] + sin_m.all(),
                   tq2.all())
                tt(k, "pool", qTs[hi].ap[64:96, 0:NQ], tq1.ap[64:96, 0:NQ], tq2.ap[64:96, 0:NQ], ALU.add, tq1.all() + tq2.all(),
                   qTs[hi].all())

        q_assemble(0)
        for qi in range(5):
            s0, NQ, qblk0 = qtile(qi)
            nqb = NQ // 128
            qT = qTb[qi % 2]
            if qi + 1 < 5:
                q_assemble(qi + 1)
            for hi, h in enumerate(heads):
                pair, mem = divmod(hi, 2)
                vcol0 = pair * 192 + mem * 64
                pob = 6 + hi % 2
                po = ps[pob]
                nkb = qblk0 + nqb
                groups = []
                kb = 0
                while kb < nkb:
                    if kb + 1 < qblk0:
                        groups.append((kb, kb + 1))
                        kb += 2
                    else:
                        groups.append((kb,))
                        kb += 1
                pend = None

                def pv(pd, nkb=nkb, po=po, pob=pob, vcol0=vcol0, NQ=NQ):
                    for (kb_, qlo_, n_, ptap_, ptk_) in pd:
                        mm(k, po[:, qlo_:NQ], Vc.ap[:, kb_, vcol0:vcol0 + 128], ptap_, kb_ == 0, kb_ == nkb - 1,
                           Vc.all() + ptk_, [psk[pob]])

                for gi, grp in enumerate(groups):
                    slot = gi % 2
                    pt = PTb[slot]
                    banks = (2 + 2 * slot, 3 + 2 * slot)
                    cur = []
                    for j_, kb in enumerate(grp):
                        r = kb - qblk0
                        q_lo = max(r, 0) * 128
                        n = NQ - q_lo
                        sb = banks[j_]
                        mm(k, ps[sb][:, 0:n], KT.ap[:, hi, kb * 128:(kb + 1) * 128], qT[hi].ap[:, q_lo:NQ], True, True,
                           [KT.k(hi)] + qT[hi].all(), [psk[sb]])
                        cur.append((kb, q_lo, n, pt.ap[:, j_ * 512:j_ * 512 + n], pt.all()))
                    if len(grp) == 2 and NQ == 512:
                        act(k, pt.ap, ps2[1 + slot], AF.Exp, [psk[banks[0]], psk[banks[1]]], pt.all(), scale=scale)
                    else:
                        for j_, (kb, q_lo, n, ptap, _) in enumerate(cur):
                            act(k, ptap, ps[banks[j_]][:, 0:n], AF.Exp, [psk[banks[j_]]], pt.all(), scale=scale)
                            if kb - qblk0 >= 0:
                                tt(k, "pool", ptap[:, 0:128], ptap[:, 0:128], c.causal.ap, ALU.mult, pt.all() + c.causal.all(),
                                   pt.all())
                    if pend is not None:
                        pv(pend)
                    pend = cur
                pv(pend)
                if mem == 0:
                    o_rows, s_rows = slice(0, 64), slice(64, 128)
                else:
                    o_rows, s_rows = slice(64, 128), slice(0, 64)
                ts(k, "dve", rec.ap[o_rows, 0:NQ], po[s_rows, 0:NQ], 1e-30, None, ALU.add, None, [psk[pob]], rec.all())
                recip(k, rec.ap[o_rows, 0:NQ], rec.ap[o_rows, 0:NQ], rec.all(), rec.all())
                tt(k, "dve", P.omlaT.ap[o_rows, 2 * hh + pair, s0:s0 + NQ], po[o_rows, 0:NQ], rec.ap[o_rows, 0:NQ], ALU.mult,
                   [psk[pob]] + rec.all(), P.omlaT.all())
    if k.dbg and k.stage == "A2":
        dump(k, "omlaT", P.omlaT.ap, [128, 4, NST], P.omlaT.all(), BF16)
    S.release(w_uq, w_uqr, w_ukv, KT, Vc, *qTb[0], *qTb[1], *PTb, tq1, tq2, ta, tb, tc, cos_m, sin_m, pq, rec)


def _norm_to_hT(k, xb, hT, sd, rstd, gcol, n):
    c = k.c
    squares8(k, hT, xb, n)
    rms_stats(k, lambda i: hT.ap[:, i, 0:n], 8, D, 0, sd, rstd, n, hT.all())
    for ci in range(8):
        stt(k, hT.ap[:, ci, 0:n], xb.ap[:, ci, 0:n], c.gv.ap[:, gcol + ci:gcol + ci + 1], rstd.ap[:, 0:n], ALU.mult, ALU.mult,
            xb.all() + rstd.all() + c.gv.all(), hT.all())


def phase_A3X(k):
    S, d, c, P, ps, psk = k.S, k.d, k.c, k.P, k.ps, k.psk
    xbs = [S.alloc(f"xa{i}", [128, 8, 512], F32) for i in range(2)]
    hT = S.alloc("hTa", [128, 8, 512], BF16)
    rstd = S.alloc("rstda", [128, 512], F32)
    sd = rstd
    qxT = S.alloc("qxT", [128, 8, 512], BF16, nsub=8)
    PTx = [S.alloc(f"PTx{i}", [128, 512], BF16) for i in range(2)]
    oxT = S.alloc("oxT", [128, 8, 512], BF16)
    rec = S.alloc("recx", [128, 512], F32)
    xTv = d["xT"].rearrange("(c p) t -> p c t", p=128)
    x2v = d["x2s"].rearrange("(c p) t -> p c t", p=128)
    tiles = [(0, 128)] + [(128 + 512 * i, 512) for i in range(4)]
    nt = len(tiles)

    def xload(ti_):
        s0_, N_ = tiles[ti_]
        S.dma("sp", xbs[ti_ % 2].ap[:, :, 0:N_], xTv[:, :, HALO0 + s0_:HALO0 + s0_ + N_], W=xbs[ti_ % 2].all(), key=f"xa{ti_ % 2}")

    def stage_w(ti_):
        s0, N = tiles[ti_]
        xb = xbs[ti_ % 2]
        for cb in range(8):
            pi = 1 + cb % 2
            cs = slice(cb * 128, (cb + 1) * 128)
            for j in range(4):
                mm(k, ps[pi][:, 0:N], P.w_out.ap[:, j, cs], P.omlaT.ap[:, j, s0:s0 + N], j == 0, False,
                   P.w_out.all() + P.omlaT.all(), [psk[pi]])
            for j in range(4):
                mm(k, ps[pi][:, 0:N], P.w_out.ap[:, 4 + j, cs], P.oretT.ap[:, j, s0:s0 + N], False, j == 3,
                   P.w_out.all() + P.oretT.all(), [psk[pi]])
            tt(k, "dve", xb.ap[:, cb, 0:N], xb.ap[:, cb, 0:N], ps[pi][:, 0:N], ALU.add, xb.all() + [psk[pi]], xb.all())
        if k.dbg and k.stage == "A3X":
            dumpx(k, "x1", xb, s0, N)

    def stage_n1(ti_):
        s0, N = tiles[ti_]
        xb = xbs[ti_ % 2]
        squares8(k, hT, xb, N)
        rms_stats(k, lambda i: hT.ap[:, i, 0:N], 8, D, 0, sd, rstd, N, hT.all())

    def stage_n2(ti_, ci):
        s0, N = tiles[ti_]
        xb = xbs[ti_ % 2]
        stt(k, hT.ap[:, ci, 0:N], xb.ap[:, ci, 0:N], c.gv.ap[:, c.g_xattn + ci:c.g_xattn + ci + 1], rstd.ap[:, 0:N], ALU.mult,
            ALU.mult, xb.all() + rstd.all() + c.gv.all(), hT.all())

    xload(0)
    xload(1)
    stage_w(0)
    stage_n1(0)
    for ci in range(8):
        stage_n2(0, ci)
    for ti_, (s0, N) in enumerate(tiles):
        xb = xbs[ti_ % 2]
        for blk in range(8):
            pi = 1 + blk % 2
            for ci in range(8):
                mm(k, ps[pi][:, 0:N], P.w_xq.ap[:, ci, blk * 128:(blk + 1) * 128], hT.ap[:, ci, 0:N], ci == 0, ci == 7,
                   P.w_xq.all() + hT.all(), [psk[pi]])
            cp(k, "act", qxT.ap[:, blk, 0:N], ps[pi][:, 0:N], [psk[pi]], [qxT.k(blk)])
        if ti_ + 1 < nt:
            stage_w(ti_ + 1)
            stage_n1(ti_ + 1)
            if ti_ + 1 == nt - 1 and k.stage != "A3X":
                S.release(P.oretT, P.omlaT, P.w_out)
                k.a3x_early = True
                load_w1_pieces(k, True)
        for h in range(4):
            for kb2 in range(2):
                sb = 3 + kb2
                for dc in range(2):
                    mm(k, ps[sb][:, 0:N], P.memKT.ap[:, 2 * h + dc, kb2 * 128:(kb2 + 1) * 128], qxT.ap[:, 2 * h + dc, 0:N],
                       dc == 0, dc == 1, P.memKT.all() + [qxT.k(2 * h + dc)], [psk[sb]])
                act(k, PTx[kb2].ap[:, 0:N], ps[sb][:, 0:N], AF.Exp, [psk[sb]], PTx[kb2].all(), scale=1.0 / 16.0)
            for kb2 in range(2):
                mm(k, ps[5][:, 0:N], c.ones.ap, PTx[kb2].ap[:, 0:N], kb2 == 0, kb2 == 1, c.ones.all() + PTx[kb2].all(), [psk[5]])
            act(k, rec.ap[:, 0:N], ps[5][:, 0:N], AF.Ln, [psk[5]], rec.all())
            act(k, rec.ap[:, 0:N], rec.ap[:, 0:N], AF.Exp, rec.all(), rec.all(), scale=-1.0)
            for eb in range(2):
                pi = 6 + eb
                for kb2 in range(2):
                    mm(k, ps[pi][:, 0:N], P.memV.ap[:, kb2, h * 256 + eb * 128:h * 256 + (eb + 1) * 128], PTx[kb2].ap[:, 0:N],
                       kb2 == 0, kb2 == 1, P.memV.all() + PTx[kb2].all(), [psk[pi]])
                tt(k, "dve", oxT.ap[:, 2 * h + eb, 0:N], ps[pi][:, 0:N], rec.ap[:, 0:N], ALU.mult, [psk[pi]] + rec.all(), oxT.all())
            if ti_ + 1 < nt:
                stage_n2(ti_ + 1, 2 * h)
                stage_n2(ti_ + 1, 2 * h + 1)
        for cb in range(8):
            pi = 1 + cb % 2
            for j in range(8):
                mm(k, ps[pi][:, 0:N], P.w_xo.ap[:, j, cb * 128:(cb + 1) * 128], oxT.ap[:, j, 0:N], j == 0, j == 7,
                   P.w_xo.all() + oxT.all(), [psk[pi]])
            tt(k, "dve", xb.ap[:, cb, 0:N], xb.ap[:, cb, 0:N], ps[pi][:, 0:N], ALU.add, xb.all() + [psk[pi]], xb.all())
        S.dma("sp", x2v[:, :, s0:s0 + N], xb.ap[:, :, 0:N], R=xb.all(), W=[("x2s", s0)], key=f"xs{ti_ % 2}")
        if k.dbg and k.stage == "A3X":
            dumpx(k, "x2", xb, s0, N)
        if ti_ + 2 < nt:
            xload(ti_ + 2)
    S.release(*xbs, hT, rstd, qxT, *PTx, oxT, rec)


def dumpx(k, name, xb, s0, N):
    if name not in k.dbg_out:
        k.dbg_out[name] = k.nc.dram_tensor("dbg_" + name, [D, NST], F32, kind="ExternalOutput").ap()
    t = k.dbg_out[name].rearrange("(c p) t -> p c t", p=128)
    k.S.dma("sp", t[:, :, s0:s0 + N], xb.ap[:, :, 0:N], R=xb.all(), W=[("dbg", name, s0)], key="dbg")


def phase_F(k):
    S, d, c, P, ps, psk = k.S, k.d, k.c, k.P, k.ps, k.psk
    xf = S.alloc("xf", [128, 8, 512], F32)
    hT = S.alloc("hTf", [128, 8, 512], BF16)
    sd, rstd = S.alloc("sdf", [128, 512], F32), S.alloc("rstdf", [128, 512], F32)
    aT = S.alloc("aT", [128, NFB, 512], BF16, nsub=NFB)
    gsb = [S.alloc(f"gsb{i}", [128, 48 + 512], F32) for i in range(2)]
    cv = [S.alloc(f"cv{i}", [128, 512], F32) for i in range(2)]
    ghalo = S.alloc("ghalo", [128, NFB, 48], F32, nsub=NFB)
    x2v = d["x2s"].rearrange("(c p) t -> p c t", p=128)
    outv = d["outT"].rearrange("(c p) t -> p c t", p=128)
    ostg = S.arena[0:128, aT.off:aT.off + 8 * 512 * 4].bitcast(F32).rearrange("p (a b) -> p a b", a=8)
    S.dma("sp", xf.ap[:, :, 0:128], x2v[:, :, 0:128], R=[("x2s", 0)], W=xf.all(), key="xf")
    _norm_to_hT(k, xf, hT, sd, rstd, c.g_ffn, 128)
    for j in range(NFB):
        pi = 1 + j % 2
        for ci in range(8):
            wl, wk = w1_slice(k, True, j, ci)
            mm(k, ps[pi][:, 0:128], wl, hT.ap[:, ci, 0:128], ci == 0, ci == 7, wk + hT.all(), [psk[pi]])
        ts(k, "dve", ghalo.ap[:, j, :], ps[pi][:, 80:128], c.flags.ap[:, 1:2], None, ALU.mult, None, [psk[pi]] + c.flags.all(),
           [ghalo.k(j)])
    for ti in range(4):
        s0 = 128 + 512 * ti
        S.dma("sp", xf.ap, x2v[:, :, s0:s0 + 512], R=[("x2s", s0)], W=xf.all(), key="xf")
        _norm_to_hT(k, xf, hT, sd, rstd, c.g_ffn, 512)
        for j in range(NFB):
            pg, pu = 1 + (j % 2), (3, 4, 7)[j % 3]
            for ci in range(8):
                wl, wk = w1_slice(k, True, j, ci)
                mm(k, ps[pg], wl, hT.ap[:, ci, :], ci == 0, ci == 7, wk + hT.all(), [psk[pg]])
            for ci in range(8):
                wl, wk = w1_slice(k, False, j, ci)
                mm(k, ps[pu], wl, hT.ap[:, ci, :], ci == 0, ci == 7, wk + hT.all(), [psk[pu]])
            g, cvb = gsb[j % 2], cv[j % 2]
            cw = c.convp.ap
            cp(k, "act", g.ap[:, 48:560], ps[pg], [psk[pg]], g.all())
            cp(k, "pool", g.ap[:, 0:48], ghalo.ap[:, j, :], [ghalo.k(j)], g.all())
            k.S.op("act", lambda e, g=g, cvb=cvb, j=j, pg=pg: e.activation(out=cvb.ap, in_=ps[pg], func=AF.Identity,
                                                                     scale=cw[:, j, 2:3], bias=cw[:, j, 3:4]),
                   [psk[pg]] + c.convp.all(), cvb.all())
            stt(k, cvb.ap, g.ap[:, 47:559], cw[:, j, 1:2], cvb.ap, ALU.mult, ALU.add, g.all() + cvb.all() + c.convp.all(), cvb.all())
            stt(k, cvb.ap, g.ap[:, 46:558], cw[:, j, 0:1], cvb.ap, ALU.mult, ALU.add, g.all() + cvb.all() + c.convp.all(), cvb.all())
            cp(k, "pool", ghalo.ap[:, j, :], g.ap[:, 512:560], g.all(), [ghalo.k(j)])
            act(k, cvb.ap, cvb.ap, AF.Silu, cvb.all(), cvb.all())
            tt(k, "dve", aT.ap[:, j, :], cvb.ap, ps[pu], ALU.mult, cvb.all() + [psk[pu]], [aT.k(j)])
        for cb in range(8):
            pi = 5 + cb % 2
            for j in range(NFB):
                mm(k, ps[pi], P.w_ffn_out.ap[:, j, cb * 128:(cb + 1) * 128], aT.ap[:, j, :], j == 0, j == NFB - 1,
                   P.w_ffn_out.all() + [aT.k(j)], [psk[pi]])
            tt(k, "dve", xf.ap[:, cb, :], xf.ap[:, cb, :], ps[pi], ALU.add, xf.all() + [psk[pi]], xf.all())
        squares8(k, hT, xf, 512)
        rms_stats(k, lambda i: hT.ap[:, i, :], 8, D, 0, sd, rstd, 512, hT.all())
        for cb in range(8):
            stt(k, ostg[:, cb, :], xf.ap[:, cb, :], c.gv.ap[:, c.g_final + cb:c.g_final + cb + 1], rstd.ap, ALU.mult, ALU.mult,
                xf.all() + rstd.all() + c.gv.all(), aT.all())
        S.dma("sp", outv[:, :, 512 * ti:512 * (ti + 1)], ostg, R=aT.all(), W=[("out", ti)], key="out")
    S.release(xf, hT, sd, rstd, aT, *gsb, *cv, ghalo, *P.w1p.values())


def _consts():
    f32 = np.float32
    H, L = 4, 128
    log_gamma = np.log(f32(1.0) - f32(2.0) ** (f32(-5.0) - np.arange(H, dtype=f32))).astype(f32)
    j = np.arange(L, dtype=f32)
    diff = j[:, None] - j[None, :]
    intra = np.where(diff[None] >= 0, np.exp(np.maximum(diff, 0.0)[None] * log_gamma[:, None, None]), 0.0).astype(f32)
    k_to_end = np.exp((L - 1 - j)[:, None] * log_gamma[None, :]).astype(f32)
    q_from_start = np.exp((j + 1)[:, None] * log_gamma[None, :]).astype(f32)
    chunk_decay = np.exp(f32(L) * log_gamma).astype(f32)
    dk = f32(128.0 ** -0.5)
    c_intra = np.zeros((128, 512), f32)
    for h in range(H):
        c_intra[:, h * 128:(h + 1) * 128] = intra[h].T * dk
    c_qfs = np.zeros((128, 512), f32)
    c_decay = np.zeros((128, 512), f32)
    for h in range(H):
        c_qfs[:, h * 128:(h + 1) * 128] = q_from_start[:, h][None, :]
        c_decay[:, h * 128:(h + 1) * 128] = chunk_decay[h]
    c_small = np.zeros((128, 8), f32)
    invf_r = (1.0 / (f32(10000.0) ** (np.arange(0, 128, 2, dtype=f32) / f32(128)))).astype(f32)
    invf_m = (1.0 / (f32(10000.0) ** (np.arange(0, 32, 2, dtype=f32) / f32(32)))).astype(f32)
    p = np.arange(128)
    c_small[:, 0] = invf_r[p % 64]
    c_small[:, 1] = invf_m[p % 16]
    c_small[:, 2:6] = k_to_end * dk
    c_ident = np.eye(128, dtype=f32)
    c_rot = np.zeros((128, 128), f32)
    for m in range(64):
        c_rot[m + 64, m] = -1.0
    for m in range(64, 128):
        c_rot[m - 64, m] = 1.0
    kk = np.arange(128)
    c_causal = (kk[None, :] >= kk[:, None]).astype(f32)
    return dict(c_small=c_small, c_ident=c_ident, c_rot=c_rot, c_causal=c_causal, c_intra=c_intra, c_qfs=c_qfs,
                c_decay=c_decay)


def make_in_maps(inputs):
    f32 = np.float32
    x = np.asarray(inputs["x"], f32)
    mem = np.asarray(inputs["mem"], f32)
    pos = np.asarray(inputs["positions"], np.int32)

    def col(g):
        g = np.asarray(g, f32).reshape(-1, 128)
        return np.ascontiguousarray(g.T)

    gv = np.concatenate([col(inputs["g_mix"][0]), col(inputs["g_xattn"][0]), col(inputs["g_mem"][0]),
                         col(inputs["g_ffn"][0]), col(inputs["g_final"]), col(inputs["g_q_lat"][0]),
                         col(inputs["g_kv_lat"][0])], axis=1)
    cw = np.asarray(inputs["conv_w"][0], f32)
    cb = np.asarray(inputs["conv_b"][0], f32)
    convp = np.zeros((128, NFB, 4), f32)
    for i in range(3):
        convp[:, :, i] = cw[i].reshape(NFB, 128).T
    convp[:, :, 3] = cb.reshape(NFB, 128).T
    shared = dict(
        w_in=np.ascontiguousarray(inputs["w_in"][0], f32), w_uq=np.ascontiguousarray(inputs["w_uq"][0], f32),
        w_ukv=np.ascontiguousarray(inputs["w_ukv"][0], f32), w_out=np.ascontiguousarray(inputs["w_out"][0], f32),
        w_xq=np.ascontiguousarray(inputs["w_xq"][0], f32), w_xkv=np.ascontiguousarray(inputs["w_xkv"][0], f32),
        w_xo=np.ascontiguousarray(inputs["w_xo"][0], f32), w_ffn_in=np.ascontiguousarray(inputs["w_ffn_in"][0], f32),
        w_ffn_out=np.ascontiguousarray(inputs["w_ffn_out"][0], f32), gv=np.ascontiguousarray(gv),
        convp=np.ascontiguousarray(convp.reshape(128, NFB * 4)), **_consts())
    maps = []
    for core in range(8):
        b, hf = core // 2, core % 2
        xT = np.zeros((D, NTOK), f32)
        pp = np.zeros((1, NTOK), np.int32)
        if hf == 0:
            xT[:, NPRE:] = x[b, :NOWN].T
            pp[0, NPRE:] = pos[b, :NOWN]
        else:
            xT[:, :] = x[b].T
            pp[0, :] = pos[b]
        flags = np.full((128, 2), float(hf), f32)
        m = dict(shared)
        m.update(xT=xT, posi=pp, memT=np.ascontiguousarray(mem[b].T), flags=flags)
        maps.append(m)
    return maps


_CACHE = {}


def kernel(**inputs):
    if "nc" not in _CACHE:
        _CACHE["nc"] = build("full")[0]
    nc = _CACHE["nc"]
    maps = make_in_maps(inputs)
    res = run_bass_kernel_spmd(nc, maps, core_ids=list(range(8)))
    out = np.zeros((NB, SEQ, D), np.float32)
    for core in range(8):
        b, hf = core // 2, core % 2
        out[b, hf * NOWN:(hf + 1) * NOWN, :] = res.results[core]["outT"].T
    return out
```
